# Optimizing a Trainium2 kernel written in Bass

```python
import jax, jax.numpy as jnp
from jax import lax
import numpy as np

D_MODEL = 2048
BATCH = 4
SEQ = 4096
DEPTH = 2

GRID_W = 64
CTX_LEN = 256
HEAD_DIM = 128
BLOCK = 128
WINDOW = 128
A_HEADS = 6
A_KV = 2
B_GROUPS = 4
B_CHUNK = 128
C_HEADS = 6
C_KV = 2
A_WIDTH = A_HEADS * HEAD_DIM
B_WIDTH = B_GROUPS * HEAD_DIM
C_WIDTH = C_HEADS * HEAD_DIM
MIX_WIDTH = A_WIDTH + B_WIDTH + C_WIDTH
IN_SPLITS = (A_WIDTH, A_KV * HEAD_DIM, A_KV * HEAD_DIM, B_WIDTH, B_WIDTH, C_WIDTH, C_KV * HEAD_DIM, C_KV * HEAD_DIM)
IN_WIDTH = A_WIDTH + 2 * A_KV * HEAD_DIM + 2 * B_WIDTH + C_WIDTH + 2 * C_KV * HEAD_DIM
OUT_SPLITS = (A_WIDTH, B_WIDTH, C_WIDTH)
N_EXPERTS = 16
EC_CAPACITY = 2
D_EXPERT = 2048
N_MOD = 6
ROPE_THETA = 10000.0
EPS = 1e-6
NEG_INF = -1e30

kernel_name = "hymba_style_diffusion_trunk_ec_moe"


def _split(x, sizes):
    idx = [int(i) for i in np.cumsum(sizes)[:-1]]
    return jnp.split(x, idx, axis=-1)


def rms_norm(x, g):
    xf = x.astype(jnp.float32)
    y = xf * lax.rsqrt(jnp.mean(xf * xf, axis=-1, keepdims=True) + EPS)
    return (y * g.astype(jnp.float32)).astype(x.dtype)


def modulate(h, shift, scale):
    return h * (1 + scale) + shift


def axial_angles(n):
    rows = n // GRID_W
    row = jnp.repeat(jnp.arange(rows, dtype=jnp.float32), GRID_W)
    col = (jnp.arange(n) % GRID_W).astype(jnp.float32)
    n_freq = HEAD_DIM // 4
    inv = ROPE_THETA ** (-jnp.arange(n_freq, dtype=jnp.float32) / n_freq)
    return row[:, None] * inv, col[:, None] * inv


def rope_half(x, ang):
    f = ang.shape[-1]
    cos = jnp.cos(ang)[None, :, None, :].astype(x.dtype)
    sin = jnp.sin(ang)[None, :, None, :].astype(x.dtype)
    x1, x2 = x[..., :f], x[..., f:]
    return jnp.concatenate([x1 * cos - x2 * sin, x2 * cos + x1 * sin], axis=-1)


def rope_2d(x, ang_r, ang_c):
    h = x.shape[-1] // 2
    return jnp.concatenate([rope_half(x[..., :h], ang_r), rope_half(x[..., h:], ang_c)], axis=-1)


def attn_heads(p_q, p_k, p_v, n_q, n_kv, qn, kn, ang):
    b, n = p_q.shape[:2]
    q = rms_norm(p_q.reshape(b, n, n_q, HEAD_DIM), qn)
    k = rms_norm(p_k.reshape(b, n, n_kv, HEAD_DIM), kn)
    if ang is not None:
        q = rope_2d(q, *ang)
        k = rope_2d(k, *ang)
    v = p_v.reshape(b, n, n_kv, HEAD_DIM)
    return q.reshape(b, n, n_kv, n_q // n_kv, HEAD_DIM), k, v


def dense_attn(q, k, v, sink):
    s = jnp.einsum('bqkgd,bskd->bkgqs', q, k).astype(jnp.float32) * (HEAD_DIM ** -0.5)
    if sink is not None:
        sk = jnp.broadcast_to(sink[None, :, :, None, None].astype(jnp.float32), s.shape[:-1] + (1,))
        s = jnp.concatenate([sk, s], axis=-1)
    p = jax.nn.softmax(s, axis=-1)
    if sink is not None:
        p = p[..., 1:]
    return jnp.einsum('bkgqs,bskd->bqkgd', p.astype(v.dtype), v)


def window_attn_latent(q, k, v, k_ctx, v_ctx, sink):
    b, n, kv, g, dh = q.shape
    nb = n // BLOCK
    L = k_ctx.shape[1]
    qb = q.reshape(b, nb, BLOCK, kv, g, dh)
    pad = ((0, 0), (BLOCK, BLOCK), (0, 0), (0, 0))
    kp, vp = jnp.pad(k, pad), jnp.pad(v, pad)
    kb = jnp.concatenate([kp[:, j * BLOCK:j * BLOCK + n].reshape(b, nb, BLOCK, kv, dh) for j in range(3)], axis=2)
    vb = jnp.concatenate([vp[:, j * BLOCK:j * BLOCK + n].reshape(b, nb, BLOCK, kv, dh) for j in range(3)], axis=2)
    scale = HEAD_DIM ** -0.5
    s_band = jnp.einsum('bnqkgd,bnskd->bnkgqs', qb, kb).astype(jnp.float32) * scale
    s_ctx = jnp.einsum('bnqkgd,bskd->bnkgqs', qb, k_ctx).astype(jnp.float32) * scale
    blk = jnp.arange(nb)[:, None, None]
    qpos = blk * BLOCK + jnp.arange(BLOCK)[None, :, None]
    kpos = (blk - 1) * BLOCK + jnp.arange(3 * BLOCK)[None, None, :]
    valid = (jnp.abs(kpos - qpos) <= WINDOW) & (kpos >= 0) & (kpos < n)
    s_band = jnp.where(valid[None, :, None, None], s_band, NEG_INF)
    sk = jnp.broadcast_to(sink[None, None, :, :, None, None].astype(jnp.float32), s_band.shape[:-1] + (1,))
    p = jax.nn.softmax(jnp.concatenate([sk, s_ctx, s_band], axis=-1), axis=-1).astype(v.dtype)
    o = (jnp.einsum('bnkgqs,bskd->bnqkgd', p[..., 1:1 + L], v_ctx)
         + jnp.einsum('bnkgqs,bnskd->bnqkgd', p[..., 1 + L:], vb))
    return o.reshape(b, n, kv * g * dh)


def global_attn_latent(q, k_all, v_all):
    b, n, kv, g, dh = q.shape
    nb = n // BLOCK
    qb = q.reshape(b, nb, BLOCK, kv, g, dh).transpose(1, 0, 2, 3, 4, 5)
    o = lax.map(lambda qi: dense_attn(qi, k_all, v_all, None), qb)
    return o.transpose(1, 0, 2, 3, 4, 5).reshape(b, n, kv * g * dh)


def chunk_mlp(u, v, vn_g, w_s, b_s):
    b, n, _ = u.shape
    nc = n // B_CHUNK
    u = jax.nn.gelu(u).reshape(b, nc, B_CHUNK, B_GROUPS, HEAD_DIM)
    v = rms_norm(jax.nn.gelu(v).reshape(b, n, B_GROUPS, HEAD_DIM), vn_g.reshape(B_GROUPS, HEAD_DIM))
    v = v.reshape(b, nc, B_CHUNK, B_GROUPS, HEAD_DIM)
    mixed = jnp.einsum('gpq,bnqgd->bnpgd', w_s, v) + b_s.T[None, None, :, :, None]
    return (u * mixed).reshape(b, n, B_WIDTH)


def merge_heads(o_a, o_b, o_c, out_g, w_out):
    g_a, g_b, g_c = _split(out_g, OUT_SPLITS)
    y = jnp.concatenate([rms_norm(o_a, g_a), rms_norm(o_b, g_b), rms_norm(o_c, g_c)], axis=-1)
    return y @ w_out


def token_mixers(hx, hc, ang, w_in, qn_a, kn_a, sink_a, vn_b, w_s, b_s, qn_c, kn_c, out_g, w_out, need_ctx):
    qa_x, ka_x, va_x, ub_x, vb_x, qc_x, kc_x, vc_x = _split(hx @ w_in, IN_SPLITS)
    qa_c, ka_c, va_c, ub_c, vb_c, qc_c, kc_c, vc_c = _split(hc @ w_in, IN_SPLITS)
    sink = sink_a.reshape(A_KV, A_HEADS // A_KV)
    q_a, k_a, v_a = attn_heads(qa_x, ka_x, va_x, A_HEADS, A_KV, qn_a, kn_a, ang)
    q_ac, k_ac, v_ac = attn_heads(qa_c, ka_c, va_c, A_HEADS, A_KV, qn_a, kn_a, None)
    o_a = window_attn_latent(q_a, k_a, v_a, k_ac, v_ac, sink)
    o_b = chunk_mlp(ub_x, vb_x, vn_b, w_s, b_s)
    q_c, k_c, v_c = attn_heads(qc_x, kc_x, vc_x, C_HEADS, C_KV, qn_c, kn_c, ang)
    q_cc, k_cc, v_cc = attn_heads(qc_c, kc_c, vc_c, C_HEADS, C_KV, qn_c, kn_c, None)
    k_all = jnp.concatenate([k_cc, k_c], axis=1)
    v_all = jnp.concatenate([v_cc, v_c], axis=1)
    o_c = global_attn_latent(q_c, k_all, v_all)
    y_x = merge_heads(o_a, o_b, o_c, out_g, w_out)
    y_c = None
    if need_ctx:
        bsz, L = hc.shape[:2]
        o_ac = dense_attn(q_ac, k_ac, v_ac, sink).reshape(bsz, L, A_WIDTH)
        o_bc = chunk_mlp(ub_c, vb_c, vn_b, w_s, b_s)
        o_cc = dense_attn(q_cc, k_cc, v_cc, None).reshape(bsz, L, C_WIDTH)
        y_c = merge_heads(o_ac, o_bc, o_cc, out_g, w_out)
    return y_x, y_c


def expert_choice_ffn(h, w_router, w_gate, w_up, w_down):
    n, d = h.shape[1], h.shape[2]
    cap = EC_CAPACITY * n // N_EXPERTS

    def per_sample(hs):
        aff = jax.nn.softmax((hs @ w_router).astype(jnp.float32), axis=-1)
        gate, idx = lax.top_k(aff.T, cap)
        xg = hs[idx]
        a = jnp.einsum('ecd,edf->ecf', xg, w_gate)
        u = jnp.einsum('ecd,edf->ecf', xg, w_up)
        y = jnp.einsum('ecf,efd->ecd', jax.nn.silu(a) * u, w_down)
        y = y * gate[..., None].astype(y.dtype)
        return jnp.zeros_like(hs).at[idx.reshape(-1)].add(y.reshape(-1, d))

    return jax.vmap(per_sample)(h)


def setup_inputs(seed: int = 0) -> dict:
    key = jax.random.key(seed)
    ks = jax.random.split(key, 24)
    f32 = jnp.float32
    nrm = lambda k, shape, s: (jax.random.normal(k, shape, f32) * s)
    gain = lambda k, shape: 1.0 + 0.02 * jax.random.normal(k, shape, f32)
    return {
        "x": nrm(ks[0], (BATCH, SEQ, D_MODEL), 1.0),
        "c": nrm(ks[1], (BATCH, D_MODEL), 1.0),
        "ctx": nrm(ks[2], (BATCH, CTX_LEN, D_MODEL), 1.0),
        "c_ctx": nrm(ks[3], (D_MODEL,), 1.0),
        "w_mod": nrm(ks[4], (DEPTH, D_MODEL, N_MOD * D_MODEL), 0.5 * D_MODEL ** -0.5),
        "b_mod": nrm(ks[5], (DEPTH, N_MOD * D_MODEL), 0.01),
        "norm1_g": gain(ks[6], (DEPTH, D_MODEL)),
        "norm2_g": gain(ks[7], (DEPTH, D_MODEL)),
        "w_in": nrm(ks[8], (DEPTH, D_MODEL, IN_WIDTH), D_MODEL ** -0.5),
        "qn_a": gain(ks[9], (DEPTH, HEAD_DIM)),
        "kn_a": gain(ks[10], (DEPTH, HEAD_DIM)),
        "sink_a": nrm(ks[11], (DEPTH, A_HEADS), 0.5),
        "vn_b": gain(ks[12], (DEPTH, B_WIDTH)),
        "w_s": nrm(ks[13], (DEPTH, B_GROUPS, B_CHUNK, B_CHUNK), 0.5 * B_CHUNK ** -0.5),
        "b_s": 1.0 + nrm(ks[14], (DEPTH, B_GROUPS, B_CHUNK), 0.1),
        "qn_c": gain(ks[15], (DEPTH, HEAD_DIM)),
        "kn_c": gain(ks[16], (DEPTH, HEAD_DIM)),
        "out_g": gain(ks[17], (DEPTH, MIX_WIDTH)),
        "w_out": nrm(ks[18], (DEPTH, MIX_WIDTH, D_MODEL), MIX_WIDTH ** -0.5),
        "w_router": nrm(ks[19], (DEPTH, D_MODEL, N_EXPERTS), D_MODEL ** -0.5),
        "w_gate": nrm(ks[20], (DEPTH, N_EXPERTS, D_MODEL, D_EXPERT), D_MODEL ** -0.5),
        "w_up": nrm(ks[21], (DEPTH, N_EXPERTS, D_MODEL, D_EXPERT), D_MODEL ** -0.5),
        "w_down": nrm(ks[22], (DEPTH, N_EXPERTS, D_EXPERT, D_MODEL), D_EXPERT ** -0.5),
    }


def reference(x, c, ctx, c_ctx, w_mod, b_mod, norm1_g, norm2_g, w_in, qn_a, kn_a, sink_a, vn_b,
              w_s, b_s, qn_c, kn_c, out_g, w_out, w_router, w_gate, w_up, w_down):
    ang = axial_angles(x.shape[1])
    xc = ctx
    for i in range(DEPTH):
        last = i == DEPTH - 1
        mod_x = (jax.nn.silu(c) @ w_mod[i] + b_mod[i])[:, None, :]
        mod_c = (jax.nn.silu(c_ctx) @ w_mod[i] + b_mod[i])[None, None, :]
        sh1, sc1, g1, sh2, sc2, g2 = jnp.split(mod_x, N_MOD, axis=-1)
        sh1c, sc1c, g1c, sh2c, sc2c, g2c = jnp.split(mod_c, N_MOD, axis=-1)
        hx = modulate(rms_norm(x, norm1_g[i]), sh1, sc1)
        hc = modulate(rms_norm(xc, norm1_g[i]), sh1c, sc1c)
        y_x, y_c = token_mixers(hx, hc, ang, w_in[i], qn_a[i], kn_a[i], sink_a[i], vn_b[i], w_s[i], b_s[i],
                                qn_c[i], kn_c[i], out_g[i], w_out[i], not last)
        x = x + g1 * y_x
        hx = modulate(rms_norm(x, norm2_g[i]), sh2, sc2)
        x = x + g2 * expert_choice_ffn(hx, w_router[i], w_gate[i], w_up[i], w_down[i])
        if not last:
            xc = xc + g1c * y_c
            hc = modulate(rms_norm(xc, norm2_g[i]), sh2c, sc2c)
            xc = xc + g2c * expert_choice_ffn(hc, w_router[i], w_gate[i], w_up[i], w_down[i])
    return x
```

```python
import contextlib
import numpy as np
import ml_dtypes
import concourse.bass as bass
import concourse.mybir as mybir
from concourse.bass_utils import run_bass_kernel_spmd

F32 = mybir.dt.float32
BF16 = mybir.dt.bfloat16
I32 = mybir.dt.int32
AF = mybir.ActivationFunctionType
ALU = mybir.AluOpType
AX = mybir.AxisListType

D = 2048
SEQ = 4096
CTX = 256
NT = 18
NTOK = NT * 128
EPS = 1e-6
NE = 16

ENGS = ("sync", "scalar", "vector", "gpsimd", "tensor")
SEM_CHUNK = 3000
NDMA_SEMS = 12


class _Op:
    __slots__ = ("eng", "fn", "deps", "is_dma", "has_dep", "sem", "val", "prev_dma")

    def __init__(self, eng, fn, is_dma):
        self.eng = eng
        self.fn = fn
        self.is_dma = is_dma
        self.deps = []
        self.has_dep = False
        self.sem = None
        self.val = None
        self.prev_dma = None


class Prog:
    def __init__(self, nc):
        self.nc = nc
        self.ops = {e: [] for e in ENGS}
        self.last_w = {}
        self.readers = {}
        self.ndma = {e: 0 for e in ENGS}
        self.dma_last = {}
        self.all_dmas = []

    def op(self, eng, fn, reads=(), writes=(), dma=False):
        o = _Op(eng, fn, dma)
        deps = []
        for r in reads:
            w = self.last_w.get(r)
            if w is not None:
                deps.append((w, "raw"))
        for k in writes:
            w = self.last_w.get(k)
            if w is not None:
                deps.append((w, "waw"))
            for rd in self.readers.get(k, ()):
                deps.append((rd, "war"))
        for (d, kind) in deps:
            if d is o:
                continue
            if not d.is_dma and not dma and d.eng == eng:
                if kind != "raw" or eng == "tensor":
                    continue
            if d not in o.deps:
                o.deps.append(d)
                d.has_dep = True
        for r in reads:
            self.readers.setdefault(r, []).append(o)
        for k in writes:
            self.last_w[k] = o
            self.readers[k] = []
        if dma:
            j = self.ndma[eng]
            self.ndma[eng] += 1
            slot = (eng, j % NDMA_SEMS)
            o.prev_dma = self.dma_last.get(slot)
            self.dma_last[slot] = o
            o.sem = slot
            o.val = 16 * (j // NDMA_SEMS + 1)
            self.all_dmas.append(o)
        self.ops[eng].append(o)
        return o

    def v(self, fn, r=(), w=()):
        return self.op("vector", fn, r, w)

    def a(self, fn, r=(), w=()):
        return self.op("scalar", fn, r, w)

    def g(self, fn, r=(), w=()):
        return self.op("gpsimd", fn, r, w)

    def t(self, fn, r=(), w=()):
        return self.op("tensor", fn, r, w)

    def dma(self, fn, r=(), w=(), q="sync"):
        return self.op(q, fn, r, w, dma=True)

    def emit(self, final_wait_eng="sync"):
        nc = self.nc
        sem_names = set()
        for e in ENGS:
            cnt = 0
            for o in self.ops[e]:
                if o.is_dma:
                    sem_names.add(o.sem)
                    continue
                if o.has_dep:
                    o.sem = (e, "c", cnt // SEM_CHUNK)
                    o.val = cnt % SEM_CHUNK + 1
                    sem_names.add(o.sem)
                    cnt += 1
        sem_names = sorted(sem_names, key=str)
        with contextlib.ExitStack() as st:
            sems = {}
            for n in sem_names:
                sems[n] = st.enter_context(nc.semaphore("s_" + "_".join(str(x) for x in n)))
            block = st.enter_context(nc.Block())
            prog = self

            def run(e, eng):
                seen = {}
                for o in prog.ops[e]:
                    waits = []
                    if o.is_dma and o.prev_dma is not None:
                        waits.append(o.prev_dma)
                    waits.extend(o.deps)
                    for d in waits:
                        if seen.get(d.sem, 0) >= d.val:
                            continue
                        eng.wait_ge(sems[d.sem], d.val)
                        seen[d.sem] = d.val
                    ins = o.fn(eng)
                    if o.is_dma:
                        ins.then_inc(sems[o.sem], 16)
                    elif o.has_dep:
                        ins.then_inc(sems[o.sem], 1)
                if e == final_wait_eng:
                    last = {}
                    for o in prog.all_dmas:
                        last[o.sem] = max(last.get(o.sem, 0), o.val)
                    for s, v in last.items():
                        if seen.get(s, 0) < v:
                            eng.wait_ge(sems[s], v)

            @block.sync
            def _(eng):
                run("sync", eng)

            @block.scalar
            def _(eng):
                run("scalar", eng)

            @block.vector
            def _(eng):
                run("vector", eng)

            @block.gpsimd
            def _(eng):
                run("gpsimd", eng)

            @block.tensor
            def _(eng):
                run("tensor", eng)


def _bc(ap1d, n):
    return ap1d.unsqueeze(0).to_broadcast([128, n])


MCOL = 1536


def build_mod():
    nc = bass.Bass("TRN2", target_bir_lowering=False)
    cin = nc.dram_tensor("cin", [128, 16, 5], F32, kind="ExternalInput").ap()
    w = nc.dram_tensor("w", [2, D, MCOL], F32, kind="ExternalInput").ap()
    b = nc.dram_tensor("b", [2, MCOL], F32, kind="ExternalInput").ap()
    out = nc.dram_tensor("out", [2, 5, MCOL], F32, kind="ExternalOutput").ap()
    with contextlib.ExitStack() as st:
        sb = lambda n, s, d: st.enter_context(nc.sbuf_tensor(n, s, d))
        ct = sb("ct", [128, 16, 5], F32)
        cs = sb("cs", [128, 16, 5], F32)
        wt = [sb(f"wt{i}", [128, 16, 512], F32) for i in range(2)]
        bt = sb("bt", [5, 2, MCOL], F32)
        ot = sb("ot", [5, 2, MCOL], F32)
        ps = [st.enter_context(nc.psum_tensor(f"ps{i}", [5, 512], F32)) for i in range(2)]
        P = Prog(nc)
        P.dma(lambda e: e.dma_start(out=ct[:], in_=cin), w=["ct"])
        for l in range(2):
            P.dma(lambda e, l=l: e.dma_start(out=bt[:, l, :], in_=b[l:l + 1, :].to_broadcast([5, MCOL])), w=[("bt", l)])
        P.a(lambda e: e.activation(out=cs[:], in_=ct[:], func=AF.Silu), ["ct"], ["cs"])
        gi = 0
        for l in range(2):
            for g in range(3):
                buf = gi % 2
                src = w[l, :, g * 512:(g + 1) * 512].rearrange("(kc p) n -> p kc n", p=128)
                P.dma(lambda e, buf=buf, src=src: e.dma_start(out=wt[buf][:], in_=src), w=[("wt", buf)])
                for kc in range(16):
                    P.t(lambda e, buf=buf, kc=kc: e.matmul(ps[buf][:], lhsT=cs[:, kc, :], rhs=wt[buf][:, kc, :],
                                                           start=(kc == 0), stop=(kc == 15)),
                        ["cs", ("wt", buf)], [("ps", buf)])
                P.v(lambda e, buf=buf, l=l, g=g: e.tensor_tensor(out=ot[:, l, g * 512:(g + 1) * 512], in0=ps[buf][:],
                                                                 in1=bt[:, l, g * 512:(g + 1) * 512], op=ALU.add),
                    [("ps", buf), ("bt", l)], [("ot", l, g)])
                gi += 1
        for l in range(2):
            P.dma(lambda e, l=l: e.dma_start(out=out[l], in_=ot[:, l, :]), [("ot", l, g) for g in range(3)], [("out", l)])
        P.emit()
    return nc


def run_mod(c, c_ctx, w_mod, b_mod):
    c_all = np.concatenate([c, c_ctx[None, :]], axis=0).astype(np.float32)
    cin = np.ascontiguousarray(c_all.T.reshape(16, 128, 5).transpose(1, 0, 2))
    nc = build_mod()
    in_maps = [{"cin": cin, "w": np.ascontiguousarray(w_mod[:, :, j * MCOL:(j + 1) * MCOL]),
                "b": np.ascontiguousarray(b_mod[:, j * MCOL:(j + 1) * MCOL])} for j in range(8)]
    res = run_bass_kernel_spmd(nc, in_maps, core_ids=list(range(8)))
    return np.concatenate([r["out"] for r in res.results], axis=2)


def emit_rstd(P, ss, rstd, n, kr, kw):
    P.a(lambda e: e.activation(out=rstd, in_=ss, func=AF.Sqrt, scale=1.0 / n, bias=EPS), kr, kw)
    P.v(lambda e: e.reciprocal(out=rstd, in_=rstd), kw, kw)


NQK = 16
WIN_PERM = np.concatenate([np.arange(0, 1024), np.arange(2304, 3328),
                           np.arange(1024, 1280), np.arange(3328, 3584),
                           np.arange(1280, 2304)])


def build_s1():
    nc = bass.Bass("TRN2", target_bir_lowering=False)
    di = lambda n, s, d=F32: nc.dram_tensor(n, s, d, kind="ExternalInput").ap()
    do = lambda n, s, d=F32: nc.dram_tensor(n, s, d, kind="ExternalOutput").ap()
    xin = di("xin", [NTOK, D])
    modx = di("modx", [6, D])
    modc = di("modc", [6, D])
    n1g = di("n1g", [D])
    win = di("win", [D, 3584])
    gqk = di("gqk", [NQK * 128])
    cs_t = di("cs_t", [NTOK, 128])
    vnb = di("vnb", [512])
    wsT = di("wsT", [128, 4, 128])
    bsT = di("bsT", [128, 4])
    ogb = di("ogb", [512])
    ident_in = di("ident", [128, 128])
    QKT = do("QKT", [128, NQK, NTOK], BF16)
    Vout = do("Vout", [NTOK, 512], BF16)
    yB = do("yB", [NTOK, 512], BF16)

    with contextlib.ExitStack() as st:
        sb = lambda n, s, d=F32: st.enter_context(nc.sbuf_tensor(n, s, d))
        pt = lambda n, s, d=F32: st.enter_context(nc.psum_tensor(n, s, d))
        wbf = sb("wbf", [128, 16, 2048], BF16)
        G1 = sb("G1", [128, D])
        SH = sb("SH", [128, D])
        gn = sb("gn", [128, D])
        xt = [sb(f"xt{i}", [128, D]) for i in range(2)]
        tmp = sb("tmp", [128, D])
        hx = sb("hx", [128, D], BF16)
        hxT = sb("hxT", [128, 16, 128], BF16)
        ident = sb("ident_f", [128, 128])
        identb = sb("identb", [128, 128], BF16)
        ss = sb("ss", [128, 1])
        rs = sb("rs", [128, 1])
        Pb = sb("Pb", [128, 2048])
        gq = sb("gq", [128, NQK, 128])
        ssq = sb("ssq", [128, NQK])
        rsq = sb("rsq", [128, NQK])
        qn = sb("qn", [128, NQK, 2, 2, 32])
        cst = sb("cst", [128, 128])
        rA = sb("rA", [128, NQK, 2, 32])
        rB = sb("rB", [128, NQK, 2, 32])
        qr = sb("qr", [128, NQK, 2, 2, 32], BF16)
        qT = sb("qT", [128, NQK, 128], BF16)
        vb16 = sb("vb16", [128, 512], BF16)
        vng = sb("vng", [128, 512])
        wsb = sb("wsb", [128, 4, 128])
        wsbb = sb("wsbb", [128, 4, 128], BF16)
        bsb = sb("bsb", [128, 4])
        ogbt = sb("ogbt", [128, 512])
        t1 = sb("t1", [128, 1024])
        t2 = sb("t2", [128, 1024])
        gl = sb("gl", [128, 1024])
        ssv = sb("ssv", [128, 4])
        rsv = sb("rsv", [128, 4])
        vn = sb("vn", [128, 4, 128], BF16)
        ob = sb("ob", [128, 512])
        yb = sb("yb", [128, 512], BF16)
        pT = pt("pT", [128, 16, 128], BF16)
        pP = pt("pP", [128, 2048])
        pQ = pt("pQ", [128, NQK, 128], BF16)

        P = Prog(nc)
        P.dma(lambda e: e.dma_start(out=ident[:], in_=ident_in), w=["ident"])
        P.v(lambda e: e.tensor_copy(out=identb[:], in_=ident[:]), ["ident"], ["identb"])
        P.dma(lambda e: e.dma_start(out=gn[:], in_=_bc(n1g, D)), w=["gn"])
        P.dma(lambda e: e.dma_start(out=gq[:].rearrange("p h d -> p (h d)"), in_=_bc(gqk, NQK * 128)), w=["gq"])
        P.dma(lambda e: e.dma_start(out=vng[:], in_=_bc(vnb, 512)), w=["vng"])
        P.dma(lambda e: e.dma_start(out=ogbt[:], in_=_bc(ogb, 512)), w=["ogbt"])
        P.dma(lambda e: e.dma_start(out=wsb[:], in_=wsT), w=["wsb"])
        P.v(lambda e: e.tensor_copy(out=wsbb[:], in_=wsb[:]), ["wsb"], ["wsbb"])
        P.dma(lambda e: e.dma_start(out=bsb[:], in_=bsT), w=["bsb"])

        def load_w(c0, ncol):
            for k4 in range(4):
                src = win[k4 * 512:(k4 + 1) * 512, c0:c0 + ncol].rearrange("(kc p) n -> p kc n", p=128)
                P.dma(lambda e, k4=k4, src=src: e.dma_start(out=wbf[:, k4 * 4:(k4 + 1) * 4, 0:ncol], in_=src),
                      w=["wbf"], q="gpsimd")

        def load_mod(m):
            P.dma(lambda e: e.dma_start(out=SH[:], in_=_bc(m[0], D)), w=["SH"])
            P.dma(lambda e: e.dma_start(out=G1[:], in_=_bc(m[1], D)), w=["G1"])
            P.v(lambda e: e.scalar_tensor_tensor(out=G1[:], in0=G1[:], scalar=1.0, in1=gn[:], op0=ALU.add, op1=ALU.mult),
                ["G1", "gn"], ["G1"])

        def hx_tile(t):
            xb = xt[t % 2]
            kx = ("xt", t % 2)
            P.dma(lambda e: e.dma_start(out=xb[:], in_=xin[t * 128:(t + 1) * 128, :]), w=[kx])
            P.a(lambda e: e.activation(out=tmp[:], in_=xb[:], func=AF.Square, accum_out=ss[:]), [kx], ["tmp", "ss"])
            emit_rstd(P, ss[:], rs[:], D, ["ss"], ["rs"])
            P.v(lambda e: e.scalar_tensor_tensor(out=tmp[:], in0=xb[:], scalar=rs[:, 0:1], in1=G1[:], op0=ALU.mult, op1=ALU.mult),
                [kx, "rs", "G1"], ["tmp"])
            P.v(lambda e: e.tensor_tensor(out=hx[:], in0=tmp[:], in1=SH[:], op=ALU.add), ["tmp", "SH"], ["hx"])
            for kc in range(16):
                P.t(lambda e, kc=kc: e.transpose(pT[:, kc, :], hx[:, kc * 128:(kc + 1) * 128], identb[:]), ["hx", "identb"], ["pT"])
            P.a(lambda e: e.activation(out=hxT[:], in_=pT[:], func=AF.Copy), ["pT"], ["hxT"])

        def inproj(ncol):
            for g in range(ncol // 512):
                for kc in range(16):
                    P.t(lambda e, g=g, kc=kc: e.matmul(pP[:, g * 512:(g + 1) * 512], lhsT=hxT[:, kc, :],
                                                      rhs=wbf[:, kc, g * 512:(g + 1) * 512], start=(kc == 0), stop=(kc == 15)),
                        ["hxT", "wbf"], ["pP"])

        load_w(0, 2048)
        for t in range(NT):
            if t == 0:
                load_mod(modc)
            if t == 2:
                load_mod(modx)
            hx_tile(t)
            inproj(2048)
            P.a(lambda e: e.activation(out=Pb[:], in_=pP[:], func=AF.Copy), ["pP"], ["Pb"])
            Pv = Pb[:].rearrange("p (h d) -> p h d", h=NQK)
            P.dma(lambda e, t=t: e.dma_start(out=cst[:], in_=cs_t[t * 128:(t + 1) * 128, :]), w=["cst"])
            P.v(lambda e: e.tensor_tensor(out=tmp[:], in0=Pb[:], in1=Pb[:], op=ALU.mult), ["Pb"], ["tmp"])
            P.v(lambda e: e.tensor_reduce(out=ssq[:], in_=tmp[:].rearrange("p (h d) -> p h d", h=NQK), axis=AX.X, op=ALU.add),
                ["tmp"], ["ssq"])
            emit_rstd(P, ssq[:], rsq[:], 128, ["ssq"], ["rsq"])
            qnv = qn[:].rearrange("p h a b f -> p h (a b f)")
            P.v(lambda e: e.tensor_tensor(out=qnv, in0=Pv, in1=rsq[:].unsqueeze(2).to_broadcast([128, NQK, 128]), op=ALU.mult),
                ["Pb", "rsq"], ["qn"])
            P.v(lambda e: e.tensor_tensor(out=qnv, in0=qnv, in1=gq[:], op=ALU.mult), ["qn", "gq"], ["qn"])
            cosb = cst[:, 0:64].rearrange("p (a f) -> p a f", a=2).unsqueeze(1).to_broadcast([128, NQK, 2, 32])
            sinb = cst[:, 64:128].rearrange("p (a f) -> p a f", a=2).unsqueeze(1).to_broadcast([128, NQK, 2, 32])
            x1 = qn[:, :, :, 0, :]
            x2 = qn[:, :, :, 1, :]
            P.v(lambda e: e.tensor_tensor(out=rA[:], in0=x1, in1=cosb, op=ALU.mult), ["qn", "cst"], ["rA"])
            P.v(lambda e: e.tensor_tensor(out=rB[:], in0=x2, in1=sinb, op=ALU.mult), ["qn", "cst"], ["rB"])
            P.v(lambda e: e.tensor_tensor(out=qr[:, :, :, 0, :], in0=rA[:], in1=rB[:], op=ALU.subtract), ["rA", "rB"], ["qr"])
            P.v(lambda e: e.tensor_tensor(out=rA[:], in0=x2, in1=cosb, op=ALU.mult), ["qn", "cst", "qr"], ["rA"])
            P.v(lambda e: e.tensor_tensor(out=rB[:], in0=x1, in1=sinb, op=ALU.mult), ["qn", "cst", "qr"], ["rB"])
            P.v(lambda e: e.tensor_tensor(out=qr[:, :, :, 1, :], in0=rA[:], in1=rB[:], op=ALU.add), ["rA", "rB"], ["qr"])
            qrv = qr[:].rearrange("p h a b f -> p h (a b f)")
            for h in range(NQK):
                P.t(lambda e, h=h: e.transpose(pQ[:, h, :], qrv[:, h, :], identb[:]), ["qr", "identb"], ["pQ"])
            P.a(lambda e: e.activation(out=qT[:], in_=pQ[:], func=AF.Copy), ["pQ"], ["qT"])
            P.dma(lambda e, t=t: e.dma_start(out=QKT[:, :, t * 128:(t + 1) * 128], in_=qT[:]), ["qT"], [("QKT", t)])

        load_w(2048, 1536)
        for t in range(NT):
            if t == 0:
                load_mod(modc)
            if t == 2:
                load_mod(modx)
            hx_tile(t)
            inproj(1536)
            P.a(lambda e: e.activation(out=vb16[:], in_=pP[:, 0:512], func=AF.Copy), ["pP"], ["vb16"])
            P.dma(lambda e, t=t: e.dma_start(out=Vout[t * 128:(t + 1) * 128, :], in_=vb16[:]), ["vb16"], [("Vout", t)])
            P.a(lambda e: e.activation(out=gl[:], in_=pP[:, 512:1536], func=AF.Gelu_apprx_tanh), ["pP"], ["gl"])
            gv = gl[:, 512:1024]
            P.v(lambda e: e.tensor_tensor(out=t1[:, 0:512], in0=gv, in1=gv, op=ALU.mult), ["gl"], ["t1"])
            P.v(lambda e: e.tensor_reduce(out=ssv[:], in_=t1[:, 0:512].rearrange("p (g d) -> p g d", g=4), axis=AX.X, op=ALU.add),
                ["t1"], ["ssv"])
            emit_rstd(P, ssv[:], rsv[:], 128, ["ssv"], ["rsv"])
            P.v(lambda e: e.tensor_tensor(out=t1[:, 0:512].rearrange("p (g d) -> p g d", g=4), in0=gv.rearrange("p (g d) -> p g d", g=4),
                                          in1=rsv[:].unsqueeze(2).to_broadcast([128, 4, 128]), op=ALU.mult), ["gl", "rsv"], ["t1"])
            P.v(lambda e: e.tensor_tensor(out=vn[:].rearrange("p g d -> p (g d)"), in0=t1[:, 0:512], in1=vng[:], op=ALU.mult),
                ["t1", "vng"], ["vn"])
            for g in range(4):
                P.t(lambda e, g=g: e.matmul(pP[:, 1536 + g * 128:1536 + (g + 1) * 128], lhsT=wsbb[:, g, :], rhs=vn[:, g, :],
                                            start=True, stop=True), ["vn", "wsbb"], ["pM"])
            P.v(lambda e: e.tensor_tensor(out=ob[:].rearrange("p (g d) -> p g d", g=4),
                                          in0=pP[:, 1536:2048].rearrange("p (g d) -> p g d", g=4),
                                          in1=bsb[:].unsqueeze(2).to_broadcast([128, 4, 128]), op=ALU.add), ["pM", "bsb"], ["ob"])
            P.v(lambda e: e.tensor_tensor(out=ob[:], in0=ob[:], in1=gl[:, 0:512], op=ALU.mult), ["ob", "gl"], ["ob"])
            P.a(lambda e: e.activation(out=t2[:, 0:512], in_=ob[:], func=AF.Square, accum_out=ss[:]), ["ob"], ["t2", "ss"])
            emit_rstd(P, ss[:], rs[:], 512, ["ss"], ["rs"])
            P.v(lambda e: e.scalar_tensor_tensor(out=yb[:], in0=ob[:], scalar=rs[:, 0:1], in1=ogbt[:], op0=ALU.mult, op1=ALU.mult),
                ["ob", "rs", "ogbt"], ["yb"])
            P.dma(lambda e, t=t: e.dma_start(out=yB[t * 128:(t + 1) * 128, :], in_=yb[:]), ["yb"], [("yB", t)])
        P.emit()
    return nc


NKA = 20
NKC = 34


def build_s2a():
    nc = bass.Bass("TRN2", target_bir_lowering=False)
    di = lambda n, s, d=F32: nc.dram_tensor(n, s, d, kind="ExternalInput").ap()
    do = lambda n, s, d=F32: nc.dram_tensor(n, s, d, kind="ExternalOutput").ap()
    QKT = di("QKT", [128, NQK, NTOK], BF16)
    KA = di("KA", [128, 2, NKA * 128], BF16)
    VA = di("VA", [NKA * 128, 256], BF16)
    KC = di("KC", [128, 2, NKC * 128], BF16)
    VC = di("VC", [NKC * 128, 256], BF16)
    msk = di("msk", [4, 128, 128])
    sink = di("sink", [6])
    gac = di("gac", [1536])
    yAC = do("yAC", [NTOK, 1536], BF16)
    with contextlib.ExitStack() as st:
        sb = lambda n, s, d=F32: st.enter_context(nc.sbuf_tensor(n, s, d))
        pt = lambda n, s, d=F32: st.enter_context(nc.psum_tensor(n, s, d))
        ka = sb("ka", [128, 2, NKA * 128], BF16)
        va = sb("va", [128, NKA, 2, 129], BF16)
        kc = sb("kc", [128, 2, NKC * 128], BF16)
        vc = sb("vc", [128, NKC, 2, 129], BF16)
        mf = sb("mf", [128, 4, 128])
        mb = sb("mb", [128, 4, 128], BF16)
        sk = sb("sk", [128, 6])
        es = sb("es", [128, 6])
        gt = sb("gt", [128, 1536])
        qt = [sb(f"qt{i}", [128, NQK, 128], BF16) for i in range(2)]
        PT = [sb(f"PT{i}", [128, 3, 128], BF16) for i in range(2)]
        oo = sb("oo", [128, 2, 6, 128])
        den = sb("den", [128, 3])
        sq = sb("sq", [128, 768])
        ss = sb("ss", [128, 1])
        rs = sb("rs", [128, 1])
        yt = sb("yt", [128, 1536], BF16)
        psS = [pt(f"psS{i}", [128, 512]) for i in range(2)]
        psO = [pt(f"psO{i}", [128, 3, 129]) for i in range(2)]
        P = Prog(nc)
        P.dma(lambda e: e.dma_start(out=ka[:], in_=KA), w=["ka"])
        P.dma(lambda e: e.dma_start(out=kc[:], in_=KC), w=["kc"])
        P.v(lambda e: e.memset(va[:, :, :, 128:129], 1.0), [], ["va1"])
        P.v(lambda e: e.memset(vc[:, :, :, 128:129], 1.0), [], ["vc1"])
        for k in range(2):
            P.dma(lambda e, k=k: e.dma_start(out=va[:, :, k, 0:128], in_=VA[:, k * 128:(k + 1) * 128].rearrange("(j s) d -> s j d", s=128)), w=[("va", k)])
            P.dma(lambda e, k=k: e.dma_start(out=vc[:, :, k, 0:128], in_=VC[:, k * 128:(k + 1) * 128].rearrange("(j s) d -> s j d", s=128)), w=[("vc", k)])
        P.dma(lambda e: e.dma_start(out=mf[:], in_=msk.rearrange("m s q -> s m q")), w=["mf"])
        P.v(lambda e: e.tensor_copy(out=mb[:], in_=mf[:]), ["mf"], ["mb"])
        P.dma(lambda e: e.dma_start(out=sk[:], in_=_bc(sink, 6)), w=["sk"])
        P.a(lambda e: e.activation(out=es[:], in_=sk[:], func=AF.Exp), ["sk"], ["es"])
        P.dma(lambda e: e.dma_start(out=gt[:], in_=_bc(gac, 1536)), w=["gt"])
        cnt = [0]

        def attn(q, qbase, KT, VT, kkey, vkeys, keys, use_sink, oi):
            for k in range(2):
                po = psO[k]
                for n, (j, m) in enumerate(keys):
                    sbuf = cnt[0] % 2
                    cnt[0] += 1
                    pS = psS[sbuf][:, 0:384].rearrange("p (g q) -> p g q", g=3)
                    P.t(lambda e, pS=pS, k=k, j=j: e.matmul(pS, lhsT=KT[:, k, j * 128:(j + 1) * 128],
                                                            rhs=q[:, qbase + 3 * k:qbase + 3 * k + 3, :], start=True, stop=True),
                        [kkey, "qt"], [("psS", sbuf)])
                    P.a(lambda e, pS=pS, sbuf=sbuf: e.activation(out=PT[sbuf][:], in_=pS, func=AF.Exp, scale=128.0 ** -0.5),
                        [("psS", sbuf)], [("PT", sbuf)])
                    if m is not None:
                        P.v(lambda e, sbuf=sbuf, m=m: e.tensor_tensor(out=PT[sbuf][:], in0=PT[sbuf][:],
                                                                     in1=mb[:, m, :].unsqueeze(1).to_broadcast([128, 3, 128]), op=ALU.mult),
                            [("PT", sbuf), "mb"], [("PT", sbuf)])
                    for g in range(3):
                        P.t(lambda e, po=po, sbuf=sbuf, g=g, j=j, k=k, n=n: e.matmul(po[:, g, :], lhsT=PT[sbuf][:, g, :], rhs=VT[:, j, k, :],
                                                                                   start=(n == 0 and g == 0), stop=(n == len(keys) - 1)),
                            [("PT", sbuf)] + vkeys, [("psO", k)])
                if use_sink:
                    P.v(lambda e, po=po, k=k: e.tensor_tensor(out=den[:], in0=po[:, :, 128], in1=es[:, 3 * k:3 * k + 3], op=ALU.add),
                        [("psO", k), "es"], ["den"])
                else:
                    P.v(lambda e, po=po: e.tensor_copy(out=den[:], in_=po[:, :, 128]), [("psO", k)], ["den"])
                P.v(lambda e: e.reciprocal(out=den[:], in_=den[:]), ["den"], ["den"])
                P.v(lambda e, po=po, k=k: e.tensor_tensor(out=oo[:, oi, 3 * k:3 * k + 3, :], in0=po[:, :, 0:128],
                                                          in1=den[:].unsqueeze(2).to_broadcast([128, 3, 128]), op=ALU.mult),
                    [("psO", k), "den"], [("oo", oi)])

        for t in range(NT):
            q = qt[t % 2]
            P.dma(lambda e, q=q, t=t: e.dma_start(out=q[:], in_=QKT[:, :, t * 128:(t + 1) * 128]), w=["qt"])
            if t < 2:
                keysA = [(0, None), (1, None)]
                keysC = [(0, None), (1, None)]
            else:
                i = t - 2
                keysA = [(0, None), (1, None), (2 + i, 2 if i == 0 else 0), (3 + i, None), (4 + i, 3 if i == 15 else 1)]
                keysC = [(j, None) for j in range(NKC)]
            attn(q, 0, ka, va, "ka", [("va", 0), ("va", 1), "va1"], keysA, True, 0)
            attn(q, 8, kc, vc, "kc", [("vc", 0), ("vc", 1), "vc1"], keysC, False, 1)
            for oi in range(2):
                ov = oo[:, oi, :, :].rearrange("p h d -> p (h d)")
                P.a(lambda e, ov=ov: e.activation(out=sq[:], in_=ov, func=AF.Square, accum_out=ss[:]), [("oo", oi)], ["sq", "ss"])
                emit_rstd(P, ss[:], rs[:], 768, ["ss"], ["rs"])
                P.v(lambda e, ov=ov, oi=oi: e.scalar_tensor_tensor(out=yt[:, oi * 768:(oi + 1) * 768], in0=ov, scalar=rs[:, 0:1],
                                                                  in1=gt[:, oi * 768:(oi + 1) * 768], op0=ALU.mult, op1=ALU.mult),
                    [("oo", oi), "rs", "gt"], ["yt"])
            P.dma(lambda e, t=t: e.dma_start(out=yAC[t * 128:(t + 1) * 128, :], in_=yt[:]), ["yt"], [("yAC", t)])
        P.emit()
    return nc


def build_s2b():
    nc = bass.Bass("TRN2", target_bir_lowering=False)
    di = lambda n, s, d=F32: nc.dram_tensor(n, s, d, kind="ExternalInput").ap()
    do = lambda n, s, d=F32: nc.dram_tensor(n, s, d, kind="ExternalOutput").ap()
    yAC = di("yAC", [NTOK, 1536], BF16)
    yB = di("yB", [NTOK, 512], BF16)
    xin = di("xin", [NTOK, D])
    modx = di("modx", [6, D])
    modc = di("modc", [6, D])
    wout = di("wout", [D, D])
    n2g = di("n2g", [D])
    wr = di("wr", [D, NE])
    ident_in = di("ident", [128, 128])
    x1o = do("x1o", [NTOK, D])
    hx2o = do("hx2o", [NTOK, D], BF16)
    affT = do("affT", [NE, NTOK])
    with contextlib.ExitStack() as st:
        sb = lambda n, s, d=F32: st.enter_context(nc.sbuf_tensor(n, s, d))
        pt = lambda n, s, d=F32: st.enter_context(nc.psum_tensor(n, s, d))
        wbf = sb("wbf", [128, 16, D], BF16)
        G1g = sb("G1g", [128, D])
        G2 = sb("G2", [128, D])
        SH2 = sb("SH2", [128, D])
        gn = sb("gn", [128, D])
        ident = sb("ident_f", [128, 128])
        identb = sb("identb", [128, 128], BF16)
        wrs = sb("wrs", [128, 16, NE])
        xt = sb("xt", [128, D])
        yt = sb("yt", [128, D], BF16)
        yT = sb("yT", [128, 16, 128], BF16)
        tmp = sb("tmp", [128, D])
        x1 = sb("x1", [128, D])
        h2 = sb("h2", [128, D])
        hb = sb("hb", [128, D], BF16)
        h2T = sb("h2T", [128, 16, 128])
        ss = sb("ss", [128, 1])
        rs = sb("rs", [128, 1])
        mx = sb("mx", [128, 1])
        se = sb("se", [128, 1])
        ex = sb("ex", [128, NE])
        af = sb("af", [128, NE])
        aT = sb("aT", [NE, 128])
        psT = pt("psT", [128, 16, 128], BF16)
        psX = [pt(f"psX{i}", [128, 512]) for i in range(2)]
        psR = [pt(f"psR{i}", [128, 4, 128]) for i in range(2)]
        psL = pt("psL", [128, 512])
        psA = pt("psA", [128, 512])
        P = Prog(nc)
        P.dma(lambda e: e.dma_start(out=ident[:], in_=ident_in), w=["ident"])
        P.v(lambda e: e.tensor_copy(out=identb[:], in_=ident[:]), ["ident"], ["identb"])
        P.dma(lambda e: e.dma_start(out=gn[:], in_=_bc(n2g, D)), w=["gn"])
        P.dma(lambda e: e.dma_start(out=wrs[:], in_=wr.rearrange("(kc p) n -> p kc n", p=128)), w=["wrs"])
        for k4 in range(4):
            src = wout[k4 * 512:(k4 + 1) * 512, :].rearrange("(kc p) n -> p kc n", p=128)
            P.dma(lambda e, k4=k4, src=src: e.dma_start(out=wbf[:, k4 * 4:(k4 + 1) * 4, :], in_=src), w=["wbf"], q="gpsimd")

        def load_mod(m):
            P.dma(lambda e: e.dma_start(out=G1g[:], in_=_bc(m[2], D)), w=["G1g"])
            P.dma(lambda e: e.dma_start(out=SH2[:], in_=_bc(m[3], D)), w=["SH2"])
            P.dma(lambda e: e.dma_start(out=G2[:], in_=_bc(m[4], D)), w=["G2"])
            P.v(lambda e: e.scalar_tensor_tensor(out=G2[:], in0=G2[:], scalar=1.0, in1=gn[:], op0=ALU.add, op1=ALU.mult),
                ["G2", "gn"], ["G2"])

        for t in range(NT):
            if t == 0:
                load_mod(modc)
            if t == 2:
                load_mod(modx)
            r0 = t * 128
            P.dma(lambda e, r0=r0: e.dma_start(out=yt[:, 0:768], in_=yAC[r0:r0 + 128, 0:768]), w=[("yt", 0)])
            P.dma(lambda e, r0=r0: e.dma_start(out=yt[:, 768:1280], in_=yB[r0:r0 + 128, :]), w=[("yt", 1)])
            P.dma(lambda e, r0=r0: e.dma_start(out=yt[:, 1280:2048], in_=yAC[r0:r0 + 128, 768:1536]), w=[("yt", 2)])
            P.dma(lambda e, r0=r0: e.dma_start(out=xt[:], in_=xin[r0:r0 + 128, :]), w=["xt"])
            for kc in range(16):
                P.t(lambda e, kc=kc: e.transpose(psT[:, kc, :], yt[:, kc * 128:(kc + 1) * 128], identb[:]),
                    [("yt", 0), ("yt", 1), ("yt", 2), "identb"], ["psT"])
            P.a(lambda e: e.activation(out=yT[:], in_=psT[:], func=AF.Copy), ["psT"], ["yT"])
            for g in range(4):
                px = psX[g % 2]
                for kc in range(16):
                    P.t(lambda e, px=px, g=g, kc=kc: e.matmul(px[:], lhsT=yT[:, kc, :], rhs=wbf[:, kc, g * 512:(g + 1) * 512],
                                                              start=(kc == 0), stop=(kc == 15)), ["yT", "wbf"], [("psX", g % 2)])
                P.v(lambda e, px=px, g=g: e.tensor_tensor(out=tmp[:, g * 512:(g + 1) * 512], in0=px[:], in1=G1g[:, g * 512:(g + 1) * 512],
                                                          op=ALU.mult), [("psX", g % 2), "G1g"], ["tmp"])
            P.v(lambda e: e.tensor_tensor(out=x1[:], in0=tmp[:], in1=xt[:], op=ALU.add), ["tmp", "xt"], ["x1"])
            P.dma(lambda e, r0=r0: e.dma_start(out=x1o[r0:r0 + 128, :], in_=x1[:]), ["x1"], [("x1o", t)])
            P.a(lambda e: e.activation(out=tmp[:], in_=x1[:], func=AF.Square, accum_out=ss[:]), ["x1"], ["tmp", "ss"])
            emit_rstd(P, ss[:], rs[:], D, ["ss"], ["rs"])
            P.v(lambda e: e.scalar_tensor_tensor(out=h2[:], in0=x1[:], scalar=rs[:, 0:1], in1=G2[:], op0=ALU.mult, op1=ALU.mult),
                ["x1", "rs", "G2"], ["h2"])
            P.v(lambda e: e.tensor_tensor(out=h2[:], in0=h2[:], in1=SH2[:], op=ALU.add), ["h2", "SH2"], ["h2"])
            P.a(lambda e: e.activation(out=hb[:], in_=h2[:], func=AF.Copy), ["h2"], ["hb"])
            P.dma(lambda e, r0=r0: e.dma_start(out=hx2o[r0:r0 + 128, :], in_=hb[:]), ["hb"], [("hx2o", t)])
            for q4 in range(4):
                pr = psR[q4 % 2]
                for j in range(4):
                    kc = q4 * 4 + j
                    P.t(lambda e, pr=pr, j=j, kc=kc: e.transpose(pr[:, j, :], h2[:, kc * 128:(kc + 1) * 128], ident[:]),
                        ["h2", "ident"], [("psR", q4 % 2)])
                P.a(lambda e, pr=pr, q4=q4: e.activation(out=h2T[:, q4 * 4:(q4 + 1) * 4, :], in_=pr[:], func=AF.Copy),
                    [("psR", q4 % 2)], ["h2T"])
            for kc in range(16):
                P.t(lambda e, kc=kc: e.matmul(psL[:, 0:NE], lhsT=h2T[:, kc, :], rhs=wrs[:, kc, :], start=(kc == 0), stop=(kc == 15)),
                    ["h2T", "wrs"], ["psL"])
            P.v(lambda e: e.tensor_reduce(out=mx[:], in_=psL[:, 0:NE], axis=AX.X, op=ALU.max), ["psL"], ["mx"])
            P.v(lambda e: e.tensor_scalar(out=mx[:], in0=mx[:], scalar1=-1.0, scalar2=None, op0=ALU.mult), ["mx"], ["mx"])
            P.a(lambda e: e.activation(out=ex[:], in_=psL[:, 0:NE], func=AF.Exp, bias=mx[:, 0:1], accum_out=se[:]), ["psL", "mx"], ["ex", "se"])
            P.v(lambda e: e.reciprocal(out=se[:], in_=se[:]), ["se"], ["se"])
            P.v(lambda e: e.tensor_scalar(out=af[:], in0=ex[:], scalar1=se[:, 0:1], scalar2=None, op0=ALU.mult), ["ex", "se"], ["af"])
            P.t(lambda e: e.transpose(psA[0:NE, 0:128], af[:], ident[:]), ["af", "ident"], ["psA"])
            P.a(lambda e: e.activation(out=aT[:], in_=psA[0:NE, 0:128], func=AF.Copy), ["psA"], ["aT"])
            P.dma(lambda e, r0=r0: e.dma_start(out=affT[:, r0:r0 + 128], in_=aT[:]), ["aT"], [("affT", t)])
        P.emit()
    return nc


NEL = 8
NROW = SEQ + CTX


def build_s3(with_ctx):
    nc = bass.Bass("TRN2", target_bir_lowering=False)
    di = lambda n, s, d=F32: nc.dram_tensor(n, s, d, kind="ExternalInput").ap()
    do = lambda n, s, d=F32: nc.dram_tensor(n, s, d, kind="ExternalOutput").ap()
    affL = di("affL", [128, 32, NEL])
    affC = di("affC", [128, 2, NEL])
    hx2 = di("hx2", [NROW, D], BF16)
    wg = di("wg", [NEL, D, D])
    wu = di("wu", [NEL, D, D])
    wd = di("wd", [NEL, D, D])
    consts = di("consts", [128, 128 + 128 + 128 + 512 + 34])
    delta = [do(f"delta{i}", [NROW, 512]) for i in range(4)]
    sets = [("L", 32, 512, 0, affL)]
    if with_ctx:
        sets.append(("C", 2, 32, SEQ, affC))
    with contextlib.ExitStack() as st:
        sb = lambda n, s, d=F32: st.enter_context(nc.sbuf_tensor(n, s, d))
        pt = lambda n, s, d=F32: st.enter_context(nc.psum_tensor(n, s, d))
        cst = sb("cst", [128, 930])
        identb = sb("identb", [128, 128], BF16)
        zt = sb("zt", [128, D])
        ring = [sb(f"ring{i}", [128, 16, 512], BF16) for i in range(4)]
        xg = [sb(f"xg{i}", [128, D], BF16) for i in range(2)]
        xgT = sb("xgT", [128, 16, 544], BF16)
        hT = sb("hT", [128, 16, 544], BF16)
        sil = sb("sil", [128, 544])
        yb = [sb(f"yb{i}", [128, 512]) for i in range(3)]
        S = {}
        for (nm, C, k, r0, _) in sets:
            S[nm] = dict(
                aff=sb(f"saff{nm}", [128, C, NEL]), lo=sb(f"lo{nm}", [128, NEL]), th=sb(f"th{nm}", [128, NEL, 15]),
                cmp=sb(f"cmp{nm}", [128, C, NEL, 15]), cntp=sb(f"cntp{nm}", [128, NEL, 15]), ge=sb(f"ge{nm}", [128, NEL, 15]),
                nge=sb(f"nge{nm}", [128, NEL]), mask=sb(f"mask{nm}", [128, C, NEL]), maskb=sb(f"maskb{nm}", [128, C, NEL], BF16),
                cA=sb(f"cA{nm}", [128, C, NEL]), cB=sb(f"cB{nm}", [128, C, NEL]), pos=sb(f"pos{nm}", [128, C, NEL]),
                vals=sb(f"vals{nm}", [128, C, NEL, 2]), oh=[sb(f"oh{nm}{i}", [128, k]) for i in range(2)],
                sl=sb(f"sl{nm}", [128, NEL, 4, 2]), idx=sb(f"idx{nm}", [128, NEL, 4], I32), gate=sb(f"gate{nm}", [128, NEL, 4]))
        jt = sb("jt", [128, NEL, 15])
        trib = sb("trib", [128, 128], BF16)
        onesb = sb("onesb", [128, 128], BF16)
        psA = [pt(f"psA{i}", [128, 512]) for i in range(2)]
        psU = [pt(f"psU{i}", [128, 512]) for i in range(2)]
        psT = pt("psT", [128, 16, 128], BF16)
        psY = [pt(f"psY{i}", [128, 512]) for i in range(2)]
        ident = cst[:, 0:128]
        ones = cst[:, 128:256]
        tri = cst[:, 256:384]
        iota = cst[:, 384:896]
        tokid = cst[:, 896:930]
        P = Prog(nc)
        P.dma(lambda e: e.dma_start(out=cst[:], in_=consts), w=["cst"])
        P.v(lambda e: e.tensor_copy(out=identb[:], in_=ident), ["cst"], ["identb"])
        P.v(lambda e: e.tensor_copy(out=trib[:], in_=tri), ["cst"], ["trib"])
        P.v(lambda e: e.tensor_copy(out=onesb[:], in_=ones), ["cst"], ["onesb"])
        for j in range(15):
            P.v(lambda e, j=j: e.memset(jt[:, :, j:j + 1], float(j + 1)), [], ["jt"])
        P.v(lambda e: e.memset(zt[:], 0.0), [], ["zt"])
        for cb in range(4):
            for r in range(NROW // 128):
                P.dma(lambda e, r=r, cb=cb: e.dma_start(out=delta[cb][r * 128:(r + 1) * 128, :], in_=zt[:, 0:512]), ["zt"], [("delta", cb)])

        def select(nm, C, k, r0, affd):
            s = S[nm]
            kk = lambda x: (nm, x)
            P.dma(lambda e, s=s, affd=affd: e.dma_start(out=s["aff"][:], in_=affd), w=[kk("aff")])
            P.v(lambda e, s=s: e.memset(s["lo"][:], 0.0), [], [kk("lo")])
            affb = s["aff"][:].unsqueeze(3).to_broadcast([128, C, NEL, 15])
            for it in range(8):
                step = 16.0 ** -(it + 1)
                P.v(lambda e, s=s, step=step: e.scalar_tensor_tensor(out=s["th"][:], in0=jt[:], scalar=step,
                                                                     in1=s["lo"][:].unsqueeze(2).to_broadcast([128, NEL, 15]),
                                                                     op0=ALU.mult, op1=ALU.add), ["jt", kk("lo")], [kk("th")])
                P.v(lambda e, s=s: e.tensor_tensor(out=s["cmp"][:], in0=affb, in1=s["th"][:].unsqueeze(1).to_broadcast([128, C, NEL, 15]),
                                                   op=ALU.is_ge), [kk("aff"), kk("th")], [kk("cmp")])
                P.v(lambda e, s=s: e.tensor_reduce(out=s["cntp"][:], in_=s["cmp"][:].rearrange("p c e j -> p e j c"), axis=AX.X, op=ALU.add),
                    [kk("cmp")], [kk("cntp")])
                P.t(lambda e, s=s: e.matmul(psY[0][:, 0:NEL * 15], lhsT=ones, rhs=s["cntp"][:].rearrange("p e j -> p (e j)"), start=True, stop=True),
                    ["cst", kk("cntp")], [("psY", 0)])
                P.v(lambda e, s=s, k=k: e.tensor_scalar(out=s["ge"][:].rearrange("p e j -> p (e j)"), in0=psY[0][:, 0:NEL * 15], scalar1=float(k) - 0.5,
                                                        scalar2=None, op0=ALU.is_ge), [("psY", 0)], [kk("ge")])
                P.v(lambda e, s=s: e.tensor_reduce(out=s["nge"][:], in_=s["ge"][:], axis=AX.X, op=ALU.add), [kk("ge")], [kk("nge")])
                P.v(lambda e, s=s, step=step: e.scalar_tensor_tensor(out=s["lo"][:], in0=s["nge"][:], scalar=step, in1=s["lo"][:],
                                                                     op0=ALU.mult, op1=ALU.add), [kk("nge"), kk("lo")], [kk("lo")])
            P.v(lambda e, s=s: e.tensor_tensor(out=s["mask"][:], in0=s["aff"][:], in1=s["lo"][:].unsqueeze(1).to_broadcast([128, C, NEL]),
                                               op=ALU.is_ge), [kk("aff"), kk("lo")], [kk("mask")])
            P.v(lambda e, s=s: e.tensor_copy(out=s["maskb"][:], in_=s["mask"][:]), [kk("mask")], [kk("maskb")])
            mb2 = s["maskb"][:].rearrange("p c e -> p (c e)")
            P.t(lambda e: e.matmul(psY[0][:, 0:C * NEL], lhsT=trib[:], rhs=mb2, start=True, stop=True), ["trib", kk("maskb")], [("psY", 0)])
            P.t(lambda e: e.matmul(psY[1][:, 0:C * NEL], lhsT=onesb[:], rhs=mb2, start=True, stop=True), ["onesb", kk("maskb")], [("psY", 1)])
            P.v(lambda e, s=s: e.tensor_copy(out=s["cA"][:].rearrange("p c e -> p (c e)"), in_=psY[1][:, 0:C * NEL]), [("psY", 1)], [kk("cA")])
            cur, nxt = "cA", "cB"
            sh = 1
            while sh < C:
                P.v(lambda e, s=s, cur=cur, nxt=nxt, sh=sh: e.tensor_copy(out=s[nxt][:, 0:sh, :], in_=s[cur][:, 0:sh, :]), [kk(cur)], [kk(nxt)])
                P.v(lambda e, s=s, cur=cur, nxt=nxt, sh=sh: e.tensor_tensor(out=s[nxt][:, sh:C, :], in0=s[cur][:, sh:C, :], in1=s[cur][:, 0:C - sh, :],
                                                                          op=ALU.add), [kk(cur)], [kk(nxt)])
                cur, nxt = nxt, cur
                sh *= 2
            P.v(lambda e, s=s, cur=cur: e.tensor_tensor(out=s["pos"][:].rearrange("p c e -> p (c e)"), in0=s[cur][:].rearrange("p c e -> p (c e)"),
                                                        in1=psY[1][:, 0:C * NEL], op=ALU.subtract), [kk(cur), ("psY", 1)], [kk("pos")])
            P.v(lambda e, s=s: e.tensor_tensor(out=s["pos"][:].rearrange("p c e -> p (c e)"), in0=s["pos"][:].rearrange("p c e -> p (c e)"),
                                               in1=psY[0][:, 0:C * NEL], op=ALU.add), [kk("pos"), ("psY", 0)], [kk("pos")])
            P.v(lambda e, s=s: e.tensor_tensor(out=s["pos"][:], in0=s["pos"][:], in1=s["mask"][:], op=ALU.mult), [kk("pos"), kk("mask")], [kk("pos")])
            P.v(lambda e, s=s: e.tensor_scalar(out=s["pos"][:], in0=s["pos"][:], scalar1=-1.0, scalar2=None, op0=ALU.add), [kk("pos")], [kk("pos")])
            P.v(lambda e, s=s, r0=r0: e.tensor_scalar(out=s["vals"][:, :, :, 0], in0=tokid[:, 0:C].unsqueeze(2).to_broadcast([128, C, NEL]),
                                                      scalar1=float(r0), scalar2=None, op0=ALU.add), ["cst"], [kk("vals")])
            P.v(lambda e, s=s: e.tensor_copy(out=s["vals"][:, :, :, 1], in_=s["aff"][:]), [kk("aff")], [kk("vals")])
            nst = (k + 127) // 128
            sw = min(k, 128)
            n_oh = 0
            for el in range(NEL):
                for c in range(C):
                    ohb = s["oh"][n_oh % 2]
                    kb = (nm, "oh", n_oh % 2)
                    n_oh += 1
                    P.v(lambda e, ohb=ohb, s=s, c=c, el=el, k=k: e.tensor_scalar(out=ohb[:], in0=iota[:, 0:k], scalar1=s["pos"][:, c, el:el + 1],
                                                                                scalar2=None, op0=ALU.is_equal), ["cst", kk("pos")], [kb])
                    for sti in range(nst):
                        P.t(lambda e, ohb=ohb, s=s, c=c, el=el, sti=sti, sw=sw: e.matmul(
                            psU[0][0:sw, (el * 4 + sti) * 2:(el * 4 + sti) * 2 + 2], lhsT=ohb[:, sti * 128:sti * 128 + sw],
                            rhs=s["vals"][:, c, el, :], start=(c == 0 and el == 0 and sti == 0), stop=(c == C - 1)),
                            [kb, kk("vals")], [("psU", 0)])
            P.v(lambda e, s=s, sw=sw: e.tensor_copy(out=s["sl"][0:sw].rearrange("p e s t -> p (e s t)"), in_=psU[0][0:sw, 0:NEL * 8]), [("psU", 0)], [kk("sl")])
            P.v(lambda e, s=s, sw=sw: e.tensor_copy(out=s["idx"][0:sw], in_=s["sl"][0:sw, :, :, 0]), [kk("sl")], [kk("idx")])
            P.v(lambda e, s=s, sw=sw: e.tensor_copy(out=s["gate"][0:sw], in_=s["sl"][0:sw, :, :, 1]), [kk("sl")], [kk("gate")])

        for sargs in sets:
            select(*sargs)

        tiles = [("L", sti, 128, sti * 128) for sti in range(4)]
        if with_ctx:
            tiles.append(("C", 0, 32, 512))
        ncols = 544 if with_ctx else 512
        ring_n = [0]
        ny = [0]

        def load_unit(wsrc, el, cb):
            rb = ring_n[0] % 4
            ring_n[0] += 1
            for k4 in range(4):
                src = wsrc[el, k4 * 512:(k4 + 1) * 512, cb * 512:(cb + 1) * 512].rearrange("(kc p) n -> p kc n", p=128)
                P.dma(lambda e, rb=rb, k4=k4, src=src: e.dma_start(out=ring[rb][:, k4 * 4:(k4 + 1) * 4, :], in_=src), w=[("ring", rb)], q="gpsimd")
            return rb

        for el in range(NEL):
            for ti, (nm, sti, n, c0) in enumerate(tiles):
                s = S[nm]
                xb = xg[ti % 2]
                kx = ("xg", ti % 2)
                P.op("gpsimd", lambda e, xb=xb, s=s, el=el, sti=sti, n=n: e.indirect_dma_start(
                    out=xb[0:n, :], out_offset=None, in_=hx2[:, :],
                    in_offset=bass.IndirectOffsetOnAxis(ap=s["idx"][0:n, el, sti:sti + 1], axis=0)), [(nm, "idx")], [kx], dma=True)
                for kc in range(16):
                    P.t(lambda e, xb=xb, kc=kc, n=n: e.transpose(psT[:, kc, 0:n], xb[0:n, kc * 128:(kc + 1) * 128], identb[0:n, 0:n]), [kx, "identb"], ["psT"])
                P.a(lambda e, c0=c0, n=n: e.activation(out=xgT[:, :, c0:c0 + n], in_=psT[:, :, 0:n], func=AF.Copy), ["psT"], ["xgT"])
            for cb in range(4):
                rg = load_unit(wg, el, cb)
                ru = load_unit(wu, el, cb)
                for f4 in range(4):
                    fc = cb * 4 + f4
                    pa = psA[fc % 2]
                    pu = psU[fc % 2]
                    for (pp, rbuf, key) in ((pa, rg, "psA"), (pu, ru, "psU")):
                        for kc in range(16):
                            P.t(lambda e, pp=pp, rbuf=rbuf, kc=kc, f4=f4: e.matmul(pp[:, 0:512], lhsT=ring[rbuf][:, kc, f4 * 128:(f4 + 1) * 128],
                                                                                  rhs=xgT[:, kc, 0:512], start=(kc == 0), stop=(kc == 15)),
                                [("ring", rbuf), "xgT"], [(key, fc % 2)])
                    P.a(lambda e, pa=pa: e.activation(out=sil[:, 0:512], in_=pa[:, 0:512], func=AF.Silu), [("psA", fc % 2)], ["sil"])
                    P.v(lambda e, pu=pu, fc=fc: e.tensor_tensor(out=hT[:, fc, 0:512], in0=sil[:, 0:512], in1=pu[:, 0:512], op=ALU.mult),
                        ["sil", ("psU", fc % 2)], ["hT"])
                    if with_ctx:
                        for (pp, rbuf, key) in ((pa, rg, "psA"), (pu, ru, "psU")):
                            for kc in range(16):
                                P.t(lambda e, pp=pp, rbuf=rbuf, kc=kc, f4=f4: e.matmul(pp[:, 0:32], lhsT=ring[rbuf][:, kc, f4 * 128:(f4 + 1) * 128],
                                                                                      rhs=xgT[:, kc, 512:544], start=(kc == 0), stop=(kc == 15)),
                                    [("ring", rbuf), "xgT"], [(key, fc % 2)])
                        P.a(lambda e, pa=pa: e.activation(out=sil[:, 512:544], in_=pa[:, 0:32], func=AF.Silu), [("psA", fc % 2)], ["sil"])
                        P.v(lambda e, pu=pu, fc=fc: e.tensor_tensor(out=hT[:, fc, 512:544], in0=sil[:, 512:544], in1=pu[:, 0:32], op=ALU.mult),
                            ["sil", ("psU", fc % 2)], ["hT"])
            for cb in range(4):
                rd = load_unit(wd, el, cb)
                for ti, (nm, sti, n, c0) in enumerate(tiles):
                    s = S[nm]
                    py = psY[ny[0] % 2]
                    ky = ("psY", ny[0] % 2)
                    ybuf = yb[ny[0] % 3]
                    kyb = ("yb", ny[0] % 3)
                    ny[0] += 1
                    for fc in range(16):
                        P.t(lambda e, py=py, rd=rd, fc=fc, c0=c0, n=n: e.matmul(py[0:n, :], lhsT=hT[:, fc, c0:c0 + n], rhs=ring[rd][:, fc, :],
                                                                               start=(fc == 0), stop=(fc == 15)), ["hT", ("ring", rd)], [ky])
                    P.v(lambda e, py=py, ybuf=ybuf, s=s, el=el, sti=sti, n=n: e.tensor_scalar(out=ybuf[0:n, :], in0=py[0:n, :],
                                                                                              scalar1=s["gate"][0:n, el, sti:sti + 1], scalar2=None, op0=ALU.mult),
                        [ky, (nm, "gate")], [kyb])
                    P.op("gpsimd", lambda e, ybuf=ybuf, s=s, el=el, sti=sti, n=n, cb=cb: e.indirect_dma_start(
                        out=delta[cb][:, :], out_offset=bass.IndirectOffsetOnAxis(ap=s["idx"][0:n, el, sti:sti + 1], axis=0),
                        in_=ybuf[0:n, :], in_offset=None, compute_op=ALU.add), [kyb, (nm, "idx"), ("delta", cb)], [("delta", cb)], dma=True)
        P.emit()
    return nc


def s3_consts():
    c = np.zeros((128, 930), np.float32)
    c[:, 0:128] = np.eye(128)
    c[:, 128:256] = 1.0
    c[:, 256:384] = np.triu(np.ones((128, 128)))
    c[:, 384:896] = np.arange(512)[None, :]
    c[:, 896:930] = np.arange(34)[None, :] * 128 + np.arange(128)[:, None]
    return c


def build_s4():
    nc = bass.Bass("TRN2", target_bir_lowering=False)
    di = lambda n, s, d=F32: nc.dram_tensor(n, s, d, kind="ExternalInput").ap()
    x1 = di("x1", [NTOK, D])
    dA = di("dA", [NTOK, D])
    dB = di("dB", [NTOK, D])
    modx = di("modx", [6, D])
    modc = di("modc", [6, D])
    x2 = nc.dram_tensor("x2", [NTOK, D], F32, kind="ExternalOutput").ap()
    with contextlib.ExitStack() as st:
        sb = lambda n, s, d=F32: st.enter_context(nc.sbuf_tensor(n, s, d))
        G = sb("G", [128, D])
        xa = [sb(f"xa{i}", [128, D]) for i in range(2)]
        da = [sb(f"da{i}", [128, D]) for i in range(2)]
        db = [sb(f"db{i}", [128, D]) for i in range(2)]
        P = Prog(nc)
        for t in range(NT):
            i = t % 2
            if t == 0:
                P.dma(lambda e: e.dma_start(out=G[:], in_=_bc(modc[5], D)), w=["G"])
            if t == 2:
                P.dma(lambda e: e.dma_start(out=G[:], in_=_bc(modx[5], D)), w=["G"])
            r0 = t * 128
            P.dma(lambda e, i=i, r0=r0: e.dma_start(out=xa[i][:], in_=x1[r0:r0 + 128, :]), w=[("xa", i)])
            P.dma(lambda e, i=i, r0=r0: e.dma_start(out=da[i][:], in_=dA[r0:r0 + 128, :]), w=[("da", i)])
            P.dma(lambda e, i=i, r0=r0: e.dma_start(out=db[i][:], in_=dB[r0:r0 + 128, :]), w=[("db", i)])
            P.v(lambda e, i=i: e.tensor_tensor(out=da[i][:], in0=da[i][:], in1=db[i][:], op=ALU.add), [("da", i), ("db", i)], [("da", i)])
            P.v(lambda e, i=i: e.tensor_tensor(out=da[i][:], in0=da[i][:], in1=G[:], op=ALU.mult), [("da", i), "G"], [("da", i)])
            P.v(lambda e, i=i: e.tensor_tensor(out=xa[i][:], in0=xa[i][:], in1=da[i][:], op=ALU.add), [("xa", i), ("da", i)], [("xa", i)])
            P.dma(lambda e, i=i, r0=r0: e.dma_start(out=x2[r0:r0 + 128, :], in_=xa[i][:]), [("xa", i)], [("x2", t)])
        P.emit()
    return nc


def _rope_tables(h):
    t = np.arange(h * 2048, (h + 1) * 2048)
    row = (t // 64).astype(np.float32)
    col = (t % 64).astype(np.float32)
    inv = (np.float32(10000.0) ** (-np.arange(32, dtype=np.float32) / np.float32(32))).astype(np.float32)
    ar = row[:, None] * inv
    ac = col[:, None] * inv
    tab = np.zeros((NTOK, 128), np.float32)
    tab[:256, :64] = 1.0
    tab[256:, :64] = np.concatenate([np.cos(ar), np.cos(ac)], 1)
    tab[256:, 64:] = np.concatenate([np.sin(ar), np.sin(ac)], 1)
    return tab


def _masks(h):
    s = np.arange(128)[:, None]
    q = np.arange(128)[None, :]
    mp = (s >= q).astype(np.float32)
    mn = (s <= q).astype(np.float32)
    return np.stack([mp, mn, mp * (0.0 if h == 0 else 1.0), mn * (0.0 if h == 1 else 1.0)])


def _run(nc, in_maps):
    res = run_bass_kernel_spmd(nc, in_maps, core_ids=list(range(8)))
    return res.results


def kernel(x, c, ctx, c_ctx, w_mod, b_mod, norm1_g, norm2_g, w_in, qn_a, kn_a, sink_a, vn_b,
           w_s, b_s, qn_c, kn_c, out_g, w_out, w_router, w_gate, w_up, w_down):
    f32 = np.float32
    x = np.asarray(x, f32)
    ctx = np.asarray(ctx, f32)
    mod = run_mod(np.asarray(c, f32), np.asarray(c_ctx, f32), np.asarray(w_mod, f32), np.asarray(b_mod, f32))
    cores = [(k // 2, k % 2) for k in range(8)]
    xin = [np.ascontiguousarray(np.concatenate([ctx[b], x[b, h * 2048:(h + 1) * 2048]], 0)) for (b, h) in cores]
    ident = np.eye(128, dtype=f32)
    consts = s3_consts()
    ropes = [_rope_tables(h) for h in range(2)]
    masks = [_masks(h) for h in range(2)]
    zK = None
    for L in range(2):
        modx = [np.ascontiguousarray(mod[L, b].reshape(6, D)) for (b, h) in cores]
        modc = np.ascontiguousarray(mod[L, 4].reshape(6, D))
        win = np.ascontiguousarray(np.asarray(w_in[L], f32)[:, WIN_PERM])
        gqk = np.concatenate([np.tile(qn_a[L], 6), np.tile(kn_a[L], 2), np.tile(qn_c[L], 6), np.tile(kn_c[L], 2)]).astype(f32)
        wsT = np.ascontiguousarray(np.asarray(w_s[L], f32).transpose(2, 0, 1))
        bsT = np.ascontiguousarray(np.asarray(b_s[L], f32).T)
        ims = [{"xin": xin[k], "modx": modx[k], "modc": modc, "n1g": np.asarray(norm1_g[L], f32), "win": win, "gqk": gqk,
                "cs_t": ropes[cores[k][1]], "vnb": np.asarray(vn_b[L], f32), "wsT": wsT, "bsT": bsT,
                "ogb": np.ascontiguousarray(np.asarray(out_g[L], f32)[768:1280]), "ident": ident} for k in range(8)]
        r1 = _run(build_s1(), ims)
        ims = []
        for k, (b, h) in enumerate(cores):
            me = r1[k]
            pa = r1[2 * b + (1 - h)]
            Q, V = me["QKT"], me["Vout"]
            Qp, Vp = pa["QKT"], pa["Vout"]
            zk = np.zeros_like(Q[:, 6:8, 0:128])
            zv = np.zeros_like(V[0:128, 0:256])
            if h == 0:
                KA = np.concatenate([Q[:, 6:8, 0:256], zk, Q[:, 6:8, 256:], Qp[:, 6:8, 256:384]], 2)
                VA = np.concatenate([V[0:256, 0:256], zv, V[256:, 0:256], Vp[256:384, 0:256]], 0)
                KC = np.concatenate([Q[:, 14:16, :], Qp[:, 14:16, 256:]], 2)
                VC = np.concatenate([V[:, 256:512], Vp[256:, 256:512]], 0)
            else:
                KA = np.concatenate([Q[:, 6:8, 0:256], Qp[:, 6:8, 2176:2304], Q[:, 6:8, 256:], zk], 2)
                VA = np.concatenate([V[0:256, 0:256], Vp[2176:2304, 0:256], V[256:, 0:256], zv], 0)
                KC = np.concatenate([Q[:, 14:16, 0:256], Qp[:, 14:16, 256:], Q[:, 14:16, 256:]], 2)
                VC = np.concatenate([V[0:256, 256:512], Vp[256:, 256:512], V[256:, 256:512]], 0)
            ims.append({"QKT": Q, "KA": np.ascontiguousarray(KA), "VA": np.ascontiguousarray(VA), "KC": np.ascontiguousarray(KC),
                        "VC": np.ascontiguousarray(VC), "msk": masks[h], "sink": np.asarray(sink_a[L], f32),
                        "gac": np.concatenate([np.asarray(out_g[L], f32)[:768], np.asarray(out_g[L], f32)[1280:]])})
        r2a = _run(build_s2a(), ims)
        ims = [{"yAC": r2a[k]["yAC"], "yB": r1[k]["yB"], "xin": xin[k], "modx": modx[k], "modc": modc,
                "wout": np.asarray(w_out[L], f32), "n2g": np.asarray(norm2_g[L], f32), "wr": np.asarray(w_router[L], f32),
                "ident": ident} for k in range(8)]
        r2b = _run(build_s2b(), ims)
        del r1, r2a
        with_ctx = (L == 0)
        ims = []
        for k, (b, h) in enumerate(cores):
            a0, a1 = r2b[2 * b]["affT"], r2b[2 * b + 1]["affT"]
            affl = np.concatenate([a0[:, 256:], a1[:, 256:]], 1)[h * 8:(h + 1) * 8]
            affL = np.ascontiguousarray(affl.reshape(8, 32, 128).transpose(2, 1, 0))
            affC = np.ascontiguousarray(a0[h * 8:(h + 1) * 8, 0:256].reshape(8, 2, 128).transpose(2, 1, 0))
            h0, h1 = r2b[2 * b]["hx2o"], r2b[2 * b + 1]["hx2o"]
            hx2 = np.ascontiguousarray(np.concatenate([h0[256:], h1[256:], h0[:256]], 0))
            ims.append({"affL": affL, "affC": affC, "hx2": hx2,
                        "wg": np.asarray(w_gate[L][h * 8:(h + 1) * 8], f32), "wu": np.asarray(w_up[L][h * 8:(h + 1) * 8], f32),
                        "wd": np.asarray(w_down[L][h * 8:(h + 1) * 8], f32), "consts": consts})
        r3 = _run(build_s3(with_ctx), ims)
        del ims
        ims = []
        for k, (b, h) in enumerate(cores):
            def rows(r):
                dl = np.concatenate([r[f"delta{i}"] for i in range(4)], 1)
                return np.ascontiguousarray(np.concatenate([dl[4096:], dl[h * 2048:(h + 1) * 2048]], 0))
            ims.append({"x1": r2b[k]["x1o"], "dA": rows(r3[k]), "dB": rows(r3[2 * b + (1 - h)]), "modx": modx[k], "modc": modc})
        r4 = _run(build_s4(), ims)
        xin = [np.ascontiguousarray(r4[k]["x2"]) for k in range(8)]
        del r2b, r3, r4, ims
    out = np.zeros((4, SEQ, D), f32)
    for k, (b, h) in enumerate(cores):
        out[b, h * 2048:(h + 1) * 2048] = xin[k][256:]
    return out
```

```python
import contextlib
import numpy as np
import ml_dtypes
import concourse.bass as bass
import concourse.mybir as mybir
from concourse.bass_utils import run_bass_kernel_spmd

F32 = mybir.dt.float32
BF16 = mybir.dt.bfloat16
I32 = mybir.dt.int32
AF = mybir.ActivationFunctionType
ALU = mybir.AluOpType
AX = mybir.AxisListType

D = 2048
SEQ = 4096
CTX = 256
NT = 18
NTOK = NT * 128
EPS = 1e-6
NE = 16

ENGS = ("sync", "scalar", "vector", "gpsimd", "tensor")
SEM_CHUNK = 3000
NDMA_SEMS = 12


class _Op:
    __slots__ = ("eng", "fn", "deps", "is_dma", "has_dep", "sem", "val", "prev_dma")

    def __init__(self, eng, fn, is_dma):
        self.eng = eng
        self.fn = fn
        self.is_dma = is_dma
        self.deps = []
        self.has_dep = False
        self.sem = None
        self.val = None
        self.prev_dma = None


class Prog:
    def __init__(self, nc):
        self.nc = nc
        self.ops = {e: [] for e in ENGS}
        self.last_w = {}
        self.readers = {}
        self.ndma = {e: 0 for e in ENGS}
        self.dma_last = {}
        self.all_dmas = []

    def op(self, eng, fn, reads=(), writes=(), dma=False):
        o = _Op(eng, fn, dma)
        deps = []
        for r in reads:
            w = self.last_w.get(r)
            if w is not None:
                deps.append((w, "raw"))
        for k in writes:
            w = self.last_w.get(k)
            if w is not None:
                deps.append((w, "waw"))
            for rd in self.readers.get(k, ()):
                deps.append((rd, "war"))
        for (d, kind) in deps:
            if d is o:
                continue
            if not d.is_dma and not dma and d.eng == eng:
                if kind != "raw" or eng == "tensor":
                    continue
            if d not in o.deps:
                o.deps.append(d)
                d.has_dep = True
        for r in reads:
            self.readers.setdefault(r, []).append(o)
        for k in writes:
            self.last_w[k] = o
            self.readers[k] = []
        if dma:
            j = self.ndma[eng]
            self.ndma[eng] += 1
            slot = (eng, j % NDMA_SEMS)
            o.prev_dma = self.dma_last.get(slot)
            self.dma_last[slot] = o
            o.sem = slot
            o.val = 16 * (j // NDMA_SEMS + 1)
            self.all_dmas.append(o)
        self.ops[eng].append(o)
        return o

    def v(self, fn, r=(), w=()):
        return self.op("vector", fn, r, w)

    def a(self, fn, r=(), w=()):
        return self.op("scalar", fn, r, w)

    def g(self, fn, r=(), w=()):
        return self.op("gpsimd", fn, r, w)

    def t(self, fn, r=(), w=()):
        return self.op("tensor", fn, r, w)

    def dma(self, fn, r=(), w=(), q="sync"):
        return self.op(q, fn, r, w, dma=True)

    def emit(self, final_wait_eng="sync", fused=False):
        nc = self.nc
        sem_names = set()
        for e in ENGS:
            cnt = 0
            for o in self.ops[e]:
                if o.is_dma:
                    sem_names.add(o.sem)
                    continue
                if o.has_dep:
                    o.sem = (e, "c", cnt // SEM_CHUNK)
                    o.val = cnt % SEM_CHUNK + 1
                    sem_names.add(o.sem)
                    cnt += 1
        sem_names = sorted(sem_names, key=str)
        with contextlib.ExitStack() as st:
            sems = {}
            for n in sem_names:
                if fused:
                    _UID[0] += 1
                    sems[n] = nc.alloc_semaphore(name="f%d_" % _UID[0] + "_".join(str(x) for x in n))
                else:
                    sems[n] = st.enter_context(nc.semaphore("s_" + "_".join(str(x) for x in n)))
            block = st.enter_context(nc.Block())
            prog = self

            def run(e, eng):
                seen = {}
                for o in prog.ops[e]:
                    waits = []
                    if o.is_dma and o.prev_dma is not None:
                        waits.append(o.prev_dma)
                    waits.extend(o.deps)
                    for d in waits:
                        if seen.get(d.sem, 0) >= d.val:
                            continue
                        eng.wait_ge(sems[d.sem], d.val)
                        seen[d.sem] = d.val
                    ins = o.fn(eng)
                    if o.is_dma:
                        ins.then_inc(sems[o.sem], 16)
                    elif o.has_dep:
                        ins.then_inc(sems[o.sem], 1)
                if e == final_wait_eng:
                    last = {}
                    for o in prog.all_dmas:
                        last[o.sem] = max(last.get(o.sem, 0), o.val)
                    for s, v in last.items():
                        if seen.get(s, 0) < v:
                            eng.wait_ge(sems[s], v)

            @block.sync
            def _(eng):
                run("sync", eng)

            @block.scalar
            def _(eng):
                run("scalar", eng)

            @block.vector
            def _(eng):
                run("vector", eng)

            @block.gpsimd
            def _(eng):
                run("gpsimd", eng)

            @block.tensor
            def _(eng):
                run("tensor", eng)


_UID = [0]


@contextlib.contextmanager
def stage_ctx(nc, fused):
    if not fused:
        with contextlib.ExitStack() as st:
            yield st
    else:
        with nc.cleanup_on_exit():
            with contextlib.ExitStack() as st:
                yield st
            nc.all_engine_barrier()


def _mk(nc, io, pfx):
    fused = io is not None
    if fused:
        di = lambda n, s, d=F32: io[n]
        do = lambda n, s, d=F32: io[n]
    else:
        di = lambda n, s, d=F32: nc.dram_tensor(n, s, d, kind="ExternalInput").ap()
        do = lambda n, s, d=F32: nc.dram_tensor(n, s, d, kind="ExternalOutput").ap()
    return fused, di, do


def _bc(ap1d, n):
    return ap1d.unsqueeze(0).to_broadcast([128, n])


MCOL = 1536


def build_mod():
    nc = bass.Bass("TRN2", target_bir_lowering=False)
    cin = nc.dram_tensor("cin", [128, 16, 5], F32, kind="ExternalInput").ap()
    w = nc.dram_tensor("w", [2, D, MCOL], F32, kind="ExternalInput").ap()
    b = nc.dram_tensor("b", [2, MCOL], F32, kind="ExternalInput").ap()
    out = nc.dram_tensor("out", [2, 5, MCOL], F32, kind="ExternalOutput").ap()
    with contextlib.ExitStack() as st:
        sb = lambda n, s, d: st.enter_context(nc.sbuf_tensor(n, s, d))
        ct = sb("ct", [128, 16, 5], F32)
        cs = sb("cs", [128, 16, 5], F32)
        wt = [sb(f"wt{i}", [128, 16, 512], F32) for i in range(2)]
        bt = sb("bt", [5, 2, MCOL], F32)
        ot = sb("ot", [5, 2, MCOL], F32)
        ps = [st.enter_context(nc.psum_tensor(f"ps{i}", [5, 512], F32)) for i in range(2)]
        P = Prog(nc)
        P.dma(lambda e: e.dma_start(out=ct[:], in_=cin), w=["ct"])
        for l in range(2):
            P.dma(lambda e, l=l: e.dma_start(out=bt[:, l, :], in_=b[l:l + 1, :].to_broadcast([5, MCOL])), w=[("bt", l)])
        P.a(lambda e: e.activation(out=cs[:], in_=ct[:], func=AF.Silu), ["ct"], ["cs"])
        gi = 0
        for l in range(2):
            for g in range(3):
                buf = gi % 2
                src = w[l, :, g * 512:(g + 1) * 512].rearrange("(kc p) n -> p kc n", p=128)
                P.dma(lambda e, buf=buf, src=src: e.dma_start(out=wt[buf][:], in_=src), w=[("wt", buf)])
                for kc in range(16):
                    P.t(lambda e, buf=buf, kc=kc: e.matmul(ps[buf][:], lhsT=cs[:, kc, :], rhs=wt[buf][:, kc, :],
                                                           start=(kc == 0), stop=(kc == 15)),
                        ["cs", ("wt", buf)], [("ps", buf)])
                P.v(lambda e, buf=buf, l=l, g=g: e.tensor_tensor(out=ot[:, l, g * 512:(g + 1) * 512], in0=ps[buf][:],
                                                                 in1=bt[:, l, g * 512:(g + 1) * 512], op=ALU.add),
                    [("ps", buf), ("bt", l)], [("ot", l, g)])
                gi += 1
        for l in range(2):
            P.dma(lambda e, l=l: e.dma_start(out=out[l], in_=ot[:, l, :]), [("ot", l, g) for g in range(3)], [("out", l)])
        P.emit()
    return nc


def run_mod(c, c_ctx, w_mod, b_mod):
    c_all = np.concatenate([c, c_ctx[None, :]], axis=0).astype(np.float32)
    cin = np.ascontiguousarray(c_all.T.reshape(16, 128, 5).transpose(1, 0, 2))
    nc = build_mod()
    in_maps = [{"cin": cin, "w": np.ascontiguousarray(w_mod[:, :, j * MCOL:(j + 1) * MCOL]),
                "b": np.ascontiguousarray(b_mod[:, j * MCOL:(j + 1) * MCOL])} for j in range(8)]
    res = run_bass_kernel_spmd(nc, in_maps, core_ids=list(range(8)))
    return np.concatenate([r["out"] for r in res.results], axis=2)


def emit_rstd(P, ss, rstd, n, kr, kw):
    P.a(lambda e: e.activation(out=rstd, in_=ss, func=AF.Sqrt, scale=1.0 / n, bias=EPS), kr, kw)
    P.v(lambda e: e.reciprocal(out=rstd, in_=rstd), kw, kw)


NQK = 16
WIN_PERM = np.concatenate([np.arange(0, 1024), np.arange(2304, 3328),
                           np.arange(1024, 1280), np.arange(3328, 3584),
                           np.arange(1280, 2304)])


def build_s1(nc=None, io=None, pfx=""):
    if nc is None:
        nc = bass.Bass("TRN2", target_bir_lowering=False)
    fused, di, do = _mk(nc, io, pfx)
    xin = di("xin", [NTOK, D])
    modx = di("modx", [6, D])
    modc = di("modc", [6, D])
    n1g = di("n1g", [D])
    win = di("win", [D, 3584])
    gqk = di("gqk", [NQK * 128])
    cs_t = di("cs_t", [NTOK, 128])
    vnb = di("vnb", [512])
    wsT = di("wsT", [128, 4, 128])
    bsT = di("bsT", [128, 4])
    ogb = di("ogb", [512])
    ident_in = di("ident", [128, 128])
    QKT = do("QKT", [128, NQK, NTOK], BF16)
    Vout = do("Vout", [NTOK, 512], BF16)
    yB = do("yB", [NTOK, 512], BF16)

    with stage_ctx(nc, fused) as st:
        sb = lambda n, s, d=F32: st.enter_context(nc.sbuf_tensor(pfx + n, s, d))
        pt = lambda n, s, d=F32: st.enter_context(nc.psum_tensor(pfx + n, s, d))
        wbf = sb("wbf", [128, 16, 2048], BF16)
        G1 = sb("G1", [128, D])
        SH = sb("SH", [128, D])
        gn = sb("gn", [128, D])
        xt = [sb(f"xt{i}", [128, D]) for i in range(2)]
        tmp = sb("tmp", [128, D])
        hx = sb("hx", [128, D], BF16)
        hxT = sb("hxT", [128, 16, 128], BF16)
        ident = sb("ident_f", [128, 128])
        identb = sb("identb", [128, 128], BF16)
        ss = sb("ss", [128, 1])
        rs = sb("rs", [128, 1])
        Pb = sb("Pb", [128, 2048])
        gq = sb("gq", [128, NQK, 128])
        ssq = sb("ssq", [128, NQK])
        rsq = sb("rsq", [128, NQK])
        qn = sb("qn", [128, NQK, 2, 2, 32])
        cst = sb("cst", [128, 128])
        rA = sb("rA", [128, NQK, 2, 32])
        rB = sb("rB", [128, NQK, 2, 32])
        qr = sb("qr", [128, NQK, 2, 2, 32], BF16)
        qT = sb("qT", [128, NQK, 128], BF16)
        vb16 = sb("vb16", [128, 512], BF16)
        vng = sb("vng", [128, 512])
        wsb = sb("wsb", [128, 4, 128])
        wsbb = sb("wsbb", [128, 4, 128], BF16)
        bsb = sb("bsb", [128, 4])
        ogbt = sb("ogbt", [128, 512])
        t1 = sb("t1", [128, 1024])
        t2 = sb("t2", [128, 1024])
        gl = sb("gl", [128, 1024])
        ssv = sb("ssv", [128, 4])
        rsv = sb("rsv", [128, 4])
        vn = sb("vn", [128, 4, 128], BF16)
        ob = sb("ob", [128, 512])
        yb = sb("yb", [128, 512], BF16)
        pT = pt("pT", [128, 16, 128], BF16)
        pP = pt("pP", [128, 2048])
        pQ = pt("pQ", [128, NQK, 128], BF16)

        P = Prog(nc)
        P.dma(lambda e: e.dma_start(out=ident[:], in_=ident_in), w=["ident"])
        P.v(lambda e: e.tensor_copy(out=identb[:], in_=ident[:]), ["ident"], ["identb"])
        P.dma(lambda e: e.dma_start(out=gn[:], in_=_bc(n1g, D)), w=["gn"])
        P.dma(lambda e: e.dma_start(out=gq[:].rearrange("p h d -> p (h d)"), in_=_bc(gqk, NQK * 128)), w=["gq"])
        P.dma(lambda e: e.dma_start(out=vng[:], in_=_bc(vnb, 512)), w=["vng"])
        P.dma(lambda e: e.dma_start(out=ogbt[:], in_=_bc(ogb, 512)), w=["ogbt"])
        P.dma(lambda e: e.dma_start(out=wsb[:], in_=wsT), w=["wsb"])
        P.v(lambda e: e.tensor_copy(out=wsbb[:], in_=wsb[:]), ["wsb"], ["wsbb"])
        P.dma(lambda e: e.dma_start(out=bsb[:], in_=bsT), w=["bsb"])

        def load_w(c0, ncol):
            for k4 in range(4):
                src = win[k4 * 512:(k4 + 1) * 512, c0:c0 + ncol].rearrange("(kc p) n -> p kc n", p=128)
                P.dma(lambda e, k4=k4, src=src: e.dma_start(out=wbf[:, k4 * 4:(k4 + 1) * 4, 0:ncol], in_=src),
                      w=["wbf"], q="gpsimd")

        def load_mod(m):
            P.dma(lambda e: e.dma_start(out=SH[:], in_=_bc(m[0], D)), w=["SH"])
            P.dma(lambda e: e.dma_start(out=G1[:], in_=_bc(m[1], D)), w=["G1"])
            P.v(lambda e: e.scalar_tensor_tensor(out=G1[:], in0=G1[:], scalar=1.0, in1=gn[:], op0=ALU.add, op1=ALU.mult),
                ["G1", "gn"], ["G1"])

        def hx_tile(t):
            xb = xt[t % 2]
            kx = ("xt", t % 2)
            P.dma(lambda e: e.dma_start(out=xb[:], in_=xin[t * 128:(t + 1) * 128, :]), w=[kx])
            P.a(lambda e: e.activation(out=tmp[:], in_=xb[:], func=AF.Square, accum_out=ss[:]), [kx], ["tmp", "ss"])
            emit_rstd(P, ss[:], rs[:], D, ["ss"], ["rs"])
            P.v(lambda e: e.scalar_tensor_tensor(out=tmp[:], in0=xb[:], scalar=rs[:, 0:1], in1=G1[:], op0=ALU.mult, op1=ALU.mult),
                [kx, "rs", "G1"], ["tmp"])
            P.v(lambda e: e.tensor_tensor(out=hx[:], in0=tmp[:], in1=SH[:], op=ALU.add), ["tmp", "SH"], ["hx"])
            for kc in range(16):
                P.t(lambda e, kc=kc: e.transpose(pT[:, kc, :], hx[:, kc * 128:(kc + 1) * 128], identb[:]), ["hx", "identb"], ["pT"])
            P.a(lambda e: e.activation(out=hxT[:], in_=pT[:], func=AF.Copy), ["pT"], ["hxT"])

        def inproj(ncol):
            for g in range(ncol // 512):
                for kc in range(16):
                    P.t(lambda e, g=g, kc=kc: e.matmul(pP[:, g * 512:(g + 1) * 512], lhsT=hxT[:, kc, :],
                                                      rhs=wbf[:, kc, g * 512:(g + 1) * 512], start=(kc == 0), stop=(kc == 15)),
                        ["hxT", "wbf"], ["pP"])

        load_w(0, 2048)
        for t in range(NT):
            if t == 0:
                load_mod(modc)
            if t == 2:
                load_mod(modx)
            hx_tile(t)
            inproj(2048)
            P.a(lambda e: e.activation(out=Pb[:], in_=pP[:], func=AF.Copy), ["pP"], ["Pb"])
            Pv = Pb[:].rearrange("p (h d) -> p h d", h=NQK)
            P.dma(lambda e, t=t: e.dma_start(out=cst[:], in_=cs_t[t * 128:(t + 1) * 128, :]), w=["cst"])
            P.v(lambda e: e.tensor_tensor(out=tmp[:], in0=Pb[:], in1=Pb[:], op=ALU.mult), ["Pb"], ["tmp"])
            P.v(lambda e: e.tensor_reduce(out=ssq[:], in_=tmp[:].rearrange("p (h d) -> p h d", h=NQK), axis=AX.X, op=ALU.add),
                ["tmp"], ["ssq"])
            emit_rstd(P, ssq[:], rsq[:], 128, ["ssq"], ["rsq"])
            qnv = qn[:].rearrange("p h a b f -> p h (a b f)")
            P.v(lambda e: e.tensor_tensor(out=qnv, in0=Pv, in1=rsq[:].unsqueeze(2).to_broadcast([128, NQK, 128]), op=ALU.mult),
                ["Pb", "rsq"], ["qn"])
            P.v(lambda e: e.tensor_tensor(out=qnv, in0=qnv, in1=gq[:], op=ALU.mult), ["qn", "gq"], ["qn"])
            cosb = cst[:, 0:64].rearrange("p (a f) -> p a f", a=2).unsqueeze(1).to_broadcast([128, NQK, 2, 32])
            sinb = cst[:, 64:128].rearrange("p (a f) -> p a f", a=2).unsqueeze(1).to_broadcast([128, NQK, 2, 32])
            x1 = qn[:, :, :, 0, :]
            x2 = qn[:, :, :, 1, :]
            P.v(lambda e: e.tensor_tensor(out=rA[:], in0=x1, in1=cosb, op=ALU.mult), ["qn", "cst"], ["rA"])
            P.v(lambda e: e.tensor_tensor(out=rB[:], in0=x2, in1=sinb, op=ALU.mult), ["qn", "cst"], ["rB"])
            P.v(lambda e: e.tensor_tensor(out=qr[:, :, :, 0, :], in0=rA[:], in1=rB[:], op=ALU.subtract), ["rA", "rB"], ["qr"])
            P.v(lambda e: e.tensor_tensor(out=rA[:], in0=x2, in1=cosb, op=ALU.mult), ["qn", "cst", "qr"], ["rA"])
            P.v(lambda e: e.tensor_tensor(out=rB[:], in0=x1, in1=sinb, op=ALU.mult), ["qn", "cst", "qr"], ["rB"])
            P.v(lambda e: e.tensor_tensor(out=qr[:, :, :, 1, :], in0=rA[:], in1=rB[:], op=ALU.add), ["rA", "rB"], ["qr"])
            qrv = qr[:].rearrange("p h a b f -> p h (a b f)")
            for h in range(NQK):
                P.t(lambda e, h=h: e.transpose(pQ[:, h, :], qrv[:, h, :], identb[:]), ["qr", "identb"], ["pQ"])
            P.a(lambda e: e.activation(out=qT[:], in_=pQ[:], func=AF.Copy), ["pQ"], ["qT"])
            P.dma(lambda e, t=t: e.dma_start(out=QKT[:, :, t * 128:(t + 1) * 128], in_=qT[:]), ["qT"], [("QKT", t)])
            if fused and t >= 2:
                ktv = io["KTx"].rearrange("(h d) t -> d h t", d=128)
                c0 = (t - 2) * 128
                P.dma(lambda e, c0=c0: e.dma_start(out=ktv[:, 0:2, c0:c0 + 128], in_=qT[:, 6:8, :]), ["qT"], [("KTx", t, 0)])
                P.dma(lambda e, c0=c0: e.dma_start(out=ktv[:, 2:4, c0:c0 + 128], in_=qT[:, 14:16, :]), ["qT"], [("KTx", t, 1)])

        load_w(2048, 1536)
        for t in range(NT):
            if t == 0:
                load_mod(modc)
            if t == 2:
                load_mod(modx)
            hx_tile(t)
            inproj(1536)
            P.a(lambda e: e.activation(out=vb16[:], in_=pP[:, 0:512], func=AF.Copy), ["pP"], ["vb16"])
            P.dma(lambda e, t=t: e.dma_start(out=Vout[t * 128:(t + 1) * 128, :], in_=vb16[:]), ["vb16"], [("Vout", t)])
            if fused and t >= 2:
                P.dma(lambda e, t=t: e.dma_start(out=io["Vx"][(t - 2) * 128:(t - 1) * 128, :], in_=vb16[:]), ["vb16"], [("Vx", t)])
            P.a(lambda e: e.activation(out=gl[:], in_=pP[:, 512:1536], func=AF.Gelu_apprx_tanh), ["pP"], ["gl"])
            gv = gl[:, 512:1024]
            P.v(lambda e: e.tensor_tensor(out=t1[:, 0:512], in0=gv, in1=gv, op=ALU.mult), ["gl"], ["t1"])
            P.v(lambda e: e.tensor_reduce(out=ssv[:], in_=t1[:, 0:512].rearrange("p (g d) -> p g d", g=4), axis=AX.X, op=ALU.add),
                ["t1"], ["ssv"])
            emit_rstd(P, ssv[:], rsv[:], 128, ["ssv"], ["rsv"])
            P.v(lambda e: e.tensor_tensor(out=t1[:, 0:512].rearrange("p (g d) -> p g d", g=4), in0=gv.rearrange("p (g d) -> p g d", g=4),
                                          in1=rsv[:].unsqueeze(2).to_broadcast([128, 4, 128]), op=ALU.mult), ["gl", "rsv"], ["t1"])
            P.v(lambda e: e.tensor_tensor(out=vn[:].rearrange("p g d -> p (g d)"), in0=t1[:, 0:512], in1=vng[:], op=ALU.mult),
                ["t1", "vng"], ["vn"])
            for g in range(4):
                P.t(lambda e, g=g: e.matmul(pP[:, 1536 + g * 128:1536 + (g + 1) * 128], lhsT=wsbb[:, g, :], rhs=vn[:, g, :],
                                            start=True, stop=True), ["vn", "wsbb"], ["pM"])
            P.v(lambda e: e.tensor_tensor(out=ob[:].rearrange("p (g d) -> p g d", g=4),
                                          in0=pP[:, 1536:2048].rearrange("p (g d) -> p g d", g=4),
                                          in1=bsb[:].unsqueeze(2).to_broadcast([128, 4, 128]), op=ALU.add), ["pM", "bsb"], ["ob"])
            P.v(lambda e: e.tensor_tensor(out=ob[:], in0=ob[:], in1=gl[:, 0:512], op=ALU.mult), ["ob", "gl"], ["ob"])
            P.a(lambda e: e.activation(out=t2[:, 0:512], in_=ob[:], func=AF.Square, accum_out=ss[:]), ["ob"], ["t2", "ss"])
            emit_rstd(P, ss[:], rs[:], 512, ["ss"], ["rs"])
            P.v(lambda e: e.scalar_tensor_tensor(out=yb[:], in0=ob[:], scalar=rs[:, 0:1], in1=ogbt[:], op0=ALU.mult, op1=ALU.mult),
                ["ob", "rs", "ogbt"], ["yb"])
            P.dma(lambda e, t=t: e.dma_start(out=yB[t * 128:(t + 1) * 128, :], in_=yb[:]), ["yb"], [("yB", t)])
        P.emit(fused=fused)
    return nc


NKA = 20
NKC = 34


def build_s2a(nc=None, io=None, pfx=""):
    if nc is None:
        nc = bass.Bass("TRN2", target_bir_lowering=False)
    fused, di, do = _mk(nc, io, pfx)
    QKT = di("QKT", [128, NQK, NTOK], BF16)
    KA = None if fused else di("KA", [128, 2, NKA * 128], BF16)
    VA = None if fused else di("VA", [NKA * 128, 256], BF16)
    KC = None if fused else di("KC", [128, 2, NKC * 128], BF16)
    VC = None if fused else di("VC", [NKC * 128, 256], BF16)
    msk = di("msk", [4, 128, 128])
    sink = di("sink", [6])
    gac = di("gac", [1536])
    yAC = do("yAC", [NTOK, 1536], BF16)
    with stage_ctx(nc, fused) as st:
        sb = lambda n, s, d=F32: st.enter_context(nc.sbuf_tensor(pfx + n, s, d))
        pt = lambda n, s, d=F32: st.enter_context(nc.psum_tensor(pfx + n, s, d))
        ka = sb("ka", [128, 2, NKA * 128], BF16)
        va = sb("va", [128, NKA, 2, 129], BF16)
        kc = sb("kc", [128, 2, NKC * 128], BF16)
        vc = sb("vc", [128, NKC, 2, 129], BF16)
        mf = sb("mf", [128, 4, 128])
        mb = sb("mb", [128, 4, 128], BF16)
        sk = sb("sk", [128, 6])
        es = sb("es", [128, 6])
        gt = sb("gt", [128, 1536])
        qt = [sb(f"qt{i}", [128, NQK, 128], BF16) for i in range(2)]
        PT = [sb(f"PT{i}", [128, 3, 128], BF16) for i in range(2)]
        oo = sb("oo", [128, 2, 6, 128])
        den = sb("den", [128, 3])
        sq = sb("sq", [128, 768])
        ss = sb("ss", [128, 1])
        rs = sb("rs", [128, 1])
        yt = sb("yt", [128, 1536], BF16)
        psS = [pt(f"psS{i}", [128, 512]) for i in range(2)]
        psO = [pt(f"psO{i}", [128, 3, 129]) for i in range(2)]
        P = Prog(nc)
        if not fused:
            P.dma(lambda e: e.dma_start(out=ka[:], in_=KA), w=["ka"])
            P.dma(lambda e: e.dma_start(out=kc[:], in_=KC), w=["kc"])
        else:
            KTg = io["KTg"].rearrange("(r h d) t -> d r h t", r=2, h=4)
            Vg = io["Vg"]
            Vo = io["Vout"]
            for k in range(2):
                P.dma(lambda e, k=k: e.dma_start(out=ka[:, k, 0:256], in_=QKT[:, 6 + k, 0:256]), w=["ka"])
                P.dma(lambda e, k=k: e.dma_start(out=ka[:, k, 256:384], in_=KTg[:, 0, k, 1920:2048]), w=["ka"])
                P.dma(lambda e, k=k: e.dma_start(out=ka[:, k, 384:2432], in_=QKT[:, 6 + k, 256:2304]), w=["ka"])
                P.dma(lambda e, k=k: e.dma_start(out=ka[:, k, 2432:2560], in_=KTg[:, 1, k, 0:128]), w=["ka"])
                P.dma(lambda e, k=k: e.dma_start(out=kc[:, k, 0:256], in_=QKT[:, 14 + k, 0:256]), w=["kc"])
                for r in range(2):
                    P.dma(lambda e, k=k, r=r: e.dma_start(out=kc[:, k, 256 + r * 2048:256 + (r + 1) * 2048], in_=KTg[:, r, 2 + k, :]), w=["kc"])
        P.v(lambda e: e.memset(va[:, :, :, 128:129], 1.0), [], ["va1"])
        P.v(lambda e: e.memset(vc[:, :, :, 128:129], 1.0), [], ["vc1"])
        for k in range(2):
            if not fused:
                P.dma(lambda e, k=k: e.dma_start(out=va[:, :, k, 0:128], in_=VA[:, k * 128:(k + 1) * 128].rearrange("(j s) d -> s j d", s=128)), w=[("va", k)])
                P.dma(lambda e, k=k: e.dma_start(out=vc[:, :, k, 0:128], in_=VC[:, k * 128:(k + 1) * 128].rearrange("(j s) d -> s j d", s=128)), w=[("vc", k)])
            else:
                ca = slice(k * 128, (k + 1) * 128)
                cc = slice(256 + k * 128, 256 + (k + 1) * 128)
                tl = lambda ap: ap.rearrange("(j s) d -> s j d", s=128)
                P.dma(lambda e, k=k, ca=ca: e.dma_start(out=va[:, 0:2, k, 0:128], in_=tl(Vo[0:256, ca])), w=[("va", k)])
                P.dma(lambda e, k=k, ca=ca: e.dma_start(out=va[:, 2:3, k, 0:128], in_=tl(Vg[1920:2048, ca])), w=[("va", k)])
                P.dma(lambda e, k=k, ca=ca: e.dma_start(out=va[:, 3:19, k, 0:128], in_=tl(Vo[256:2304, ca])), w=[("va", k)])
                P.dma(lambda e, k=k, ca=ca: e.dma_start(out=va[:, 19:20, k, 0:128], in_=tl(Vg[2048:2176, ca])), w=[("va", k)])
                P.dma(lambda e, k=k, cc=cc: e.dma_start(out=vc[:, 0:2, k, 0:128], in_=tl(Vo[0:256, cc])), w=[("vc", k)])
                P.dma(lambda e, k=k, cc=cc: e.dma_start(out=vc[:, 2:34, k, 0:128], in_=tl(Vg[:, cc])), w=[("vc", k)])
        P.dma(lambda e: e.dma_start(out=mf[:], in_=msk.rearrange("m s q -> s m q")), w=["mf"])
        P.v(lambda e: e.tensor_copy(out=mb[:], in_=mf[:]), ["mf"], ["mb"])
        P.dma(lambda e: e.dma_start(out=sk[:], in_=_bc(sink, 6)), w=["sk"])
        P.a(lambda e: e.activation(out=es[:], in_=sk[:], func=AF.Exp), ["sk"], ["es"])
        P.dma(lambda e: e.dma_start(out=gt[:], in_=_bc(gac, 1536)), w=["gt"])
        cnt = [0]

        def attn(q, qbase, KT, VT, kkey, vkeys, keys, use_sink, oi):
            for k in range(2):
                po = psO[k]
                for n, (j, m) in enumerate(keys):
                    sbuf = cnt[0] % 2
                    cnt[0] += 1
                    pS = psS[sbuf][:, 0:384].rearrange("p (g q) -> p g q", g=3)
                    P.t(lambda e, pS=pS, k=k, j=j: e.matmul(pS, lhsT=KT[:, k, j * 128:(j + 1) * 128],
                                                            rhs=q[:, qbase + 3 * k:qbase + 3 * k + 3, :], start=True, stop=True),
                        [kkey, "qt"], [("psS", sbuf)])
                    P.a(lambda e, pS=pS, sbuf=sbuf: e.activation(out=PT[sbuf][:], in_=pS, func=AF.Exp, scale=128.0 ** -0.5),
                        [("psS", sbuf)], [("PT", sbuf)])
                    if m is not None:
                        P.v(lambda e, sbuf=sbuf, m=m: e.tensor_tensor(out=PT[sbuf][:], in0=PT[sbuf][:],
                                                                     in1=mb[:, m, :].unsqueeze(1).to_broadcast([128, 3, 128]), op=ALU.mult),
                            [("PT", sbuf), "mb"], [("PT", sbuf)])
                    for g in range(3):
                        P.t(lambda e, po=po, sbuf=sbuf, g=g, j=j, k=k, n=n: e.matmul(po[:, g, :], lhsT=PT[sbuf][:, g, :], rhs=VT[:, j, k, :],
                                                                                   start=(n == 0 and g == 0), stop=(n == len(keys) - 1)),
                            [("PT", sbuf)] + vkeys, [("psO", k)])
                if use_sink:
                    P.v(lambda e, po=po, k=k: e.tensor_tensor(out=den[:], in0=po[:, :, 128], in1=es[:, 3 * k:3 * k + 3], op=ALU.add),
                        [("psO", k), "es"], ["den"])
                else:
                    P.v(lambda e, po=po: e.tensor_copy(out=den[:], in_=po[:, :, 128]), [("psO", k)], ["den"])
                P.v(lambda e: e.reciprocal(out=den[:], in_=den[:]), ["den"], ["den"])
                P.v(lambda e, po=po, k=k: e.tensor_tensor(out=oo[:, oi, 3 * k:3 * k + 3, :], in0=po[:, :, 0:128],
                                                          in1=den[:].unsqueeze(2).to_broadcast([128, 3, 128]), op=ALU.mult),
                    [("psO", k), "den"], [("oo", oi)])

        for t in range(NT):
            q = qt[t % 2]
            P.dma(lambda e, q=q, t=t: e.dma_start(out=q[:], in_=QKT[:, :, t * 128:(t + 1) * 128]), w=["qt"])
            if t < 2:
                keysA = [(0, None), (1, None)]
                keysC = [(0, None), (1, None)]
            else:
                i = t - 2
                keysA = [(0, None), (1, None), (2 + i, 2 if i == 0 else 0), (3 + i, None), (4 + i, 3 if i == 15 else 1)]
                keysC = [(j, None) for j in range(NKC)]
            attn(q, 0, ka, va, "ka", [("va", 0), ("va", 1), "va1"], keysA, True, 0)
            attn(q, 8, kc, vc, "kc", [("vc", 0), ("vc", 1), "vc1"], keysC, False, 1)
            for oi in range(2):
                ov = oo[:, oi, :, :].rearrange("p h d -> p (h d)")
                P.a(lambda e, ov=ov: e.activation(out=sq[:], in_=ov, func=AF.Square, accum_out=ss[:]), [("oo", oi)], ["sq", "ss"])
                emit_rstd(P, ss[:], rs[:], 768, ["ss"], ["rs"])
                P.v(lambda e, ov=ov, oi=oi: e.scalar_tensor_tensor(out=yt[:, oi * 768:(oi + 1) * 768], in0=ov, scalar=rs[:, 0:1],
                                                                  in1=gt[:, oi * 768:(oi + 1) * 768], op0=ALU.mult, op1=ALU.mult),
                    [("oo", oi), "rs", "gt"], ["yt"])
            P.dma(lambda e, t=t: e.dma_start(out=yAC[t * 128:(t + 1) * 128, :], in_=yt[:]), ["yt"], [("yAC", t)])
        P.emit(fused=fused)
    return nc


def build_s2b(nc=None, io=None, pfx=""):
    if nc is None:
        nc = bass.Bass("TRN2", target_bir_lowering=False)
    fused, di, do = _mk(nc, io, pfx)
    yAC = di("yAC", [NTOK, 1536], BF16)
    yB = di("yB", [NTOK, 512], BF16)
    xin = di("xin", [NTOK, D])
    modx = di("modx", [6, D])
    modc = di("modc", [6, D])
    wout = di("wout", [D, D])
    n2g = di("n2g", [D])
    wr = di("wr", [D, NE])
    ident_in = di("ident", [128, 128])
    x1o = do("x1o", [NTOK, D])
    hx2o = do("hx2o", [NTOK, D], BF16)
    affT = None if fused else do("affT", [NE, NTOK])
    with stage_ctx(nc, fused) as st:
        sb = lambda n, s, d=F32: st.enter_context(nc.sbuf_tensor(pfx + n, s, d))
        pt = lambda n, s, d=F32: st.enter_context(nc.psum_tensor(pfx + n, s, d))
        wbf = sb("wbf", [128, 16, D], BF16)
        G1g = sb("G1g", [128, D])
        G2 = sb("G2", [128, D])
        SH2 = sb("SH2", [128, D])
        gn = sb("gn", [128, D])
        ident = sb("ident_f", [128, 128])
        identb = sb("identb", [128, 128], BF16)
        wrs = sb("wrs", [128, 16, NE])
        xt = sb("xt", [128, D])
        yt = sb("yt", [128, D], BF16)
        yT = sb("yT", [128, 16, 128], BF16)
        tmp = sb("tmp", [128, D])
        x1 = sb("x1", [128, D])
        h2 = sb("h2", [128, D])
        hb = sb("hb", [128, D], BF16)
        h2T = sb("h2T", [128, 16, 128])
        ss = sb("ss", [128, 1])
        rs = sb("rs", [128, 1])
        mx = sb("mx", [128, 1])
        se = sb("se", [128, 1])
        ex = sb("ex", [128, NE])
        af = sb("af", [128, NE])
        aT = sb("aT", [NE, 128])
        psT = pt("psT", [128, 16, 128], BF16)
        psX = [pt(f"psX{i}", [128, 512]) for i in range(2)]
        psR = [pt(f"psR{i}", [128, 4, 128]) for i in range(2)]
        psL = pt("psL", [128, 512])
        psA = pt("psA", [128, 512])
        P = Prog(nc)
        P.dma(lambda e: e.dma_start(out=ident[:], in_=ident_in), w=["ident"])
        P.v(lambda e: e.tensor_copy(out=identb[:], in_=ident[:]), ["ident"], ["identb"])
        P.dma(lambda e: e.dma_start(out=gn[:], in_=_bc(n2g, D)), w=["gn"])
        P.dma(lambda e: e.dma_start(out=wrs[:], in_=wr.rearrange("(kc p) n -> p kc n", p=128)), w=["wrs"])
        for k4 in range(4):
            src = wout[k4 * 512:(k4 + 1) * 512, :].rearrange("(kc p) n -> p kc n", p=128)
            P.dma(lambda e, k4=k4, src=src: e.dma_start(out=wbf[:, k4 * 4:(k4 + 1) * 4, :], in_=src), w=["wbf"], q="gpsimd")

        def load_mod(m):
            P.dma(lambda e: e.dma_start(out=G1g[:], in_=_bc(m[2], D)), w=["G1g"])
            P.dma(lambda e: e.dma_start(out=SH2[:], in_=_bc(m[3], D)), w=["SH2"])
            P.dma(lambda e: e.dma_start(out=G2[:], in_=_bc(m[4], D)), w=["G2"])
            P.v(lambda e: e.scalar_tensor_tensor(out=G2[:], in0=G2[:], scalar=1.0, in1=gn[:], op0=ALU.add, op1=ALU.mult),
                ["G2", "gn"], ["G2"])

        for t in range(NT):
            if t == 0:
                load_mod(modc)
            if t == 2:
                load_mod(modx)
            r0 = t * 128
            P.dma(lambda e, r0=r0: e.dma_start(out=yt[:, 0:768], in_=yAC[r0:r0 + 128, 0:768]), w=[("yt", 0)])
            P.dma(lambda e, r0=r0: e.dma_start(out=yt[:, 768:1280], in_=yB[r0:r0 + 128, :]), w=[("yt", 1)])
            P.dma(lambda e, r0=r0: e.dma_start(out=yt[:, 1280:2048], in_=yAC[r0:r0 + 128, 768:1536]), w=[("yt", 2)])
            P.dma(lambda e, r0=r0: e.dma_start(out=xt[:], in_=xin[r0:r0 + 128, :]), w=["xt"])
            for kc in range(16):
                P.t(lambda e, kc=kc: e.transpose(psT[:, kc, :], yt[:, kc * 128:(kc + 1) * 128], identb[:]),
                    [("yt", 0), ("yt", 1), ("yt", 2), "identb"], ["psT"])
            P.a(lambda e: e.activation(out=yT[:], in_=psT[:], func=AF.Copy), ["psT"], ["yT"])
            for g in range(4):
                px = psX[g % 2]
                for kc in range(16):
                    P.t(lambda e, px=px, g=g, kc=kc: e.matmul(px[:], lhsT=yT[:, kc, :], rhs=wbf[:, kc, g * 512:(g + 1) * 512],
                                                              start=(kc == 0), stop=(kc == 15)), ["yT", "wbf"], [("psX", g % 2)])
                P.v(lambda e, px=px, g=g: e.tensor_tensor(out=tmp[:, g * 512:(g + 1) * 512], in0=px[:], in1=G1g[:, g * 512:(g + 1) * 512],
                                                          op=ALU.mult), [("psX", g % 2), "G1g"], ["tmp"])
            P.v(lambda e: e.tensor_tensor(out=x1[:], in0=tmp[:], in1=xt[:], op=ALU.add), ["tmp", "xt"], ["x1"])
            P.dma(lambda e, r0=r0: e.dma_start(out=x1o[r0:r0 + 128, :], in_=x1[:]), ["x1"], [("x1o", t)])
            P.a(lambda e: e.activation(out=tmp[:], in_=x1[:], func=AF.Square, accum_out=ss[:]), ["x1"], ["tmp", "ss"])
            emit_rstd(P, ss[:], rs[:], D, ["ss"], ["rs"])
            P.v(lambda e: e.scalar_tensor_tensor(out=h2[:], in0=x1[:], scalar=rs[:, 0:1], in1=G2[:], op0=ALU.mult, op1=ALU.mult),
                ["x1", "rs", "G2"], ["h2"])
            P.v(lambda e: e.tensor_tensor(out=h2[:], in0=h2[:], in1=SH2[:], op=ALU.add), ["h2", "SH2"], ["h2"])
            P.a(lambda e: e.activation(out=hb[:], in_=h2[:], func=AF.Copy), ["h2"], ["hb"])
            P.dma(lambda e, r0=r0: e.dma_start(out=hx2o[r0:r0 + 128, :], in_=hb[:]), ["hb"], [("hx2o", t)])
            for q4 in range(4):
                pr = psR[q4 % 2]
                for j in range(4):
                    kc = q4 * 4 + j
                    P.t(lambda e, pr=pr, j=j, kc=kc: e.transpose(pr[:, j, :], h2[:, kc * 128:(kc + 1) * 128], ident[:]),
                        ["h2", "ident"], [("psR", q4 % 2)])
                P.a(lambda e, pr=pr, q4=q4: e.activation(out=h2T[:, q4 * 4:(q4 + 1) * 4, :], in_=pr[:], func=AF.Copy),
                    [("psR", q4 % 2)], ["h2T"])
            for kc in range(16):
                P.t(lambda e, kc=kc: e.matmul(psL[:, 0:NE], lhsT=h2T[:, kc, :], rhs=wrs[:, kc, :], start=(kc == 0), stop=(kc == 15)),
                    ["h2T", "wrs"], ["psL"])
            P.v(lambda e: e.tensor_reduce(out=mx[:], in_=psL[:, 0:NE], axis=AX.X, op=ALU.max), ["psL"], ["mx"])
            P.v(lambda e: e.tensor_scalar(out=mx[:], in0=mx[:], scalar1=-1.0, scalar2=None, op0=ALU.mult), ["mx"], ["mx"])
            P.a(lambda e: e.activation(out=ex[:], in_=psL[:, 0:NE], func=AF.Exp, bias=mx[:, 0:1], accum_out=se[:]), ["psL", "mx"], ["ex", "se"])
            P.v(lambda e: e.reciprocal(out=se[:], in_=se[:]), ["se"], ["se"])
            P.v(lambda e: e.tensor_scalar(out=af[:], in0=ex[:], scalar1=se[:, 0:1], scalar2=None, op0=ALU.mult), ["ex", "se"], ["af"])
            if fused:
                adst = io["affC"][r0:r0 + 128, :] if t < 2 else io["affIn"][r0 - 256:r0 - 128, :]
                P.dma(lambda e, adst=adst: e.dma_start(out=adst, in_=af[:]), ["af"], [("affo", t)])
            else:
                P.t(lambda e: e.transpose(psA[0:NE, 0:128], af[:], ident[:]), ["af", "ident"], ["psA"])
                P.a(lambda e: e.activation(out=aT[:], in_=psA[0:NE, 0:128], func=AF.Copy), ["psA"], ["aT"])
                P.dma(lambda e, r0=r0: e.dma_start(out=affT[:, r0:r0 + 128], in_=aT[:]), ["aT"], [("affT", t)])
        P.emit(fused=fused)
    return nc


NEL = 8
NROW = SEQ + CTX


def build_s3(with_ctx, nc=None, io=None, pfx=""):
    if nc is None:
        nc = bass.Bass("TRN2", target_bir_lowering=False)
    fused, di, do = _mk(nc, io, pfx)
    affL = di("affL", [128, 32, NEL])
    affC = di("affC", [128, 2, NEL])
    hx2 = di("hx2", [NROW, D], BF16)
    wg = di("wg", [NEL, D, D])
    wu = di("wu", [NEL, D, D])
    wd = di("wd", [NEL, D, D])
    consts = di("consts", [128, 128 + 128 + 128 + 512 + 34])
    delta = [do(f"delta{i}", [NROW, 512]) for i in range(4)]
    sets = [("L", 32, 512, 0, affL)]
    if with_ctx:
        sets.append(("C", 2, 32, SEQ, affC))
    with stage_ctx(nc, fused) as st:
        sb = lambda n, s, d=F32: st.enter_context(nc.sbuf_tensor(pfx + n, s, d))
        pt = lambda n, s, d=F32: st.enter_context(nc.psum_tensor(pfx + n, s, d))
        cst = sb("cst", [128, 930])
        identb = sb("identb", [128, 128], BF16)
        zt = sb("zt", [128, D])
        ring = [sb(f"ring{i}", [128, 16, 512], BF16) for i in range(4)]
        xg = [sb(f"xg{i}", [128, D], BF16) for i in range(2)]
        xgT = sb("xgT", [128, 16, 544], BF16)
        hT = sb("hT", [128, 16, 544], BF16)
        sil = sb("sil", [128, 544])
        yb = [sb(f"yb{i}", [128, 512]) for i in range(3)]
        S = {}
        for (nm, C, k, r0, _) in sets:
            S[nm] = dict(
                aff=sb(f"saff{nm}", [128, C, NEL]), lo=sb(f"lo{nm}", [128, NEL]), th=sb(f"th{nm}", [128, NEL, 15]),
                cmp=sb(f"cmp{nm}", [128, C, NEL, 15]), cntp=sb(f"cntp{nm}", [128, NEL, 15]), ge=sb(f"ge{nm}", [128, NEL, 15]),
                nge=sb(f"nge{nm}", [128, NEL]), mask=sb(f"mask{nm}", [128, C, NEL]), maskb=sb(f"maskb{nm}", [128, C, NEL], BF16),
                cA=sb(f"cA{nm}", [128, C, NEL]), cB=sb(f"cB{nm}", [128, C, NEL]), pos=sb(f"pos{nm}", [128, C, NEL]),
                vals=sb(f"vals{nm}", [128, C, NEL, 2]), oh=[sb(f"oh{nm}{i}", [128, k]) for i in range(2)],
                sl=sb(f"sl{nm}", [128, NEL, 4, 2]), idx=sb(f"idx{nm}", [128, NEL, 4], I32), gate=sb(f"gate{nm}", [128, NEL, 4]))
        jt = sb("jt", [128, NEL, 15])
        trib = sb("trib", [128, 128], BF16)
        onesb = sb("onesb", [128, 128], BF16)
        psA = [pt(f"psA{i}", [128, 512]) for i in range(2)]
        psU = [pt(f"psU{i}", [128, 512]) for i in range(2)]
        psT = pt("psT", [128, 16, 128], BF16)
        psY = [pt(f"psY{i}", [128, 512]) for i in range(2)]
        ident = cst[:, 0:128]
        ones = cst[:, 128:256]
        tri = cst[:, 256:384]
        iota = cst[:, 384:896]
        tokid = cst[:, 896:930]
        P = Prog(nc)
        P.dma(lambda e: e.dma_start(out=cst[:], in_=consts), w=["cst"])
        P.v(lambda e: e.tensor_copy(out=identb[:], in_=ident), ["cst"], ["identb"])
        P.v(lambda e: e.tensor_copy(out=trib[:], in_=tri), ["cst"], ["trib"])
        P.v(lambda e: e.tensor_copy(out=onesb[:], in_=ones), ["cst"], ["onesb"])
        for j in range(15):
            P.v(lambda e, j=j: e.memset(jt[:, :, j:j + 1], float(j + 1)), [], ["jt"])
        P.v(lambda e: e.memset(zt[:], 0.0), [], ["zt"])
        for cb in range(4):
            for r in range(NROW // 128):
                P.dma(lambda e, r=r, cb=cb: e.dma_start(out=delta[cb][r * 128:(r + 1) * 128, :], in_=zt[:, 0:512]), ["zt"], [("delta", cb)])

        def select(nm, C, k, r0, affd):
            s = S[nm]
            kk = lambda x: (nm, x)
            P.dma(lambda e, s=s, affd=affd: e.dma_start(out=s["aff"][:], in_=affd), w=[kk("aff")])
            P.v(lambda e, s=s: e.memset(s["lo"][:], 0.0), [], [kk("lo")])
            affb = s["aff"][:].unsqueeze(3).to_broadcast([128, C, NEL, 15])
            for it in range(8):
                step = 16.0 ** -(it + 1)
                P.v(lambda e, s=s, step=step: e.scalar_tensor_tensor(out=s["th"][:], in0=jt[:], scalar=step,
                                                                     in1=s["lo"][:].unsqueeze(2).to_broadcast([128, NEL, 15]),
                                                                     op0=ALU.mult, op1=ALU.add), ["jt", kk("lo")], [kk("th")])
                P.v(lambda e, s=s: e.tensor_tensor(out=s["cmp"][:], in0=affb, in1=s["th"][:].unsqueeze(1).to_broadcast([128, C, NEL, 15]),
                                                   op=ALU.is_ge), [kk("aff"), kk("th")], [kk("cmp")])
                P.v(lambda e, s=s: e.tensor_reduce(out=s["cntp"][:], in_=s["cmp"][:].rearrange("p c e j -> p e j c"), axis=AX.X, op=ALU.add),
                    [kk("cmp")], [kk("cntp")])
                P.t(lambda e, s=s: e.matmul(psY[0][:, 0:NEL * 15], lhsT=ones, rhs=s["cntp"][:].rearrange("p e j -> p (e j)"), start=True, stop=True),
                    ["cst", kk("cntp")], [("psY", 0)])
                P.v(lambda e, s=s, k=k: e.tensor_scalar(out=s["ge"][:].rearrange("p e j -> p (e j)"), in0=psY[0][:, 0:NEL * 15], scalar1=float(k) - 0.5,
                                                        scalar2=None, op0=ALU.is_ge), [("psY", 0)], [kk("ge")])
                P.v(lambda e, s=s: e.tensor_reduce(out=s["nge"][:], in_=s["ge"][:], axis=AX.X, op=ALU.add), [kk("ge")], [kk("nge")])
                P.v(lambda e, s=s, step=step: e.scalar_tensor_tensor(out=s["lo"][:], in0=s["nge"][:], scalar=step, in1=s["lo"][:],
                                                                     op0=ALU.mult, op1=ALU.add), [kk("nge"), kk("lo")], [kk("lo")])
            P.v(lambda e, s=s: e.tensor_tensor(out=s["mask"][:], in0=s["aff"][:], in1=s["lo"][:].unsqueeze(1).to_broadcast([128, C, NEL]),
                                               op=ALU.is_ge), [kk("aff"), kk("lo")], [kk("mask")])
            P.v(lambda e, s=s: e.tensor_copy(out=s["maskb"][:], in_=s["mask"][:]), [kk("mask")], [kk("maskb")])
            mb2 = s["maskb"][:].rearrange("p c e -> p (c e)")
            P.t(lambda e: e.matmul(psY[0][:, 0:C * NEL], lhsT=trib[:], rhs=mb2, start=True, stop=True), ["trib", kk("maskb")], [("psY", 0)])
            P.t(lambda e: e.matmul(psY[1][:, 0:C * NEL], lhsT=onesb[:], rhs=mb2, start=True, stop=True), ["onesb", kk("maskb")], [("psY", 1)])
            P.v(lambda e, s=s: e.tensor_copy(out=s["cA"][:].rearrange("p c e -> p (c e)"), in_=psY[1][:, 0:C * NEL]), [("psY", 1)], [kk("cA")])
            cur, nxt = "cA", "cB"
            sh = 1
            while sh < C:
                P.v(lambda e, s=s, cur=cur, nxt=nxt, sh=sh: e.tensor_copy(out=s[nxt][:, 0:sh, :], in_=s[cur][:, 0:sh, :]), [kk(cur)], [kk(nxt)])
                P.v(lambda e, s=s, cur=cur, nxt=nxt, sh=sh: e.tensor_tensor(out=s[nxt][:, sh:C, :], in0=s[cur][:, sh:C, :], in1=s[cur][:, 0:C - sh, :],
                                                                          op=ALU.add), [kk(cur)], [kk(nxt)])
                cur, nxt = nxt, cur
                sh *= 2
            P.v(lambda e, s=s, cur=cur: e.tensor_tensor(out=s["pos"][:].rearrange("p c e -> p (c e)"), in0=s[cur][:].rearrange("p c e -> p (c e)"),
                                                        in1=psY[1][:, 0:C * NEL], op=ALU.subtract), [kk(cur), ("psY", 1)], [kk("pos")])
            P.v(lambda e, s=s: e.tensor_tensor(out=s["pos"][:].rearrange("p c e -> p (c e)"), in0=s["pos"][:].rearrange("p c e -> p (c e)"),
                                               in1=psY[0][:, 0:C * NEL], op=ALU.add), [kk("pos"), ("psY", 0)], [kk("pos")])
            P.v(lambda e, s=s: e.tensor_tensor(out=s["pos"][:], in0=s["pos"][:], in1=s["mask"][:], op=ALU.mult), [kk("pos"), kk("mask")], [kk("pos")])
            P.v(lambda e, s=s: e.tensor_scalar(out=s["pos"][:], in0=s["pos"][:], scalar1=-1.0, scalar2=None, op0=ALU.add), [kk("pos")], [kk("pos")])
            P.v(lambda e, s=s, r0=r0: e.tensor_scalar(out=s["vals"][:, :, :, 0], in0=tokid[:, 0:C].unsqueeze(2).to_broadcast([128, C, NEL]),
                                                      scalar1=float(r0), scalar2=None, op0=ALU.add), ["cst"], [kk("vals")])
            P.v(lambda e, s=s: e.tensor_copy(out=s["vals"][:, :, :, 1], in_=s["aff"][:]), [kk("aff")], [kk("vals")])
            nst = (k + 127) // 128
            sw = min(k, 128)
            n_oh = 0
            for el in range(NEL):
                for c in range(C):
                    ohb = s["oh"][n_oh % 2]
                    kb = (nm, "oh", n_oh % 2)
                    n_oh += 1
                    P.v(lambda e, ohb=ohb, s=s, c=c, el=el, k=k: e.tensor_scalar(out=ohb[:], in0=iota[:, 0:k], scalar1=s["pos"][:, c, el:el + 1],
                                                                                scalar2=None, op0=ALU.is_equal), ["cst", kk("pos")], [kb])
                    for sti in range(nst):
                        P.t(lambda e, ohb=ohb, s=s, c=c, el=el, sti=sti, sw=sw: e.matmul(
                            psU[0][0:sw, (el * 4 + sti) * 2:(el * 4 + sti) * 2 + 2], lhsT=ohb[:, sti * 128:sti * 128 + sw],
                            rhs=s["vals"][:, c, el, :], start=(c == 0 and el == 0 and sti == 0), stop=(c == C - 1)),
                            [kb, kk("vals")], [("psU", 0)])
            P.v(lambda e, s=s, sw=sw: e.tensor_copy(out=s["sl"][0:sw].rearrange("p e s t -> p (e s t)"), in_=psU[0][0:sw, 0:NEL * 8]), [("psU", 0)], [kk("sl")])
            P.v(lambda e, s=s, sw=sw: e.tensor_copy(out=s["idx"][0:sw], in_=s["sl"][0:sw, :, :, 0]), [kk("sl")], [kk("idx")])
            P.v(lambda e, s=s, sw=sw: e.tensor_copy(out=s["gate"][0:sw], in_=s["sl"][0:sw, :, :, 1]), [kk("sl")], [kk("gate")])

        for sargs in sets:
            select(*sargs)

        tiles = [("L", sti, 128, sti * 128) for sti in range(4)]
        if with_ctx:
            tiles.append(("C", 0, 32, 512))
        ncols = 544 if with_ctx else 512
        ring_n = [0]
        ny = [0]

        def load_unit(wsrc, el, cb):
            rb = ring_n[0] % 4
            ring_n[0] += 1
            for k4 in range(4):
                src = wsrc[el, k4 * 512:(k4 + 1) * 512, cb * 512:(cb + 1) * 512].rearrange("(kc p) n -> p kc n", p=128)
                P.dma(lambda e, rb=rb, k4=k4, src=src: e.dma_start(out=ring[rb][:, k4 * 4:(k4 + 1) * 4, :], in_=src), w=[("ring", rb)], q="gpsimd")
            return rb

        for el in range(NEL):
            for ti, (nm, sti, n, c0) in enumerate(tiles):
                s = S[nm]
                xb = xg[ti % 2]
                kx = ("xg", ti % 2)
                P.op("gpsimd", lambda e, xb=xb, s=s, el=el, sti=sti, n=n: e.indirect_dma_start(
                    out=xb[0:n, :], out_offset=None, in_=hx2[:, :],
                    in_offset=bass.IndirectOffsetOnAxis(ap=s["idx"][0:n, el, sti:sti + 1], axis=0)), [(nm, "idx")], [kx], dma=True)
                for kc in range(16):
                    P.t(lambda e, xb=xb, kc=kc, n=n: e.transpose(psT[:, kc, 0:n], xb[0:n, kc * 128:(kc + 1) * 128], identb[0:n, 0:n]), [kx, "identb"], ["psT"])
                P.a(lambda e, c0=c0, n=n: e.activation(out=xgT[:, :, c0:c0 + n], in_=psT[:, :, 0:n], func=AF.Copy), ["psT"], ["xgT"])
            for cb in range(4):
                rg = load_unit(wg, el, cb)
                ru = load_unit(wu, el, cb)
                for f4 in range(4):
                    fc = cb * 4 + f4
                    pa = psA[fc % 2]
                    pu = psU[fc % 2]
                    for (pp, rbuf, key) in ((pa, rg, "psA"), (pu, ru, "psU")):
                        for kc in range(16):
                            P.t(lambda e, pp=pp, rbuf=rbuf, kc=kc, f4=f4: e.matmul(pp[:, 0:512], lhsT=ring[rbuf][:, kc, f4 * 128:(f4 + 1) * 128],
                                                                                  rhs=xgT[:, kc, 0:512], start=(kc == 0), stop=(kc == 15)),
                                [("ring", rbuf), "xgT"], [(key, fc % 2)])
                    P.a(lambda e, pa=pa: e.activation(out=sil[:, 0:512], in_=pa[:, 0:512], func=AF.Silu), [("psA", fc % 2)], ["sil"])
                    P.v(lambda e, pu=pu, fc=fc: e.tensor_tensor(out=hT[:, fc, 0:512], in0=sil[:, 0:512], in1=pu[:, 0:512], op=ALU.mult),
                        ["sil", ("psU", fc % 2)], ["hT"])
                    if with_ctx:
                        for (pp, rbuf, key) in ((pa, rg, "psA"), (pu, ru, "psU")):
                            for kc in range(16):
                                P.t(lambda e, pp=pp, rbuf=rbuf, kc=kc, f4=f4: e.matmul(pp[:, 0:32], lhsT=ring[rbuf][:, kc, f4 * 128:(f4 + 1) * 128],
                                                                                      rhs=xgT[:, kc, 512:544], start=(kc == 0), stop=(kc == 15)),
                                    [("ring", rbuf), "xgT"], [(key, fc % 2)])
                        P.a(lambda e, pa=pa: e.activation(out=sil[:, 512:544], in_=pa[:, 0:32], func=AF.Silu), [("psA", fc % 2)], ["sil"])
                        P.v(lambda e, pu=pu, fc=fc: e.tensor_tensor(out=hT[:, fc, 512:544], in0=sil[:, 512:544], in1=pu[:, 0:32], op=ALU.mult),
                            ["sil", ("psU", fc % 2)], ["hT"])
            for cb in range(4):
                rd = load_unit(wd, el, cb)
                for ti, (nm, sti, n, c0) in enumerate(tiles):
                    s = S[nm]
                    py = psY[ny[0] % 2]
                    ky = ("psY", ny[0] % 2)
                    ybuf = yb[ny[0] % 3]
                    kyb = ("yb", ny[0] % 3)
                    ny[0] += 1
                    for fc in range(16):
                        P.t(lambda e, py=py, rd=rd, fc=fc, c0=c0, n=n: e.matmul(py[0:n, :], lhsT=hT[:, fc, c0:c0 + n], rhs=ring[rd][:, fc, :],
                                                                               start=(fc == 0), stop=(fc == 15)), ["hT", ("ring", rd)], [ky])
                    P.v(lambda e, py=py, ybuf=ybuf, s=s, el=el, sti=sti, n=n: e.tensor_scalar(out=ybuf[0:n, :], in0=py[0:n, :],
                                                                                              scalar1=s["gate"][0:n, el, sti:sti + 1], scalar2=None, op0=ALU.mult),
                        [ky, (nm, "gate")], [kyb])
                    P.op("gpsimd", lambda e, ybuf=ybuf, s=s, el=el, sti=sti, n=n, cb=cb: e.indirect_dma_start(
                        out=delta[cb][:, :], out_offset=bass.IndirectOffsetOnAxis(ap=s["idx"][0:n, el, sti:sti + 1], axis=0),
                        in_=ybuf[0:n, :], in_offset=None, compute_op=ALU.add), [kyb, (nm, "idx"), ("delta", cb)], [("delta", cb)], dma=True)
        P.emit(fused=fused)
    return nc


def s3_consts():
    c = np.zeros((128, 930), np.float32)
    c[:, 0:128] = np.eye(128)
    c[:, 128:256] = 1.0
    c[:, 256:384] = np.triu(np.ones((128, 128)))
    c[:, 384:896] = np.arange(512)[None, :]
    c[:, 896:930] = np.arange(34)[None, :] * 128 + np.arange(128)[:, None]
    return c


def build_s4(nc=None, io=None, pfx=""):
    if nc is None:
        nc = bass.Bass("TRN2", target_bir_lowering=False)
    fused, di, do = _mk(nc, io, pfx)
    x1 = di("x1", [NTOK, D])
    dA = di("dA", [NTOK, D])
    dB = None if fused else di("dB", [NTOK, D])
    modx = di("modx", [6, D])
    modc = di("modc", [6, D])
    x2 = do("x2", [NTOK, D])
    with stage_ctx(nc, fused) as st:
        sb = lambda n, s, d=F32: st.enter_context(nc.sbuf_tensor(pfx + n, s, d))
        G = sb("G", [128, D])
        xa = [sb(f"xa{i}", [128, D]) for i in range(2)]
        da = [sb(f"da{i}", [128, D]) for i in range(2)]
        db = [sb(f"db{i}", [128, D]) for i in range(2)]
        P = Prog(nc)
        for t in range(NT):
            i = t % 2
            if t == 0:
                P.dma(lambda e: e.dma_start(out=G[:], in_=_bc(modc[5], D)), w=["G"])
            if t == 2:
                P.dma(lambda e: e.dma_start(out=G[:], in_=_bc(modx[5], D)), w=["G"])
            r0 = t * 128
            P.dma(lambda e, i=i, r0=r0: e.dma_start(out=xa[i][:], in_=x1[r0:r0 + 128, :]), w=[("xa", i)])
            P.dma(lambda e, i=i, r0=r0: e.dma_start(out=da[i][:], in_=dA[r0:r0 + 128, :]), w=[("da", i)])
            if not fused:
                P.dma(lambda e, i=i, r0=r0: e.dma_start(out=db[i][:], in_=dB[r0:r0 + 128, :]), w=[("db", i)])
                P.v(lambda e, i=i: e.tensor_tensor(out=da[i][:], in0=da[i][:], in1=db[i][:], op=ALU.add), [("da", i), ("db", i)], [("da", i)])
            P.v(lambda e, i=i: e.tensor_tensor(out=da[i][:], in0=da[i][:], in1=G[:], op=ALU.mult), [("da", i), "G"], [("da", i)])
            P.v(lambda e, i=i: e.tensor_tensor(out=xa[i][:], in0=xa[i][:], in1=da[i][:], op=ALU.add), [("xa", i), ("da", i)], [("xa", i)])
            P.dma(lambda e, i=i, r0=r0: e.dma_start(out=x2[r0:r0 + 128, :], in_=xa[i][:]), [("xa", i)], [("x2", t)])
        P.emit(fused=fused)
    return nc


def _rope_tables(h):
    t = np.arange(h * 2048, (h + 1) * 2048)
    row = (t // 64).astype(np.float32)
    col = (t % 64).astype(np.float32)
    inv = (np.float32(10000.0) ** (-np.arange(32, dtype=np.float32) / np.float32(32))).astype(np.float32)
    ar = row[:, None] * inv
    ac = col[:, None] * inv
    tab = np.zeros((NTOK, 128), np.float32)
    tab[:256, :64] = 1.0
    tab[256:, :64] = np.concatenate([np.cos(ar), np.cos(ac)], 1)
    tab[256:, 64:] = np.concatenate([np.sin(ar), np.sin(ac)], 1)
    return tab


def _masks(h):
    s = np.arange(128)[:, None]
    q = np.arange(128)[None, :]
    mp = (s >= q).astype(np.float32)
    mn = (s <= q).astype(np.float32)
    return np.stack([mp, mn, mp * (0.0 if h == 0 else 1.0), mn * (0.0 if h == 1 else 1.0)])


def _run(nc, in_maps):
    res = run_bass_kernel_spmd(nc, in_maps, core_ids=list(range(8)))
    return res.results


PAIRS = [[0, 1], [2, 3], [4, 5], [6, 7]]
NXR = NTOK + 128
CAPL = 384
NCST = 931


def f_consts():
    c = np.zeros((128, NCST), np.float32)
    c[:, 0:930] = s3_consts()
    c[:, 930] = NTOK + np.arange(128)
    return c


def stage_mod(nc, io, pfx):
    cin, w, b, modS = io["cin"], io["w_mod"], io["b_mod"], io["modS"]
    with stage_ctx(nc, True) as st:
        sb = lambda n, s, d=F32: st.enter_context(nc.sbuf_tensor(pfx + n, s, d))
        ct = sb("ct", [128, 16, 2])
        cs = sb("cs", [128, 16, 2])
        wt = [sb(f"wt{i}", [128, 16, 512]) for i in range(3)]
        bt = [sb(f"bt{i}", [2, 512]) for i in range(3)]
        ot = [sb(f"ot{i}", [2, 512]) for i in range(3)]
        ps = [st.enter_context(nc.psum_tensor(pfx + f"ps{i}", [2, 512], F32)) for i in range(2)]
        P = Prog(nc)
        P.dma(lambda e: e.dma_start(out=ct[:], in_=cin), w=["ct"])
        P.a(lambda e: e.activation(out=cs[:], in_=ct[:], func=AF.Silu), ["ct"], ["cs"])
        gi = 0
        for l in range(2):
            for g in range(24):
                i3 = gi % 3
                i2 = gi % 2
                gi += 1
                cols = slice(g * 512, (g + 1) * 512)
                src = w[l, :, cols].rearrange("(kc p) n -> p kc n", p=128)
                P.dma(lambda e, i3=i3, src=src: e.dma_start(out=wt[i3][:], in_=src), w=[("wt", i3)])
                P.dma(lambda e, i3=i3, l=l, cols=cols: e.dma_start(out=bt[i3][:], in_=b[l:l + 1, cols].to_broadcast([2, 512])), w=[("bt", i3)])
                for kc in range(16):
                    P.t(lambda e, i2=i2, i3=i3, kc=kc: e.matmul(ps[i2][:], lhsT=cs[:, kc, :], rhs=wt[i3][:, kc, :], start=(kc == 0), stop=(kc == 15)),
                        ["cs", ("wt", i3)], [("ps", i2)])
                P.v(lambda e, i2=i2, i3=i3: e.tensor_tensor(out=ot[i3][:], in0=ps[i2][:], in1=bt[i3][:], op=ALU.add), [("ps", i2), ("bt", i3)], [("ot", i3)])
                P.dma(lambda e, i3=i3, l=l, cols=cols: e.dma_start(out=modS[l, :, cols], in_=ot[i3][:]), [("ot", i3)], [("modS", l, g)])
        P.emit(fused=True)


def stage_cc(nc, pairs):
    with nc.cleanup_on_exit():
        _UID[0] += 1
        sem = nc.alloc_semaphore(name="cc%d" % _UID[0])
        with nc.Block() as block:
            @block.gpsimd
            def _(g):
                for i, (a, b) in enumerate(pairs):
                    g.collective_compute("AllGather", ALU.bypass, replica_groups=PAIRS, ins=[a], outs=[b]).then_inc(sem, 1)
                    g.wait_ge(sem, i + 1)
        nc.all_engine_barrier()


def stage_s3a(nc, io, with_ctx, pfx):
    affAll, affIn, affC, consts = io["affAll"], io["affIn"], io["affC"], io["consts"]
    idxT, gateT, delta, hx2 = io["idxT"], io["gateT"], io["delta"], io["hx2o"]
    with stage_ctx(nc, True) as st:
        sb = lambda n, s, d=F32: st.enter_context(nc.sbuf_tensor(pfx + n, s, d))
        pt = lambda n, s, d=F32: st.enter_context(nc.psum_tensor(pfx + n, s, d))
        cst = sb("cst", [128, NCST])
        trib = sb("trib", [128, 128], BF16)
        onesb = sb("onesb", [128, 128], BF16)
        jt = sb("jt", [128, NE, 15])
        zt = sb("zt", [128, D])
        ztb = sb("ztb", [128, D], BF16)
        sl = sb("sl", [128, NE, 4, 3])
        idxf = sb("idxf", [128, NE, 4])
        idx = sb("idx", [128, NE, 4], I32)
        gate = sb("gate", [128, NE, 4])
        psC = pt("psC", [128, 512])
        psP = [pt(f"psP{i}", [128, 512]) for i in range(2)]
        psS = [pt(f"psS{i}", [128, 512]) for i in range(2)]
        ones = cst[:, 128:256]
        tri = cst[:, 256:384]
        iota = cst[:, 384:896]
        tokid = cst[:, 896:930]
        dummy = cst[:, 930:931]
        P = Prog(nc)
        P.dma(lambda e: e.dma_start(out=cst[:], in_=consts), w=["cst"])
        P.v(lambda e: e.tensor_copy(out=trib[:], in_=tri), ["cst"], ["trib"])
        P.v(lambda e: e.tensor_copy(out=onesb[:], in_=ones), ["cst"], ["onesb"])
        for j in range(15):
            P.v(lambda e, j=j: e.memset(jt[:, :, j:j + 1], float(j + 1)), [], ["jt"])
        P.v(lambda e: e.memset(zt[:], 0.0), [], ["zt"])
        P.v(lambda e: e.memset(ztb[:], 0.0), [], ["ztb"])
        P.v(lambda e: e.memset(sl[:], 0.0), [], ["sl"])
        for r in range(NXR // 128):
            P.dma(lambda e, r=r: e.dma_start(out=delta[r * 128:(r + 1) * 128, :], in_=zt[:]), ["zt"], [("delta", r)])
        P.dma(lambda e: e.dma_start(out=hx2[NTOK:NXR, :], in_=ztb[:]), ["ztb"], ["hx2d"])

        def select(nm, Cth, thr_ap, Cm, mask_ap, k, r0, cap, st0, psSl):
            kk = lambda x: (nm, x)
            athr = sb(f"athr{nm}", [128, Cth, NE])
            am = athr if mask_ap is None else sb(f"am{nm}", [128, Cm, NE])
            kam = kk("athr") if mask_ap is None else kk("am")
            lo = sb(f"lo{nm}", [128, NE])
            th = sb(f"th{nm}", [128, NE, 15])
            cmpb = sb(f"cmp{nm}", [128, Cth, NE, 15], BF16)
            cntp = sb(f"cntp{nm}", [128, NE, 15])
            ge = sb(f"ge{nm}", [128, NE, 15])
            nge = sb(f"nge{nm}", [128, NE])
            mask = sb(f"mask{nm}", [128, Cm, NE])
            maskb = sb(f"maskb{nm}", [128, Cm, NE], BF16)
            cA = sb(f"cA{nm}", [128, Cm, NE])
            cB = sb(f"cB{nm}", [128, Cm, NE])
            pos = sb(f"pos{nm}", [128, Cm, NE])
            vals = sb(f"vals{nm}", [128, Cm, NE, 3])
            oh = [sb(f"oh{nm}{i}", [128, cap]) for i in range(2)]
            bufs = {"cA": cA, "cB": cB}
            P.dma(lambda e: e.dma_start(out=athr[:], in_=thr_ap.rearrange("(c p) e -> p c e", p=128)), w=[kk("athr")])
            if mask_ap is not None:
                P.dma(lambda e: e.dma_start(out=am[:], in_=mask_ap.rearrange("(c p) e -> p c e", p=128)), w=[kk("am")])
            P.v(lambda e: e.memset(lo[:], 0.0), [], [kk("lo")])
            affb = athr[:].unsqueeze(3).to_broadcast([128, Cth, NE, 15])
            for it in range(8):
                step = 16.0 ** -(it + 1)
                P.v(lambda e, step=step: e.scalar_tensor_tensor(out=th[:], in0=jt[:], scalar=step, in1=lo[:].unsqueeze(2).to_broadcast([128, NE, 15]),
                                                                op0=ALU.mult, op1=ALU.add), ["jt", kk("lo")], [kk("th")])
                P.v(lambda e: e.tensor_tensor(out=cmpb[:], in0=affb, in1=th[:].unsqueeze(1).to_broadcast([128, Cth, NE, 15]), op=ALU.is_ge),
                    [kk("athr"), kk("th")], [kk("cmp")])
                P.v(lambda e: e.tensor_reduce(out=cntp[:], in_=cmpb[:].rearrange("p c e j -> p e j c"), axis=AX.X, op=ALU.add), [kk("cmp")], [kk("cntp")])
                P.t(lambda e: e.matmul(psC[:, 0:NE * 15], lhsT=ones, rhs=cntp[:].rearrange("p e j -> p (e j)"), start=True, stop=True),
                    ["cst", kk("cntp")], ["psC"])
                P.v(lambda e: e.tensor_scalar(out=ge[:].rearrange("p e j -> p (e j)"), in0=psC[:, 0:NE * 15], scalar1=float(k) - 0.5, scalar2=None,
                                              op0=ALU.is_ge), ["psC"], [kk("ge")])
                P.v(lambda e: e.tensor_reduce(out=nge[:], in_=ge[:], axis=AX.X, op=ALU.add), [kk("ge")], [kk("nge")])
                P.v(lambda e, step=step: e.scalar_tensor_tensor(out=lo[:], in0=nge[:], scalar=step, in1=lo[:], op0=ALU.mult, op1=ALU.add),
                    [kk("nge"), kk("lo")], [kk("lo")])
            P.v(lambda e: e.tensor_tensor(out=mask[:], in0=am[:], in1=lo[:].unsqueeze(1).to_broadcast([128, Cm, NE]), op=ALU.is_ge),
                [kam, kk("lo")], [kk("mask")])
            P.v(lambda e: e.tensor_copy(out=maskb[:], in_=mask[:]), [kk("mask")], [kk("maskb")])
            mb2 = maskb[:].rearrange("p c e -> p (c e)")
            ncol = Cm * NE
            P.t(lambda e: e.matmul(psP[0][:, 0:ncol], lhsT=trib[:], rhs=mb2, start=True, stop=True), ["trib", kk("maskb")], [("psP", 0)])
            P.t(lambda e: e.matmul(psP[1][:, 0:ncol], lhsT=onesb[:], rhs=mb2, start=True, stop=True), ["onesb", kk("maskb")], [("psP", 1)])
            P.v(lambda e: e.tensor_copy(out=cA[:].rearrange("p c e -> p (c e)"), in_=psP[1][:, 0:ncol]), [("psP", 1)], [kk("cA")])
            cur, nxt = "cA", "cB"
            sh = 1
            while sh < Cm:
                P.v(lambda e, cur=cur, nxt=nxt, sh=sh: e.tensor_copy(out=bufs[nxt][:, 0:sh, :], in_=bufs[cur][:, 0:sh, :]), [kk(cur)], [kk(nxt)])
                P.v(lambda e, cur=cur, nxt=nxt, sh=sh: e.tensor_tensor(out=bufs[nxt][:, sh:Cm, :], in0=bufs[cur][:, sh:Cm, :], in1=bufs[cur][:, 0:Cm - sh, :],
                                                                  op=ALU.add), [kk(cur)], [kk(nxt)])
                cur, nxt = nxt, cur
                sh *= 2
            pos2 = pos[:].rearrange("p c e -> p (c e)")
            P.v(lambda e, cur=cur: e.tensor_tensor(out=pos2, in0=bufs[cur][:].rearrange("p c e -> p (c e)"), in1=psP[1][:, 0:ncol], op=ALU.subtract),
                [kk(cur), ("psP", 1)], [kk("pos")])
            P.v(lambda e: e.tensor_tensor(out=pos2, in0=pos2, in1=psP[0][:, 0:ncol], op=ALU.add), [kk("pos"), ("psP", 0)], [kk("pos")])
            P.v(lambda e: e.tensor_tensor(out=pos[:], in0=pos[:], in1=mask[:], op=ALU.mult), [kk("pos"), kk("mask")], [kk("pos")])
            P.v(lambda e: e.tensor_scalar(out=pos[:], in0=pos[:], scalar1=-1.0, scalar2=None, op0=ALU.add), [kk("pos")], [kk("pos")])
            P.v(lambda e: e.tensor_scalar(out=vals[:, :, :, 0], in0=tokid[:, 0:Cm].unsqueeze(2).to_broadcast([128, Cm, NE]), scalar1=float(r0), scalar2=None,
                                          op0=ALU.add), ["cst"], [kk("vals")])
            P.v(lambda e: e.tensor_copy(out=vals[:, :, :, 1], in_=am[:]), [kam], [kk("vals")])
            P.v(lambda e: e.memset(vals[:, :, :, 2], 1.0), [], [kk("vals")])
            nst = (cap + 127) // 128
            sw = min(cap, 128)
            n_oh = 0
            first = True
            for el in range(NE):
                for c in range(Cm):
                    ohb = oh[n_oh % 2]
                    kb = (nm, "oh", n_oh % 2)
                    n_oh += 1
                    P.v(lambda e, ohb=ohb, c=c, el=el: e.tensor_scalar(out=ohb[:], in0=iota[:, 0:cap], scalar1=pos[:, c, el:el + 1], scalar2=None,
                                                                      op0=ALU.is_equal), ["cst", kk("pos")], [kb])
                    for sti in range(nst):
                        col = (el * 4 + st0 + sti) * 3
                        P.t(lambda e, ohb=ohb, c=c, el=el, sti=sti, col=col, first=first: e.matmul(
                            psSl[0:sw, col:col + 3], lhsT=ohb[:, sti * 128:sti * 128 + sw], rhs=vals[:, c, el, :],
                            start=first, stop=(c == Cm - 1)), [kb, kk("vals")], [kk("psSl")])
                        first = False
            for sti in range(nst):
                P.v(lambda e, sti=sti: e.tensor_copy(out=sl[0:sw, :, st0 + sti, :],
                                                     in_=psSl[0:sw, 0:NE * 12].rearrange("p (e s t) -> p e s t", e=NE, s=4)[:, :, st0 + sti, :]),
                    [kk("psSl")], ["sl"])

        select("L", 32, affAll, 16, affIn, 512, 256, CAPL, 0, psS[0])
        if with_ctx:
            select("C", 2, affC, 2, None, 32, 0, 32, 3, psS[1])
        P.v(lambda e: e.tensor_scalar(out=idxf[:], in0=sl[:, :, :, 2], scalar1=-1.0, scalar2=dummy, op0=ALU.add, op1=ALU.mult), ["sl", "cst"], ["idxf"])
        P.v(lambda e: e.tensor_tensor(out=idxf[:], in0=sl[:, :, :, 0], in1=idxf[:], op=ALU.subtract), ["sl", "idxf"], ["idxf"])
        P.v(lambda e: e.tensor_copy(out=idx[:], in_=idxf[:]), ["idxf"], ["idx"])
        P.v(lambda e: e.tensor_copy(out=gate[:], in_=sl[:, :, :, 1]), ["sl"], ["gate"])
        P.dma(lambda e: e.dma_start(out=idxT, in_=idx[:]), ["idx"], ["idxT"])
        P.dma(lambda e: e.dma_start(out=gateT, in_=gate[:]), ["gate"], ["gateT"])
        P.emit(fused=True)


def stage_s3b(nc, io, L, with_ctx, pfx):
    idxT, gateT, delta, hx2, consts = io["idxT"], io["gateT"], io["delta"], io["hx2o"], io["consts"]
    wg, wu, wd = io["w_gate"], io["w_up"], io["w_down"]
    with stage_ctx(nc, True) as st:
        sb = lambda n, s, d=F32: st.enter_context(nc.sbuf_tensor(pfx + n, s, d))
        pt = lambda n, s, d=F32: st.enter_context(nc.psum_tensor(pfx + n, s, d))
        ncols = CAPL + (32 if with_ctx else 0)
        cst = sb("cst", [128, 128])
        identb = sb("identb", [128, 128], BF16)
        idx = sb("idx", [128, NE, 4], I32)
        gate = sb("gate", [128, NE, 4])
        ring = [sb(f"ring{i}", [128, 16, 512], BF16) for i in range(4)]
        xg = [sb(f"xg{i}", [128, D], BF16) for i in range(2)]
        xgT = sb("xgT", [128, 16, ncols], BF16)
        hT = sb("hT", [128, 16, ncols], BF16)
        sil = sb("sil", [128, ncols])
        ybig = [sb(f"ybig{i}", [128, D]) for i in range(4)]
        psA = [pt(f"psA{i}", [128, 512]) for i in range(2)]
        psU = [pt(f"psU{i}", [128, 512]) for i in range(2)]
        psT = pt("psT", [128, 16, 128], BF16)
        psY = [pt(f"psY{i}", [128, 512]) for i in range(2)]
        P = Prog(nc)
        P.dma(lambda e: e.dma_start(out=cst[:], in_=consts[:, 0:128]), w=["cst"])
        P.v(lambda e: e.tensor_copy(out=identb[:], in_=cst[:]), ["cst"], ["identb"])
        P.dma(lambda e: e.dma_start(out=idx[:], in_=idxT), w=["idx"])
        P.dma(lambda e: e.dma_start(out=gate[:], in_=gateT), w=["gate"])
        tiles = [(sti, 128, sti * 128) for sti in range(3)]
        if with_ctx:
            tiles.append((3, 32, CAPL))
        ring_n = [0]
        ny = [0]

        def load_unit(wsrc, el, cb):
            rb = ring_n[0] % 4
            ring_n[0] += 1
            for k4 in range(4):
                src = wsrc[L, el, k4 * 512:(k4 + 1) * 512, cb * 512:(cb + 1) * 512].rearrange("(kc p) n -> p kc n", p=128)
                P.dma(lambda e, rb=rb, k4=k4, src=src: e.dma_start(out=ring[rb][:, k4 * 4:(k4 + 1) * 4, :], in_=src), w=[("ring", rb)], q="gpsimd")
            return rb

        for el in range(NE):
            for ti, (sti, n, c0) in enumerate(tiles):
                xb = xg[ti % 2]
                kx = ("xg", ti % 2)
                P.op("gpsimd", lambda e, xb=xb, el=el, sti=sti, n=n: e.indirect_dma_start(
                    out=xb[0:n, :], out_offset=None, in_=hx2[:, :],
                    in_offset=bass.IndirectOffsetOnAxis(ap=idx[0:n, el, sti:sti + 1], axis=0)), ["idx"], [kx], dma=True)
                for kc in range(16):
                    P.t(lambda e, xb=xb, kc=kc, n=n: e.transpose(psT[:, kc, 0:n], xb[0:n, kc * 128:(kc + 1) * 128], identb[0:n, 0:n]), [kx, "identb"], ["psT"])
                P.a(lambda e, c0=c0, n=n: e.activation(out=xgT[:, :, c0:c0 + n], in_=psT[:, :, 0:n], func=AF.Copy), ["psT"], ["xgT"])
            for cb in range(4):
                rg = load_unit(wg, el, cb)
                ru = load_unit(wu, el, cb)
                for f4 in range(4):
                    fc = cb * 4 + f4
                    pa = psA[fc % 2]
                    pu = psU[fc % 2]
                    for (pp, rbuf, key) in ((pa, rg, "psA"), (pu, ru, "psU")):
                        for kc in range(16):
                            P.t(lambda e, pp=pp, rbuf=rbuf, kc=kc, f4=f4: e.matmul(pp[:, 0:ncols], lhsT=ring[rbuf][:, kc, f4 * 128:(f4 + 1) * 128],
                                                                                  rhs=xgT[:, kc, :], start=(kc == 0), stop=(kc == 15)),
                                [("ring", rbuf), "xgT"], [(key, fc % 2)])
                    P.a(lambda e, pa=pa: e.activation(out=sil[:], in_=pa[:, 0:ncols], func=AF.Silu), [("psA", fc % 2)], ["sil"])
                    P.v(lambda e, pu=pu, fc=fc: e.tensor_tensor(out=hT[:, fc, :], in0=sil[:], in1=pu[:, 0:ncols], op=ALU.mult),
                        ["sil", ("psU", fc % 2)], ["hT"])
            for cb in range(4):
                rd = load_unit(wd, el, cb)
                for ti, (sti, n, c0) in enumerate(tiles):
                    py = psY[ny[0] % 2]
                    ky = ("psY", ny[0] % 2)
                    ny[0] += 1
                    for fc in range(16):
                        P.t(lambda e, py=py, rd=rd, fc=fc, c0=c0, n=n: e.matmul(py[0:n, :], lhsT=hT[:, fc, c0:c0 + n], rhs=ring[rd][:, fc, :],
                                                                               start=(fc == 0), stop=(fc == 15)), ["hT", ("ring", rd)], [ky])
                    P.v(lambda e, py=py, ti=ti, el=el, sti=sti, n=n, cb=cb: e.tensor_scalar(out=ybig[ti][0:n, cb * 512:(cb + 1) * 512], in0=py[0:n, :],
                                                                                           scalar1=gate[0:n, el, sti:sti + 1], scalar2=None, op0=ALU.mult),
                        [ky, "gate"], [("ybig", ti)])
            for ti, (sti, n, c0) in enumerate(tiles):
                P.op("gpsimd", lambda e, ti=ti, el=el, sti=sti, n=n: e.indirect_dma_start(
                    out=delta[:, :], out_offset=bass.IndirectOffsetOnAxis(ap=idx[0:n, el, sti:sti + 1], axis=0),
                    in_=ybig[ti][0:n, :], in_offset=None, compute_op=ALU.add), [("ybig", ti), "idx", "delta"], ["delta"], dma=True)
        P.emit(fused=True)


def stage_copy_out(nc, src, dst, pfx):
    with stage_ctx(nc, True) as st:
        bufs = [st.enter_context(nc.sbuf_tensor(pfx + f"cb{i}", [128, D], F32)) for i in range(3)]
        P = Prog(nc)
        for t in range(16):
            b = bufs[t % 3]
            P.dma(lambda e, b=b, t=t: e.dma_start(out=b[:], in_=src[256 + t * 128:256 + (t + 1) * 128, :]), w=[("cb", t % 3)])
            P.dma(lambda e, b=b, t=t: e.dma_start(out=dst[t * 128:(t + 1) * 128, :], in_=b[:]), [("cb", t % 3)], [("out", t)])
        P.emit(fused=True)


def build_fused(ncores=8):
    global PAIRS
    PAIRS = [[2 * i, 2 * i + 1] for i in range(ncores // 2)]
    nc = bass.Bass("TRN2", target_bir_lowering=False)
    ein = lambda n, s, d=F32: nc.dram_tensor(n, s, d, kind="ExternalInput").ap()
    scr = lambda n, s, d=F32: nc.dram_tensor(n, s, d, kind="Internal").ap()
    scrl = lambda n, s, d=F32: nc.dram_tensor(n, s, d, kind="Internal", addr_space="Local").ap()
    X = {}
    X["xin0"] = ein("xin", [NTOK, D])
    X["cin"] = ein("cin", [128, 16, 2])
    X["w_mod"] = ein("w_mod", [2, D, 6 * D])
    X["b_mod"] = ein("b_mod", [2, 6 * D])
    n1g = ein("n1g", [2, D]); n2g = ein("n2g", [2, D])
    win = ein("win", [2, D, 3584])
    gqk = ein("gqk", [2, NQK * 128])
    X["cs_t"] = ein("cs_t", [NTOK, 128])
    vnb = ein("vnb", [2, 512]); wsT = ein("wsT", [2, 128, 4, 128]); bsT = ein("bsT", [2, 128, 4]); ogb = ein("ogb", [2, 512])
    X["ident"] = ein("ident", [128, 128])
    X["msk"] = ein("msk", [4, 128, 128])
    sink = ein("sink", [2, 6]); gac = ein("gac", [2, 1536])
    wout = ein("wout", [2, D, D]); wr = ein("wr", [2, D, NE])
    X["w_gate"] = ein("w_gate", [2, NE, D, D]); X["w_up"] = ein("w_up", [2, NE, D, D]); X["w_down"] = ein("w_down", [2, NE, D, D])
    X["consts"] = ein("consts", [128, NCST])
    out = nc.dram_tensor("out", [2048, D], F32, kind="ExternalOutput").ap()
    modS = scr("modS", [2, 2, 6 * D])
    X["modS"] = modS
    X["QKT"] = scr("QKT", [128, NQK, NTOK], BF16); X["Vout"] = scr("Vout", [NTOK, 512], BF16)
    X["yB"] = scr("yB", [NTOK, 512], BF16); X["yAC"] = scr("yAC", [NTOK, 1536], BF16)
    X["KTx"] = scr("KTx", [512, 2048], BF16); X["KTg"] = scrl("KTg", [1024, 2048], BF16)
    X["Vx"] = scr("Vx", [2048, 512], BF16); X["Vg"] = scrl("Vg", [4096, 512], BF16)
    xs = scr("xs", [NXR, D]); X["hx2o"] = scr("hx2s", [NXR, D], BF16); X["delta"] = scr("delta", [NXR, D])
    X["affIn"] = scr("affIn", [2048, NE]); X["affC"] = scr("affC", [256, NE]); X["affAll"] = scrl("affAll", [4096, NE])
    X["idxT"] = scr("idxT", [128, NE, 4], I32); X["gateT"] = scr("gateT", [128, NE, 4])

    stage_mod(nc, X, "m_")
    for L in range(2):
        io = dict(X)
        io["xin"] = X["xin0"] if L == 0 else xs
        io["modx"] = modS[L, 0].rearrange("(s d) -> s d", s=6)
        io["modc"] = modS[L, 1].rearrange("(s d) -> s d", s=6)
        io.update(n1g=n1g[L], n2g=n2g[L], win=win[L], gqk=gqk[L], vnb=vnb[L], wsT=wsT[L], bsT=bsT[L], ogb=ogb[L],
                  sink=sink[L], gac=gac[L], wout=wout[L], wr=wr[L], x1o=xs, x1=xs, dA=X["delta"], x2=xs)
        build_s1(nc=nc, io=io, pfx=f"L{L}a_")
        stage_cc(nc, [(X["KTx"], X["KTg"]), (X["Vx"], X["Vg"])])
        build_s2a(nc=nc, io=io, pfx=f"L{L}b_")
        build_s2b(nc=nc, io=io, pfx=f"L{L}c_")
        stage_cc(nc, [(X["affIn"], X["affAll"])])
        stage_s3a(nc, io, L == 0, f"L{L}d_")
        stage_s3b(nc, io, L, L == 0, f"L{L}e_")
        build_s4(nc=nc, io=io, pfx=f"L{L}f_")
    stage_copy_out(nc, xs, out, "o_")
    return nc


def kernel(x, c, ctx, c_ctx, w_mod, b_mod, norm1_g, norm2_g, w_in, qn_a, kn_a, sink_a, vn_b,
           w_s, b_s, qn_c, kn_c, out_g, w_out, w_router, w_gate, w_up, w_down):
    f32 = np.float32
    A = lambda a: np.ascontiguousarray(np.asarray(a, f32))
    x = A(x); ctx = A(ctx); c = A(c); c_ctx = A(c_ctx)
    cores = [(k // 2, k % 2) for k in range(8)]
    og = A(out_g)
    shared = {
        "w_mod": A(w_mod), "b_mod": A(b_mod), "n1g": A(norm1_g), "n2g": A(norm2_g),
        "win": A(np.asarray(w_in, f32)[:, :, WIN_PERM]),
        "gqk": A(np.stack([np.concatenate([np.tile(qn_a[L], 6), np.tile(kn_a[L], 2), np.tile(qn_c[L], 6), np.tile(kn_c[L], 2)]) for L in range(2)])),
        "vnb": A(vn_b), "wsT": A(np.asarray(w_s, f32).transpose(0, 3, 1, 2)), "bsT": A(np.asarray(b_s, f32).transpose(0, 2, 1)),
        "ogb": A(og[:, 768:1280]), "ident": np.eye(128, dtype=f32), "sink": A(sink_a),
        "gac": A(np.concatenate([og[:, :768], og[:, 1280:]], 1)), "wout": A(w_out), "wr": A(w_router),
        "w_gate": A(w_gate), "w_up": A(w_up), "w_down": A(w_down), "consts": f_consts(),
    }
    ims = []
    for (b, h) in cores:
        d = dict(shared)
        d["xin"] = A(np.concatenate([ctx[b], x[b, h * 2048:(h + 1) * 2048]], 0))
        c2 = np.stack([c[b], c_ctx], 0)
        d["cin"] = A(c2.T.reshape(16, 128, 2).transpose(1, 0, 2))
        d["cs_t"] = _rope_tables(h)
        d["msk"] = _masks(h)
        ims.append(d)
    ncores = _NCORES[0]
    nc = build_fused(ncores)
    res = run_bass_kernel_spmd(nc, ims[:ncores], core_ids=list(range(ncores)))
    out = np.zeros((4, SEQ, D), f32)
    for k, (b, h) in enumerate(cores[:ncores]):
        out[b, h * 2048:(h + 1) * 2048] = res.results[k]["out"]
    return out


_NCORES = [8]
```

```python
import contextlib
import numpy as np
import ml_dtypes
import concourse.bass as bass
import concourse.mybir as mybir
from concourse.bass_utils import run_bass_kernel_spmd

F32 = mybir.dt.float32
BF16 = mybir.dt.bfloat16
I32 = mybir.dt.int32
AF = mybir.ActivationFunctionType
ALU = mybir.AluOpType
AX = mybir.AxisListType

D = 2048
SEQ = 4096
CTX = 256
NT = 18
NTOK = NT * 128
EPS = 1e-6
NE = 16

ENGS = ("sync", "scalar", "vector", "gpsimd", "tensor")
SEM_CHUNK = 3000
NDMA_SEMS = 12


class _Op:
    __slots__ = ("eng", "fn", "deps", "is_dma", "has_dep", "sem", "val", "prev_dma")

    def __init__(self, eng, fn, is_dma):
        self.eng = eng
        self.fn = fn
        self.is_dma = is_dma
        self.deps = []
        self.has_dep = False
        self.sem = None
        self.val = None
        self.prev_dma = None


class Prog:
    def __init__(self, nc):
        self.nc = nc
        self.ops = {e: [] for e in ENGS}
        self.last_w = {}
        self.readers = {}
        self.ndma = {e: 0 for e in ENGS}
        self.dma_last = {}
        self.all_dmas = []

    def op(self, eng, fn, reads=(), writes=(), dma=False):
        o = _Op(eng, fn, dma)
        deps = []
        for r in reads:
            w = self.last_w.get(r)
            if w is not None:
                deps.append((w, "raw"))
        for k in writes:
            w = self.last_w.get(k)
            if w is not None:
                deps.append((w, "waw"))
            for rd in self.readers.get(k, ()):
                deps.append((rd, "war"))
        for (d, kind) in deps:
            if d is o:
                continue
            if not d.is_dma and not dma and d.eng == eng:
                if kind != "raw" or eng == "tensor":
                    continue
            if d not in o.deps:
                o.deps.append(d)
                d.has_dep = True
        for r in reads:
            self.readers.setdefault(r, []).append(o)
        for k in writes:
            self.last_w[k] = o
            self.readers[k] = []
        if dma:
            j = self.ndma[eng]
            self.ndma[eng] += 1
            slot = (eng, j % NDMA_SEMS)
            o.prev_dma = self.dma_last.get(slot)
            self.dma_last[slot] = o
            o.sem = slot
            o.val = 16 * (j // NDMA_SEMS + 1)
            self.all_dmas.append(o)
        self.ops[eng].append(o)
        return o

    def v(self, fn, r=(), w=()):
        return self.op("vector", fn, r, w)

    def a(self, fn, r=(), w=()):
        return self.op("scalar", fn, r, w)

    def g(self, fn, r=(), w=()):
        return self.op("gpsimd", fn, r, w)

    def t(self, fn, r=(), w=()):
        return self.op("tensor", fn, r, w)

    def dma(self, fn, r=(), w=(), q="sync"):
        return self.op(q, fn, r, w, dma=True)

    def emit(self, final_wait_eng="sync", fused=False):
        nc = self.nc
        sem_names = set()
        for e in ENGS:
            cnt = 0
            for o in self.ops[e]:
                if o.is_dma:
                    sem_names.add(o.sem)
                    continue
                if o.has_dep:
                    o.sem = (e, "c", cnt // SEM_CHUNK)
                    o.val = cnt % SEM_CHUNK + 1
                    sem_names.add(o.sem)
                    cnt += 1
        sem_names = sorted(sem_names, key=str)
        with contextlib.ExitStack() as st:
            sems = {}
            for n in sem_names:
                if fused:
                    _UID[0] += 1
                    sems[n] = nc.alloc_semaphore(name="f%d_" % _UID[0] + "_".join(str(x) for x in n))
                else:
                    sems[n] = st.enter_context(nc.semaphore("s_" + "_".join(str(x) for x in n)))
            block = st.enter_context(nc.Block())
            prog = self

            def run(e, eng):
                seen = {}
                for o in prog.ops[e]:
                    waits = []
                    if o.is_dma and o.prev_dma is not None:
                        waits.append(o.prev_dma)
                    waits.extend(o.deps)
                    for d in waits:
                        if seen.get(d.sem, 0) >= d.val:
                            continue
                        eng.wait_ge(sems[d.sem], d.val)
                        seen[d.sem] = d.val
                    ins = o.fn(eng)
                    if o.is_dma:
                        ins.then_inc(sems[o.sem], 16)
                    elif o.has_dep:
                        ins.then_inc(sems[o.sem], 1)
                if e == final_wait_eng:
                    last = {}
                    for o in prog.all_dmas:
                        last[o.sem] = max(last.get(o.sem, 0), o.val)
                    for s, v in last.items():
                        if seen.get(s, 0) < v:
                            eng.wait_ge(sems[s], v)

            @block.sync
            def _(eng):
                run("sync", eng)

            @block.scalar
            def _(eng):
                run("scalar", eng)

            @block.vector
            def _(eng):
                run("vector", eng)

            @block.gpsimd
            def _(eng):
                run("gpsimd", eng)

            @block.tensor
            def _(eng):
                run("tensor", eng)


_UID = [0]


@contextlib.contextmanager
def stage_ctx(nc, fused):
    if not fused:
        with contextlib.ExitStack() as st:
            yield st
    else:
        with nc.cleanup_on_exit():
            with contextlib.ExitStack() as st:
                yield st
            nc.all_engine_barrier()


def _mk(nc, io, pfx):
    fused = io is not None
    if fused:
        di = lambda n, s, d=F32: io[n]
        do = lambda n, s, d=F32: io[n]
    else:
        di = lambda n, s, d=F32: nc.dram_tensor(n, s, d, kind="ExternalInput").ap()
        do = lambda n, s, d=F32: nc.dram_tensor(n, s, d, kind="ExternalOutput").ap()
    return fused, di, do


def _bc(ap1d, n):
    return ap1d.unsqueeze(0).to_broadcast([128, n])


MCOL = 1536


def build_mod():
    nc = bass.Bass("TRN2", target_bir_lowering=False)
    cin = nc.dram_tensor("cin", [128, 16, 5], F32, kind="ExternalInput").ap()
    w = nc.dram_tensor("w", [2, D, MCOL], F32, kind="ExternalInput").ap()
    b = nc.dram_tensor("b", [2, MCOL], F32, kind="ExternalInput").ap()
    out = nc.dram_tensor("out", [2, 5, MCOL], F32, kind="ExternalOutput").ap()
    with contextlib.ExitStack() as st:
        sb = lambda n, s, d: st.enter_context(nc.sbuf_tensor(n, s, d))
        ct = sb("ct", [128, 16, 5], F32)
        cs = sb("cs", [128, 16, 5], F32)
        wt = [sb(f"wt{i}", [128, 16, 512], F32) for i in range(2)]
        bt = sb("bt", [5, 2, MCOL], F32)
        ot = sb("ot", [5, 2, MCOL], F32)
        ps = [st.enter_context(nc.psum_tensor(f"ps{i}", [5, 512], F32)) for i in range(2)]
        P = Prog(nc)
        P.dma(lambda e: e.dma_start(out=ct[:], in_=cin), w=["ct"])
        for l in range(2):
            P.dma(lambda e, l=l: e.dma_start(out=bt[:, l, :], in_=b[l:l + 1, :].to_broadcast([5, MCOL])), w=[("bt", l)])
        P.a(lambda e: e.activation(out=cs[:], in_=ct[:], func=AF.Silu), ["ct"], ["cs"])
        gi = 0
        for l in range(2):
            for g in range(3):
                buf = gi % 2
                src = w[l, :, g * 512:(g + 1) * 512].rearrange("(kc p) n -> p kc n", p=128)
                P.dma(lambda e, buf=buf, src=src: e.dma_start(out=wt[buf][:], in_=src), w=[("wt", buf)])
                for kc in range(16):
                    P.t(lambda e, buf=buf, kc=kc: e.matmul(ps[buf][:], lhsT=cs[:, kc, :], rhs=wt[buf][:, kc, :],
                                                           start=(kc == 0), stop=(kc == 15)),
                        ["cs", ("wt", buf)], [("ps", buf)])
                P.v(lambda e, buf=buf, l=l, g=g: e.tensor_tensor(out=ot[:, l, g * 512:(g + 1) * 512], in0=ps[buf][:],
                                                                 in1=bt[:, l, g * 512:(g + 1) * 512], op=ALU.add),
                    [("ps", buf), ("bt", l)], [("ot", l, g)])
                gi += 1
        for l in range(2):
            P.dma(lambda e, l=l: e.dma_start(out=out[l], in_=ot[:, l, :]), [("ot", l, g) for g in range(3)], [("out", l)])
        P.emit()
    return nc


def run_mod(c, c_ctx, w_mod, b_mod):
    c_all = np.concatenate([c, c_ctx[None, :]], axis=0).astype(np.float32)
    cin = np.ascontiguousarray(c_all.T.reshape(16, 128, 5).transpose(1, 0, 2))
    nc = build_mod()
    in_maps = [{"cin": cin, "w": np.ascontiguousarray(w_mod[:, :, j * MCOL:(j + 1) * MCOL]),
                "b": np.ascontiguousarray(b_mod[:, j * MCOL:(j + 1) * MCOL])} for j in range(8)]
    res = run_bass_kernel_spmd(nc, in_maps, core_ids=list(range(8)))
    return np.concatenate([r["out"] for r in res.results], axis=2)


def emit_rstd(P, ss, rstd, n, kr, kw):
    P.a(lambda e: e.activation(out=rstd, in_=ss, func=AF.Sqrt, scale=1.0 / n, bias=EPS), kr, kw)
    P.v(lambda e: e.reciprocal(out=rstd, in_=rstd), kw, kw)


NQK = 16
WIN_PERM = np.concatenate([np.arange(0, 1024), np.arange(2304, 3328),
                           np.arange(1024, 1280), np.arange(3328, 3584),
                           np.arange(1280, 2304)])


def build_s1(nc=None, io=None, pfx=""):
    if nc is None:
        nc = bass.Bass("TRN2", target_bir_lowering=False)
    fused, di, do = _mk(nc, io, pfx)
    xin = di("xin", [NTOK, D])
    modx = di("modx", [6, D])
    modc = di("modc", [6, D])
    n1g = di("n1g", [D])
    win = di("win", [D, 3584])
    gqk = di("gqk", [NQK * 128])
    cs_t = di("cs_t", [NTOK, 128])
    vnb = di("vnb", [512])
    wsT = di("wsT", [128, 4, 128])
    bsT = di("bsT", [128, 4])
    ogb = di("ogb", [512])
    ident_in = di("ident", [128, 128])
    QKT = do("QKT", [128, NQK, NTOK], BF16)
    Vout = do("Vout", [NTOK, 512], BF16)
    yB = do("yB", [NTOK, 512], BF16)

    with stage_ctx(nc, fused) as st:
        sb = lambda n, s, d=F32: st.enter_context(nc.sbuf_tensor(pfx + n, s, d))
        pt = lambda n, s, d=F32: st.enter_context(nc.psum_tensor(pfx + n, s, d))
        wbf = sb("wbf", [128, 16, 2048], BF16)
        G1 = sb("G1", [128, D])
        SH = sb("SH", [128, D])
        gn = sb("gn", [128, D])
        xt = [sb(f"xt{i}", [128, D]) for i in range(2)]
        tmp = sb("tmp", [128, D])
        hx = sb("hx", [128, D], BF16)
        hxT = sb("hxT", [128, 16, 128], BF16)
        ident = sb("ident_f", [128, 128])
        identb = sb("identb", [128, 128], BF16)
        ss = sb("ss", [128, 1])
        rs = sb("rs", [128, 1])
        Pb = sb("Pb", [128, 2048])
        gq = sb("gq", [128, NQK, 128])
        ssq = sb("ssq", [128, NQK])
        rsq = sb("rsq", [128, NQK])
        qn = sb("qn", [128, NQK, 2, 2, 32])
        cst = sb("cst", [128, 128])
        rA = sb("rA", [128, NQK, 2, 32])
        rB = sb("rB", [128, NQK, 2, 32])
        qr = sb("qr", [128, NQK, 2, 2, 32], BF16)
        qT = sb("qT", [128, NQK, 128], BF16)
        vb16 = sb("vb16", [128, 512], BF16)
        vng = sb("vng", [128, 512])
        wsb = sb("wsb", [128, 4, 128])
        wsbb = sb("wsbb", [128, 4, 128], BF16)
        bsb = sb("bsb", [128, 4])
        ogbt = sb("ogbt", [128, 512])
        t1 = sb("t1", [128, 1024])
        t2 = sb("t2", [128, 1024])
        gl = sb("gl", [128, 1024])
        ssv = sb("ssv", [128, 4])
        rsv = sb("rsv", [128, 4])
        vn = sb("vn", [128, 4, 128], BF16)
        ob = sb("ob", [128, 512])
        yb = sb("yb", [128, 512], BF16)
        pT = pt("pT", [128, 16, 128], BF16)
        pP = pt("pP", [128, 2048])
        pQ = pt("pQ", [128, NQK, 128], BF16)

        P = Prog(nc)
        P.dma(lambda e: e.dma_start(out=ident[:], in_=ident_in), w=["ident"])
        P.v(lambda e: e.tensor_copy(out=identb[:], in_=ident[:]), ["ident"], ["identb"])
        P.dma(lambda e: e.dma_start(out=gn[:], in_=_bc(n1g, D)), w=["gn"])
        P.dma(lambda e: e.dma_start(out=gq[:].rearrange("p h d -> p (h d)"), in_=_bc(gqk, NQK * 128)), w=["gq"])
        P.dma(lambda e: e.dma_start(out=vng[:], in_=_bc(vnb, 512)), w=["vng"])
        P.dma(lambda e: e.dma_start(out=ogbt[:], in_=_bc(ogb, 512)), w=["ogbt"])
        P.dma(lambda e: e.dma_start(out=wsb[:], in_=wsT), w=["wsb"])
        P.v(lambda e: e.tensor_copy(out=wsbb[:], in_=wsb[:]), ["wsb"], ["wsbb"])
        P.dma(lambda e: e.dma_start(out=bsb[:], in_=bsT), w=["bsb"])

        def load_w(c0, ncol):
            for k4 in range(4):
                src = win[k4 * 512:(k4 + 1) * 512, c0:c0 + ncol].rearrange("(kc p) n -> p kc n", p=128)
                P.dma(lambda e, k4=k4, src=src: e.dma_start(out=wbf[:, k4 * 4:(k4 + 1) * 4, 0:ncol], in_=src),
                      w=["wbf"], q="gpsimd")

        def load_mod(m):
            P.dma(lambda e: e.dma_start(out=SH[:], in_=_bc(m[0], D)), w=["SH"])
            P.dma(lambda e: e.dma_start(out=G1[:], in_=_bc(m[1], D)), w=["G1"])
            P.v(lambda e: e.scalar_tensor_tensor(out=G1[:], in0=G1[:], scalar=1.0, in1=gn[:], op0=ALU.add, op1=ALU.mult),
                ["G1", "gn"], ["G1"])

        def hx_tile(t):
            xb = xt[t % 2]
            kx = ("xt", t % 2)
            P.dma(lambda e: e.dma_start(out=xb[:], in_=xin[t * 128:(t + 1) * 128, :]), w=[kx])
            P.a(lambda e: e.activation(out=tmp[:], in_=xb[:], func=AF.Square, accum_out=ss[:]), [kx], ["tmp", "ss"])
            emit_rstd(P, ss[:], rs[:], D, ["ss"], ["rs"])
            P.v(lambda e: e.scalar_tensor_tensor(out=tmp[:], in0=xb[:], scalar=rs[:, 0:1], in1=G1[:], op0=ALU.mult, op1=ALU.mult),
                [kx, "rs", "G1"], ["tmp"])
            P.v(lambda e: e.tensor_tensor(out=hx[:], in0=tmp[:], in1=SH[:], op=ALU.add), ["tmp", "SH"], ["hx"])
            for kc in range(16):
                P.t(lambda e, kc=kc: e.transpose(pT[:, kc, :], hx[:, kc * 128:(kc + 1) * 128], identb[:]), ["hx", "identb"], ["pT"])
            P.a(lambda e: e.activation(out=hxT[:], in_=pT[:], func=AF.Copy), ["pT"], ["hxT"])

        def inproj(ncol):
            for g in range(ncol // 512):
                for kc in range(16):
                    P.t(lambda e, g=g, kc=kc: e.matmul(pP[:, g * 512:(g + 1) * 512], lhsT=hxT[:, kc, :],
                                                      rhs=wbf[:, kc, g * 512:(g + 1) * 512], start=(kc == 0), stop=(kc == 15)),
                        ["hxT", "wbf"], ["pP"])

        load_w(0, 2048)
        for t in range(NT):
            if t == 0:
                load_mod(modc)
            if t == 2:
                load_mod(modx)
            hx_tile(t)
            inproj(2048)
            P.a(lambda e: e.activation(out=Pb[:], in_=pP[:], func=AF.Copy), ["pP"], ["Pb"])
            Pv = Pb[:].rearrange("p (h d) -> p h d", h=NQK)
            P.dma(lambda e, t=t: e.dma_start(out=cst[:], in_=cs_t[t * 128:(t + 1) * 128, :]), w=["cst"])
            P.v(lambda e: e.tensor_tensor(out=tmp[:], in0=Pb[:], in1=Pb[:], op=ALU.mult), ["Pb"], ["tmp"])
            P.v(lambda e: e.tensor_reduce(out=ssq[:], in_=tmp[:].rearrange("p (h d) -> p h d", h=NQK), axis=AX.X, op=ALU.add),
                ["tmp"], ["ssq"])
            emit_rstd(P, ssq[:], rsq[:], 128, ["ssq"], ["rsq"])
            qnv = qn[:].rearrange("p h a b f -> p h (a b f)")
            P.v(lambda e: e.tensor_tensor(out=qnv, in0=Pv, in1=rsq[:].unsqueeze(2).to_broadcast([128, NQK, 128]), op=ALU.mult),
                ["Pb", "rsq"], ["qn"])
            P.v(lambda e: e.tensor_tensor(out=qnv, in0=qnv, in1=gq[:], op=ALU.mult), ["qn", "gq"], ["qn"])
            cosb = cst[:, 0:64].rearrange("p (a f) -> p a f", a=2).unsqueeze(1).to_broadcast([128, NQK, 2, 32])
            sinb = cst[:, 64:128].rearrange("p (a f) -> p a f", a=2).unsqueeze(1).to_broadcast([128, NQK, 2, 32])
            x1 = qn[:, :, :, 0, :]
            x2 = qn[:, :, :, 1, :]
            P.v(lambda e: e.tensor_tensor(out=rA[:], in0=x1, in1=cosb, op=ALU.mult), ["qn", "cst"], ["rA"])
            P.v(lambda e: e.tensor_tensor(out=rB[:], in0=x2, in1=sinb, op=ALU.mult), ["qn", "cst"], ["rB"])
            P.v(lambda e: e.tensor_tensor(out=qr[:, :, :, 0, :], in0=rA[:], in1=rB[:], op=ALU.subtract), ["rA", "rB"], ["qr"])
            P.v(lambda e: e.tensor_tensor(out=rA[:], in0=x2, in1=cosb, op=ALU.mult), ["qn", "cst", "qr"], ["rA"])
            P.v(lambda e: e.tensor_tensor(out=rB[:], in0=x1, in1=sinb, op=ALU.mult), ["qn", "cst", "qr"], ["rB"])
            P.v(lambda e: e.tensor_tensor(out=qr[:, :, :, 1, :], in0=rA[:], in1=rB[:], op=ALU.add), ["rA", "rB"], ["qr"])
            qrv = qr[:].rearrange("p h a b f -> p h (a b f)")
            for h in range(NQK):
                P.t(lambda e, h=h: e.transpose(pQ[:, h, :], qrv[:, h, :], identb[:]), ["qr", "identb"], ["pQ"])
            P.a(lambda e: e.activation(out=qT[:], in_=pQ[:], func=AF.Copy), ["pQ"], ["qT"])
            P.dma(lambda e, t=t: e.dma_start(out=QKT[:, :, t * 128:(t + 1) * 128], in_=qT[:]), ["qT"], [("QKT", t)])
            if fused and t >= 2:
                ktv = io["KTx"].rearrange("(h d) t -> d h t", d=128)
                c0 = (t - 2) * 128
                P.dma(lambda e, c0=c0: e.dma_start(out=ktv[:, 0:2, c0:c0 + 128], in_=qT[:, 6:8, :]), ["qT"], [("KTx", t, 0)])
                P.dma(lambda e, c0=c0: e.dma_start(out=ktv[:, 2:4, c0:c0 + 128], in_=qT[:, 14:16, :]), ["qT"], [("KTx", t, 1)])

        load_w(2048, 1536)
        for t in range(NT):
            if t == 0:
                load_mod(modc)
            if t == 2:
                load_mod(modx)
            hx_tile(t)
            inproj(1536)
            P.a(lambda e: e.activation(out=vb16[:], in_=pP[:, 0:512], func=AF.Copy), ["pP"], ["vb16"])
            P.dma(lambda e, t=t: e.dma_start(out=Vout[t * 128:(t + 1) * 128, :], in_=vb16[:]), ["vb16"], [("Vout", t)])
            if fused and t >= 2:
                P.dma(lambda e, t=t: e.dma_start(out=io["Vx"][(t - 2) * 128:(t - 1) * 128, :], in_=vb16[:]), ["vb16"], [("Vx", t)])
            P.a(lambda e: e.activation(out=gl[:], in_=pP[:, 512:1536], func=AF.Gelu_apprx_tanh), ["pP"], ["gl"])
            gv = gl[:, 512:1024]
            P.v(lambda e: e.tensor_tensor(out=t1[:, 0:512], in0=gv, in1=gv, op=ALU.mult), ["gl"], ["t1"])
            P.v(lambda e: e.tensor_reduce(out=ssv[:], in_=t1[:, 0:512].rearrange("p (g d) -> p g d", g=4), axis=AX.X, op=ALU.add),
                ["t1"], ["ssv"])
            emit_rstd(P, ssv[:], rsv[:], 128, ["ssv"], ["rsv"])
            P.v(lambda e: e.tensor_tensor(out=t1[:, 0:512].rearrange("p (g d) -> p g d", g=4), in0=gv.rearrange("p (g d) -> p g d", g=4),
                                          in1=rsv[:].unsqueeze(2).to_broadcast([128, 4, 128]), op=ALU.mult), ["gl", "rsv"], ["t1"])
            P.v(lambda e: e.tensor_tensor(out=vn[:].rearrange("p g d -> p (g d)"), in0=t1[:, 0:512], in1=vng[:], op=ALU.mult),
                ["t1", "vng"], ["vn"])
            for g in range(4):
                P.t(lambda e, g=g: e.matmul(pP[:, 1536 + g * 128:1536 + (g + 1) * 128], lhsT=wsbb[:, g, :], rhs=vn[:, g, :],
                                            start=True, stop=True), ["vn", "wsbb"], ["pM"])
            P.v(lambda e: e.tensor_tensor(out=ob[:].rearrange("p (g d) -> p g d", g=4),
                                          in0=pP[:, 1536:2048].rearrange("p (g d) -> p g d", g=4),
                                          in1=bsb[:].unsqueeze(2).to_broadcast([128, 4, 128]), op=ALU.add), ["pM", "bsb"], ["ob"])
            P.v(lambda e: e.tensor_tensor(out=ob[:], in0=ob[:], in1=gl[:, 0:512], op=ALU.mult), ["ob", "gl"], ["ob"])
            P.a(lambda e: e.activation(out=t2[:, 0:512], in_=ob[:], func=AF.Square, accum_out=ss[:]), ["ob"], ["t2", "ss"])
            emit_rstd(P, ss[:], rs[:], 512, ["ss"], ["rs"])
            P.v(lambda e: e.scalar_tensor_tensor(out=yb[:], in0=ob[:], scalar=rs[:, 0:1], in1=ogbt[:], op0=ALU.mult, op1=ALU.mult),
                ["ob", "rs", "ogbt"], ["yb"])
            P.dma(lambda e, t=t: e.dma_start(out=yB[t * 128:(t + 1) * 128, :], in_=yb[:]), ["yb"], [("yB", t)])
        P.emit(fused=fused)
    return nc


NKA = 20
NKC = 34


def build_s2a(nc=None, io=None, pfx=""):
    if nc is None:
        nc = bass.Bass("TRN2", target_bir_lowering=False)
    fused, di, do = _mk(nc, io, pfx)
    QKT = di("QKT", [128, NQK, NTOK], BF16)
    KA = None if fused else di("KA", [128, 2, NKA * 128], BF16)
    VA = None if fused else di("VA", [NKA * 128, 256], BF16)
    KC = None if fused else di("KC", [128, 2, NKC * 128], BF16)
    VC = None if fused else di("VC", [NKC * 128, 256], BF16)
    msk = di("msk", [4, 128, 128])
    sink = di("sink", [6])
    gac = di("gac", [1536])
    yAC = do("yAC", [NTOK, 1536], BF16)
    with stage_ctx(nc, fused) as st:
        sb = lambda n, s, d=F32: st.enter_context(nc.sbuf_tensor(pfx + n, s, d))
        pt = lambda n, s, d=F32: st.enter_context(nc.psum_tensor(pfx + n, s, d))
        ka = sb("ka", [128, 2, NKA * 128], BF16)
        va = sb("va", [128, NKA, 2, 129], BF16)
        kc = sb("kc", [128, 2, NKC * 128], BF16)
        vc = sb("vc", [128, NKC, 2, 129], BF16)
        mf = sb("mf", [128, 4, 128])
        mb = sb("mb", [128, 4, 128], BF16)
        sk = sb("sk", [128, 6])
        es = sb("es", [128, 6])
        gt = sb("gt", [128, 1536])
        qt = [sb(f"qt{i}", [128, NQK, 128], BF16) for i in range(2)]
        PT = [sb(f"PT{i}", [128, 3, 128], BF16) for i in range(3)]
        oo = sb("oo", [128, 2, 6, 128])
        den = sb("den", [128, 3])
        sq = sb("sq", [128, 768])
        ss = sb("ss", [128, 1])
        rs = sb("rs", [128, 1])
        yt = sb("yt", [128, 1536], BF16)
        psS = [pt(f"psS{i}", [128, 512]) for i in range(3)]
        psO = [pt(f"psO{i}", [128, 3, 129]) for i in range(2)]
        P = Prog(nc)
        if not fused:
            P.dma(lambda e: e.dma_start(out=ka[:], in_=KA), w=["ka"])
            P.dma(lambda e: e.dma_start(out=kc[:], in_=KC), w=["kc"])
        else:
            KTg = io["KTg"].rearrange("(r h d) t -> d r h t", r=2, h=4)
            Vg = io["Vg"]
            Vo = io["Vout"]
            for k in range(2):
                P.dma(lambda e, k=k: e.dma_start(out=ka[:, k, 0:256], in_=QKT[:, 6 + k, 0:256]), w=["ka"])
                P.dma(lambda e, k=k: e.dma_start(out=ka[:, k, 256:384], in_=KTg[:, 0, k, 1920:2048]), w=["ka"])
                P.dma(lambda e, k=k: e.dma_start(out=ka[:, k, 384:2432], in_=QKT[:, 6 + k, 256:2304]), w=["ka"])
                P.dma(lambda e, k=k: e.dma_start(out=ka[:, k, 2432:2560], in_=KTg[:, 1, k, 0:128]), w=["ka"])
                P.dma(lambda e, k=k: e.dma_start(out=kc[:, k, 0:256], in_=QKT[:, 14 + k, 0:256]), w=["kc"])
                for r in range(2):
                    P.dma(lambda e, k=k, r=r: e.dma_start(out=kc[:, k, 256 + r * 2048:256 + (r + 1) * 2048], in_=KTg[:, r, 2 + k, :]), w=["kc"])
        P.v(lambda e: e.memset(va[:, :, :, 128:129], 1.0), [], ["va1"])
        P.v(lambda e: e.memset(vc[:, :, :, 128:129], 1.0), [], ["vc1"])
        for k in range(2):
            if not fused:
                P.dma(lambda e, k=k: e.dma_start(out=va[:, :, k, 0:128], in_=VA[:, k * 128:(k + 1) * 128].rearrange("(j s) d -> s j d", s=128)), w=[("va", k)])
                P.dma(lambda e, k=k: e.dma_start(out=vc[:, :, k, 0:128], in_=VC[:, k * 128:(k + 1) * 128].rearrange("(j s) d -> s j d", s=128)), w=[("vc", k)])
            else:
                ca = slice(k * 128, (k + 1) * 128)
                cc = slice(256 + k * 128, 256 + (k + 1) * 128)
                tl = lambda ap: ap.rearrange("(j s) d -> s j d", s=128)
                P.dma(lambda e, k=k, ca=ca: e.dma_start(out=va[:, 0:2, k, 0:128], in_=tl(Vo[0:256, ca])), w=[("va", k)])
                P.dma(lambda e, k=k, ca=ca: e.dma_start(out=va[:, 2:3, k, 0:128], in_=tl(Vg[1920:2048, ca])), w=[("va", k)])
                P.dma(lambda e, k=k, ca=ca: e.dma_start(out=va[:, 3:19, k, 0:128], in_=tl(Vo[256:2304, ca])), w=[("va", k)])
                P.dma(lambda e, k=k, ca=ca: e.dma_start(out=va[:, 19:20, k, 0:128], in_=tl(Vg[2048:2176, ca])), w=[("va", k)])
                P.dma(lambda e, k=k, cc=cc: e.dma_start(out=vc[:, 0:2, k, 0:128], in_=tl(Vo[0:256, cc])), w=[("vc", k)])
                P.dma(lambda e, k=k, cc=cc: e.dma_start(out=vc[:, 2:34, k, 0:128], in_=tl(Vg[:, cc])), w=[("vc", k)])
        P.dma(lambda e: e.dma_start(out=mf[:], in_=msk.rearrange("m s q -> s m q")), w=["mf"])
        P.v(lambda e: e.tensor_copy(out=mb[:], in_=mf[:]), ["mf"], ["mb"])
        P.dma(lambda e: e.dma_start(out=sk[:], in_=_bc(sink, 6)), w=["sk"])
        P.a(lambda e: e.activation(out=es[:], in_=sk[:], func=AF.Exp), ["sk"], ["es"])
        P.dma(lambda e: e.dma_start(out=gt[:], in_=_bc(gac, 1536)), w=["gt"])
        PF = 2
        NBUF = PF + 1
        steps = []
        for t in range(NT):
            if t < 2:
                keysA = [(0, None), (1, None)]
                keysC = [(0, None), (1, None)]
            else:
                i = t - 2
                keysA = [(0, None), (1, None), (2 + i, 2 if i == 0 else 0), (3 + i, None), (4 + i, 3 if i == 15 else 1)]
                keysC = [(j, None) for j in range(NKC)]
            for (oi, qbase, KT, VT, kkey, vkeys, keys, use_sink) in (
                    (0, 0, ka, va, "ka", [("va", 0), ("va", 1), "va1"], keysA, True),
                    (1, 8, kc, vc, "kc", [("vc", 0), ("vc", 1), "vc1"], keysC, False)):
                for k in range(2):
                    for n, (j, m) in enumerate(keys):
                        steps.append(dict(t=t, oi=oi, qbase=qbase, KT=KT, VT=VT, kkey=kkey, vkeys=vkeys, k=k, n=n, j=j, m=m,
                                          last=(n == len(keys) - 1), use_sink=use_sink,
                                          tile_first=(oi == 0 and k == 0 and n == 0), tile_last=(oi == 1 and k == 1 and n == len(keys) - 1)))
        ngrp = [0]

        def load_q(t):
            q = qt[t % 2]
            P.dma(lambda e, q=q, t=t: e.dma_start(out=q[:], in_=QKT[:, :, t * 128:(t + 1) * 128]), w=[("qt", t % 2)])

        def s1(i):
            sp = steps[i]
            t, k, j, m = sp["t"], sp["k"], sp["j"], sp["m"]
            if sp["tile_first"]:
                if t == 0:
                    load_q(0)
                if t + 1 < NT:
                    load_q(t + 1)
            q = qt[t % 2]
            b = i % NBUF
            KT, qbase = sp["KT"], sp["qbase"]
            pS = psS[b][:, 0:384].rearrange("p (g q) -> p g q", g=3)
            P.t(lambda e: e.matmul(pS, lhsT=KT[:, k, j * 128:(j + 1) * 128], rhs=q[:, qbase + 3 * k:qbase + 3 * k + 3, :], start=True, stop=True),
                [sp["kkey"], ("qt", t % 2)], [("psS", b)])
            P.a(lambda e: e.activation(out=PT[b][:], in_=pS, func=AF.Exp, scale=128.0 ** -0.5), [("psS", b)], [("PT", b)])
            if m is not None:
                P.v(lambda e: e.tensor_tensor(out=PT[b][:], in0=PT[b][:], in1=mb[:, m, :].unsqueeze(1).to_broadcast([128, 3, 128]), op=ALU.mult),
                    [("PT", b), "mb"], [("PT", b)])

        def s2(i):
            sp = steps[i]
            t, k, j, n, oi = sp["t"], sp["k"], sp["j"], sp["n"], sp["oi"]
            b = i % NBUF
            VT = sp["VT"]
            pb = ngrp[0] % 2
            po = psO[pb]
            for g in range(3):
                P.t(lambda e, g=g: e.matmul(po[:, g, :], lhsT=PT[b][:, g, :], rhs=VT[:, j, k, :], start=(n == 0 and g == 0), stop=sp["last"]),
                    [("PT", b)] + sp["vkeys"], [("psO", pb)])
            if sp["last"]:
                ngrp[0] += 1
                if sp["use_sink"]:
                    P.v(lambda e: e.tensor_tensor(out=den[:], in0=po[:, :, 128], in1=es[:, 3 * k:3 * k + 3], op=ALU.add), [("psO", pb), "es"], ["den"])
                else:
                    P.v(lambda e: e.tensor_copy(out=den[:], in_=po[:, :, 128]), [("psO", pb)], ["den"])
                P.v(lambda e: e.reciprocal(out=den[:], in_=den[:]), ["den"], ["den"])
                P.v(lambda e: e.tensor_tensor(out=oo[:, oi, 3 * k:3 * k + 3, :], in0=po[:, :, 0:128], in1=den[:].unsqueeze(2).to_broadcast([128, 3, 128]),
                                              op=ALU.mult), [("psO", pb), "den"], [("oo", oi)])
            if sp["tile_last"]:
                for o2 in range(2):
                    ov = oo[:, o2, :, :].rearrange("p h d -> p (h d)")
                    P.v(lambda e, ov=ov: e.tensor_tensor(out=sq[:], in0=ov, in1=ov, op=ALU.mult), [("oo", o2)], ["sq"])
                    P.v(lambda e: e.tensor_reduce(out=ss[:], in_=sq[:], axis=AX.X, op=ALU.add), ["sq"], ["ss"])
                    emit_rstd(P, ss[:], rs[:], 768, ["ss"], ["rs"])
                    P.v(lambda e, ov=ov, o2=o2: e.scalar_tensor_tensor(out=yt[:, o2 * 768:(o2 + 1) * 768], in0=ov, scalar=rs[:, 0:1],
                                                                      in1=gt[:, o2 * 768:(o2 + 1) * 768], op0=ALU.mult, op1=ALU.mult),
                        [("oo", o2), "rs", "gt"], ["yt"])
                P.dma(lambda e: e.dma_start(out=yAC[t * 128:(t + 1) * 128, :], in_=yt[:]), ["yt"], [("yAC", t)])

        for i in range(min(PF, len(steps))):
            s1(i)
        for i in range(len(steps)):
            if i + PF < len(steps):
                s1(i + PF)
            s2(i)
        P.emit(fused=fused)
    return nc


def build_s2b(nc=None, io=None, pfx=""):
    if nc is None:
        nc = bass.Bass("TRN2", target_bir_lowering=False)
    fused, di, do = _mk(nc, io, pfx)
    yAC = di("yAC", [NTOK, 1536], BF16)
    yB = di("yB", [NTOK, 512], BF16)
    xin = di("xin", [NTOK, D])
    modx = di("modx", [6, D])
    modc = di("modc", [6, D])
    wout = di("wout", [D, D])
    n2g = di("n2g", [D])
    wr = di("wr", [D, NE])
    ident_in = di("ident", [128, 128])
    x1o = do("x1o", [NTOK, D])
    hx2o = do("hx2o", [NTOK, D], BF16)
    affT = None if fused else do("affT", [NE, NTOK])
    with stage_ctx(nc, fused) as st:
        sb = lambda n, s, d=F32: st.enter_context(nc.sbuf_tensor(pfx + n, s, d))
        pt = lambda n, s, d=F32: st.enter_context(nc.psum_tensor(pfx + n, s, d))
        wbf = sb("wbf", [128, 16, D], BF16)
        G1g = sb("G1g", [128, D])
        G2 = sb("G2", [128, D])
        SH2 = sb("SH2", [128, D])
        gn = sb("gn", [128, D])
        ident = sb("ident_f", [128, 128])
        identb = sb("identb", [128, 128], BF16)
        wrs = sb("wrs", [128, 16, NE])
        xt = sb("xt", [128, D])
        yt = sb("yt", [128, D], BF16)
        yT = sb("yT", [128, 16, 128], BF16)
        tmp = sb("tmp", [128, D])
        x1 = sb("x1", [128, D])
        h2 = sb("h2", [128, D])
        hb = sb("hb", [128, D], BF16)
        h2T = sb("h2T", [128, 16, 128])
        ss = sb("ss", [128, 1])
        rs = sb("rs", [128, 1])
        mx = sb("mx", [128, 1])
        se = sb("se", [128, 1])
        ex = sb("ex", [128, NE])
        af = sb("af", [128, NE])
        aT = sb("aT", [NE, 128])
        psT = pt("psT", [128, 16, 128], BF16)
        psX = [pt(f"psX{i}", [128, 512]) for i in range(2)]
        psR = [pt(f"psR{i}", [128, 4, 128]) for i in range(2)]
        psL = pt("psL", [128, 512])
        psA = pt("psA", [128, 512])
        P = Prog(nc)
        P.dma(lambda e: e.dma_start(out=ident[:], in_=ident_in), w=["ident"])
        P.v(lambda e: e.tensor_copy(out=identb[:], in_=ident[:]), ["ident"], ["identb"])
        P.dma(lambda e: e.dma_start(out=gn[:], in_=_bc(n2g, D)), w=["gn"])
        P.dma(lambda e: e.dma_start(out=wrs[:], in_=wr.rearrange("(kc p) n -> p kc n", p=128)), w=["wrs"])
        for k4 in range(4):
            src = wout[k4 * 512:(k4 + 1) * 512, :].rearrange("(kc p) n -> p kc n", p=128)
            P.dma(lambda e, k4=k4, src=src: e.dma_start(out=wbf[:, k4 * 4:(k4 + 1) * 4, :], in_=src), w=["wbf"], q="gpsimd")

        def load_mod(m):
            P.dma(lambda e: e.dma_start(out=G1g[:], in_=_bc(m[2], D)), w=["G1g"])
            P.dma(lambda e: e.dma_start(out=SH2[:], in_=_bc(m[3], D)), w=["SH2"])
            P.dma(lambda e: e.dma_start(out=G2[:], in_=_bc(m[4], D)), w=["G2"])
            P.v(lambda e: e.scalar_tensor_tensor(out=G2[:], in0=G2[:], scalar=1.0, in1=gn[:], op0=ALU.add, op1=ALU.mult),
                ["G2", "gn"], ["G2"])

        for t in range(NT):
            if t == 0:
                load_mod(modc)
            if t == 2:
                load_mod(modx)
            r0 = t * 128
            P.dma(lambda e, r0=r0: e.dma_start(out=yt[:, 0:768], in_=yAC[r0:r0 + 128, 0:768]), w=[("yt", 0)])
            P.dma(lambda e, r0=r0: e.dma_start(out=yt[:, 768:1280], in_=yB[r0:r0 + 128, :]), w=[("yt", 1)])
            P.dma(lambda e, r0=r0: e.dma_start(out=yt[:, 1280:2048], in_=yAC[r0:r0 + 128, 768:1536]), w=[("yt", 2)])
            P.dma(lambda e, r0=r0: e.dma_start(out=xt[:], in_=xin[r0:r0 + 128, :]), w=["xt"])
            for kc in range(16):
                P.t(lambda e, kc=kc: e.transpose(psT[:, kc, :], yt[:, kc * 128:(kc + 1) * 128], identb[:]),
                    [("yt", 0), ("yt", 1), ("yt", 2), "identb"], ["psT"])
            P.a(lambda e: e.activation(out=yT[:], in_=psT[:], func=AF.Copy), ["psT"], ["yT"])
            for g in range(4):
                px = psX[g % 2]
                for kc in range(16):
                    P.t(lambda e, px=px, g=g, kc=kc: e.matmul(px[:], lhsT=yT[:, kc, :], rhs=wbf[:, kc, g * 512:(g + 1) * 512],
                                                              start=(kc == 0), stop=(kc == 15)), ["yT", "wbf"], [("psX", g % 2)])
                P.v(lambda e, px=px, g=g: e.tensor_tensor(out=tmp[:, g * 512:(g + 1) * 512], in0=px[:], in1=G1g[:, g * 512:(g + 1) * 512],
                                                          op=ALU.mult), [("psX", g % 2), "G1g"], ["tmp"])
            P.v(lambda e: e.tensor_tensor(out=x1[:], in0=tmp[:], in1=xt[:], op=ALU.add), ["tmp", "xt"], ["x1"])
            P.dma(lambda e, r0=r0: e.dma_start(out=x1o[r0:r0 + 128, :], in_=x1[:]), ["x1"], [("x1o", t)])
            P.a(lambda e: e.activation(out=tmp[:], in_=x1[:], func=AF.Square, accum_out=ss[:]), ["x1"], ["tmp", "ss"])
            emit_rstd(P, ss[:], rs[:], D, ["ss"], ["rs"])
            P.v(lambda e: e.scalar_tensor_tensor(out=h2[:], in0=x1[:], scalar=rs[:, 0:1], in1=G2[:], op0=ALU.mult, op1=ALU.mult),
                ["x1", "rs", "G2"], ["h2"])
            P.v(lambda e: e.tensor_tensor(out=h2[:], in0=h2[:], in1=SH2[:], op=ALU.add), ["h2", "SH2"], ["h2"])
            P.a(lambda e: e.activation(out=hb[:], in_=h2[:], func=AF.Copy), ["h2"], ["hb"])
            P.dma(lambda e, r0=r0: e.dma_start(out=hx2o[r0:r0 + 128, :], in_=hb[:]), ["hb"], [("hx2o", t)])
            for q4 in range(4):
                pr = psR[q4 % 2]
                for j in range(4):
                    kc = q4 * 4 + j
                    P.t(lambda e, pr=pr, j=j, kc=kc: e.transpose(pr[:, j, :], h2[:, kc * 128:(kc + 1) * 128], ident[:]),
                        ["h2", "ident"], [("psR", q4 % 2)])
                P.a(lambda e, pr=pr, q4=q4: e.activation(out=h2T[:, q4 * 4:(q4 + 1) * 4, :], in_=pr[:], func=AF.Copy),
                    [("psR", q4 % 2)], ["h2T"])
            for kc in range(16):
                P.t(lambda e, kc=kc: e.matmul(psL[:, 0:NE], lhsT=h2T[:, kc, :], rhs=wrs[:, kc, :], start=(kc == 0), stop=(kc == 15)),
                    ["h2T", "wrs"], ["psL"])
            P.v(lambda e: e.tensor_reduce(out=mx[:], in_=psL[:, 0:NE], axis=AX.X, op=ALU.max), ["psL"], ["mx"])
            P.v(lambda e: e.tensor_scalar(out=mx[:], in0=mx[:], scalar1=-1.0, scalar2=None, op0=ALU.mult), ["mx"], ["mx"])
            P.a(lambda e: e.activation(out=ex[:], in_=psL[:, 0:NE], func=AF.Exp, bias=mx[:, 0:1], accum_out=se[:]), ["psL", "mx"], ["ex", "se"])
            P.v(lambda e: e.reciprocal(out=se[:], in_=se[:]), ["se"], ["se"])
            P.v(lambda e: e.tensor_scalar(out=af[:], in0=ex[:], scalar1=se[:, 0:1], scalar2=None, op0=ALU.mult), ["ex", "se"], ["af"])
            if fused:
                adst = io["affC"][r0:r0 + 128, :] if t < 2 else io["affIn"][r0 - 256:r0 - 128, :]
                P.dma(lambda e, adst=adst: e.dma_start(out=adst, in_=af[:]), ["af"], [("affo", t)])
            else:
                P.t(lambda e: e.transpose(psA[0:NE, 0:128], af[:], ident[:]), ["af", "ident"], ["psA"])
                P.a(lambda e: e.activation(out=aT[:], in_=psA[0:NE, 0:128], func=AF.Copy), ["psA"], ["aT"])
                P.dma(lambda e, r0=r0: e.dma_start(out=affT[:, r0:r0 + 128], in_=aT[:]), ["aT"], [("affT", t)])
        P.emit(fused=fused)
    return nc


NEL = 8
NROW = SEQ + CTX


def build_s3(with_ctx, nc=None, io=None, pfx=""):
    if nc is None:
        nc = bass.Bass("TRN2", target_bir_lowering=False)
    fused, di, do = _mk(nc, io, pfx)
    affL = di("affL", [128, 32, NEL])
    affC = di("affC", [128, 2, NEL])
    hx2 = di("hx2", [NROW, D], BF16)
    wg = di("wg", [NEL, D, D])
    wu = di("wu", [NEL, D, D])
    wd = di("wd", [NEL, D, D])
    consts = di("consts", [128, 128 + 128 + 128 + 512 + 34])
    delta = [do(f"delta{i}", [NROW, 512]) for i in range(4)]
    sets = [("L", 32, 512, 0, affL)]
    if with_ctx:
        sets.append(("C", 2, 32, SEQ, affC))
    with stage_ctx(nc, fused) as st:
        sb = lambda n, s, d=F32: st.enter_context(nc.sbuf_tensor(pfx + n, s, d))
        pt = lambda n, s, d=F32: st.enter_context(nc.psum_tensor(pfx + n, s, d))
        cst = sb("cst", [128, 930])
        identb = sb("identb", [128, 128], BF16)
        zt = sb("zt", [128, D])
        ring = [sb(f"ring{i}", [128, 16, 512], BF16) for i in range(4)]
        xg = [sb(f"xg{i}", [128, D], BF16) for i in range(2)]
        xgT = sb("xgT", [128, 16, 544], BF16)
        hT = sb("hT", [128, 16, 544], BF16)
        sil = sb("sil", [128, 544])
        yb = [sb(f"yb{i}", [128, 512]) for i in range(3)]
        S = {}
        for (nm, C, k, r0, _) in sets:
            S[nm] = dict(
                aff=sb(f"saff{nm}", [128, C, NEL]), lo=sb(f"lo{nm}", [128, NEL]), th=sb(f"th{nm}", [128, NEL, 15]),
                cmp=sb(f"cmp{nm}", [128, C, NEL, 15]), cntp=sb(f"cntp{nm}", [128, NEL, 15]), ge=sb(f"ge{nm}", [128, NEL, 15]),
                nge=sb(f"nge{nm}", [128, NEL]), mask=sb(f"mask{nm}", [128, C, NEL]), maskb=sb(f"maskb{nm}", [128, C, NEL], BF16),
                cA=sb(f"cA{nm}", [128, C, NEL]), cB=sb(f"cB{nm}", [128, C, NEL]), pos=sb(f"pos{nm}", [128, C, NEL]),
                vals=sb(f"vals{nm}", [128, C, NEL, 2]), oh=[sb(f"oh{nm}{i}", [128, k]) for i in range(2)],
                sl=sb(f"sl{nm}", [128, NEL, 4, 2]), idx=sb(f"idx{nm}", [128, NEL, 4], I32), gate=sb(f"gate{nm}", [128, NEL, 4]))
        jt = sb("jt", [128, NEL, 15])
        trib = sb("trib", [128, 128], BF16)
        onesb = sb("onesb", [128, 128], BF16)
        psA = [pt(f"psA{i}", [128, 512]) for i in range(2)]
        psU = [pt(f"psU{i}", [128, 512]) for i in range(2)]
        psT = pt("psT", [128, 16, 128], BF16)
        psY = [pt(f"psY{i}", [128, 512]) for i in range(2)]
        ident = cst[:, 0:128]
        ones = cst[:, 128:256]
        tri = cst[:, 256:384]
        iota = cst[:, 384:896]
        tokid = cst[:, 896:930]
        P = Prog(nc)
        P.dma(lambda e: e.dma_start(out=cst[:], in_=consts), w=["cst"])
        P.v(lambda e: e.tensor_copy(out=identb[:], in_=ident), ["cst"], ["identb"])
        P.v(lambda e: e.tensor_copy(out=trib[:], in_=tri), ["cst"], ["trib"])
        P.v(lambda e: e.tensor_copy(out=onesb[:], in_=ones), ["cst"], ["onesb"])
        for j in range(15):
            P.v(lambda e, j=j: e.memset(jt[:, :, j:j + 1], float(j + 1)), [], ["jt"])
        P.v(lambda e: e.memset(zt[:], 0.0), [], ["zt"])
        for cb in range(4):
            for r in range(NROW // 128):
                P.dma(lambda e, r=r, cb=cb: e.dma_start(out=delta[cb][r * 128:(r + 1) * 128, :], in_=zt[:, 0:512]), ["zt"], [("delta", cb)])

        def select(nm, C, k, r0, affd):
            s = S[nm]
            kk = lambda x: (nm, x)
            P.dma(lambda e, s=s, affd=affd: e.dma_start(out=s["aff"][:], in_=affd), w=[kk("aff")])
            P.v(lambda e, s=s: e.memset(s["lo"][:], 0.0), [], [kk("lo")])
            affb = s["aff"][:].unsqueeze(3).to_broadcast([128, C, NEL, 15])
            for it in range(8):
                step = 16.0 ** -(it + 1)
                P.v(lambda e, s=s, step=step: e.scalar_tensor_tensor(out=s["th"][:], in0=jt[:], scalar=step,
                                                                     in1=s["lo"][:].unsqueeze(2).to_broadcast([128, NEL, 15]),
                                                                     op0=ALU.mult, op1=ALU.add), ["jt", kk("lo")], [kk("th")])
                P.v(lambda e, s=s: e.tensor_tensor(out=s["cmp"][:], in0=affb, in1=s["th"][:].unsqueeze(1).to_broadcast([128, C, NEL, 15]),
                                                   op=ALU.is_ge), [kk("aff"), kk("th")], [kk("cmp")])
                P.v(lambda e, s=s: e.tensor_reduce(out=s["cntp"][:], in_=s["cmp"][:].rearrange("p c e j -> p e j c"), axis=AX.X, op=ALU.add),
                    [kk("cmp")], [kk("cntp")])
                P.t(lambda e, s=s: e.matmul(psY[0][:, 0:NEL * 15], lhsT=ones, rhs=s["cntp"][:].rearrange("p e j -> p (e j)"), start=True, stop=True),
                    ["cst", kk("cntp")], [("psY", 0)])
                P.v(lambda e, s=s, k=k: e.tensor_scalar(out=s["ge"][:].rearrange("p e j -> p (e j)"), in0=psY[0][:, 0:NEL * 15], scalar1=float(k) - 0.5,
                                                        scalar2=None, op0=ALU.is_ge), [("psY", 0)], [kk("ge")])
                P.v(lambda e, s=s: e.tensor_reduce(out=s["nge"][:], in_=s["ge"][:], axis=AX.X, op=ALU.add), [kk("ge")], [kk("nge")])
                P.v(lambda e, s=s, step=step: e.scalar_tensor_tensor(out=s["lo"][:], in0=s["nge"][:], scalar=step, in1=s["lo"][:],
                                                                     op0=ALU.mult, op1=ALU.add), [kk("nge"), kk("lo")], [kk("lo")])
            P.v(lambda e, s=s: e.tensor_tensor(out=s["mask"][:], in0=s["aff"][:], in1=s["lo"][:].unsqueeze(1).to_broadcast([128, C, NEL]),
                                               op=ALU.is_ge), [kk("aff"), kk("lo")], [kk("mask")])
            P.v(lambda e, s=s: e.tensor_copy(out=s["maskb"][:], in_=s["mask"][:]), [kk("mask")], [kk("maskb")])
            mb2 = s["maskb"][:].rearrange("p c e -> p (c e)")
            P.t(lambda e: e.matmul(psY[0][:, 0:C * NEL], lhsT=trib[:], rhs=mb2, start=True, stop=True), ["trib", kk("maskb")], [("psY", 0)])
            P.t(lambda e: e.matmul(psY[1][:, 0:C * NEL], lhsT=onesb[:], rhs=mb2, start=True, stop=True), ["onesb", kk("maskb")], [("psY", 1)])
            P.v(lambda e, s=s: e.tensor_copy(out=s["cA"][:].rearrange("p c e -> p (c e)"), in_=psY[1][:, 0:C * NEL]), [("psY", 1)], [kk("cA")])
            cur, nxt = "cA", "cB"
            sh = 1
            while sh < C:
                P.v(lambda e, s=s, cur=cur, nxt=nxt, sh=sh: e.tensor_copy(out=s[nxt][:, 0:sh, :], in_=s[cur][:, 0:sh, :]), [kk(cur)], [kk(nxt)])
                P.v(lambda e, s=s, cur=cur, nxt=nxt, sh=sh: e.tensor_tensor(out=s[nxt][:, sh:C, :], in0=s[cur][:, sh:C, :], in1=s[cur][:, 0:C - sh, :],
                                                                          op=ALU.add), [kk(cur)], [kk(nxt)])
                cur, nxt = nxt, cur
                sh *= 2
            P.v(lambda e, s=s, cur=cur: e.tensor_tensor(out=s["pos"][:].rearrange("p c e -> p (c e)"), in0=s[cur][:].rearrange("p c e -> p (c e)"),
                                                        in1=psY[1][:, 0:C * NEL], op=ALU.subtract), [kk(cur), ("psY", 1)], [kk("pos")])
            P.v(lambda e, s=s: e.tensor_tensor(out=s["pos"][:].rearrange("p c e -> p (c e)"), in0=s["pos"][:].rearrange("p c e -> p (c e)"),
                                               in1=psY[0][:, 0:C * NEL], op=ALU.add), [kk("pos"), ("psY", 0)], [kk("pos")])
            P.v(lambda e, s=s: e.tensor_tensor(out=s["pos"][:], in0=s["pos"][:], in1=s["mask"][:], op=ALU.mult), [kk("pos"), kk("mask")], [kk("pos")])
            P.v(lambda e, s=s: e.tensor_scalar(out=s["pos"][:], in0=s["pos"][:], scalar1=-1.0, scalar2=None, op0=ALU.add), [kk("pos")], [kk("pos")])
            P.v(lambda e, s=s, r0=r0: e.tensor_scalar(out=s["vals"][:, :, :, 0], in0=tokid[:, 0:C].unsqueeze(2).to_broadcast([128, C, NEL]),
                                                      scalar1=float(r0), scalar2=None, op0=ALU.add), ["cst"], [kk("vals")])
            P.v(lambda e, s=s: e.tensor_copy(out=s["vals"][:, :, :, 1], in_=s["aff"][:]), [kk("aff")], [kk("vals")])
            nst = (k + 127) // 128
            sw = min(k, 128)
            n_oh = 0
            for el in range(NEL):
                for c in range(C):
                    ohb = s["oh"][n_oh % 2]
                    kb = (nm, "oh", n_oh % 2)
                    n_oh += 1
                    P.v(lambda e, ohb=ohb, s=s, c=c, el=el, k=k: e.tensor_scalar(out=ohb[:], in0=iota[:, 0:k], scalar1=s["pos"][:, c, el:el + 1],
                                                                                scalar2=None, op0=ALU.is_equal), ["cst", kk("pos")], [kb])
                    for sti in range(nst):
                        P.t(lambda e, ohb=ohb, s=s, c=c, el=el, sti=sti, sw=sw: e.matmul(
                            psU[0][0:sw, (el * 4 + sti) * 2:(el * 4 + sti) * 2 + 2], lhsT=ohb[:, sti * 128:sti * 128 + sw],
                            rhs=s["vals"][:, c, el, :], start=(c == 0 and el == 0 and sti == 0), stop=(c == C - 1)),
                            [kb, kk("vals")], [("psU", 0)])
            P.v(lambda e, s=s, sw=sw: e.tensor_copy(out=s["sl"][0:sw].rearrange("p e s t -> p (e s t)"), in_=psU[0][0:sw, 0:NEL * 8]), [("psU", 0)], [kk("sl")])
            P.v(lambda e, s=s, sw=sw: e.tensor_copy(out=s["idx"][0:sw], in_=s["sl"][0:sw, :, :, 0]), [kk("sl")], [kk("idx")])
            P.v(lambda e, s=s, sw=sw: e.tensor_copy(out=s["gate"][0:sw], in_=s["sl"][0:sw, :, :, 1]), [kk("sl")], [kk("gate")])

        for sargs in sets:
            select(*sargs)

        tiles = [("L", sti, 128, sti * 128) for sti in range(4)]
        if with_ctx:
            tiles.append(("C", 0, 32, 512))
        ncols = 544 if with_ctx else 512
        ring_n = [0]
        ny = [0]

        def load_unit(wsrc, el, cb):
            rb = ring_n[0] % 4
            ring_n[0] += 1
            for k4 in range(4):
                src = wsrc[el, k4 * 512:(k4 + 1) * 512, cb * 512:(cb + 1) * 512].rearrange("(kc p) n -> p kc n", p=128)
                P.dma(lambda e, rb=rb, k4=k4, src=src: e.dma_start(out=ring[rb][:, k4 * 4:(k4 + 1) * 4, :], in_=src), w=[("ring", rb)], q="gpsimd")
            return rb

        for el in range(NEL):
            for ti, (nm, sti, n, c0) in enumerate(tiles):
                s = S[nm]
                xb = xg[ti % 2]
                kx = ("xg", ti % 2)
                P.op("gpsimd", lambda e, xb=xb, s=s, el=el, sti=sti, n=n: e.indirect_dma_start(
                    out=xb[0:n, :], out_offset=None, in_=hx2[:, :],
                    in_offset=bass.IndirectOffsetOnAxis(ap=s["idx"][0:n, el, sti:sti + 1], axis=0)), [(nm, "idx")], [kx], dma=True)
                for kc in range(16):
                    P.t(lambda e, xb=xb, kc=kc, n=n: e.transpose(psT[:, kc, 0:n], xb[0:n, kc * 128:(kc + 1) * 128], identb[0:n, 0:n]), [kx, "identb"], ["psT"])
                P.a(lambda e, c0=c0, n=n: e.activation(out=xgT[:, :, c0:c0 + n], in_=psT[:, :, 0:n], func=AF.Copy), ["psT"], ["xgT"])
            for cb in range(4):
                rg = load_unit(wg, el, cb)
                ru = load_unit(wu, el, cb)
                for f4 in range(4):
                    fc = cb * 4 + f4
                    pa = psA[fc % 2]
                    pu = psU[fc % 2]
                    for (pp, rbuf, key) in ((pa, rg, "psA"), (pu, ru, "psU")):
                        for kc in range(16):
                            P.t(lambda e, pp=pp, rbuf=rbuf, kc=kc, f4=f4: e.matmul(pp[:, 0:512], lhsT=ring[rbuf][:, kc, f4 * 128:(f4 + 1) * 128],
                                                                                  rhs=xgT[:, kc, 0:512], start=(kc == 0), stop=(kc == 15)),
                                [("ring", rbuf), "xgT"], [(key, fc % 2)])
                    P.a(lambda e, pa=pa: e.activation(out=sil[:, 0:512], in_=pa[:, 0:512], func=AF.Silu), [("psA", fc % 2)], ["sil"])
                    P.v(lambda e, pu=pu, fc=fc: e.tensor_tensor(out=hT[:, fc, 0:512], in0=sil[:, 0:512], in1=pu[:, 0:512], op=ALU.mult),
                        ["sil", ("psU", fc % 2)], ["hT"])
                    if with_ctx:
                        for (pp, rbuf, key) in ((pa, rg, "psA"), (pu, ru, "psU")):
                            for kc in range(16):
                                P.t(lambda e, pp=pp, rbuf=rbuf, kc=kc, f4=f4: e.matmul(pp[:, 0:32], lhsT=ring[rbuf][:, kc, f4 * 128:(f4 + 1) * 128],
                                                                                      rhs=xgT[:, kc, 512:544], start=(kc == 0), stop=(kc == 15)),
                                    [("ring", rbuf), "xgT"], [(key, fc % 2)])
                        P.a(lambda e, pa=pa: e.activation(out=sil[:, 512:544], in_=pa[:, 0:32], func=AF.Silu), [("psA", fc % 2)], ["sil"])
                        P.v(lambda e, pu=pu, fc=fc: e.tensor_tensor(out=hT[:, fc, 512:544], in0=sil[:, 512:544], in1=pu[:, 0:32], op=ALU.mult),
                            ["sil", ("psU", fc % 2)], ["hT"])
            for cb in range(4):
                rd = load_unit(wd, el, cb)
                for ti, (nm, sti, n, c0) in enumerate(tiles):
                    s = S[nm]
                    py = psY[ny[0] % 2]
                    ky = ("psY", ny[0] % 2)
                    ybuf = yb[ny[0] % 3]
                    kyb = ("yb", ny[0] % 3)
                    ny[0] += 1
                    for fc in range(16):
                        P.t(lambda e, py=py, rd=rd, fc=fc, c0=c0, n=n: e.matmul(py[0:n, :], lhsT=hT[:, fc, c0:c0 + n], rhs=ring[rd][:, fc, :],
                                                                               start=(fc == 0), stop=(fc == 15)), ["hT", ("ring", rd)], [ky])
                    P.v(lambda e, py=py, ybuf=ybuf, s=s, el=el, sti=sti, n=n: e.tensor_scalar(out=ybuf[0:n, :], in0=py[0:n, :],
                                                                                              scalar1=s["gate"][0:n, el, sti:sti + 1], scalar2=None, op0=ALU.mult),
                        [ky, (nm, "gate")], [kyb])
                    P.op("gpsimd", lambda e, ybuf=ybuf, s=s, el=el, sti=sti, n=n, cb=cb: e.indirect_dma_start(
                        out=delta[cb][:, :], out_offset=bass.IndirectOffsetOnAxis(ap=s["idx"][0:n, el, sti:sti + 1], axis=0),
                        in_=ybuf[0:n, :], in_offset=None, compute_op=ALU.add), [kyb, (nm, "idx"), ("delta", cb)], [("delta", cb)], dma=True)
        P.emit(fused=fused)
    return nc


def s3_consts():
    c = np.zeros((128, 930), np.float32)
    c[:, 0:128] = np.eye(128)
    c[:, 128:256] = 1.0
    c[:, 256:384] = np.triu(np.ones((128, 128)))
    c[:, 384:896] = np.arange(512)[None, :]
    c[:, 896:930] = np.arange(34)[None, :] * 128 + np.arange(128)[:, None]
    return c


def build_s4(nc=None, io=None, pfx=""):
    if nc is None:
        nc = bass.Bass("TRN2", target_bir_lowering=False)
    fused, di, do = _mk(nc, io, pfx)
    x1 = di("x1", [NTOK, D])
    dA = di("dA", [NTOK, D])
    dB = None if fused else di("dB", [NTOK, D])
    modx = di("modx", [6, D])
    modc = di("modc", [6, D])
    x2 = do("x2", [NTOK, D])
    with stage_ctx(nc, fused) as st:
        sb = lambda n, s, d=F32: st.enter_context(nc.sbuf_tensor(pfx + n, s, d))
        G = sb("G", [128, D])
        xa = [sb(f"xa{i}", [128, D]) for i in range(2)]
        da = [sb(f"da{i}", [128, D]) for i in range(2)]
        db = [sb(f"db{i}", [128, D]) for i in range(2)]
        P = Prog(nc)
        for t in range(NT):
            i = t % 2
            if t == 0:
                P.dma(lambda e: e.dma_start(out=G[:], in_=_bc(modc[5], D)), w=["G"])
            if t == 2:
                P.dma(lambda e: e.dma_start(out=G[:], in_=_bc(modx[5], D)), w=["G"])
            r0 = t * 128
            P.dma(lambda e, i=i, r0=r0: e.dma_start(out=xa[i][:], in_=x1[r0:r0 + 128, :]), w=[("xa", i)])
            P.dma(lambda e, i=i, r0=r0: e.dma_start(out=da[i][:], in_=dA[r0:r0 + 128, :]), w=[("da", i)])
            if not fused:
                P.dma(lambda e, i=i, r0=r0: e.dma_start(out=db[i][:], in_=dB[r0:r0 + 128, :]), w=[("db", i)])
                P.v(lambda e, i=i: e.tensor_tensor(out=da[i][:], in0=da[i][:], in1=db[i][:], op=ALU.add), [("da", i), ("db", i)], [("da", i)])
            P.v(lambda e, i=i: e.tensor_tensor(out=da[i][:], in0=da[i][:], in1=G[:], op=ALU.mult), [("da", i), "G"], [("da", i)])
            P.v(lambda e, i=i: e.tensor_tensor(out=xa[i][:], in0=xa[i][:], in1=da[i][:], op=ALU.add), [("xa", i), ("da", i)], [("xa", i)])
            P.dma(lambda e, i=i, r0=r0: e.dma_start(out=x2[r0:r0 + 128, :], in_=xa[i][:]), [("xa", i)], [("x2", t)])
        P.emit(fused=fused)
    return nc


def _rope_tables(h):
    t = np.arange(h * 2048, (h + 1) * 2048)
    row = (t // 64).astype(np.float32)
    col = (t % 64).astype(np.float32)
    inv = (np.float32(10000.0) ** (-np.arange(32, dtype=np.float32) / np.float32(32))).astype(np.float32)
    ar = row[:, None] * inv
    ac = col[:, None] * inv
    tab = np.zeros((NTOK, 128), np.float32)
    tab[:256, :64] = 1.0
    tab[256:, :64] = np.concatenate([np.cos(ar), np.cos(ac)], 1)
    tab[256:, 64:] = np.concatenate([np.sin(ar), np.sin(ac)], 1)
    return tab


def _masks(h):
    s = np.arange(128)[:, None]
    q = np.arange(128)[None, :]
    mp = (s >= q).astype(np.float32)
    mn = (s <= q).astype(np.float32)
    return np.stack([mp, mn, mp * (0.0 if h == 0 else 1.0), mn * (0.0 if h == 1 else 1.0)])


def _run(nc, in_maps):
    res = run_bass_kernel_spmd(nc, in_maps, core_ids=list(range(8)))
    return res.results


PAIRS = [[0, 1], [2, 3], [4, 5], [6, 7]]
NXR = NTOK + 128
CAPL = 384
NCST = 931


def f_consts():
    c = np.zeros((128, NCST), np.float32)
    c[:, 0:930] = s3_consts()
    c[:, 930] = NTOK + np.arange(128)
    return c


def stage_mod(nc, io, pfx):
    cin, w, b, modS = io["cin"], io["w_mod"], io["b_mod"], io["modS"]
    with stage_ctx(nc, True) as st:
        sb = lambda n, s, d=F32: st.enter_context(nc.sbuf_tensor(pfx + n, s, d))
        ct = sb("ct", [128, 16, 2])
        cs = sb("cs", [128, 16, 2])
        wt = [sb(f"wt{i}", [128, 16, 512]) for i in range(3)]
        bt = [sb(f"bt{i}", [2, 512]) for i in range(3)]
        ot = [sb(f"ot{i}", [2, 512]) for i in range(3)]
        ps = [st.enter_context(nc.psum_tensor(pfx + f"ps{i}", [2, 512], F32)) for i in range(2)]
        P = Prog(nc)
        P.dma(lambda e: e.dma_start(out=ct[:], in_=cin), w=["ct"])
        P.a(lambda e: e.activation(out=cs[:], in_=ct[:], func=AF.Silu), ["ct"], ["cs"])
        gi = 0
        for l in range(2):
            for g in range(24):
                i3 = gi % 3
                i2 = gi % 2
                gi += 1
                cols = slice(g * 512, (g + 1) * 512)
                src = w[l, :, cols].rearrange("(kc p) n -> p kc n", p=128)
                P.dma(lambda e, i3=i3, src=src: e.dma_start(out=wt[i3][:], in_=src), w=[("wt", i3)])
                P.dma(lambda e, i3=i3, l=l, cols=cols: e.dma_start(out=bt[i3][:], in_=b[l:l + 1, cols].to_broadcast([2, 512])), w=[("bt", i3)])
                for kc in range(16):
                    P.t(lambda e, i2=i2, i3=i3, kc=kc: e.matmul(ps[i2][:], lhsT=cs[:, kc, :], rhs=wt[i3][:, kc, :], start=(kc == 0), stop=(kc == 15)),
                        ["cs", ("wt", i3)], [("ps", i2)])
                P.v(lambda e, i2=i2, i3=i3: e.tensor_tensor(out=ot[i3][:], in0=ps[i2][:], in1=bt[i3][:], op=ALU.add), [("ps", i2), ("bt", i3)], [("ot", i3)])
                P.dma(lambda e, i3=i3, l=l, cols=cols: e.dma_start(out=modS[l, :, cols], in_=ot[i3][:]), [("ot", i3)], [("modS", l, g)])
        P.emit(fused=True)


def stage_cc(nc, pairs):
    with nc.cleanup_on_exit():
        _UID[0] += 1
        sem = nc.alloc_semaphore(name="cc%d" % _UID[0])
        with nc.Block() as block:
            @block.gpsimd
            def _(g):
                for i, (a, b) in enumerate(pairs):
                    g.collective_compute("AllGather", ALU.bypass, replica_groups=PAIRS, ins=[a], outs=[b]).then_inc(sem, 1)
                    g.wait_ge(sem, i + 1)
        nc.all_engine_barrier()


def stage_s3a(nc, io, with_ctx, pfx):
    affAll, affIn, affC, consts = io["affAll"], io["affIn"], io["affC"], io["consts"]
    idxT, gateT, delta, hx2 = io["idxT"], io["gateT"], io["delta"], io["hx2o"]
    with stage_ctx(nc, True) as st:
        sb = lambda n, s, d=F32: st.enter_context(nc.sbuf_tensor(pfx + n, s, d))
        pt = lambda n, s, d=F32: st.enter_context(nc.psum_tensor(pfx + n, s, d))
        cst = sb("cst", [128, NCST])
        trib = sb("trib", [128, 128], BF16)
        onesb = sb("onesb", [128, 128], BF16)
        jt = sb("jt", [128, NE, 15])
        zt = sb("zt", [128, D])
        ztb = sb("ztb", [128, D], BF16)
        sl = sb("sl", [128, NE, 4, 3])
        idxf = sb("idxf", [128, NE, 4])
        idx = sb("idx", [128, NE, 4], I32)
        gate = sb("gate", [128, NE, 4])
        psC = pt("psC", [128, 512])
        psP = [pt(f"psP{i}", [128, 512]) for i in range(2)]
        psS = [pt(f"psS{i}", [128, 512]) for i in range(2)]
        ones = cst[:, 128:256]
        tri = cst[:, 256:384]
        iota = cst[:, 384:896]
        tokid = cst[:, 896:930]
        dummy = cst[:, 930:931]
        P = Prog(nc)
        P.dma(lambda e: e.dma_start(out=cst[:], in_=consts), w=["cst"])
        P.v(lambda e: e.tensor_copy(out=trib[:], in_=tri), ["cst"], ["trib"])
        P.v(lambda e: e.tensor_copy(out=onesb[:], in_=ones), ["cst"], ["onesb"])
        for j in range(15):
            P.v(lambda e, j=j: e.memset(jt[:, :, j:j + 1], float(j + 1)), [], ["jt"])
        P.v(lambda e: e.memset(zt[:], 0.0), [], ["zt"])
        P.v(lambda e: e.memset(ztb[:], 0.0), [], ["ztb"])
        P.v(lambda e: e.memset(sl[:], 0.0), [], ["sl"])
        for r in range(NXR // 128):
            P.dma(lambda e, r=r: e.dma_start(out=delta[r * 128:(r + 1) * 128, :], in_=zt[:]), ["zt"], [("delta", r)])
        P.dma(lambda e: e.dma_start(out=hx2[NTOK:NXR, :], in_=ztb[:]), ["ztb"], ["hx2d"])

        def select(nm, Cth, thr_ap, Cm, mask_ap, k, r0, cap, st0, psSl):
            kk = lambda x: (nm, x)
            athr = sb(f"athr{nm}", [128, Cth, NE])
            am = athr if mask_ap is None else sb(f"am{nm}", [128, Cm, NE])
            kam = kk("athr") if mask_ap is None else kk("am")
            lo = sb(f"lo{nm}", [128, NE])
            th = sb(f"th{nm}", [128, NE, 15])
            cmpb = sb(f"cmp{nm}", [128, Cth, NE, 15], BF16)
            cntp = sb(f"cntp{nm}", [128, NE, 15])
            ge = sb(f"ge{nm}", [128, NE, 15])
            nge = sb(f"nge{nm}", [128, NE])
            mask = sb(f"mask{nm}", [128, Cm, NE])
            maskb = sb(f"maskb{nm}", [128, Cm, NE], BF16)
            cA = sb(f"cA{nm}", [128, Cm, NE])
            cB = sb(f"cB{nm}", [128, Cm, NE])
            pos = sb(f"pos{nm}", [128, Cm, NE])
            vals = sb(f"vals{nm}", [128, Cm, NE, 3])
            oh = [sb(f"oh{nm}{i}", [128, cap]) for i in range(2)]
            bufs = {"cA": cA, "cB": cB}
            P.dma(lambda e: e.dma_start(out=athr[:], in_=thr_ap.rearrange("(c p) e -> p c e", p=128)), w=[kk("athr")])
            if mask_ap is not None:
                P.dma(lambda e: e.dma_start(out=am[:], in_=mask_ap.rearrange("(c p) e -> p c e", p=128)), w=[kk("am")])
            P.v(lambda e: e.memset(lo[:], 0.0), [], [kk("lo")])
            affb = athr[:].unsqueeze(3).to_broadcast([128, Cth, NE, 15])
            for it in range(8):
                step = 16.0 ** -(it + 1)
                P.v(lambda e, step=step: e.scalar_tensor_tensor(out=th[:], in0=jt[:], scalar=step, in1=lo[:].unsqueeze(2).to_broadcast([128, NE, 15]),
                                                                op0=ALU.mult, op1=ALU.add), ["jt", kk("lo")], [kk("th")])
                P.v(lambda e: e.tensor_tensor(out=cmpb[:], in0=affb, in1=th[:].unsqueeze(1).to_broadcast([128, Cth, NE, 15]), op=ALU.is_ge),
                    [kk("athr"), kk("th")], [kk("cmp")])
                P.v(lambda e: e.tensor_reduce(out=cntp[:], in_=cmpb[:].rearrange("p c e j -> p e j c"), axis=AX.X, op=ALU.add), [kk("cmp")], [kk("cntp")])
                P.t(lambda e: e.matmul(psC[:, 0:NE * 15], lhsT=ones, rhs=cntp[:].rearrange("p e j -> p (e j)"), start=True, stop=True),
                    ["cst", kk("cntp")], ["psC"])
                P.v(lambda e: e.tensor_scalar(out=ge[:].rearrange("p e j -> p (e j)"), in0=psC[:, 0:NE * 15], scalar1=float(k) - 0.5, scalar2=None,
                                              op0=ALU.is_ge), ["psC"], [kk("ge")])
                P.v(lambda e: e.tensor_reduce(out=nge[:], in_=ge[:], axis=AX.X, op=ALU.add), [kk("ge")], [kk("nge")])
                P.v(lambda e, step=step: e.scalar_tensor_tensor(out=lo[:], in0=nge[:], scalar=step, in1=lo[:], op0=ALU.mult, op1=ALU.add),
                    [kk("nge"), kk("lo")], [kk("lo")])
            P.v(lambda e: e.tensor_tensor(out=mask[:], in0=am[:], in1=lo[:].unsqueeze(1).to_broadcast([128, Cm, NE]), op=ALU.is_ge),
                [kam, kk("lo")], [kk("mask")])
            P.v(lambda e: e.tensor_copy(out=maskb[:], in_=mask[:]), [kk("mask")], [kk("maskb")])
            mb2 = maskb[:].rearrange("p c e -> p (c e)")
            ncol = Cm * NE
            P.t(lambda e: e.matmul(psP[0][:, 0:ncol], lhsT=trib[:], rhs=mb2, start=True, stop=True), ["trib", kk("maskb")], [("psP", 0)])
            P.t(lambda e: e.matmul(psP[1][:, 0:ncol], lhsT=onesb[:], rhs=mb2, start=True, stop=True), ["onesb", kk("maskb")], [("psP", 1)])
            P.v(lambda e: e.tensor_copy(out=cA[:].rearrange("p c e -> p (c e)"), in_=psP[1][:, 0:ncol]), [("psP", 1)], [kk("cA")])
            cur, nxt = "cA", "cB"
            sh = 1
            while sh < Cm:
                P.v(lambda e, cur=cur, nxt=nxt, sh=sh: e.tensor_copy(out=bufs[nxt][:, 0:sh, :], in_=bufs[cur][:, 0:sh, :]), [kk(cur)], [kk(nxt)])
                P.v(lambda e, cur=cur, nxt=nxt, sh=sh: e.tensor_tensor(out=bufs[nxt][:, sh:Cm, :], in0=bufs[cur][:, sh:Cm, :], in1=bufs[cur][:, 0:Cm - sh, :],
                                                                  op=ALU.add), [kk(cur)], [kk(nxt)])
                cur, nxt = nxt, cur
                sh *= 2
            pos2 = pos[:].rearrange("p c e -> p (c e)")
            P.v(lambda e, cur=cur: e.tensor_tensor(out=pos2, in0=bufs[cur][:].rearrange("p c e -> p (c e)"), in1=psP[1][:, 0:ncol], op=ALU.subtract),
                [kk(cur), ("psP", 1)], [kk("pos")])
            P.v(lambda e: e.tensor_tensor(out=pos2, in0=pos2, in1=psP[0][:, 0:ncol], op=ALU.add), [kk("pos"), ("psP", 0)], [kk("pos")])
            P.v(lambda e: e.tensor_tensor(out=pos[:], in0=pos[:], in1=mask[:], op=ALU.mult), [kk("pos"), kk("mask")], [kk("pos")])
            P.v(lambda e: e.tensor_scalar(out=pos[:], in0=pos[:], scalar1=-1.0, scalar2=None, op0=ALU.add), [kk("pos")], [kk("pos")])
            P.v(lambda e: e.tensor_scalar(out=vals[:, :, :, 0], in0=tokid[:, 0:Cm].unsqueeze(2).to_broadcast([128, Cm, NE]), scalar1=float(r0), scalar2=None,
                                          op0=ALU.add), ["cst"], [kk("vals")])
            P.v(lambda e: e.tensor_copy(out=vals[:, :, :, 1], in_=am[:]), [kam], [kk("vals")])
            P.v(lambda e: e.memset(vals[:, :, :, 2], 1.0), [], [kk("vals")])
            nst = (cap + 127) // 128
            sw = min(cap, 128)
            n_oh = 0
            first = True
            for el in range(NE):
                for c in range(Cm):
                    ohb = oh[n_oh % 2]
                    kb = (nm, "oh", n_oh % 2)
                    n_oh += 1
                    P.v(lambda e, ohb=ohb, c=c, el=el: e.tensor_scalar(out=ohb[:], in0=iota[:, 0:cap], scalar1=pos[:, c, el:el + 1], scalar2=None,
                                                                      op0=ALU.is_equal), ["cst", kk("pos")], [kb])
                    for sti in range(nst):
                        col = (el * 4 + st0 + sti) * 3
                        P.t(lambda e, ohb=ohb, c=c, el=el, sti=sti, col=col, first=first: e.matmul(
                            psSl[0:sw, col:col + 3], lhsT=ohb[:, sti * 128:sti * 128 + sw], rhs=vals[:, c, el, :],
                            start=first, stop=(c == Cm - 1)), [kb, kk("vals")], [kk("psSl")])
                        first = False
            for sti in range(nst):
                P.v(lambda e, sti=sti: e.tensor_copy(out=sl[0:sw, :, st0 + sti, :],
                                                     in_=psSl[0:sw, 0:NE * 12].rearrange("p (e s t) -> p e s t", e=NE, s=4)[:, :, st0 + sti, :]),
                    [kk("psSl")], ["sl"])

        select("L", 32, affAll, 16, affIn, 512, 256, CAPL, 0, psS[0])
        if with_ctx:
            select("C", 2, affC, 2, None, 32, 0, 32, 3, psS[1])
        P.v(lambda e: e.tensor_scalar(out=idxf[:], in0=sl[:, :, :, 2], scalar1=-1.0, scalar2=dummy, op0=ALU.add, op1=ALU.mult), ["sl", "cst"], ["idxf"])
        P.v(lambda e: e.tensor_tensor(out=idxf[:], in0=sl[:, :, :, 0], in1=idxf[:], op=ALU.subtract), ["sl", "idxf"], ["idxf"])
        P.v(lambda e: e.tensor_copy(out=idx[:], in_=idxf[:]), ["idxf"], ["idx"])
        P.v(lambda e: e.tensor_copy(out=gate[:], in_=sl[:, :, :, 1]), ["sl"], ["gate"])
        P.dma(lambda e: e.dma_start(out=idxT, in_=idx[:]), ["idx"], ["idxT"])
        P.dma(lambda e: e.dma_start(out=gateT, in_=gate[:]), ["gate"], ["gateT"])
        P.emit(fused=True)


def stage_s3b(nc, io, L, with_ctx, pfx):
    idxT, gateT, delta, hx2, consts = io["idxT"], io["gateT"], io["delta"], io["hx2o"], io["consts"]
    wg, wu, wd = io["w_gate"], io["w_up"], io["w_down"]
    with stage_ctx(nc, True) as st:
        sb = lambda n, s, d=F32: st.enter_context(nc.sbuf_tensor(pfx + n, s, d))
        pt = lambda n, s, d=F32: st.enter_context(nc.psum_tensor(pfx + n, s, d))
        ncols = CAPL + (32 if with_ctx else 0)
        cst = sb("cst", [128, 128])
        identb = sb("identb", [128, 128], BF16)
        idx = sb("idx", [128, NE, 4], I32)
        gate = sb("gate", [128, NE, 4])
        ring = [sb(f"ring{i}", [128, 16, 512], BF16) for i in range(4)]
        xg = [sb(f"xg{i}", [128, D], BF16) for i in range(2)]
        xgT = sb("xgT", [128, 16, ncols], BF16)
        hT = sb("hT", [128, 16, ncols], BF16)
        sil = sb("sil", [128, ncols])
        ybig = [sb(f"ybig{i}", [128, D]) for i in range(4)]
        psA = [pt(f"psA{i}", [128, 512]) for i in range(2)]
        psU = [pt(f"psU{i}", [128, 512]) for i in range(2)]
        psT = pt("psT", [128, 16, 128], BF16)
        psY = [pt(f"psY{i}", [128, 512]) for i in range(2)]
        P = Prog(nc)
        P.dma(lambda e: e.dma_start(out=cst[:], in_=consts[:, 0:128]), w=["cst"])
        P.v(lambda e: e.tensor_copy(out=identb[:], in_=cst[:]), ["cst"], ["identb"])
        P.dma(lambda e: e.dma_start(out=idx[:], in_=idxT), w=["idx"])
        P.dma(lambda e: e.dma_start(out=gate[:], in_=gateT), w=["gate"])
        tiles = [(sti, 128, sti * 128) for sti in range(3)]
        if with_ctx:
            tiles.append((3, 32, CAPL))
        ring_n = [0]
        ny = [0]

        def load_unit(wsrc, el, cb):
            rb = ring_n[0] % 4
            ring_n[0] += 1
            for k4 in range(4):
                src = wsrc[L, el, k4 * 512:(k4 + 1) * 512, cb * 512:(cb + 1) * 512].rearrange("(kc p) n -> p kc n", p=128)
                P.dma(lambda e, rb=rb, k4=k4, src=src: e.dma_start(out=ring[rb][:, k4 * 4:(k4 + 1) * 4, :], in_=src), w=[("ring", rb)], q="gpsimd")
            return rb

        for el in range(NE):
            for ti, (sti, n, c0) in enumerate(tiles):
                xb = xg[ti % 2]
                kx = ("xg", ti % 2)
                P.op("gpsimd", lambda e, xb=xb, el=el, sti=sti, n=n: e.indirect_dma_start(
                    out=xb[0:n, :], out_offset=None, in_=hx2[:, :],
                    in_offset=bass.IndirectOffsetOnAxis(ap=idx[0:n, el, sti:sti + 1], axis=0)), ["idx"], [kx], dma=True)
                for kc in range(16):
                    P.t(lambda e, xb=xb, kc=kc, n=n: e.transpose(psT[:, kc, 0:n], xb[0:n, kc * 128:(kc + 1) * 128], identb[0:n, 0:n]), [kx, "identb"], ["psT"])
                P.a(lambda e, c0=c0, n=n: e.activation(out=xgT[:, :, c0:c0 + n], in_=psT[:, :, 0:n], func=AF.Copy), ["psT"], ["xgT"])
            for cb in range(4):
                rg = load_unit(wg, el, cb)
                ru = load_unit(wu, el, cb)
                for f4 in range(4):
                    fc = cb * 4 + f4
                    pa = psA[fc % 2]
                    pu = psU[fc % 2]
                    for (pp, rbuf, key) in ((pa, rg, "psA"), (pu, ru, "psU")):
                        for kc in range(16):
                            P.t(lambda e, pp=pp, rbuf=rbuf, kc=kc, f4=f4: e.matmul(pp[:, 0:ncols], lhsT=ring[rbuf][:, kc, f4 * 128:(f4 + 1) * 128],
                                                                                  rhs=xgT[:, kc, :], start=(kc == 0), stop=(kc == 15)),
                                [("ring", rbuf), "xgT"], [(key, fc % 2)])
                    P.a(lambda e, pa=pa: e.activation(out=sil[:], in_=pa[:, 0:ncols], func=AF.Silu), [("psA", fc % 2)], ["sil"])
                    P.v(lambda e, pu=pu, fc=fc: e.tensor_tensor(out=hT[:, fc, :], in0=sil[:], in1=pu[:, 0:ncols], op=ALU.mult),
                        ["sil", ("psU", fc % 2)], ["hT"])
            for cb in range(4):
                rd = load_unit(wd, el, cb)
                for ti, (sti, n, c0) in enumerate(tiles):
                    py = psY[ny[0] % 2]
                    ky = ("psY", ny[0] % 2)
                    ny[0] += 1
                    for fc in range(16):
                        P.t(lambda e, py=py, rd=rd, fc=fc, c0=c0, n=n: e.matmul(py[0:n, :], lhsT=hT[:, fc, c0:c0 + n], rhs=ring[rd][:, fc, :],
                                                                               start=(fc == 0), stop=(fc == 15)), ["hT", ("ring", rd)], [ky])
                    P.v(lambda e, py=py, ti=ti, el=el, sti=sti, n=n, cb=cb: e.tensor_scalar(out=ybig[ti][0:n, cb * 512:(cb + 1) * 512], in0=py[0:n, :],
                                                                                           scalar1=gate[0:n, el, sti:sti + 1], scalar2=None, op0=ALU.mult),
                        [ky, "gate"], [("ybig", ti)])
            for ti, (sti, n, c0) in enumerate(tiles):
                P.op("gpsimd", lambda e, ti=ti, el=el, sti=sti, n=n: e.indirect_dma_start(
                    out=delta[:, :], out_offset=bass.IndirectOffsetOnAxis(ap=idx[0:n, el, sti:sti + 1], axis=0),
                    in_=ybig[ti][0:n, :], in_offset=None, compute_op=ALU.add), [("ybig", ti), "idx", "delta"], ["delta"], dma=True)
        P.emit(fused=True)


def stage_copy_out(nc, src, dst, pfx):
    with stage_ctx(nc, True) as st:
        bufs = [st.enter_context(nc.sbuf_tensor(pfx + f"cb{i}", [128, D], F32)) for i in range(3)]
        P = Prog(nc)
        for t in range(16):
            b = bufs[t % 3]
            P.dma(lambda e, b=b, t=t: e.dma_start(out=b[:], in_=src[256 + t * 128:256 + (t + 1) * 128, :]), w=[("cb", t % 3)])
            P.dma(lambda e, b=b, t=t: e.dma_start(out=dst[t * 128:(t + 1) * 128, :], in_=b[:]), [("cb", t % 3)], [("out", t)])
        P.emit(fused=True)


def build_fused(ncores=8):
    global PAIRS
    PAIRS = [[2 * i, 2 * i + 1] for i in range(ncores // 2)]
    nc = bass.Bass("TRN2", target_bir_lowering=False)
    ein = lambda n, s, d=F32: nc.dram_tensor(n, s, d, kind="ExternalInput").ap()
    scr = lambda n, s, d=F32: nc.dram_tensor(n, s, d, kind="Internal").ap()
    scrl = lambda n, s, d=F32: nc.dram_tensor(n, s, d, kind="Internal", addr_space="Local").ap()
    X = {}
    X["xin0"] = ein("xin", [NTOK, D])
    X["cin"] = ein("cin", [128, 16, 2])
    X["w_mod"] = ein("w_mod", [2, D, 6 * D])
    X["b_mod"] = ein("b_mod", [2, 6 * D])
    n1g = ein("n1g", [2, D]); n2g = ein("n2g", [2, D])
    win = ein("win", [2, D, 3584])
    gqk = ein("gqk", [2, NQK * 128])
    X["cs_t"] = ein("cs_t", [NTOK, 128])
    vnb = ein("vnb", [2, 512]); wsT = ein("wsT", [2, 128, 4, 128]); bsT = ein("bsT", [2, 128, 4]); ogb = ein("ogb", [2, 512])
    X["ident"] = ein("ident", [128, 128])
    X["msk"] = ein("msk", [4, 128, 128])
    sink = ein("sink", [2, 6]); gac = ein("gac", [2, 1536])
    wout = ein("wout", [2, D, D]); wr = ein("wr", [2, D, NE])
    X["w_gate"] = ein("w_gate", [2, NE, D, D]); X["w_up"] = ein("w_up", [2, NE, D, D]); X["w_down"] = ein("w_down", [2, NE, D, D])
    X["consts"] = ein("consts", [128, NCST])
    out = nc.dram_tensor("out", [2048, D], F32, kind="ExternalOutput").ap()
    modS = scr("modS", [2, 2, 6 * D])
    X["modS"] = modS
    X["QKT"] = scr("QKT", [128, NQK, NTOK], BF16); X["Vout"] = scr("Vout", [NTOK, 512], BF16)
    X["yB"] = scr("yB", [NTOK, 512], BF16); X["yAC"] = scr("yAC", [NTOK, 1536], BF16)
    X["KTx"] = scr("KTx", [512, 2048], BF16); X["KTg"] = scrl("KTg", [1024, 2048], BF16)
    X["Vx"] = scr("Vx", [2048, 512], BF16); X["Vg"] = scrl("Vg", [4096, 512], BF16)
    xs = scr("xs", [NXR, D]); X["hx2o"] = scr("hx2s", [NXR, D], BF16); X["delta"] = scr("delta", [NXR, D])
    X["affIn"] = scr("affIn", [2048, NE]); X["affC"] = scr("affC", [256, NE]); X["affAll"] = scrl("affAll", [4096, NE])
    X["idxT"] = scr("idxT", [128, NE, 4], I32); X["gateT"] = scr("gateT", [128, NE, 4])

    stage_mod(nc, X, "m_")
    for L in range(2):
        io = dict(X)
        io["xin"] = X["xin0"] if L == 0 else xs
        io["modx"] = modS[L, 0].rearrange("(s d) -> s d", s=6)
        io["modc"] = modS[L, 1].rearrange("(s d) -> s d", s=6)
        io.update(n1g=n1g[L], n2g=n2g[L], win=win[L], gqk=gqk[L], vnb=vnb[L], wsT=wsT[L], bsT=bsT[L], ogb=ogb[L],
                  sink=sink[L], gac=gac[L], wout=wout[L], wr=wr[L], x1o=xs, x1=xs, dA=X["delta"], x2=xs)
        build_s1(nc=nc, io=io, pfx=f"L{L}a_")
        stage_cc(nc, [(X["KTx"], X["KTg"]), (X["Vx"], X["Vg"])])
        build_s2a(nc=nc, io=io, pfx=f"L{L}b_")
        build_s2b(nc=nc, io=io, pfx=f"L{L}c_")
        stage_cc(nc, [(X["affIn"], X["affAll"])])
        stage_s3a(nc, io, L == 0, f"L{L}d_")
        stage_s3b(nc, io, L, L == 0, f"L{L}e_")
        build_s4(nc=nc, io=io, pfx=f"L{L}f_")
    stage_copy_out(nc, xs, out, "o_")
    return nc


def kernel(x, c, ctx, c_ctx, w_mod, b_mod, norm1_g, norm2_g, w_in, qn_a, kn_a, sink_a, vn_b,
           w_s, b_s, qn_c, kn_c, out_g, w_out, w_router, w_gate, w_up, w_down):
    f32 = np.float32
    A = lambda a: np.ascontiguousarray(np.asarray(a, f32))
    x = A(x); ctx = A(ctx); c = A(c); c_ctx = A(c_ctx)
    cores = [(k // 2, k % 2) for k in range(8)]
    og = A(out_g)
    shared = {
        "w_mod": A(w_mod), "b_mod": A(b_mod), "n1g": A(norm1_g), "n2g": A(norm2_g),
        "win": A(np.asarray(w_in, f32)[:, :, WIN_PERM]),
        "gqk": A(np.stack([np.concatenate([np.tile(qn_a[L], 6), np.tile(kn_a[L], 2), np.tile(qn_c[L], 6), np.tile(kn_c[L], 2)]) for L in range(2)])),
        "vnb": A(vn_b), "wsT": A(np.asarray(w_s, f32).transpose(0, 3, 1, 2)), "bsT": A(np.asarray(b_s, f32).transpose(0, 2, 1)),
        "ogb": A(og[:, 768:1280]), "ident": np.eye(128, dtype=f32), "sink": A(sink_a),
        "gac": A(np.concatenate([og[:, :768], og[:, 1280:]], 1)), "wout": A(w_out), "wr": A(w_router),
        "w_gate": A(w_gate), "w_up": A(w_up), "w_down": A(w_down), "consts": f_consts(),
    }
    ims = []
    for (b, h) in cores:
        d = dict(shared)
        d["xin"] = A(np.concatenate([ctx[b], x[b, h * 2048:(h + 1) * 2048]], 0))
        c2 = np.stack([c[b], c_ctx], 0)
        d["cin"] = A(c2.T.reshape(16, 128, 2).transpose(1, 0, 2))
        d["cs_t"] = _rope_tables(h)
        d["msk"] = _masks(h)
        ims.append(d)
    ncores = _NCORES[0]
    nc = build_fused(ncores)
    res = run_bass_kernel_spmd(nc, ims[:ncores], core_ids=list(range(ncores)))
    out = np.zeros((4, SEQ, D), f32)
    for k, (b, h) in enumerate(cores[:ncores]):
        out[b, h * 2048:(h + 1) * 2048] = res.results[k]["out"]
    return out


_NCORES = [8]
```

```python
import contextlib
import numpy as np
import ml_dtypes
import concourse.bass as bass
import concourse.mybir as mybir
from concourse.bass_utils import run_bass_kernel_spmd

F32 = mybir.dt.float32
BF16 = mybir.dt.bfloat16
I32 = mybir.dt.int32
AF = mybir.ActivationFunctionType
ALU = mybir.AluOpType
AX = mybir.AxisListType

D = 2048
SEQ = 4096
CTX = 256
NT = 18
NTOK = NT * 128
EPS = 1e-6
NE = 16

ENGS = ("sync", "scalar", "vector", "gpsimd", "tensor")
SEM_CHUNK = 3000
NDMA_SEMS = 12


class _Op:
    __slots__ = ("eng", "fn", "deps", "is_dma", "has_dep", "sem", "val", "prev_dma")

    def __init__(self, eng, fn, is_dma):
        self.eng = eng
        self.fn = fn
        self.is_dma = is_dma
        self.deps = []
        self.has_dep = False
        self.sem = None
        self.val = None
        self.prev_dma = None


class Prog:
    def __init__(self, nc):
        self.nc = nc
        self.ops = {e: [] for e in ENGS}
        self.last_w = {}
        self.readers = {}
        self.ndma = {e: 0 for e in ENGS}
        self.dma_last = {}
        self.all_dmas = []

    def op(self, eng, fn, reads=(), writes=(), dma=False):
        o = _Op(eng, fn, dma)
        deps = []
        for r in reads:
            w = self.last_w.get(r)
            if w is not None:
                deps.append((w, "raw"))
        for k in writes:
            w = self.last_w.get(k)
            if w is not None:
                deps.append((w, "waw"))
            for rd in self.readers.get(k, ()):
                deps.append((rd, "war"))
        for (d, kind) in deps:
            if d is o:
                continue
            if not d.is_dma and not dma and d.eng == eng:
                if kind != "raw" or eng == "tensor":
                    continue
            if d not in o.deps:
                o.deps.append(d)
                d.has_dep = True
        for r in reads:
            self.readers.setdefault(r, []).append(o)
        for k in writes:
            self.last_w[k] = o
            self.readers[k] = []
        if dma:
            j = self.ndma[eng]
            self.ndma[eng] += 1
            slot = (eng, j % NDMA_SEMS)
            o.prev_dma = self.dma_last.get(slot)
            self.dma_last[slot] = o
            o.sem = slot
            o.val = 16 * (j // NDMA_SEMS + 1)
            self.all_dmas.append(o)
        self.ops[eng].append(o)
        return o

    def v(self, fn, r=(), w=()):
        return self.op("vector", fn, r, w)

    def a(self, fn, r=(), w=()):
        return self.op("scalar", fn, r, w)

    def g(self, fn, r=(), w=()):
        return self.op("gpsimd", fn, r, w)

    def t(self, fn, r=(), w=()):
        return self.op("tensor", fn, r, w)

    def dma(self, fn, r=(), w=(), q="sync"):
        return self.op(q, fn, r, w, dma=True)

    def emit(self, final_wait_eng="sync", fused=False):
        nc = self.nc
        sem_names = set()
        for e in ENGS:
            cnt = 0
            for o in self.ops[e]:
                if o.is_dma:
                    sem_names.add(o.sem)
                    continue
                if o.has_dep:
                    o.sem = (e, "c", cnt // SEM_CHUNK)
                    o.val = cnt % SEM_CHUNK + 1
                    sem_names.add(o.sem)
                    cnt += 1
        sem_names = sorted(sem_names, key=str)
        with contextlib.ExitStack() as st:
            sems = {}
            for n in sem_names:
                if fused:
                    _UID[0] += 1
                    sems[n] = nc.alloc_semaphore(name="f%d_" % _UID[0] + "_".join(str(x) for x in n))
                else:
                    sems[n] = st.enter_context(nc.semaphore("s_" + "_".join(str(x) for x in n)))
            block = st.enter_context(nc.Block())
            prog = self

            def run(e, eng):
                seen = {}
                for o in prog.ops[e]:
                    waits = []
                    if o.is_dma and o.prev_dma is not None:
                        waits.append(o.prev_dma)
                    waits.extend(o.deps)
                    for d in waits:
                        if seen.get(d.sem, 0) >= d.val:
                            continue
                        eng.wait_ge(sems[d.sem], d.val)
                        seen[d.sem] = d.val
                    ins = o.fn(eng)
                    if o.is_dma:
                        ins.then_inc(sems[o.sem], 16)
                    elif o.has_dep:
                        ins.then_inc(sems[o.sem], 1)
                if e == final_wait_eng:
                    last = {}
                    for o in prog.all_dmas:
                        last[o.sem] = max(last.get(o.sem, 0), o.val)
                    for s, v in last.items():
                        if seen.get(s, 0) < v:
                            eng.wait_ge(sems[s], v)

            @block.sync
            def _(eng):
                run("sync", eng)

            @block.scalar
            def _(eng):
                run("scalar", eng)

            @block.vector
            def _(eng):
                run("vector", eng)

            @block.gpsimd
            def _(eng):
                run("gpsimd", eng)

            @block.tensor
            def _(eng):
                run("tensor", eng)


_UID = [0]


@contextlib.contextmanager
def stage_ctx(nc, fused):
    if not fused:
        with contextlib.ExitStack() as st:
            yield st
    else:
        with nc.cleanup_on_exit():
            with contextlib.ExitStack() as st:
                yield st
            nc.all_engine_barrier()


def _mk(nc, io, pfx):
    fused = io is not None
    if fused:
        di = lambda n, s, d=F32: io[n]
        do = lambda n, s, d=F32: io[n]
    else:
        di = lambda n, s, d=F32: nc.dram_tensor(n, s, d, kind="ExternalInput").ap()
        do = lambda n, s, d=F32: nc.dram_tensor(n, s, d, kind="ExternalOutput").ap()
    return fused, di, do


def _bc(ap1d, n):
    return ap1d.unsqueeze(0).to_broadcast([128, n])


MCOL = 1536


def build_mod():
    nc = bass.Bass("TRN2", target_bir_lowering=False)
    cin = nc.dram_tensor("cin", [128, 16, 5], F32, kind="ExternalInput").ap()
    w = nc.dram_tensor("w", [2, D, MCOL], F32, kind="ExternalInput").ap()
    b = nc.dram_tensor("b", [2, MCOL], F32, kind="ExternalInput").ap()
    out = nc.dram_tensor("out", [2, 5, MCOL], F32, kind="ExternalOutput").ap()
    with contextlib.ExitStack() as st:
        sb = lambda n, s, d: st.enter_context(nc.sbuf_tensor(n, s, d))
        ct = sb("ct", [128, 16, 5], F32)
        cs = sb("cs", [128, 16, 5], F32)
        wt = [sb(f"wt{i}", [128, 16, 512], F32) for i in range(2)]
        bt = sb("bt", [5, 2, MCOL], F32)
        ot = sb("ot", [5, 2, MCOL], F32)
        ps = [st.enter_context(nc.psum_tensor(f"ps{i}", [5, 512], F32)) for i in range(2)]
        P = Prog(nc)
        P.dma(lambda e: e.dma_start(out=ct[:], in_=cin), w=["ct"])
        for l in range(2):
            P.dma(lambda e, l=l: e.dma_start(out=bt[:, l, :], in_=b[l:l + 1, :].to_broadcast([5, MCOL])), w=[("bt", l)])
        P.a(lambda e: e.activation(out=cs[:], in_=ct[:], func=AF.Silu), ["ct"], ["cs"])
        gi = 0
        for l in range(2):
            for g in range(3):
                buf = gi % 2
                src = w[l, :, g * 512:(g + 1) * 512].rearrange("(kc p) n -> p kc n", p=128)
                P.dma(lambda e, buf=buf, src=src: e.dma_start(out=wt[buf][:], in_=src), w=[("wt", buf)])
                for kc in range(16):
                    P.t(lambda e, buf=buf, kc=kc: e.matmul(ps[buf][:], lhsT=cs[:, kc, :], rhs=wt[buf][:, kc, :],
                                                           start=(kc == 0), stop=(kc == 15)),
                        ["cs", ("wt", buf)], [("ps", buf)])
                P.v(lambda e, buf=buf, l=l, g=g: e.tensor_tensor(out=ot[:, l, g * 512:(g + 1) * 512], in0=ps[buf][:],
                                                                 in1=bt[:, l, g * 512:(g + 1) * 512], op=ALU.add),
                    [("ps", buf), ("bt", l)], [("ot", l, g)])
                gi += 1
        for l in range(2):
            P.dma(lambda e, l=l: e.dma_start(out=out[l], in_=ot[:, l, :]), [("ot", l, g) for g in range(3)], [("out", l)])
        P.emit()
    return nc


def run_mod(c, c_ctx, w_mod, b_mod):
    c_all = np.concatenate([c, c_ctx[None, :]], axis=0).astype(np.float32)
    cin = np.ascontiguousarray(c_all.T.reshape(16, 128, 5).transpose(1, 0, 2))
    nc = build_mod()
    in_maps = [{"cin": cin, "w": np.ascontiguousarray(w_mod[:, :, j * MCOL:(j + 1) * MCOL]),
                "b": np.ascontiguousarray(b_mod[:, j * MCOL:(j + 1) * MCOL])} for j in range(8)]
    res = run_bass_kernel_spmd(nc, in_maps, core_ids=list(range(8)))
    return np.concatenate([r["out"] for r in res.results], axis=2)


def emit_rstd(P, ss, rstd, n, kr, kw):
    P.a(lambda e: e.activation(out=rstd, in_=ss, func=AF.Sqrt, scale=1.0 / n, bias=EPS), kr, kw)
    P.v(lambda e: e.reciprocal(out=rstd, in_=rstd), kw, kw)


NQK = 16
WIN_PERM = np.concatenate([np.arange(0, 1024), np.arange(2304, 3328),
                           np.arange(1024, 1280), np.arange(3328, 3584),
                           np.arange(1280, 2304)])


def build_s1(nc=None, io=None, pfx=""):
    if nc is None:
        nc = bass.Bass("TRN2", target_bir_lowering=False)
    fused, di, do = _mk(nc, io, pfx)
    xin = di("xin", [NTOK, D])
    modx = di("modx", [6, D])
    modc = di("modc", [6, D])
    n1g = di("n1g", [D])
    win = di("win", [D, 3584])
    gqk = di("gqk", [NQK * 128])
    cs_t = di("cs_t", [NTOK, 128])
    vnb = di("vnb", [512])
    wsT = di("wsT", [128, 4, 128])
    bsT = di("bsT", [128, 4])
    ogb = di("ogb", [512])
    ident_in = di("ident", [128, 128])
    QKT = do("QKT", [128, NQK, NTOK], BF16)
    Vout = do("Vout", [NTOK, 512], BF16)
    yB = do("yB", [NTOK, 512], BF16)

    with stage_ctx(nc, fused) as st:
        sb = lambda n, s, d=F32: st.enter_context(nc.sbuf_tensor(pfx + n, s, d))
        pt = lambda n, s, d=F32: st.enter_context(nc.psum_tensor(pfx + n, s, d))
        wbf = sb("wbf", [128, 16, 2048], BF16)
        G1 = sb("G1", [128, D])
        SH = sb("SH", [128, D])
        gn = sb("gn", [128, D])
        xt = [sb(f"xt{i}", [128, D]) for i in range(2)]
        tmp = sb("tmp", [128, D])
        hx = sb("hx", [128, D], BF16)
        hxT = sb("hxT", [128, 16, 128], BF16)
        ident = sb("ident_f", [128, 128])
        identb = sb("identb", [128, 128], BF16)
        ss = sb("ss", [128, 1])
        rs = sb("rs", [128, 1])
        Pb = sb("Pb", [128, 2048])
        gq = sb("gq", [128, NQK, 128])
        ssq = sb("ssq", [128, NQK])
        rsq = sb("rsq", [128, NQK])
        qn = sb("qn", [128, NQK, 2, 2, 32])
        cst = sb("cst", [128, 128])
        rA = sb("rA", [128, NQK, 2, 32])
        rB = sb("rB", [128, NQK, 2, 32])
        qr = sb("qr", [128, NQK, 2, 2, 32], BF16)
        qT = sb("qT", [128, NQK, 128], BF16)
        vb16 = sb("vb16", [128, 512], BF16)
        vng = sb("vng", [128, 512])
        wsb = sb("wsb", [128, 4, 128])
        wsbb = sb("wsbb", [128, 4, 128], BF16)
        bsb = sb("bsb", [128, 4])
        ogbt = sb("ogbt", [128, 512])
        t1 = sb("t1", [128, 1024])
        t2 = sb("t2", [128, 1024])
        gl = sb("gl", [128, 1024])
        ssv = sb("ssv", [128, 4])
        rsv = sb("rsv", [128, 4])
        vn = sb("vn", [128, 4, 128], BF16)
        ob = sb("ob", [128, 512])
        yb = sb("yb", [128, 512], BF16)
        pT = pt("pT", [128, 16, 128], BF16)
        pP = pt("pP", [128, 2048])
        pQ = pt("pQ", [128, NQK, 128], BF16)

        P = Prog(nc)
        P.dma(lambda e: e.dma_start(out=ident[:], in_=ident_in), w=["ident"])
        P.v(lambda e: e.tensor_copy(out=identb[:], in_=ident[:]), ["ident"], ["identb"])
        P.dma(lambda e: e.dma_start(out=gn[:], in_=_bc(n1g, D)), w=["gn"])
        P.dma(lambda e: e.dma_start(out=gq[:].rearrange("p h d -> p (h d)"), in_=_bc(gqk, NQK * 128)), w=["gq"])
        P.dma(lambda e: e.dma_start(out=vng[:], in_=_bc(vnb, 512)), w=["vng"])
        P.dma(lambda e: e.dma_start(out=ogbt[:], in_=_bc(ogb, 512)), w=["ogbt"])
        P.dma(lambda e: e.dma_start(out=wsb[:], in_=wsT), w=["wsb"])
        P.v(lambda e: e.tensor_copy(out=wsbb[:], in_=wsb[:]), ["wsb"], ["wsbb"])
        P.dma(lambda e: e.dma_start(out=bsb[:], in_=bsT), w=["bsb"])

        def load_w(c0, ncol):
            for k4 in range(4):
                src = win[k4 * 512:(k4 + 1) * 512, c0:c0 + ncol].rearrange("(kc p) n -> p kc n", p=128)
                P.dma(lambda e, k4=k4, src=src: e.dma_start(out=wbf[:, k4 * 4:(k4 + 1) * 4, 0:ncol], in_=src),
                      w=["wbf"], q="gpsimd")

        def load_mod(m):
            P.dma(lambda e: e.dma_start(out=SH[:], in_=_bc(m[0], D)), w=["SH"])
            P.dma(lambda e: e.dma_start(out=G1[:], in_=_bc(m[1], D)), w=["G1"])
            P.v(lambda e: e.scalar_tensor_tensor(out=G1[:], in0=G1[:], scalar=1.0, in1=gn[:], op0=ALU.add, op1=ALU.mult),
                ["G1", "gn"], ["G1"])

        def hx_tile(t):
            xb = xt[t % 2]
            kx = ("xt", t % 2)
            P.dma(lambda e: e.dma_start(out=xb[:], in_=xin[t * 128:(t + 1) * 128, :]), w=[kx])
            P.a(lambda e: e.activation(out=tmp[:], in_=xb[:], func=AF.Square, accum_out=ss[:]), [kx], ["tmp", "ss"])
            emit_rstd(P, ss[:], rs[:], D, ["ss"], ["rs"])
            P.v(lambda e: e.scalar_tensor_tensor(out=tmp[:], in0=xb[:], scalar=rs[:, 0:1], in1=G1[:], op0=ALU.mult, op1=ALU.mult),
                [kx, "rs", "G1"], ["tmp"])
            P.v(lambda e: e.tensor_tensor(out=hx[:], in0=tmp[:], in1=SH[:], op=ALU.add), ["tmp", "SH"], ["hx"])
            for kc in range(16):
                P.t(lambda e, kc=kc: e.transpose(pT[:, kc, :], hx[:, kc * 128:(kc + 1) * 128], identb[:]), ["hx", "identb"], ["pT"])
            P.a(lambda e: e.activation(out=hxT[:], in_=pT[:], func=AF.Copy), ["pT"], ["hxT"])

        def inproj(ncol):
            for g in range(ncol // 512):
                for kc in range(16):
                    P.t(lambda e, g=g, kc=kc: e.matmul(pP[:, g * 512:(g + 1) * 512], lhsT=hxT[:, kc, :],
                                                      rhs=wbf[:, kc, g * 512:(g + 1) * 512], start=(kc == 0), stop=(kc == 15)),
                        ["hxT", "wbf"], ["pP"])

        load_w(0, 2048)
        for t in range(NT):
            if t == 0:
                load_mod(modc)
            if t == 2:
                load_mod(modx)
            hx_tile(t)
            inproj(2048)
            P.a(lambda e: e.activation(out=Pb[:], in_=pP[:], func=AF.Copy), ["pP"], ["Pb"])
            Pv = Pb[:].rearrange("p (h d) -> p h d", h=NQK)
            P.dma(lambda e, t=t: e.dma_start(out=cst[:], in_=cs_t[t * 128:(t + 1) * 128, :]), w=["cst"])
            P.v(lambda e: e.tensor_tensor(out=tmp[:], in0=Pb[:], in1=Pb[:], op=ALU.mult), ["Pb"], ["tmp"])
            P.v(lambda e: e.tensor_reduce(out=ssq[:], in_=tmp[:].rearrange("p (h d) -> p h d", h=NQK), axis=AX.X, op=ALU.add),
                ["tmp"], ["ssq"])
            emit_rstd(P, ssq[:], rsq[:], 128, ["ssq"], ["rsq"])
            qnv = qn[:].rearrange("p h a b f -> p h (a b f)")
            P.v(lambda e: e.tensor_tensor(out=qnv, in0=Pv, in1=rsq[:].unsqueeze(2).to_broadcast([128, NQK, 128]), op=ALU.mult),
                ["Pb", "rsq"], ["qn"])
            P.v(lambda e: e.tensor_tensor(out=qnv, in0=qnv, in1=gq[:], op=ALU.mult), ["qn", "gq"], ["qn"])
            cosb = cst[:, 0:64].rearrange("p (a f) -> p a f", a=2).unsqueeze(1).to_broadcast([128, NQK, 2, 32])
            sinb = cst[:, 64:128].rearrange("p (a f) -> p a f", a=2).unsqueeze(1).to_broadcast([128, NQK, 2, 32])
            x1 = qn[:, :, :, 0, :]
            x2 = qn[:, :, :, 1, :]
            P.v(lambda e: e.tensor_tensor(out=rA[:], in0=x1, in1=cosb, op=ALU.mult), ["qn", "cst"], ["rA"])
            P.v(lambda e: e.tensor_tensor(out=rB[:], in0=x2, in1=sinb, op=ALU.mult), ["qn", "cst"], ["rB"])
            P.v(lambda e: e.tensor_tensor(out=qr[:, :, :, 0, :], in0=rA[:], in1=rB[:], op=ALU.subtract), ["rA", "rB"], ["qr"])
            P.v(lambda e: e.tensor_tensor(out=rA[:], in0=x2, in1=cosb, op=ALU.mult), ["qn", "cst", "qr"], ["rA"])
            P.v(lambda e: e.tensor_tensor(out=rB[:], in0=x1, in1=sinb, op=ALU.mult), ["qn", "cst", "qr"], ["rB"])
            P.v(lambda e: e.tensor_tensor(out=qr[:, :, :, 1, :], in0=rA[:], in1=rB[:], op=ALU.add), ["rA", "rB"], ["qr"])
            qrv = qr[:].rearrange("p h a b f -> p h (a b f)")
            for h in range(NQK):
                P.t(lambda e, h=h: e.transpose(pQ[:, h, :], qrv[:, h, :], identb[:]), ["qr", "identb"], ["pQ"])
            P.a(lambda e: e.activation(out=qT[:], in_=pQ[:], func=AF.Copy), ["pQ"], ["qT"])
            P.dma(lambda e, t=t: e.dma_start(out=QKT[:, :, t * 128:(t + 1) * 128], in_=qT[:]), ["qT"], [("QKT", t)])
            if fused and t >= 2:
                ktv = io["KTx"].rearrange("(h d) t -> d h t", d=128)
                c0 = (t - 2) * 128
                P.dma(lambda e, c0=c0: e.dma_start(out=ktv[:, 0:2, c0:c0 + 128], in_=qT[:, 6:8, :]), ["qT"], [("KTx", t, 0)])
                P.dma(lambda e, c0=c0: e.dma_start(out=ktv[:, 2:4, c0:c0 + 128], in_=qT[:, 14:16, :]), ["qT"], [("KTx", t, 1)])

        load_w(2048, 1536)
        for t in range(NT):
            if t == 0:
                load_mod(modc)
            if t == 2:
                load_mod(modx)
            hx_tile(t)
            inproj(1536)
            P.a(lambda e: e.activation(out=vb16[:], in_=pP[:, 0:512], func=AF.Copy), ["pP"], ["vb16"])
            P.dma(lambda e, t=t: e.dma_start(out=Vout[t * 128:(t + 1) * 128, :], in_=vb16[:]), ["vb16"], [("Vout", t)])
            if fused and t >= 2:
                P.dma(lambda e, t=t: e.dma_start(out=io["Vx"][(t - 2) * 128:(t - 1) * 128, :], in_=vb16[:]), ["vb16"], [("Vx", t)])
            P.a(lambda e: e.activation(out=gl[:], in_=pP[:, 512:1536], func=AF.Gelu_apprx_tanh), ["pP"], ["gl"])
            gv = gl[:, 512:1024]
            P.v(lambda e: e.tensor_tensor(out=t1[:, 0:512], in0=gv, in1=gv, op=ALU.mult), ["gl"], ["t1"])
            P.v(lambda e: e.tensor_reduce(out=ssv[:], in_=t1[:, 0:512].rearrange("p (g d) -> p g d", g=4), axis=AX.X, op=ALU.add),
                ["t1"], ["ssv"])
            emit_rstd(P, ssv[:], rsv[:], 128, ["ssv"], ["rsv"])
            P.v(lambda e: e.tensor_tensor(out=t1[:, 0:512].rearrange("p (g d) -> p g d", g=4), in0=gv.rearrange("p (g d) -> p g d", g=4),
                                          in1=rsv[:].unsqueeze(2).to_broadcast([128, 4, 128]), op=ALU.mult), ["gl", "rsv"], ["t1"])
            P.v(lambda e: e.tensor_tensor(out=vn[:].rearrange("p g d -> p (g d)"), in0=t1[:, 0:512], in1=vng[:], op=ALU.mult),
                ["t1", "vng"], ["vn"])
            for g in range(4):
                P.t(lambda e, g=g: e.matmul(pP[:, 1536 + g * 128:1536 + (g + 1) * 128], lhsT=wsbb[:, g, :], rhs=vn[:, g, :],
                                            start=True, stop=True), ["vn", "wsbb"], ["pM"])
            P.v(lambda e: e.tensor_tensor(out=ob[:].rearrange("p (g d) -> p g d", g=4),
                                          in0=pP[:, 1536:2048].rearrange("p (g d) -> p g d", g=4),
                                          in1=bsb[:].unsqueeze(2).to_broadcast([128, 4, 128]), op=ALU.add), ["pM", "bsb"], ["ob"])
            P.v(lambda e: e.tensor_tensor(out=ob[:], in0=ob[:], in1=gl[:, 0:512], op=ALU.mult), ["ob", "gl"], ["ob"])
            P.a(lambda e: e.activation(out=t2[:, 0:512], in_=ob[:], func=AF.Square, accum_out=ss[:]), ["ob"], ["t2", "ss"])
            emit_rstd(P, ss[:], rs[:], 512, ["ss"], ["rs"])
            P.v(lambda e: e.scalar_tensor_tensor(out=yb[:], in0=ob[:], scalar=rs[:, 0:1], in1=ogbt[:], op0=ALU.mult, op1=ALU.mult),
                ["ob", "rs", "ogbt"], ["yb"])
            P.dma(lambda e, t=t: e.dma_start(out=yB[t * 128:(t + 1) * 128, :], in_=yb[:]), ["yb"], [("yB", t)])
        P.emit(fused=fused)
    return nc


NKA = 20
NKC = 34


def build_s2a(nc=None, io=None, pfx=""):
    if nc is None:
        nc = bass.Bass("TRN2", target_bir_lowering=False)
    fused, di, do = _mk(nc, io, pfx)
    QKT = di("QKT", [128, NQK, NTOK], BF16)
    KA = None if fused else di("KA", [128, 2, NKA * 128], BF16)
    VA = None if fused else di("VA", [NKA * 128, 256], BF16)
    KC = None if fused else di("KC", [128, 2, NKC * 128], BF16)
    VC = None if fused else di("VC", [NKC * 128, 256], BF16)
    msk = di("msk", [4, 128, 128])
    sink = di("sink", [6])
    gac = di("gac", [1536])
    yAC = do("yAC", [NTOK, 1536], BF16)
    with stage_ctx(nc, fused) as st:
        sb = lambda n, s, d=F32: st.enter_context(nc.sbuf_tensor(pfx + n, s, d))
        pt = lambda n, s, d=F32: st.enter_context(nc.psum_tensor(pfx + n, s, d))
        ka = sb("ka", [128, 2, NKA * 128], BF16)
        va = sb("va", [128, NKA, 2, 129], BF16)
        kc = sb("kc", [128, 2, NKC * 128], BF16)
        vc = sb("vc", [128, NKC, 2, 129], BF16)
        mf = sb("mf", [128, 4, 128])
        mb = sb("mb", [128, 4, 128], BF16)
        sk = sb("sk", [128, 6])
        es = sb("es", [128, 6])
        gt = sb("gt", [128, 1536])
        qt = [sb(f"qt{i}", [128, NQK, 128], BF16) for i in range(2)]
        PT = [sb(f"PT{i}", [128, 3, 128], BF16) for i in range(3)]
        oo = sb("oo", [128, 2, 6, 128])
        den = sb("den", [128, 3])
        sq = sb("sq", [128, 768])
        ss = sb("ss", [128, 1])
        rs = sb("rs", [128, 1])
        yt = sb("yt", [128, 1536], BF16)
        psS = [pt(f"psS{i}", [128, 512]) for i in range(3)]
        psO = [pt(f"psO{i}", [128, 3, 129]) for i in range(2)]
        P = Prog(nc)
        if not fused:
            P.dma(lambda e: e.dma_start(out=ka[:], in_=KA), w=["ka"])
            P.dma(lambda e: e.dma_start(out=kc[:], in_=KC), w=["kc"])
        else:
            KTg = io["KTg"].rearrange("(r h d) t -> d r h t", r=2, h=4)
            Vg = io["Vg"]
            Vo = io["Vout"]
            for k in range(2):
                P.dma(lambda e, k=k: e.dma_start(out=ka[:, k, 0:256], in_=QKT[:, 6 + k, 0:256]), w=["ka"])
                P.dma(lambda e, k=k: e.dma_start(out=ka[:, k, 256:384], in_=KTg[:, 0, k, 1920:2048]), w=["ka"])
                P.dma(lambda e, k=k: e.dma_start(out=ka[:, k, 384:2432], in_=QKT[:, 6 + k, 256:2304]), w=["ka"])
                P.dma(lambda e, k=k: e.dma_start(out=ka[:, k, 2432:2560], in_=KTg[:, 1, k, 0:128]), w=["ka"])
                P.dma(lambda e, k=k: e.dma_start(out=kc[:, k, 0:256], in_=QKT[:, 14 + k, 0:256]), w=["kc"])
                for r in range(2):
                    P.dma(lambda e, k=k, r=r: e.dma_start(out=kc[:, k, 256 + r * 2048:256 + (r + 1) * 2048], in_=KTg[:, r, 2 + k, :]), w=["kc"])
        P.v(lambda e: e.memset(va[:, :, :, 128:129], 1.0), [], ["va1"])
        P.v(lambda e: e.memset(vc[:, :, :, 128:129], 1.0), [], ["vc1"])
        for k in range(2):
            if not fused:
                P.dma(lambda e, k=k: e.dma_start(out=va[:, :, k, 0:128], in_=VA[:, k * 128:(k + 1) * 128].rearrange("(j s) d -> s j d", s=128)), w=[("va", k)])
                P.dma(lambda e, k=k: e.dma_start(out=vc[:, :, k, 0:128], in_=VC[:, k * 128:(k + 1) * 128].rearrange("(j s) d -> s j d", s=128)), w=[("vc", k)])
            else:
                ca = slice(k * 128, (k + 1) * 128)
                cc = slice(256 + k * 128, 256 + (k + 1) * 128)
                tl = lambda ap: ap.rearrange("(j s) d -> s j d", s=128)
                P.dma(lambda e, k=k, ca=ca: e.dma_start(out=va[:, 0:2, k, 0:128], in_=tl(Vo[0:256, ca])), w=[("va", k)])
                P.dma(lambda e, k=k, ca=ca: e.dma_start(out=va[:, 2:3, k, 0:128], in_=tl(Vg[1920:2048, ca])), w=[("va", k)])
                P.dma(lambda e, k=k, ca=ca: e.dma_start(out=va[:, 3:19, k, 0:128], in_=tl(Vo[256:2304, ca])), w=[("va", k)])
                P.dma(lambda e, k=k, ca=ca: e.dma_start(out=va[:, 19:20, k, 0:128], in_=tl(Vg[2048:2176, ca])), w=[("va", k)])
                P.dma(lambda e, k=k, cc=cc: e.dma_start(out=vc[:, 0:2, k, 0:128], in_=tl(Vo[0:256, cc])), w=[("vc", k)])
                P.dma(lambda e, k=k, cc=cc: e.dma_start(out=vc[:, 2:34, k, 0:128], in_=tl(Vg[:, cc])), w=[("vc", k)])
        P.dma(lambda e: e.dma_start(out=mf[:], in_=msk.rearrange("m s q -> s m q")), w=["mf"])
        P.v(lambda e: e.tensor_copy(out=mb[:], in_=mf[:]), ["mf"], ["mb"])
        P.dma(lambda e: e.dma_start(out=sk[:], in_=_bc(sink, 6)), w=["sk"])
        P.a(lambda e: e.activation(out=es[:], in_=sk[:], func=AF.Exp), ["sk"], ["es"])
        P.dma(lambda e: e.dma_start(out=gt[:], in_=_bc(gac, 1536)), w=["gt"])
        PF = 2
        NBUF = PF + 1
        steps = []
        for t in range(NT):
            if t < 2:
                keysA = [(0, None), (1, None)]
                keysC = [(0, None), (1, None)]
            else:
                i = t - 2
                keysA = [(0, None), (1, None), (2 + i, 2 if i == 0 else 0), (3 + i, None), (4 + i, 3 if i == 15 else 1)]
                keysC = [(j, None) for j in range(NKC)]
            for (oi, qbase, KT, VT, kkey, vkeys, keys, use_sink) in (
                    (0, 0, ka, va, "ka", [("va", 0), ("va", 1), "va1"], keysA, True),
                    (1, 8, kc, vc, "kc", [("vc", 0), ("vc", 1), "vc1"], keysC, False)):
                for k in range(2):
                    for n, (j, m) in enumerate(keys):
                        steps.append(dict(t=t, oi=oi, qbase=qbase, KT=KT, VT=VT, kkey=kkey, vkeys=vkeys, k=k, n=n, j=j, m=m,
                                          last=(n == len(keys) - 1), use_sink=use_sink,
                                          tile_first=(oi == 0 and k == 0 and n == 0), tile_last=(oi == 1 and k == 1 and n == len(keys) - 1)))
        ngrp = [0]

        def load_q(t):
            q = qt[t % 2]
            P.dma(lambda e, q=q, t=t: e.dma_start(out=q[:], in_=QKT[:, :, t * 128:(t + 1) * 128]), w=[("qt", t % 2)])

        def s1(i):
            sp = steps[i]
            t, k, j, m = sp["t"], sp["k"], sp["j"], sp["m"]
            if sp["tile_first"]:
                if t == 0:
                    load_q(0)
                if t + 1 < NT:
                    load_q(t + 1)
            q = qt[t % 2]
            b = i % NBUF
            KT, qbase = sp["KT"], sp["qbase"]
            pS = psS[b][:, 0:384].rearrange("p (g q) -> p g q", g=3)
            P.t(lambda e: e.matmul(pS, lhsT=KT[:, k, j * 128:(j + 1) * 128], rhs=q[:, qbase + 3 * k:qbase + 3 * k + 3, :], start=True, stop=True),
                [sp["kkey"], ("qt", t % 2)], [("psS", b)])
            P.a(lambda e: e.activation(out=PT[b][:], in_=pS, func=AF.Exp, scale=128.0 ** -0.5), [("psS", b)], [("PT", b)])
            if m is not None:
                P.v(lambda e: e.tensor_tensor(out=PT[b][:], in0=PT[b][:], in1=mb[:, m, :].unsqueeze(1).to_broadcast([128, 3, 128]), op=ALU.mult),
                    [("PT", b), "mb"], [("PT", b)])

        def s2(i):
            sp = steps[i]
            t, k, j, n, oi = sp["t"], sp["k"], sp["j"], sp["n"], sp["oi"]
            b = i % NBUF
            VT = sp["VT"]
            pb = ngrp[0] % 2
            po = psO[pb]
            for g in range(3):
                P.t(lambda e, g=g: e.matmul(po[:, g, :], lhsT=PT[b][:, g, :], rhs=VT[:, j, k, :], start=(n == 0 and g == 0), stop=sp["last"]),
                    [("PT", b)] + sp["vkeys"], [("psO", pb)])
            if sp["last"]:
                ngrp[0] += 1
                if sp["use_sink"]:
                    P.v(lambda e: e.tensor_tensor(out=den[:], in0=po[:, :, 128], in1=es[:, 3 * k:3 * k + 3], op=ALU.add), [("psO", pb), "es"], ["den"])
                else:
                    P.v(lambda e: e.tensor_copy(out=den[:], in_=po[:, :, 128]), [("psO", pb)], ["den"])
                P.v(lambda e: e.reciprocal(out=den[:], in_=den[:]), ["den"], ["den"])
                P.v(lambda e: e.tensor_tensor(out=oo[:, oi, 3 * k:3 * k + 3, :], in0=po[:, :, 0:128], in1=den[:].unsqueeze(2).to_broadcast([128, 3, 128]),
                                              op=ALU.mult), [("psO", pb), "den"], [("oo", oi)])
            if sp["tile_last"]:
                for o2 in range(2):
                    ov = oo[:, o2, :, :].rearrange("p h d -> p (h d)")
                    P.v(lambda e, ov=ov: e.tensor_tensor(out=sq[:], in0=ov, in1=ov, op=ALU.mult), [("oo", o2)], ["sq"])
                    P.v(lambda e: e.tensor_reduce(out=ss[:], in_=sq[:], axis=AX.X, op=ALU.add), ["sq"], ["ss"])
                    emit_rstd(P, ss[:], rs[:], 768, ["ss"], ["rs"])
                    P.v(lambda e, ov=ov, o2=o2: e.scalar_tensor_tensor(out=yt[:, o2 * 768:(o2 + 1) * 768], in0=ov, scalar=rs[:, 0:1],
                                                                      in1=gt[:, o2 * 768:(o2 + 1) * 768], op0=ALU.mult, op1=ALU.mult),
                        [("oo", o2), "rs", "gt"], ["yt"])
                P.dma(lambda e: e.dma_start(out=yAC[t * 128:(t + 1) * 128, :], in_=yt[:]), ["yt"], [("yAC", t)])

        for i in range(min(PF, len(steps))):
            s1(i)
        for i in range(len(steps)):
            if i + PF < len(steps):
                s1(i + PF)
            s2(i)
        P.emit(fused=fused)
    return nc


def build_s2b(nc=None, io=None, pfx=""):
    if nc is None:
        nc = bass.Bass("TRN2", target_bir_lowering=False)
    fused, di, do = _mk(nc, io, pfx)
    yAC = di("yAC", [NTOK, 1536], BF16)
    yB = di("yB", [NTOK, 512], BF16)
    xin = di("xin", [NTOK, D])
    modx = di("modx", [6, D])
    modc = di("modc", [6, D])
    wout = di("wout", [D, D])
    n2g = di("n2g", [D])
    wr = di("wr", [D, NE])
    ident_in = di("ident", [128, 128])
    x1o = do("x1o", [NTOK, D])
    hx2o = do("hx2o", [NTOK, D], BF16)
    affT = None if fused else do("affT", [NE, NTOK])
    with stage_ctx(nc, fused) as st:
        sb = lambda n, s, d=F32: st.enter_context(nc.sbuf_tensor(pfx + n, s, d))
        pt = lambda n, s, d=F32: st.enter_context(nc.psum_tensor(pfx + n, s, d))
        wbf = sb("wbf", [128, 16, D], BF16)
        G1g = sb("G1g", [128, D])
        G2 = sb("G2", [128, D])
        SH2 = sb("SH2", [128, D])
        gn = sb("gn", [128, D])
        ident = sb("ident_f", [128, 128])
        identb = sb("identb", [128, 128], BF16)
        wrs = sb("wrs", [128, 16, NE])
        xt = sb("xt", [128, D])
        yt = sb("yt", [128, D], BF16)
        yT = sb("yT", [128, 16, 128], BF16)
        tmp = sb("tmp", [128, D])
        x1 = sb("x1", [128, D])
        h2 = sb("h2", [128, D])
        hb = sb("hb", [128, D], BF16)
        h2T = sb("h2T", [128, 16, 128])
        ss = sb("ss", [128, 1])
        rs = sb("rs", [128, 1])
        mx = sb("mx", [128, 1])
        se = sb("se", [128, 1])
        ex = sb("ex", [128, NE])
        af = sb("af", [128, NE])
        aT = sb("aT", [NE, 128])
        psT = pt("psT", [128, 16, 128], BF16)
        psX = [pt(f"psX{i}", [128, 512]) for i in range(2)]
        psR = [pt(f"psR{i}", [128, 4, 128]) for i in range(2)]
        psL = pt("psL", [128, 512])
        psA = pt("psA", [128, 512])
        P = Prog(nc)
        P.dma(lambda e: e.dma_start(out=ident[:], in_=ident_in), w=["ident"])
        P.v(lambda e: e.tensor_copy(out=identb[:], in_=ident[:]), ["ident"], ["identb"])
        P.dma(lambda e: e.dma_start(out=gn[:], in_=_bc(n2g, D)), w=["gn"])
        P.dma(lambda e: e.dma_start(out=wrs[:], in_=wr.rearrange("(kc p) n -> p kc n", p=128)), w=["wrs"])
        for k4 in range(4):
            src = wout[k4 * 512:(k4 + 1) * 512, :].rearrange("(kc p) n -> p kc n", p=128)
            P.dma(lambda e, k4=k4, src=src: e.dma_start(out=wbf[:, k4 * 4:(k4 + 1) * 4, :], in_=src), w=["wbf"], q="gpsimd")

        def load_mod(m):
            P.dma(lambda e: e.dma_start(out=G1g[:], in_=_bc(m[2], D)), w=["G1g"])
            P.dma(lambda e: e.dma_start(out=SH2[:], in_=_bc(m[3], D)), w=["SH2"])
            P.dma(lambda e: e.dma_start(out=G2[:], in_=_bc(m[4], D)), w=["G2"])
            P.v(lambda e: e.scalar_tensor_tensor(out=G2[:], in0=G2[:], scalar=1.0, in1=gn[:], op0=ALU.add, op1=ALU.mult),
                ["G2", "gn"], ["G2"])

        for t in range(NT):
            if t == 0:
                load_mod(modc)
            if t == 2:
                load_mod(modx)
            r0 = t * 128
            P.dma(lambda e, r0=r0: e.dma_start(out=yt[:, 0:768], in_=yAC[r0:r0 + 128, 0:768]), w=[("yt", 0)])
            P.dma(lambda e, r0=r0: e.dma_start(out=yt[:, 768:1280], in_=yB[r0:r0 + 128, :]), w=[("yt", 1)])
            P.dma(lambda e, r0=r0: e.dma_start(out=yt[:, 1280:2048], in_=yAC[r0:r0 + 128, 768:1536]), w=[("yt", 2)])
            P.dma(lambda e, r0=r0: e.dma_start(out=xt[:], in_=xin[r0:r0 + 128, :]), w=["xt"])
            for kc in range(16):
                P.t(lambda e, kc=kc: e.transpose(psT[:, kc, :], yt[:, kc * 128:(kc + 1) * 128], identb[:]),
                    [("yt", 0), ("yt", 1), ("yt", 2), "identb"], ["psT"])
            P.a(lambda e: e.activation(out=yT[:], in_=psT[:], func=AF.Copy), ["psT"], ["yT"])
            for g in range(4):
                px = psX[g % 2]
                for kc in range(16):
                    P.t(lambda e, px=px, g=g, kc=kc: e.matmul(px[:], lhsT=yT[:, kc, :], rhs=wbf[:, kc, g * 512:(g + 1) * 512],
                                                              start=(kc == 0), stop=(kc == 15)), ["yT", "wbf"], [("psX", g % 2)])
                P.v(lambda e, px=px, g=g: e.tensor_tensor(out=tmp[:, g * 512:(g + 1) * 512], in0=px[:], in1=G1g[:, g * 512:(g + 1) * 512],
                                                          op=ALU.mult), [("psX", g % 2), "G1g"], ["tmp"])
            P.v(lambda e: e.tensor_tensor(out=x1[:], in0=tmp[:], in1=xt[:], op=ALU.add), ["tmp", "xt"], ["x1"])
            P.dma(lambda e, r0=r0: e.dma_start(out=x1o[r0:r0 + 128, :], in_=x1[:]), ["x1"], [("x1o", t)])
            P.a(lambda e: e.activation(out=tmp[:], in_=x1[:], func=AF.Square, accum_out=ss[:]), ["x1"], ["tmp", "ss"])
            emit_rstd(P, ss[:], rs[:], D, ["ss"], ["rs"])
            P.v(lambda e: e.scalar_tensor_tensor(out=h2[:], in0=x1[:], scalar=rs[:, 0:1], in1=G2[:], op0=ALU.mult, op1=ALU.mult),
                ["x1", "rs", "G2"], ["h2"])
            P.v(lambda e: e.tensor_tensor(out=h2[:], in0=h2[:], in1=SH2[:], op=ALU.add), ["h2", "SH2"], ["h2"])
            P.a(lambda e: e.activation(out=hb[:], in_=h2[:], func=AF.Copy), ["h2"], ["hb"])
            P.dma(lambda e, r0=r0: e.dma_start(out=hx2o[r0:r0 + 128, :], in_=hb[:]), ["hb"], [("hx2o", t)])
            for q4 in range(4):
                pr = psR[q4 % 2]
                for j in range(4):
                    kc = q4 * 4 + j
                    P.t(lambda e, pr=pr, j=j, kc=kc: e.transpose(pr[:, j, :], h2[:, kc * 128:(kc + 1) * 128], ident[:]),
                        ["h2", "ident"], [("psR", q4 % 2)])
                P.a(lambda e, pr=pr, q4=q4: e.activation(out=h2T[:, q4 * 4:(q4 + 1) * 4, :], in_=pr[:], func=AF.Copy),
                    [("psR", q4 % 2)], ["h2T"])
            for kc in range(16):
                P.t(lambda e, kc=kc: e.matmul(psL[:, 0:NE], lhsT=h2T[:, kc, :], rhs=wrs[:, kc, :], start=(kc == 0), stop=(kc == 15)),
                    ["h2T", "wrs"], ["psL"])
            P.v(lambda e: e.tensor_reduce(out=mx[:], in_=psL[:, 0:NE], axis=AX.X, op=ALU.max), ["psL"], ["mx"])
            P.v(lambda e: e.tensor_scalar(out=mx[:], in0=mx[:], scalar1=-1.0, scalar2=None, op0=ALU.mult), ["mx"], ["mx"])
            P.a(lambda e: e.activation(out=ex[:], in_=psL[:, 0:NE], func=AF.Exp, bias=mx[:, 0:1], accum_out=se[:]), ["psL", "mx"], ["ex", "se"])
            P.v(lambda e: e.reciprocal(out=se[:], in_=se[:]), ["se"], ["se"])
            P.v(lambda e: e.tensor_scalar(out=af[:], in0=ex[:], scalar1=se[:, 0:1], scalar2=None, op0=ALU.mult), ["ex", "se"], ["af"])
            if fused:
                adst = io["affC"][r0:r0 + 128, :] if t < 2 else io["affIn"][r0 - 256:r0 - 128, :]
                P.dma(lambda e, adst=adst: e.dma_start(out=adst, in_=af[:]), ["af"], [("affo", t)])
            else:
                P.t(lambda e: e.transpose(psA[0:NE, 0:128], af[:], ident[:]), ["af", "ident"], ["psA"])
                P.a(lambda e: e.activation(out=aT[:], in_=psA[0:NE, 0:128], func=AF.Copy), ["psA"], ["aT"])
                P.dma(lambda e, r0=r0: e.dma_start(out=affT[:, r0:r0 + 128], in_=aT[:]), ["aT"], [("affT", t)])
        P.emit(fused=fused)
    return nc


NEL = 8
NROW = SEQ + CTX


def build_s3(with_ctx, nc=None, io=None, pfx=""):
    if nc is None:
        nc = bass.Bass("TRN2", target_bir_lowering=False)
    fused, di, do = _mk(nc, io, pfx)
    affL = di("affL", [128, 32, NEL])
    affC = di("affC", [128, 2, NEL])
    hx2 = di("hx2", [NROW, D], BF16)
    wg = di("wg", [NEL, D, D])
    wu = di("wu", [NEL, D, D])
    wd = di("wd", [NEL, D, D])
    consts = di("consts", [128, 128 + 128 + 128 + 512 + 34])
    delta = [do(f"delta{i}", [NROW, 512]) for i in range(4)]
    sets = [("L", 32, 512, 0, affL)]
    if with_ctx:
        sets.append(("C", 2, 32, SEQ, affC))
    with stage_ctx(nc, fused) as st:
        sb = lambda n, s, d=F32: st.enter_context(nc.sbuf_tensor(pfx + n, s, d))
        pt = lambda n, s, d=F32: st.enter_context(nc.psum_tensor(pfx + n, s, d))
        cst = sb("cst", [128, 930])
        identb = sb("identb", [128, 128], BF16)
        zt = sb("zt", [128, D])
        ring = [sb(f"ring{i}", [128, 16, 512], BF16) for i in range(4)]
        xg = [sb(f"xg{i}", [128, D], BF16) for i in range(2)]
        xgT = sb("xgT", [128, 16, 544], BF16)
        hT = sb("hT", [128, 16, 544], BF16)
        sil = sb("sil", [128, 544])
        yb = [sb(f"yb{i}", [128, 512]) for i in range(3)]
        S = {}
        for (nm, C, k, r0, _) in sets:
            S[nm] = dict(
                aff=sb(f"saff{nm}", [128, C, NEL]), lo=sb(f"lo{nm}", [128, NEL]), th=sb(f"th{nm}", [128, NEL, 15]),
                cmp=sb(f"cmp{nm}", [128, C, NEL, 15]), cntp=sb(f"cntp{nm}", [128, NEL, 15]), ge=sb(f"ge{nm}", [128, NEL, 15]),
                nge=sb(f"nge{nm}", [128, NEL]), mask=sb(f"mask{nm}", [128, C, NEL]), maskb=sb(f"maskb{nm}", [128, C, NEL], BF16),
                cA=sb(f"cA{nm}", [128, C, NEL]), cB=sb(f"cB{nm}", [128, C, NEL]), pos=sb(f"pos{nm}", [128, C, NEL]),
                vals=sb(f"vals{nm}", [128, C, NEL, 2]), oh=[sb(f"oh{nm}{i}", [128, k]) for i in range(2)],
                sl=sb(f"sl{nm}", [128, NEL, 4, 2]), idx=sb(f"idx{nm}", [128, NEL, 4], I32), gate=sb(f"gate{nm}", [128, NEL, 4]))
        jt = sb("jt", [128, NEL, 15])
        trib = sb("trib", [128, 128], BF16)
        onesb = sb("onesb", [128, 128], BF16)
        psA = [pt(f"psA{i}", [128, 512]) for i in range(2)]
        psU = [pt(f"psU{i}", [128, 512]) for i in range(2)]
        psT = pt("psT", [128, 16, 128], BF16)
        psY = [pt(f"psY{i}", [128, 512]) for i in range(2)]
        ident = cst[:, 0:128]
        ones = cst[:, 128:256]
        tri = cst[:, 256:384]
        iota = cst[:, 384:896]
        tokid = cst[:, 896:930]
        P = Prog(nc)
        P.dma(lambda e: e.dma_start(out=cst[:], in_=consts), w=["cst"])
        P.v(lambda e: e.tensor_copy(out=identb[:], in_=ident), ["cst"], ["identb"])
        P.v(lambda e: e.tensor_copy(out=trib[:], in_=tri), ["cst"], ["trib"])
        P.v(lambda e: e.tensor_copy(out=onesb[:], in_=ones), ["cst"], ["onesb"])
        for j in range(15):
            P.v(lambda e, j=j: e.memset(jt[:, :, j:j + 1], float(j + 1)), [], ["jt"])
        P.v(lambda e: e.memset(zt[:], 0.0), [], ["zt"])
        for cb in range(4):
            for r in range(NROW // 128):
                P.dma(lambda e, r=r, cb=cb: e.dma_start(out=delta[cb][r * 128:(r + 1) * 128, :], in_=zt[:, 0:512]), ["zt"], [("delta", cb)])

        def select(nm, C, k, r0, affd):
            s = S[nm]
            kk = lambda x: (nm, x)
            P.dma(lambda e, s=s, affd=affd: e.dma_start(out=s["aff"][:], in_=affd), w=[kk("aff")])
            P.v(lambda e, s=s: e.memset(s["lo"][:], 0.0), [], [kk("lo")])
            affb = s["aff"][:].unsqueeze(3).to_broadcast([128, C, NEL, 15])
            for it in range(8):
                step = 16.0 ** -(it + 1)
                P.v(lambda e, s=s, step=step: e.scalar_tensor_tensor(out=s["th"][:], in0=jt[:], scalar=step,
                                                                     in1=s["lo"][:].unsqueeze(2).to_broadcast([128, NEL, 15]),
                                                                     op0=ALU.mult, op1=ALU.add), ["jt", kk("lo")], [kk("th")])
                P.v(lambda e, s=s: e.tensor_tensor(out=s["cmp"][:], in0=affb, in1=s["th"][:].unsqueeze(1).to_broadcast([128, C, NEL, 15]),
                                                   op=ALU.is_ge), [kk("aff"), kk("th")], [kk("cmp")])
                P.v(lambda e, s=s: e.tensor_reduce(out=s["cntp"][:], in_=s["cmp"][:].rearrange("p c e j -> p e j c"), axis=AX.X, op=ALU.add),
                    [kk("cmp")], [kk("cntp")])
                P.t(lambda e, s=s: e.matmul(psY[0][:, 0:NEL * 15], lhsT=ones, rhs=s["cntp"][:].rearrange("p e j -> p (e j)"), start=True, stop=True),
                    ["cst", kk("cntp")], [("psY", 0)])
                P.v(lambda e, s=s, k=k: e.tensor_scalar(out=s["ge"][:].rearrange("p e j -> p (e j)"), in0=psY[0][:, 0:NEL * 15], scalar1=float(k) - 0.5,
                                                        scalar2=None, op0=ALU.is_ge), [("psY", 0)], [kk("ge")])
                P.v(lambda e, s=s: e.tensor_reduce(out=s["nge"][:], in_=s["ge"][:], axis=AX.X, op=ALU.add), [kk("ge")], [kk("nge")])
                P.v(lambda e, s=s, step=step: e.scalar_tensor_tensor(out=s["lo"][:], in0=s["nge"][:], scalar=step, in1=s["lo"][:],
                                                                     op0=ALU.mult, op1=ALU.add), [kk("nge"), kk("lo")], [kk("lo")])
            P.v(lambda e, s=s: e.tensor_tensor(out=s["mask"][:], in0=s["aff"][:], in1=s["lo"][:].unsqueeze(1).to_broadcast([128, C, NEL]),
                                               op=ALU.is_ge), [kk("aff"), kk("lo")], [kk("mask")])
            P.v(lambda e, s=s: e.tensor_copy(out=s["maskb"][:], in_=s["mask"][:]), [kk("mask")], [kk("maskb")])
            mb2 = s["maskb"][:].rearrange("p c e -> p (c e)")
            P.t(lambda e: e.matmul(psY[0][:, 0:C * NEL], lhsT=trib[:], rhs=mb2, start=True, stop=True), ["trib", kk("maskb")], [("psY", 0)])
            P.t(lambda e: e.matmul(psY[1][:, 0:C * NEL], lhsT=onesb[:], rhs=mb2, start=True, stop=True), ["onesb", kk("maskb")], [("psY", 1)])
            P.v(lambda e, s=s: e.tensor_copy(out=s["cA"][:].rearrange("p c e -> p (c e)"), in_=psY[1][:, 0:C * NEL]), [("psY", 1)], [kk("cA")])
            cur, nxt = "cA", "cB"
            sh = 1
            while sh < C:
                P.v(lambda e, s=s, cur=cur, nxt=nxt, sh=sh: e.tensor_copy(out=s[nxt][:, 0:sh, :], in_=s[cur][:, 0:sh, :]), [kk(cur)], [kk(nxt)])
                P.v(lambda e, s=s, cur=cur, nxt=nxt, sh=sh: e.tensor_tensor(out=s[nxt][:, sh:C, :], in0=s[cur][:, sh:C, :], in1=s[cur][:, 0:C - sh, :],
                                                                          op=ALU.add), [kk(cur)], [kk(nxt)])
                cur, nxt = nxt, cur
                sh *= 2
            P.v(lambda e, s=s, cur=cur: e.tensor_tensor(out=s["pos"][:].rearrange("p c e -> p (c e)"), in0=s[cur][:].rearrange("p c e -> p (c e)"),
                                                        in1=psY[1][:, 0:C * NEL], op=ALU.subtract), [kk(cur), ("psY", 1)], [kk("pos")])
            P.v(lambda e, s=s: e.tensor_tensor(out=s["pos"][:].rearrange("p c e -> p (c e)"), in0=s["pos"][:].rearrange("p c e -> p (c e)"),
                                               in1=psY[0][:, 0:C * NEL], op=ALU.add), [kk("pos"), ("psY", 0)], [kk("pos")])
            P.v(lambda e, s=s: e.tensor_tensor(out=s["pos"][:], in0=s["pos"][:], in1=s["mask"][:], op=ALU.mult), [kk("pos"), kk("mask")], [kk("pos")])
            P.v(lambda e, s=s: e.tensor_scalar(out=s["pos"][:], in0=s["pos"][:], scalar1=-1.0, scalar2=None, op0=ALU.add), [kk("pos")], [kk("pos")])
            P.v(lambda e, s=s, r0=r0: e.tensor_scalar(out=s["vals"][:, :, :, 0], in0=tokid[:, 0:C].unsqueeze(2).to_broadcast([128, C, NEL]),
                                                      scalar1=float(r0), scalar2=None, op0=ALU.add), ["cst"], [kk("vals")])
            P.v(lambda e, s=s: e.tensor_copy(out=s["vals"][:, :, :, 1], in_=s["aff"][:]), [kk("aff")], [kk("vals")])
            nst = (k + 127) // 128
            sw = min(k, 128)
            n_oh = 0
            for el in range(NEL):
                for c in range(C):
                    ohb = s["oh"][n_oh % 2]
                    kb = (nm, "oh", n_oh % 2)
                    n_oh += 1
                    P.v(lambda e, ohb=ohb, s=s, c=c, el=el, k=k: e.tensor_scalar(out=ohb[:], in0=iota[:, 0:k], scalar1=s["pos"][:, c, el:el + 1],
                                                                                scalar2=None, op0=ALU.is_equal), ["cst", kk("pos")], [kb])
                    for sti in range(nst):
                        P.t(lambda e, ohb=ohb, s=s, c=c, el=el, sti=sti, sw=sw: e.matmul(
                            psU[0][0:sw, (el * 4 + sti) * 2:(el * 4 + sti) * 2 + 2], lhsT=ohb[:, sti * 128:sti * 128 + sw],
                            rhs=s["vals"][:, c, el, :], start=(c == 0 and el == 0 and sti == 0), stop=(c == C - 1)),
                            [kb, kk("vals")], [("psU", 0)])
            P.v(lambda e, s=s, sw=sw: e.tensor_copy(out=s["sl"][0:sw].rearrange("p e s t -> p (e s t)"), in_=psU[0][0:sw, 0:NEL * 8]), [("psU", 0)], [kk("sl")])
            P.v(lambda e, s=s, sw=sw: e.tensor_copy(out=s["idx"][0:sw], in_=s["sl"][0:sw, :, :, 0]), [kk("sl")], [kk("idx")])
            P.v(lambda e, s=s, sw=sw: e.tensor_copy(out=s["gate"][0:sw], in_=s["sl"][0:sw, :, :, 1]), [kk("sl")], [kk("gate")])

        for sargs in sets:
            select(*sargs)

        tiles = [("L", sti, 128, sti * 128) for sti in range(4)]
        if with_ctx:
            tiles.append(("C", 0, 32, 512))
        ncols = 544 if with_ctx else 512
        ring_n = [0]
        ny = [0]

        def load_unit(wsrc, el, cb):
            rb = ring_n[0] % 4
            ring_n[0] += 1
            for k4 in range(4):
                src = wsrc[el, k4 * 512:(k4 + 1) * 512, cb * 512:(cb + 1) * 512].rearrange("(kc p) n -> p kc n", p=128)
                P.dma(lambda e, rb=rb, k4=k4, src=src: e.dma_start(out=ring[rb][:, k4 * 4:(k4 + 1) * 4, :], in_=src), w=[("ring", rb)], q="gpsimd")
            return rb

        for el in range(NEL):
            for ti, (nm, sti, n, c0) in enumerate(tiles):
                s = S[nm]
                xb = xg[ti % 2]
                kx = ("xg", ti % 2)
                P.op("gpsimd", lambda e, xb=xb, s=s, el=el, sti=sti, n=n: e.indirect_dma_start(
                    out=xb[0:n, :], out_offset=None, in_=hx2[:, :],
                    in_offset=bass.IndirectOffsetOnAxis(ap=s["idx"][0:n, el, sti:sti + 1], axis=0)), [(nm, "idx")], [kx], dma=True)
                for kc in range(16):
                    P.t(lambda e, xb=xb, kc=kc, n=n: e.transpose(psT[:, kc, 0:n], xb[0:n, kc * 128:(kc + 1) * 128], identb[0:n, 0:n]), [kx, "identb"], ["psT"])
                P.a(lambda e, c0=c0, n=n: e.activation(out=xgT[:, :, c0:c0 + n], in_=psT[:, :, 0:n], func=AF.Copy), ["psT"], ["xgT"])
            for cb in range(4):
                rg = load_unit(wg, el, cb)
                ru = load_unit(wu, el, cb)
                for f4 in range(4):
                    fc = cb * 4 + f4
                    pa = psA[fc % 2]
                    pu = psU[fc % 2]
                    for (pp, rbuf, key) in ((pa, rg, "psA"), (pu, ru, "psU")):
                        for kc in range(16):
                            P.t(lambda e, pp=pp, rbuf=rbuf, kc=kc, f4=f4: e.matmul(pp[:, 0:512], lhsT=ring[rbuf][:, kc, f4 * 128:(f4 + 1) * 128],
                                                                                  rhs=xgT[:, kc, 0:512], start=(kc == 0), stop=(kc == 15)),
                                [("ring", rbuf), "xgT"], [(key, fc % 2)])
                    P.a(lambda e, pa=pa: e.activation(out=sil[:, 0:512], in_=pa[:, 0:512], func=AF.Silu), [("psA", fc % 2)], ["sil"])
                    P.v(lambda e, pu=pu, fc=fc: e.tensor_tensor(out=hT[:, fc, 0:512], in0=sil[:, 0:512], in1=pu[:, 0:512], op=ALU.mult),
                        ["sil", ("psU", fc % 2)], ["hT"])
                    if with_ctx:
                        for (pp, rbuf, key) in ((pa, rg, "psA"), (pu, ru, "psU")):
                            for kc in range(16):
                                P.t(lambda e, pp=pp, rbuf=rbuf, kc=kc, f4=f4: e.matmul(pp[:, 0:32], lhsT=ring[rbuf][:, kc, f4 * 128:(f4 + 1) * 128],
                                                                                      rhs=xgT[:, kc, 512:544], start=(kc == 0), stop=(kc == 15)),
                                    [("ring", rbuf), "xgT"], [(key, fc % 2)])
                        P.a(lambda e, pa=pa: e.activation(out=sil[:, 512:544], in_=pa[:, 0:32], func=AF.Silu), [("psA", fc % 2)], ["sil"])
                        P.v(lambda e, pu=pu, fc=fc: e.tensor_tensor(out=hT[:, fc, 512:544], in0=sil[:, 512:544], in1=pu[:, 0:32], op=ALU.mult),
                            ["sil", ("psU", fc % 2)], ["hT"])
            for cb in range(4):
                rd = load_unit(wd, el, cb)
                for ti, (nm, sti, n, c0) in enumerate(tiles):
                    s = S[nm]
                    py = psY[ny[0] % 2]
                    ky = ("psY", ny[0] % 2)
                    ybuf = yb[ny[0] % 3]
                    kyb = ("yb", ny[0] % 3)
                    ny[0] += 1
                    for fc in range(16):
                        P.t(lambda e, py=py, rd=rd, fc=fc, c0=c0, n=n: e.matmul(py[0:n, :], lhsT=hT[:, fc, c0:c0 + n], rhs=ring[rd][:, fc, :],
                                                                               start=(fc == 0), stop=(fc == 15)), ["hT", ("ring", rd)], [ky])
                    P.v(lambda e, py=py, ybuf=ybuf, s=s, el=el, sti=sti, n=n: e.tensor_scalar(out=ybuf[0:n, :], in0=py[0:n, :],
                                                                                              scalar1=s["gate"][0:n, el, sti:sti + 1], scalar2=None, op0=ALU.mult),
                        [ky, (nm, "gate")], [kyb])
                    P.op("gpsimd", lambda e, ybuf=ybuf, s=s, el=el, sti=sti, n=n, cb=cb: e.indirect_dma_start(
                        out=delta[cb][:, :], out_offset=bass.IndirectOffsetOnAxis(ap=s["idx"][0:n, el, sti:sti + 1], axis=0),
                        in_=ybuf[0:n, :], in_offset=None, compute_op=ALU.add), [kyb, (nm, "idx"), ("delta", cb)], [("delta", cb)], dma=True)
        P.emit(fused=fused)
    return nc


def s3_consts():
    c = np.zeros((128, 930), np.float32)
    c[:, 0:128] = np.eye(128)
    c[:, 128:256] = 1.0
    c[:, 256:384] = np.triu(np.ones((128, 128)))
    c[:, 384:896] = np.arange(512)[None, :]
    c[:, 896:930] = np.arange(34)[None, :] * 128 + np.arange(128)[:, None]
    return c


def build_s4(nc=None, io=None, pfx=""):
    if nc is None:
        nc = bass.Bass("TRN2", target_bir_lowering=False)
    fused, di, do = _mk(nc, io, pfx)
    x1 = di("x1", [NTOK, D])
    dA = di("dA", [NTOK, D])
    dB = None if fused else di("dB", [NTOK, D])
    modx = di("modx", [6, D])
    modc = di("modc", [6, D])
    x2 = do("x2", [NTOK, D])
    with stage_ctx(nc, fused) as st:
        sb = lambda n, s, d=F32: st.enter_context(nc.sbuf_tensor(pfx + n, s, d))
        G = sb("G", [128, D])
        xa = [sb(f"xa{i}", [128, D]) for i in range(2)]
        da = [sb(f"da{i}", [128, D]) for i in range(2)]
        db = [sb(f"db{i}", [128, D]) for i in range(2)]
        P = Prog(nc)
        for t in range(NT):
            i = t % 2
            if t == 0:
                P.dma(lambda e: e.dma_start(out=G[:], in_=_bc(modc[5], D)), w=["G"])
            if t == 2:
                P.dma(lambda e: e.dma_start(out=G[:], in_=_bc(modx[5], D)), w=["G"])
            r0 = t * 128
            P.dma(lambda e, i=i, r0=r0: e.dma_start(out=xa[i][:], in_=x1[r0:r0 + 128, :]), w=[("xa", i)])
            P.dma(lambda e, i=i, r0=r0: e.dma_start(out=da[i][:], in_=dA[r0:r0 + 128, :]), w=[("da", i)])
            if not fused:
                P.dma(lambda e, i=i, r0=r0: e.dma_start(out=db[i][:], in_=dB[r0:r0 + 128, :]), w=[("db", i)])
                P.v(lambda e, i=i: e.tensor_tensor(out=da[i][:], in0=da[i][:], in1=db[i][:], op=ALU.add), [("da", i), ("db", i)], [("da", i)])
            P.v(lambda e, i=i: e.tensor_tensor(out=da[i][:], in0=da[i][:], in1=G[:], op=ALU.mult), [("da", i), "G"], [("da", i)])
            P.v(lambda e, i=i: e.tensor_tensor(out=xa[i][:], in0=xa[i][:], in1=da[i][:], op=ALU.add), [("xa", i), ("da", i)], [("xa", i)])
            P.dma(lambda e, i=i, r0=r0: e.dma_start(out=x2[r0:r0 + 128, :], in_=xa[i][:]), [("xa", i)], [("x2", t)])
        P.emit(fused=fused)
    return nc


def _rope_tables(h):
    t = np.arange(h * 2048, (h + 1) * 2048)
    row = (t // 64).astype(np.float32)
    col = (t % 64).astype(np.float32)
    inv = (np.float32(10000.0) ** (-np.arange(32, dtype=np.float32) / np.float32(32))).astype(np.float32)
    ar = row[:, None] * inv
    ac = col[:, None] * inv
    tab = np.zeros((NTOK, 128), np.float32)
    tab[:256, :64] = 1.0
    tab[256:, :64] = np.concatenate([np.cos(ar), np.cos(ac)], 1)
    tab[256:, 64:] = np.concatenate([np.sin(ar), np.sin(ac)], 1)
    return tab


def _masks(h):
    s = np.arange(128)[:, None]
    q = np.arange(128)[None, :]
    mp = (s >= q).astype(np.float32)
    mn = (s <= q).astype(np.float32)
    return np.stack([mp, mn, mp * (0.0 if h == 0 else 1.0), mn * (0.0 if h == 1 else 1.0)])


def _run(nc, in_maps):
    res = run_bass_kernel_spmd(nc, in_maps, core_ids=list(range(8)))
    return res.results


PAIRS = [[0, 1], [2, 3], [4, 5], [6, 7]]
NXR = NTOK + 128
CAPL = 384
NCST = 931


def f_consts():
    c = np.zeros((128, NCST), np.float32)
    c[:, 0:930] = s3_consts()
    c[:, 930] = NTOK + np.arange(128)
    return c


def stage_mod(nc, io, pfx):
    cin, w, b, modS = io["cin"], io["w_mod"], io["b_mod"], io["modS"]
    with stage_ctx(nc, True) as st:
        sb = lambda n, s, d=F32: st.enter_context(nc.sbuf_tensor(pfx + n, s, d))
        ct = sb("ct", [128, 16, 2])
        cs = sb("cs", [128, 16, 2])
        wt = [sb(f"wt{i}", [128, 16, 512]) for i in range(3)]
        bt = [sb(f"bt{i}", [2, 512]) for i in range(3)]
        ot = [sb(f"ot{i}", [2, 512]) for i in range(3)]
        ps = [st.enter_context(nc.psum_tensor(pfx + f"ps{i}", [2, 512], F32)) for i in range(2)]
        P = Prog(nc)
        P.dma(lambda e: e.dma_start(out=ct[:], in_=cin), w=["ct"])
        P.a(lambda e: e.activation(out=cs[:], in_=ct[:], func=AF.Silu), ["ct"], ["cs"])
        gi = 0
        for l in range(2):
            for g in range(24):
                i3 = gi % 3
                i2 = gi % 2
                gi += 1
                cols = slice(g * 512, (g + 1) * 512)
                src = w[l, :, cols].rearrange("(kc p) n -> p kc n", p=128)
                P.dma(lambda e, i3=i3, src=src: e.dma_start(out=wt[i3][:], in_=src), w=[("wt", i3)])
                P.dma(lambda e, i3=i3, l=l, cols=cols: e.dma_start(out=bt[i3][:], in_=b[l:l + 1, cols].to_broadcast([2, 512])), w=[("bt", i3)])
                for kc in range(16):
                    P.t(lambda e, i2=i2, i3=i3, kc=kc: e.matmul(ps[i2][:], lhsT=cs[:, kc, :], rhs=wt[i3][:, kc, :], start=(kc == 0), stop=(kc == 15)),
                        ["cs", ("wt", i3)], [("ps", i2)])
                P.v(lambda e, i2=i2, i3=i3: e.tensor_tensor(out=ot[i3][:], in0=ps[i2][:], in1=bt[i3][:], op=ALU.add), [("ps", i2), ("bt", i3)], [("ot", i3)])
                P.dma(lambda e, i3=i3, l=l, cols=cols: e.dma_start(out=modS[l, :, cols], in_=ot[i3][:]), [("ot", i3)], [("modS", l, g)])
        P.emit(fused=True)


def stage_cc(nc, pairs):
    with nc.cleanup_on_exit():
        _UID[0] += 1
        sem = nc.alloc_semaphore(name="cc%d" % _UID[0])
        with nc.Block() as block:
            @block.gpsimd
            def _(g):
                for i, (a, b) in enumerate(pairs):
                    g.collective_compute("AllGather", ALU.bypass, replica_groups=PAIRS, ins=[a], outs=[b]).then_inc(sem, 1)
                    g.wait_ge(sem, i + 1)
        nc.all_engine_barrier()


def stage_s3a(nc, io, with_ctx, pfx):
    affAll, affIn, affC, consts = io["affAll"], io["affIn"], io["affC"], io["consts"]
    idxT, gateT, delta, hx2 = io["idxT"], io["gateT"], io["delta"], io["hx2o"]
    with stage_ctx(nc, True) as st:
        sb = lambda n, s, d=F32: st.enter_context(nc.sbuf_tensor(pfx + n, s, d))
        pt = lambda n, s, d=F32: st.enter_context(nc.psum_tensor(pfx + n, s, d))
        cst = sb("cst", [128, NCST])
        trib = sb("trib", [128, 128], BF16)
        onesb = sb("onesb", [128, 128], BF16)
        jt = sb("jt", [128, NE, 15])
        zt = sb("zt", [128, D])
        ztb = sb("ztb", [128, D], BF16)
        sl = sb("sl", [128, NE, 4, 3])
        idxf = sb("idxf", [128, NE, 4])
        idx = sb("idx", [128, NE, 4], I32)
        gate = sb("gate", [128, NE, 4])
        psC = pt("psC", [128, 512])
        psP = [pt(f"psP{i}", [128, 512]) for i in range(2)]
        psS = [pt(f"psS{i}", [128, 512]) for i in range(2)]
        ones = cst[:, 128:256]
        tri = cst[:, 256:384]
        iota = cst[:, 384:896]
        tokid = cst[:, 896:930]
        dummy = cst[:, 930:931]
        P = Prog(nc)
        P.dma(lambda e: e.dma_start(out=cst[:], in_=consts), w=["cst"])
        P.v(lambda e: e.tensor_copy(out=trib[:], in_=tri), ["cst"], ["trib"])
        P.v(lambda e: e.tensor_copy(out=onesb[:], in_=ones), ["cst"], ["onesb"])
        for j in range(15):
            P.v(lambda e, j=j: e.memset(jt[:, :, j:j + 1], float(j + 1)), [], ["jt"])
        P.v(lambda e: e.memset(zt[:], 0.0), [], ["zt"])
        P.v(lambda e: e.memset(ztb[:], 0.0), [], ["ztb"])
        P.v(lambda e: e.memset(sl[:], 0.0), [], ["sl"])
        for r in range(NXR // 128):
            P.dma(lambda e, r=r: e.dma_start(out=delta[r * 128:(r + 1) * 128, :], in_=zt[:]), ["zt"], [("delta", r)])
        P.dma(lambda e: e.dma_start(out=hx2[NTOK:NXR, :], in_=ztb[:]), ["ztb"], ["hx2d"])

        def select(nm, Cth, thr_ap, Cm, mask_ap, k, r0, cap, st0, psSl):
            kk = lambda x: (nm, x)
            athr = sb(f"athr{nm}", [128, Cth, NE])
            am = athr if mask_ap is None else sb(f"am{nm}", [128, Cm, NE])
            kam = kk("athr") if mask_ap is None else kk("am")
            lo = sb(f"lo{nm}", [128, NE])
            th = sb(f"th{nm}", [128, NE, 15])
            cmpb = sb(f"cmp{nm}", [128, Cth, NE, 15], BF16)
            cntp = sb(f"cntp{nm}", [128, NE, 15])
            ge = sb(f"ge{nm}", [128, NE, 15])
            nge = sb(f"nge{nm}", [128, NE])
            mask = sb(f"mask{nm}", [128, Cm, NE])
            maskb = sb(f"maskb{nm}", [128, Cm, NE], BF16)
            cA = sb(f"cA{nm}", [128, Cm, NE])
            cB = sb(f"cB{nm}", [128, Cm, NE])
            pos = sb(f"pos{nm}", [128, Cm, NE])
            vals = sb(f"vals{nm}", [128, Cm, NE, 3])
            oh = [sb(f"oh{nm}{i}", [128, cap]) for i in range(2)]
            bufs = {"cA": cA, "cB": cB}
            P.dma(lambda e: e.dma_start(out=athr[:], in_=thr_ap.rearrange("(c p) e -> p c e", p=128)), w=[kk("athr")])
            if mask_ap is not None:
                P.dma(lambda e: e.dma_start(out=am[:], in_=mask_ap.rearrange("(c p) e -> p c e", p=128)), w=[kk("am")])
            P.v(lambda e: e.memset(lo[:], 0.0), [], [kk("lo")])
            affb = athr[:].unsqueeze(3).to_broadcast([128, Cth, NE, 15])
            for it in range(8):
                step = 16.0 ** -(it + 1)
                P.v(lambda e, step=step: e.scalar_tensor_tensor(out=th[:], in0=jt[:], scalar=step, in1=lo[:].unsqueeze(2).to_broadcast([128, NE, 15]),
                                                                op0=ALU.mult, op1=ALU.add), ["jt", kk("lo")], [kk("th")])
                P.v(lambda e: e.tensor_tensor(out=cmpb[:], in0=affb, in1=th[:].unsqueeze(1).to_broadcast([128, Cth, NE, 15]), op=ALU.is_ge),
                    [kk("athr"), kk("th")], [kk("cmp")])
                P.v(lambda e: e.tensor_reduce(out=cntp[:], in_=cmpb[:].rearrange("p c e j -> p e j c"), axis=AX.X, op=ALU.add), [kk("cmp")], [kk("cntp")])
                P.t(lambda e: e.matmul(psC[:, 0:NE * 15], lhsT=ones, rhs=cntp[:].rearrange("p e j -> p (e j)"), start=True, stop=True),
                    ["cst", kk("cntp")], ["psC"])
                P.v(lambda e: e.tensor_scalar(out=ge[:].rearrange("p e j -> p (e j)"), in0=psC[:, 0:NE * 15], scalar1=float(k) - 0.5, scalar2=None,
                                              op0=ALU.is_ge), ["psC"], [kk("ge")])
                P.v(lambda e: e.tensor_reduce(out=nge[:], in_=ge[:], axis=AX.X, op=ALU.add), [kk("ge")], [kk("nge")])
                P.v(lambda e, step=step: e.scalar_tensor_tensor(out=lo[:], in0=nge[:], scalar=step, in1=lo[:], op0=ALU.mult, op1=ALU.add),
                    [kk("nge"), kk("lo")], [kk("lo")])
            P.v(lambda e: e.tensor_tensor(out=mask[:], in0=am[:], in1=lo[:].unsqueeze(1).to_broadcast([128, Cm, NE]), op=ALU.is_ge),
                [kam, kk("lo")], [kk("mask")])
            P.v(lambda e: e.tensor_copy(out=maskb[:], in_=mask[:]), [kk("mask")], [kk("maskb")])
            mb2 = maskb[:].rearrange("p c e -> p (c e)")
            ncol = Cm * NE
            P.t(lambda e: e.matmul(psP[0][:, 0:ncol], lhsT=trib[:], rhs=mb2, start=True, stop=True), ["trib", kk("maskb")], [("psP", 0)])
            P.t(lambda e: e.matmul(psP[1][:, 0:ncol], lhsT=onesb[:], rhs=mb2, start=True, stop=True), ["onesb", kk("maskb")], [("psP", 1)])
            P.v(lambda e: e.tensor_copy(out=cA[:].rearrange("p c e -> p (c e)"), in_=psP[1][:, 0:ncol]), [("psP", 1)], [kk("cA")])
            cur, nxt = "cA", "cB"
            sh = 1
            while sh < Cm:
                P.v(lambda e, cur=cur, nxt=nxt, sh=sh: e.tensor_copy(out=bufs[nxt][:, 0:sh, :], in_=bufs[cur][:, 0:sh, :]), [kk(cur)], [kk(nxt)])
                P.v(lambda e, cur=cur, nxt=nxt, sh=sh: e.tensor_tensor(out=bufs[nxt][:, sh:Cm, :], in0=bufs[cur][:, sh:Cm, :], in1=bufs[cur][:, 0:Cm - sh, :],
                                                                  op=ALU.add), [kk(cur)], [kk(nxt)])
                cur, nxt = nxt, cur
                sh *= 2
            pos2 = pos[:].rearrange("p c e -> p (c e)")
            P.v(lambda e, cur=cur: e.tensor_tensor(out=pos2, in0=bufs[cur][:].rearrange("p c e -> p (c e)"), in1=psP[1][:, 0:ncol], op=ALU.subtract),
                [kk(cur), ("psP", 1)], [kk("pos")])
            P.v(lambda e: e.tensor_tensor(out=pos2, in0=pos2, in1=psP[0][:, 0:ncol], op=ALU.add), [kk("pos"), ("psP", 0)], [kk("pos")])
            P.v(lambda e: e.tensor_tensor(out=pos[:], in0=pos[:], in1=mask[:], op=ALU.mult), [kk("pos"), kk("mask")], [kk("pos")])
            P.v(lambda e: e.tensor_scalar(out=pos[:], in0=pos[:], scalar1=-1.0, scalar2=None, op0=ALU.add), [kk("pos")], [kk("pos")])
            P.v(lambda e: e.tensor_scalar(out=vals[:, :, :, 0], in0=tokid[:, 0:Cm].unsqueeze(2).to_broadcast([128, Cm, NE]), scalar1=float(r0), scalar2=None,
                                          op0=ALU.add), ["cst"], [kk("vals")])
            P.v(lambda e: e.tensor_copy(out=vals[:, :, :, 1], in_=am[:]), [kam], [kk("vals")])
            P.v(lambda e: e.memset(vals[:, :, :, 2], 1.0), [], [kk("vals")])
            nst = (cap + 127) // 128
            sw = min(cap, 128)
            n_oh = 0
            first = True
            for el in range(NE):
                for c in range(Cm):
                    ohb = oh[n_oh % 2]
                    kb = (nm, "oh", n_oh % 2)
                    n_oh += 1
                    P.v(lambda e, ohb=ohb, c=c, el=el: e.tensor_scalar(out=ohb[:], in0=iota[:, 0:cap], scalar1=pos[:, c, el:el + 1], scalar2=None,
                                                                      op0=ALU.is_equal), ["cst", kk("pos")], [kb])
                    for sti in range(nst):
                        col = (el * 4 + st0 + sti) * 3
                        P.t(lambda e, ohb=ohb, c=c, el=el, sti=sti, col=col, first=first: e.matmul(
                            psSl[0:sw, col:col + 3], lhsT=ohb[:, sti * 128:sti * 128 + sw], rhs=vals[:, c, el, :],
                            start=first, stop=(c == Cm - 1)), [kb, kk("vals")], [kk("psSl")])
                        first = False
            for sti in range(nst):
                P.v(lambda e, sti=sti: e.tensor_copy(out=sl[0:sw, :, st0 + sti, :],
                                                     in_=psSl[0:sw, 0:NE * 12].rearrange("p (e s t) -> p e s t", e=NE, s=4)[:, :, st0 + sti, :]),
                    [kk("psSl")], ["sl"])

        select("L", 32, affAll, 16, affIn, 512, 256, CAPL, 0, psS[0])
        if with_ctx:
            select("C", 2, affC, 2, None, 32, 0, 32, 3, psS[1])
        P.v(lambda e: e.tensor_scalar(out=idxf[:], in0=sl[:, :, :, 2], scalar1=-1.0, scalar2=dummy, op0=ALU.add, op1=ALU.mult), ["sl", "cst"], ["idxf"])
        P.v(lambda e: e.tensor_tensor(out=idxf[:], in0=sl[:, :, :, 0], in1=idxf[:], op=ALU.subtract), ["sl", "idxf"], ["idxf"])
        P.v(lambda e: e.tensor_copy(out=idx[:], in_=idxf[:]), ["idxf"], ["idx"])
        P.v(lambda e: e.tensor_copy(out=gate[:], in_=sl[:, :, :, 1]), ["sl"], ["gate"])
        P.dma(lambda e: e.dma_start(out=idxT, in_=idx[:]), ["idx"], ["idxT"])
        P.dma(lambda e: e.dma_start(out=gateT, in_=gate[:]), ["gate"], ["gateT"])
        P.emit(fused=True)


def stage_s3b(nc, io, L, with_ctx, pfx):
    idxT, gateT, delta, hx2, consts = io["idxT"], io["gateT"], io["delta"], io["hx2o"], io["consts"]
    wg, wu, wd = io["w_gate"], io["w_up"], io["w_down"]
    with stage_ctx(nc, True) as st:
        sb = lambda n, s, d=F32: st.enter_context(nc.sbuf_tensor(pfx + n, s, d))
        pt = lambda n, s, d=F32: st.enter_context(nc.psum_tensor(pfx + n, s, d))
        ncols = CAPL + (32 if with_ctx else 0)
        cst = sb("cst", [128, 128])
        identb = sb("identb", [128, 128], BF16)
        idx = sb("idx", [128, NE, 4], I32)
        gate = sb("gate", [128, NE, 4])
        NR, NSTG, PFD = 5, 6, 3
        ring = [sb(f"ring{i}", [128, 16, 512], BF16) for i in range(NR)]
        stg = [sb(f"stg{i}", [128, 4, 512]) for i in range(NSTG)]
        xg = [sb(f"xg{i}", [128, D], BF16) for i in range(2)]
        xgT = sb("xgT", [128, 16, ncols], BF16)
        hT = sb("hT", [128, 16, ncols], BF16)
        sil = sb("sil", [128, ncols])
        ybig = [sb(f"ybig{i}", [128, D]) for i in range(4)]
        psA = [pt(f"psA{i}", [128, 512]) for i in range(2)]
        psU = [pt(f"psU{i}", [128, 512]) for i in range(2)]
        psT = pt("psT", [128, 16, 128], BF16)
        psY = [pt(f"psY{i}", [128, 512]) for i in range(2)]
        P = Prog(nc)
        P.dma(lambda e: e.dma_start(out=cst[:], in_=consts[:, 0:128]), w=["cst"])
        P.v(lambda e: e.tensor_copy(out=identb[:], in_=cst[:]), ["cst"], ["identb"])
        P.dma(lambda e: e.dma_start(out=idx[:], in_=idxT), w=["idx"])
        P.dma(lambda e: e.dma_start(out=gate[:], in_=gateT), w=["gate"])
        tiles = [(sti, 128, sti * 128) for sti in range(3)]
        if with_ctx:
            tiles.append((3, 32, CAPL))
        ny = [0]
        seq = []
        for el_ in range(NE):
            for cb_ in range(4):
                seq.append((wg, el_, cb_))
                seq.append((wu, el_, cb_))
            for cb_ in range(4):
                seq.append((wd, el_, cb_))
        issued = [0]
        nq = [0]
        cast_eng = ["scalar", "vector", "scalar", "vector"]

        def issue(i):
            wsrc, el_, cb_ = seq[i]
            rb = i % NR
            for k4 in range(4):
                si = nq[0] % NSTG
                ce = cast_eng[nq[0] % 4]
                nq[0] += 1
                src = wsrc[L, el_, k4 * 512:(k4 + 1) * 512, cb_ * 512:(cb_ + 1) * 512].rearrange("(kc p) n -> p kc n", p=128)
                P.dma(lambda e, si=si, src=src: e.dma_start(out=stg[si][:], in_=src), w=[("stg", si)])
                dst = ring[rb][:, k4 * 4:(k4 + 1) * 4, :]
                if ce == "scalar":
                    P.op("scalar", lambda e, si=si, dst=dst: e.activation(out=dst, in_=stg[si][:], func=AF.Copy), [("stg", si)], [("ring", rb, k4)])
                else:
                    P.op(ce, lambda e, si=si, dst=dst: e.tensor_copy(out=dst, in_=stg[si][:]), [("stg", si)], [("ring", rb, k4)])

        def need(i):
            while issued[0] < min(len(seq), i + PFD + 1):
                issue(issued[0])
                issued[0] += 1
            return i % NR

        un = [0]

        def load_unit(wsrc, el, cb):
            i = un[0]
            un[0] += 1
            assert seq[i][1] == el and seq[i][2] == cb and seq[i][0] is wsrc
            return need(i)

        for el in range(NE):
            for ti, (sti, n, c0) in enumerate(tiles):
                xb = xg[ti % 2]
                kx = ("xg", ti % 2)
                P.op("gpsimd", lambda e, xb=xb, el=el, sti=sti, n=n: e.indirect_dma_start(
                    out=xb[0:n, :], out_offset=None, in_=hx2[:, :],
                    in_offset=bass.IndirectOffsetOnAxis(ap=idx[0:n, el, sti:sti + 1], axis=0)), ["idx"], [kx], dma=True)
                for kc in range(16):
                    P.t(lambda e, xb=xb, kc=kc, n=n: e.transpose(psT[:, kc, 0:n], xb[0:n, kc * 128:(kc + 1) * 128], identb[0:n, 0:n]), [kx, "identb"], ["psT"])
                P.a(lambda e, c0=c0, n=n: e.activation(out=xgT[:, :, c0:c0 + n], in_=psT[:, :, 0:n], func=AF.Copy), ["psT"], ["xgT"])
            for cb in range(4):
                rg = load_unit(wg, el, cb)
                ru = load_unit(wu, el, cb)
                for f4 in range(4):
                    fc = cb * 4 + f4
                    pa = psA[fc % 2]
                    pu = psU[fc % 2]
                    for (pp, rbuf, key) in ((pa, rg, "psA"), (pu, ru, "psU")):
                        for kc in range(16):
                            P.t(lambda e, pp=pp, rbuf=rbuf, kc=kc, f4=f4: e.matmul(pp[:, 0:ncols], lhsT=ring[rbuf][:, kc, f4 * 128:(f4 + 1) * 128],
                                                                                  rhs=xgT[:, kc, :], start=(kc == 0), stop=(kc == 15)),
                                [("ring", rbuf, kc // 4), "xgT"], [(key, fc % 2)])
                    P.a(lambda e, pa=pa: e.activation(out=sil[:], in_=pa[:, 0:ncols], func=AF.Silu), [("psA", fc % 2)], ["sil"])
                    P.v(lambda e, pu=pu, fc=fc: e.tensor_tensor(out=hT[:, fc, :], in0=sil[:], in1=pu[:, 0:ncols], op=ALU.mult),
                        ["sil", ("psU", fc % 2)], ["hT"])
            for cb in range(4):
                rd = load_unit(wd, el, cb)
                for ti, (sti, n, c0) in enumerate(tiles):
                    py = psY[ny[0] % 2]
                    ky = ("psY", ny[0] % 2)
                    ny[0] += 1
                    for fc in range(16):
                        P.t(lambda e, py=py, rd=rd, fc=fc, c0=c0, n=n: e.matmul(py[0:n, :], lhsT=hT[:, fc, c0:c0 + n], rhs=ring[rd][:, fc, :],
                                                                               start=(fc == 0), stop=(fc == 15)), ["hT", ("ring", rd, fc // 4)], [ky])
                    P.v(lambda e, py=py, ti=ti, el=el, sti=sti, n=n, cb=cb: e.tensor_scalar(out=ybig[ti][0:n, cb * 512:(cb + 1) * 512], in0=py[0:n, :],
                                                                                           scalar1=gate[0:n, el, sti:sti + 1], scalar2=None, op0=ALU.mult),
                        [ky, "gate"], [("ybig", ti)])
            for ti, (sti, n, c0) in enumerate(tiles):
                P.op("gpsimd", lambda e, ti=ti, el=el, sti=sti, n=n: e.indirect_dma_start(
                    out=delta[:, :], out_offset=bass.IndirectOffsetOnAxis(ap=idx[0:n, el, sti:sti + 1], axis=0),
                    in_=ybig[ti][0:n, :], in_offset=None, compute_op=ALU.add), [("ybig", ti), "idx", "delta"], ["delta"], dma=True)
        P.emit(fused=True)


def stage_copy_out(nc, src, dst, pfx):
    with stage_ctx(nc, True) as st:
        bufs = [st.enter_context(nc.sbuf_tensor(pfx + f"cb{i}", [128, D], F32)) for i in range(3)]
        P = Prog(nc)
        for t in range(16):
            b = bufs[t % 3]
            P.dma(lambda e, b=b, t=t: e.dma_start(out=b[:], in_=src[256 + t * 128:256 + (t + 1) * 128, :]), w=[("cb", t % 3)])
            P.dma(lambda e, b=b, t=t: e.dma_start(out=dst[t * 128:(t + 1) * 128, :], in_=b[:]), [("cb", t % 3)], [("out", t)])
        P.emit(fused=True)


def build_fused(ncores=8):
    global PAIRS
    PAIRS = [[2 * i, 2 * i + 1] for i in range(ncores // 2)]
    nc = bass.Bass("TRN2", target_bir_lowering=False)
    ein = lambda n, s, d=F32: nc.dram_tensor(n, s, d, kind="ExternalInput").ap()
    scr = lambda n, s, d=F32: nc.dram_tensor(n, s, d, kind="Internal").ap()
    scrl = lambda n, s, d=F32: nc.dram_tensor(n, s, d, kind="Internal", addr_space="Local").ap()
    X = {}
    X["xin0"] = ein("xin", [NTOK, D])
    X["cin"] = ein("cin", [128, 16, 2])
    X["w_mod"] = ein("w_mod", [2, D, 6 * D])
    X["b_mod"] = ein("b_mod", [2, 6 * D])
    n1g = ein("n1g", [2, D]); n2g = ein("n2g", [2, D])
    win = ein("win", [2, D, 3584])
    gqk = ein("gqk", [2, NQK * 128])
    X["cs_t"] = ein("cs_t", [NTOK, 128])
    vnb = ein("vnb", [2, 512]); wsT = ein("wsT", [2, 128, 4, 128]); bsT = ein("bsT", [2, 128, 4]); ogb = ein("ogb", [2, 512])
    X["ident"] = ein("ident", [128, 128])
    X["msk"] = ein("msk", [4, 128, 128])
    sink = ein("sink", [2, 6]); gac = ein("gac", [2, 1536])
    wout = ein("wout", [2, D, D]); wr = ein("wr", [2, D, NE])
    X["w_gate"] = ein("w_gate", [2, NE, D, D]); X["w_up"] = ein("w_up", [2, NE, D, D]); X["w_down"] = ein("w_down", [2, NE, D, D])
    X["consts"] = ein("consts", [128, NCST])
    out = nc.dram_tensor("out", [2048, D], F32, kind="ExternalOutput").ap()
    modS = scr("modS", [2, 2, 6 * D])
    X["modS"] = modS
    X["QKT"] = scr("QKT", [128, NQK, NTOK], BF16); X["Vout"] = scr("Vout", [NTOK, 512], BF16)
    X["yB"] = scr("yB", [NTOK, 512], BF16); X["yAC"] = scr("yAC", [NTOK, 1536], BF16)
    X["KTx"] = scr("KTx", [512, 2048], BF16); X["KTg"] = scrl("KTg", [1024, 2048], BF16)
    X["Vx"] = scr("Vx", [2048, 512], BF16); X["Vg"] = scrl("Vg", [4096, 512], BF16)
    xs = scr("xs", [NXR, D]); X["hx2o"] = scr("hx2s", [NXR, D], BF16); X["delta"] = scr("delta", [NXR, D])
    X["affIn"] = scr("affIn", [2048, NE]); X["affC"] = scr("affC", [256, NE]); X["affAll"] = scrl("affAll", [4096, NE])
    X["idxT"] = scr("idxT", [128, NE, 4], I32); X["gateT"] = scr("gateT", [128, NE, 4])

    stage_mod(nc, X, "m_")
    for L in range(2):
        io = dict(X)
        io["xin"] = X["xin0"] if L == 0 else xs
        io["modx"] = modS[L, 0].rearrange("(s d) -> s d", s=6)
        io["modc"] = modS[L, 1].rearrange("(s d) -> s d", s=6)
        io.update(n1g=n1g[L], n2g=n2g[L], win=win[L], gqk=gqk[L], vnb=vnb[L], wsT=wsT[L], bsT=bsT[L], ogb=ogb[L],
                  sink=sink[L], gac=gac[L], wout=wout[L], wr=wr[L], x1o=xs, x1=xs, dA=X["delta"], x2=xs)
        build_s1(nc=nc, io=io, pfx=f"L{L}a_")
        stage_cc(nc, [(X["KTx"], X["KTg"]), (X["Vx"], X["Vg"])])
        build_s2a(nc=nc, io=io, pfx=f"L{L}b_")
        build_s2b(nc=nc, io=io, pfx=f"L{L}c_")
        stage_cc(nc, [(X["affIn"], X["affAll"])])
        stage_s3a(nc, io, L == 0, f"L{L}d_")
        stage_s3b(nc, io, L, L == 0, f"L{L}e_")
        build_s4(nc=nc, io=io, pfx=f"L{L}f_")
    stage_copy_out(nc, xs, out, "o_")
    return nc


def kernel(x, c, ctx, c_ctx, w_mod, b_mod, norm1_g, norm2_g, w_in, qn_a, kn_a, sink_a, vn_b,
           w_s, b_s, qn_c, kn_c, out_g, w_out, w_router, w_gate, w_up, w_down):
    f32 = np.float32
    A = lambda a: np.ascontiguousarray(np.asarray(a, f32))
    x = A(x); ctx = A(ctx); c = A(c); c_ctx = A(c_ctx)
    cores = [(k // 2, k % 2) for k in range(8)]
    og = A(out_g)
    shared = {
        "w_mod": A(w_mod), "b_mod": A(b_mod), "n1g": A(norm1_g), "n2g": A(norm2_g),
        "win": A(np.asarray(w_in, f32)[:, :, WIN_PERM]),
        "gqk": A(np.stack([np.concatenate([np.tile(qn_a[L], 6), np.tile(kn_a[L], 2), np.tile(qn_c[L], 6), np.tile(kn_c[L], 2)]) for L in range(2)])),
        "vnb": A(vn_b), "wsT": A(np.asarray(w_s, f32).transpose(0, 3, 1, 2)), "bsT": A(np.asarray(b_s, f32).transpose(0, 2, 1)),
        "ogb": A(og[:, 768:1280]), "ident": np.eye(128, dtype=f32), "sink": A(sink_a),
        "gac": A(np.concatenate([og[:, :768], og[:, 1280:]], 1)), "wout": A(w_out), "wr": A(w_router),
        "w_gate": A(w_gate), "w_up": A(w_up), "w_down": A(w_down), "consts": f_consts(),
    }
    ims = []
    for (b, h) in cores:
        d = dict(shared)
        d["xin"] = A(np.concatenate([ctx[b], x[b, h * 2048:(h + 1) * 2048]], 0))
        c2 = np.stack([c[b], c_ctx], 0)
        d["cin"] = A(c2.T.reshape(16, 128, 2).transpose(1, 0, 2))
        d["cs_t"] = _rope_tables(h)
        d["msk"] = _masks(h)
        ims.append(d)
    ncores = _NCORES[0]
    nc = build_fused(ncores)
    res = run_bass_kernel_spmd(nc, ims[:ncores], core_ids=list(range(ncores)))
    out = np.zeros((4, SEQ, D), f32)
    for k, (b, h) in enumerate(cores[:ncores]):
        out[b, h * 2048:(h + 1) * 2048] = res.results[k]["out"]
    return out


_NCORES = [8]
```

```python
import contextlib
import numpy as np
import ml_dtypes
import concourse.bass as bass
import concourse.mybir as mybir
from concourse.bass_utils import run_bass_kernel_spmd

F32 = mybir.dt.float32
BF16 = mybir.dt.bfloat16
I32 = mybir.dt.int32
AF = mybir.ActivationFunctionType
ALU = mybir.AluOpType
AX = mybir.AxisListType

D = 2048
SEQ = 4096
CTX = 256
NT = 18
NTOK = NT * 128
EPS = 1e-6
NE = 16

ENGS = ("sync", "scalar", "vector", "gpsimd", "tensor")
SEM_CHUNK = 3000
NDMA_SEMS = 12


class _Op:
    __slots__ = ("eng", "fn", "deps", "is_dma", "has_dep", "sem", "val", "prev_dma")

    def __init__(self, eng, fn, is_dma):
        self.eng = eng
        self.fn = fn
        self.is_dma = is_dma
        self.deps = []
        self.has_dep = False
        self.sem = None
        self.val = None
        self.prev_dma = None


class Prog:
    def __init__(self, nc):
        self.nc = nc
        self.ops = {e: [] for e in ENGS}
        self.last_w = {}
        self.readers = {}
        self.ndma = {e: 0 for e in ENGS}
        self.dma_last = {}
        self.all_dmas = []

    def op(self, eng, fn, reads=(), writes=(), dma=False):
        o = _Op(eng, fn, dma)
        deps = []
        for r in reads:
            w = self.last_w.get(r)
            if w is not None:
                deps.append((w, "raw"))
        for k in writes:
            w = self.last_w.get(k)
            if w is not None:
                deps.append((w, "waw"))
            for rd in self.readers.get(k, ()):
                deps.append((rd, "war"))
        for (d, kind) in deps:
            if d is o:
                continue
            if not d.is_dma and not dma and d.eng == eng:
                if kind != "raw" or eng == "tensor":
                    continue
            if d not in o.deps:
                o.deps.append(d)
                d.has_dep = True
        for r in reads:
            self.readers.setdefault(r, []).append(o)
        for k in writes:
            self.last_w[k] = o
            self.readers[k] = []
        if dma:
            j = self.ndma[eng]
            self.ndma[eng] += 1
            slot = (eng, j % NDMA_SEMS)
            o.prev_dma = self.dma_last.get(slot)
            self.dma_last[slot] = o
            o.sem = slot
            o.val = 16 * (j // NDMA_SEMS + 1)
            self.all_dmas.append(o)
        self.ops[eng].append(o)
        return o

    def v(self, fn, r=(), w=()):
        return self.op("vector", fn, r, w)

    def a(self, fn, r=(), w=()):
        return self.op("scalar", fn, r, w)

    def g(self, fn, r=(), w=()):
        return self.op("gpsimd", fn, r, w)

    def t(self, fn, r=(), w=()):
        return self.op("tensor", fn, r, w)

    def dma(self, fn, r=(), w=(), q="sync"):
        return self.op(q, fn, r, w, dma=True)

    def emit(self, final_wait_eng="sync", fused=False):
        nc = self.nc
        sem_names = set()
        for e in ENGS:
            cnt = 0
            for o in self.ops[e]:
                if o.is_dma:
                    sem_names.add(o.sem)
                    continue
                if o.has_dep:
                    o.sem = (e, "c", cnt // SEM_CHUNK)
                    o.val = cnt % SEM_CHUNK + 1
                    sem_names.add(o.sem)
                    cnt += 1
        sem_names = sorted(sem_names, key=str)
        with contextlib.ExitStack() as st:
            sems = {}
            for n in sem_names:
                if fused:
                    _UID[0] += 1
                    sems[n] = nc.alloc_semaphore(name="f%d_" % _UID[0] + "_".join(str(x) for x in n))
                else:
                    sems[n] = st.enter_context(nc.semaphore("s_" + "_".join(str(x) for x in n)))
            block = st.enter_context(nc.Block())
            prog = self

            def run(e, eng):
                seen = {}
                for o in prog.ops[e]:
                    waits = []
                    if o.is_dma and o.prev_dma is not None:
                        waits.append(o.prev_dma)
                    waits.extend(o.deps)
                    for d in waits:
                        if seen.get(d.sem, 0) >= d.val:
                            continue
                        eng.wait_ge(sems[d.sem], d.val)
                        seen[d.sem] = d.val
                    ins = o.fn(eng)
                    if o.is_dma:
                        ins.then_inc(sems[o.sem], 16)
                    elif o.has_dep:
                        ins.then_inc(sems[o.sem], 1)
                if e == final_wait_eng:
                    last = {}
                    for o in prog.all_dmas:
                        last[o.sem] = max(last.get(o.sem, 0), o.val)
                    for s, v in last.items():
                        if seen.get(s, 0) < v:
                            eng.wait_ge(sems[s], v)

            @block.sync
            def _(eng):
                run("sync", eng)

            @block.scalar
            def _(eng):
                run("scalar", eng)

            @block.vector
            def _(eng):
                run("vector", eng)

            @block.gpsimd
            def _(eng):
                run("gpsimd", eng)

            @block.tensor
            def _(eng):
                run("tensor", eng)


_UID = [0]


@contextlib.contextmanager
def stage_ctx(nc, fused):
    if not fused:
        with contextlib.ExitStack() as st:
            yield st
    else:
        with nc.cleanup_on_exit():
            with contextlib.ExitStack() as st:
                yield st
            nc.all_engine_barrier()


def _mk(nc, io, pfx):
    fused = io is not None
    if fused:
        di = lambda n, s, d=F32: io[n]
        do = lambda n, s, d=F32: io[n]
    else:
        di = lambda n, s, d=F32: nc.dram_tensor(n, s, d, kind="ExternalInput").ap()
        do = lambda n, s, d=F32: nc.dram_tensor(n, s, d, kind="ExternalOutput").ap()
    return fused, di, do


def _bc(ap1d, n):
    return ap1d.unsqueeze(0).to_broadcast([128, n])


MCOL = 1536


def build_mod():
    nc = bass.Bass("TRN2", target_bir_lowering=False)
    cin = nc.dram_tensor("cin", [128, 16, 5], F32, kind="ExternalInput").ap()
    w = nc.dram_tensor("w", [2, D, MCOL], F32, kind="ExternalInput").ap()
    b = nc.dram_tensor("b", [2, MCOL], F32, kind="ExternalInput").ap()
    out = nc.dram_tensor("out", [2, 5, MCOL], F32, kind="ExternalOutput").ap()
    with contextlib.ExitStack() as st:
        sb = lambda n, s, d: st.enter_context(nc.sbuf_tensor(n, s, d))
        ct = sb("ct", [128, 16, 5], F32)
        cs = sb("cs", [128, 16, 5], F32)
        wt = [sb(f"wt{i}", [128, 16, 512], F32) for i in range(2)]
        bt = sb("bt", [5, 2, MCOL], F32)
        ot = sb("ot", [5, 2, MCOL], F32)
        ps = [st.enter_context(nc.psum_tensor(f"ps{i}", [5, 512], F32)) for i in range(2)]
        P = Prog(nc)
        P.dma(lambda e: e.dma_start(out=ct[:], in_=cin), w=["ct"])
        for l in range(2):
            P.dma(lambda e, l=l: e.dma_start(out=bt[:, l, :], in_=b[l:l + 1, :].to_broadcast([5, MCOL])), w=[("bt", l)])
        P.a(lambda e: e.activation(out=cs[:], in_=ct[:], func=AF.Silu), ["ct"], ["cs"])
        gi = 0
        for l in range(2):
            for g in range(3):
                buf = gi % 2
                src = w[l, :, g * 512:(g + 1) * 512].rearrange("(kc p) n -> p kc n", p=128)
                P.dma(lambda e, buf=buf, src=src: e.dma_start(out=wt[buf][:], in_=src), w=[("wt", buf)])
                for kc in range(16):
                    P.t(lambda e, buf=buf, kc=kc: e.matmul(ps[buf][:], lhsT=cs[:, kc, :], rhs=wt[buf][:, kc, :],
                                                           start=(kc == 0), stop=(kc == 15)),
                        ["cs", ("wt", buf)], [("ps", buf)])
                P.v(lambda e, buf=buf, l=l, g=g: e.tensor_tensor(out=ot[:, l, g * 512:(g + 1) * 512], in0=ps[buf][:],
                                                                 in1=bt[:, l, g * 512:(g + 1) * 512], op=ALU.add),
                    [("ps", buf), ("bt", l)], [("ot", l, g)])
                gi += 1
        for l in range(2):
            P.dma(lambda e, l=l: e.dma_start(out=out[l], in_=ot[:, l, :]), [("ot", l, g) for g in range(3)], [("out", l)])
        P.emit()
    return nc


def run_mod(c, c_ctx, w_mod, b_mod):
    c_all = np.concatenate([c, c_ctx[None, :]], axis=0).astype(np.float32)
    cin = np.ascontiguousarray(c_all.T.reshape(16, 128, 5).transpose(1, 0, 2))
    nc = build_mod()
    in_maps = [{"cin": cin, "w": np.ascontiguousarray(w_mod[:, :, j * MCOL:(j + 1) * MCOL]),
                "b": np.ascontiguousarray(b_mod[:, j * MCOL:(j + 1) * MCOL])} for j in range(8)]
    res = run_bass_kernel_spmd(nc, in_maps, core_ids=list(range(8)))
    return np.concatenate([r["out"] for r in res.results], axis=2)


def emit_rstd(P, ss, rstd, n, kr, kw):
    P.a(lambda e: e.activation(out=rstd, in_=ss, func=AF.Sqrt, scale=1.0 / n, bias=EPS), kr, kw)
    P.v(lambda e: e.reciprocal(out=rstd, in_=rstd), kw, kw)


NQK = 16
WIN_PERM = np.concatenate([np.arange(0, 1024), np.arange(2304, 3328),
                           np.arange(1024, 1280), np.arange(3328, 3584),
                           np.arange(1280, 2304)])


def build_s1(nc=None, io=None, pfx=""):
    if nc is None:
        nc = bass.Bass("TRN2", target_bir_lowering=False)
    fused, di, do = _mk(nc, io, pfx)
    xin = di("xin", [NTOK, D])
    modx = di("modx", [6, D])
    modc = di("modc", [6, D])
    n1g = di("n1g", [D])
    win = di("win", [D, 3584])
    gqk = di("gqk", [NQK * 128])
    cs_t = di("cs_t", [NTOK, 128])
    vnb = di("vnb", [512])
    wsT = di("wsT", [128, 4, 128])
    bsT = di("bsT", [128, 4])
    ogb = di("ogb", [512])
    ident_in = di("ident", [128, 128])
    QKT = do("QKT", [128, NQK, NTOK], BF16)
    Vout = do("Vout", [NTOK, 512], BF16)
    yB = do("yB", [NTOK, 512], BF16)

    with stage_ctx(nc, fused) as st:
        sb = lambda n, s, d=F32: st.enter_context(nc.sbuf_tensor(pfx + n, s, d))
        pt = lambda n, s, d=F32: st.enter_context(nc.psum_tensor(pfx + n, s, d))
        wbf = sb("wbf", [128, 16, 2048], BF16)
        G1 = sb("G1", [128, D])
        SH = sb("SH", [128, D])
        gn = sb("gn", [128, D])
        xt = [sb(f"xt{i}", [128, D]) for i in range(2)]
        tmp = sb("tmp", [128, D])
        hx = sb("hx", [128, D], BF16)
        hxT = sb("hxT", [128, 16, 128], BF16)
        hxb = [hx, sb("hx1", [128, D], BF16)]
        hxTb = [hxT, sb("hxT1", [128, 16, 128], BF16)]
        tmph = sb("tmph", [128, D])
        ssh = sb("ssh", [128, 1])
        rsh = sb("rsh", [128, 1])
        ident = sb("ident_f", [128, 128])
        identb = sb("identb", [128, 128], BF16)
        ss = sb("ss", [128, 1])
        rs = sb("rs", [128, 1])
        Pb = sb("Pb", [128, 2048])
        gq = sb("gq", [128, NQK, 128])
        ssq = sb("ssq", [128, NQK])
        rsq = sb("rsq", [128, NQK])
        qn = sb("qn", [128, NQK, 2, 2, 32])
        cst = sb("cst", [128, 128])
        rA = sb("rA", [128, NQK, 2, 32])
        rB = sb("rB", [128, NQK, 2, 32])
        qr = sb("qr", [128, NQK, 2, 2, 32], BF16)
        qrb = [qr, sb("qr1", [128, NQK, 2, 2, 32], BF16)]
        qT = sb("qT", [128, NQK, 128], BF16)
        vb16 = sb("vb16", [128, 512], BF16)
        vng = sb("vng", [128, 512])
        wsb = sb("wsb", [128, 4, 128])
        wsbb = sb("wsbb", [128, 4, 128], BF16)
        bsb = sb("bsb", [128, 4])
        ogbt = sb("ogbt", [128, 512])
        t1 = sb("t1", [128, 1024])
        t2 = sb("t2", [128, 1024])
        gl = sb("gl", [128, 1024])
        ssv = sb("ssv", [128, 4])
        rsv = sb("rsv", [128, 4])
        vn = sb("vn", [128, 4, 128], BF16)
        ob = sb("ob", [128, 512])
        yb = sb("yb", [128, 512], BF16)
        pT = pt("pT", [128, 16, 128], BF16)
        pP = pt("pP", [128, 2048])
        pQ = pt("pQ", [128, NQK, 128], BF16)

        P = Prog(nc)
        P.dma(lambda e: e.dma_start(out=ident[:], in_=ident_in), w=["ident"])
        P.v(lambda e: e.tensor_copy(out=identb[:], in_=ident[:]), ["ident"], ["identb"])
        P.dma(lambda e: e.dma_start(out=gn[:], in_=_bc(n1g, D)), w=["gn"])
        P.dma(lambda e: e.dma_start(out=gq[:].rearrange("p h d -> p (h d)"), in_=_bc(gqk, NQK * 128)), w=["gq"])
        P.dma(lambda e: e.dma_start(out=vng[:], in_=_bc(vnb, 512)), w=["vng"])
        P.dma(lambda e: e.dma_start(out=ogbt[:], in_=_bc(ogb, 512)), w=["ogbt"])
        P.dma(lambda e: e.dma_start(out=wsb[:], in_=wsT), w=["wsb"])
        P.v(lambda e: e.tensor_copy(out=wsbb[:], in_=wsb[:]), ["wsb"], ["wsbb"])
        P.dma(lambda e: e.dma_start(out=bsb[:], in_=bsT), w=["bsb"])

        def load_w(c0, ncol):
            for k4 in range(4):
                src = win[k4 * 512:(k4 + 1) * 512, c0:c0 + ncol].rearrange("(kc p) n -> p kc n", p=128)
                P.dma(lambda e, k4=k4, src=src: e.dma_start(out=wbf[:, k4 * 4:(k4 + 1) * 4, 0:ncol], in_=src),
                      w=["wbf"], q="gpsimd")

        def load_mod(m):
            P.dma(lambda e: e.dma_start(out=SH[:], in_=_bc(m[0], D)), w=["SH"])
            P.dma(lambda e: e.dma_start(out=G1[:], in_=_bc(m[1], D)), w=["G1"])
            P.v(lambda e: e.scalar_tensor_tensor(out=G1[:], in0=G1[:], scalar=1.0, in1=gn[:], op0=ALU.add, op1=ALU.mult),
                ["G1", "gn"], ["G1"])

        def hx_tile(t):
            xb = xt[t % 2]
            kx = ("xt", t % 2)
            hb = hxb[t % 2]
            hTb = hxTb[t % 2]
            kh = ("hx", t % 2)
            P.dma(lambda e: e.dma_start(out=xb[:], in_=xin[t * 128:(t + 1) * 128, :]), w=[kx])
            P.a(lambda e: e.activation(out=tmph[:], in_=xb[:], func=AF.Square, accum_out=ssh[:]), [kx], ["tmph", "ssh"])
            emit_rstd(P, ssh[:], rsh[:], D, ["ssh"], ["rsh"])
            P.v(lambda e: e.scalar_tensor_tensor(out=tmph[:], in0=xb[:], scalar=rsh[:, 0:1], in1=G1[:], op0=ALU.mult, op1=ALU.mult),
                [kx, "rsh", "G1"], ["tmph"])
            P.v(lambda e: e.tensor_tensor(out=hb[:], in0=tmph[:], in1=SH[:], op=ALU.add), ["tmph", "SH"], [kh])
            for kc in range(16):
                P.t(lambda e, kc=kc: e.transpose(pT[:, kc, :], hb[:, kc * 128:(kc + 1) * 128], identb[:]), [kh, "identb"], ["pT"])
            P.a(lambda e: e.activation(out=hTb[:], in_=pT[:], func=AF.Copy), ["pT"], [("hxT", t % 2)])

        def prep(t):
            if t == 0:
                load_mod(modc)
            if t == 2:
                load_mod(modx)
            hx_tile(t)

        def inproj(ncol, t):
            hTb = hxTb[t % 2]
            for g in range(ncol // 512):
                for kc in range(16):
                    P.t(lambda e, g=g, kc=kc: e.matmul(pP[:, g * 512:(g + 1) * 512], lhsT=hTb[:, kc, :],
                                                      rhs=wbf[:, kc, g * 512:(g + 1) * 512], start=(kc == 0), stop=(kc == 15)),
                        [("hxT", t % 2), "wbf"], ["pP"])

        def postA(t):
            P.a(lambda e: e.activation(out=Pb[:], in_=pP[:], func=AF.Copy), ["pP"], ["Pb"])
            Pv = Pb[:].rearrange("p (h d) -> p h d", h=NQK)
            P.dma(lambda e, t=t: e.dma_start(out=cst[:], in_=cs_t[t * 128:(t + 1) * 128, :]), w=["cst"])
            P.v(lambda e: e.tensor_tensor(out=tmp[:], in0=Pb[:], in1=Pb[:], op=ALU.mult), ["Pb"], ["tmp"])
            P.v(lambda e: e.tensor_reduce(out=ssq[:], in_=tmp[:].rearrange("p (h d) -> p h d", h=NQK), axis=AX.X, op=ALU.add),
                ["tmp"], ["ssq"])
            emit_rstd(P, ssq[:], rsq[:], 128, ["ssq"], ["rsq"])
            qnv = qn[:].rearrange("p h a b f -> p h (a b f)")
            P.v(lambda e: e.tensor_tensor(out=qnv, in0=Pv, in1=rsq[:].unsqueeze(2).to_broadcast([128, NQK, 128]), op=ALU.mult),
                ["Pb", "rsq"], ["qn"])
            P.v(lambda e: e.tensor_tensor(out=qnv, in0=qnv, in1=gq[:], op=ALU.mult), ["qn", "gq"], ["qn"])
            cosb = cst[:, 0:64].rearrange("p (a f) -> p a f", a=2).unsqueeze(1).to_broadcast([128, NQK, 2, 32])
            sinb = cst[:, 64:128].rearrange("p (a f) -> p a f", a=2).unsqueeze(1).to_broadcast([128, NQK, 2, 32])
            x1 = qn[:, :, :, 0, :]
            x2 = qn[:, :, :, 1, :]
            P.v(lambda e: e.tensor_tensor(out=rA[:], in0=x1, in1=cosb, op=ALU.mult), ["qn", "cst"], ["rA"])
            P.v(lambda e: e.tensor_tensor(out=rB[:], in0=x2, in1=sinb, op=ALU.mult), ["qn", "cst"], ["rB"])
            P.v(lambda e: e.tensor_tensor(out=qrb[t % 2][:, :, :, 0, :], in0=rA[:], in1=rB[:], op=ALU.subtract), ["rA", "rB"], [("qr", t % 2)])
            P.v(lambda e: e.tensor_tensor(out=rA[:], in0=x2, in1=cosb, op=ALU.mult), ["qn", "cst", ("qr", t % 2)], ["rA"])
            P.v(lambda e: e.tensor_tensor(out=rB[:], in0=x1, in1=sinb, op=ALU.mult), ["qn", "cst", ("qr", t % 2)], ["rB"])
            P.v(lambda e: e.tensor_tensor(out=qrb[t % 2][:, :, :, 1, :], in0=rA[:], in1=rB[:], op=ALU.add), ["rA", "rB"], [("qr", t % 2)])

        def storeA(t):
            qrv = qrb[t % 2][:].rearrange("p h a b f -> p h (a b f)")
            for h in range(NQK):
                P.t(lambda e, h=h: e.transpose(pQ[:, h, :], qrv[:, h, :], identb[:]), [("qr", t % 2), "identb"], ["pQ"])
            P.a(lambda e: e.activation(out=qT[:], in_=pQ[:], func=AF.Copy), ["pQ"], ["qT"])
            P.dma(lambda e, t=t: e.dma_start(out=QKT[:, :, t * 128:(t + 1) * 128], in_=qT[:]), ["qT"], [("QKT", t)])
            if fused and t >= 2:
                ktv = io["KTx"].rearrange("(h d) t -> d h t", d=128)
                c0 = (t - 2) * 128
                P.dma(lambda e, c0=c0: e.dma_start(out=ktv[:, 0:2, c0:c0 + 128], in_=qT[:, 6:8, :]), ["qT"], [("KTx", t, 0)])
                P.dma(lambda e, c0=c0: e.dma_start(out=ktv[:, 2:4, c0:c0 + 128], in_=qT[:, 14:16, :]), ["qT"], [("KTx", t, 1)])


        def postB(t):
            P.a(lambda e: e.activation(out=vb16[:], in_=pP[:, 0:512], func=AF.Copy), ["pP"], ["vb16"])
            P.dma(lambda e, t=t: e.dma_start(out=Vout[t * 128:(t + 1) * 128, :], in_=vb16[:]), ["vb16"], [("Vout", t)])
            if fused and t >= 2:
                P.dma(lambda e, t=t: e.dma_start(out=io["Vx"][(t - 2) * 128:(t - 1) * 128, :], in_=vb16[:]), ["vb16"], [("Vx", t)])
            P.a(lambda e: e.activation(out=gl[:], in_=pP[:, 512:1536], func=AF.Gelu_apprx_tanh), ["pP"], ["gl"])
            gv = gl[:, 512:1024]
            P.v(lambda e: e.tensor_tensor(out=t1[:, 0:512], in0=gv, in1=gv, op=ALU.mult), ["gl"], ["t1"])
            P.v(lambda e: e.tensor_reduce(out=ssv[:], in_=t1[:, 0:512].rearrange("p (g d) -> p g d", g=4), axis=AX.X, op=ALU.add),
                ["t1"], ["ssv"])
            emit_rstd(P, ssv[:], rsv[:], 128, ["ssv"], ["rsv"])
            P.v(lambda e: e.tensor_tensor(out=t1[:, 0:512].rearrange("p (g d) -> p g d", g=4), in0=gv.rearrange("p (g d) -> p g d", g=4),
                                          in1=rsv[:].unsqueeze(2).to_broadcast([128, 4, 128]), op=ALU.mult), ["gl", "rsv"], ["t1"])
            P.v(lambda e: e.tensor_tensor(out=vn[:].rearrange("p g d -> p (g d)"), in0=t1[:, 0:512], in1=vng[:], op=ALU.mult),
                ["t1", "vng"], ["vn"])
            for g in range(4):
                P.t(lambda e, g=g: e.matmul(pP[:, 1536 + g * 128:1536 + (g + 1) * 128], lhsT=wsbb[:, g, :], rhs=vn[:, g, :],
                                            start=True, stop=True), ["vn", "wsbb"], ["pM"])
            P.v(lambda e: e.tensor_tensor(out=ob[:].rearrange("p (g d) -> p g d", g=4),
                                          in0=pP[:, 1536:2048].rearrange("p (g d) -> p g d", g=4),
                                          in1=bsb[:].unsqueeze(2).to_broadcast([128, 4, 128]), op=ALU.add), ["pM", "bsb"], ["ob"])
            P.v(lambda e: e.tensor_tensor(out=ob[:], in0=ob[:], in1=gl[:, 0:512], op=ALU.mult), ["ob", "gl"], ["ob"])
            P.a(lambda e: e.activation(out=t2[:, 0:512], in_=ob[:], func=AF.Square, accum_out=ss[:]), ["ob"], ["t2", "ss"])
            emit_rstd(P, ss[:], rs[:], 512, ["ss"], ["rs"])
            P.v(lambda e: e.scalar_tensor_tensor(out=yb[:], in0=ob[:], scalar=rs[:, 0:1], in1=ogbt[:], op0=ALU.mult, op1=ALU.mult),
                ["ob", "rs", "ogbt"], ["yb"])
            P.dma(lambda e, t=t: e.dma_start(out=yB[t * 128:(t + 1) * 128, :], in_=yb[:]), ["yb"], [("yB", t)])

        load_w(0, 2048)
        prep(0)
        for t in range(NT):
            inproj(2048, t)
            if t + 1 < NT:
                prep(t + 1)
            if t >= 1:
                storeA(t - 1)
            postA(t)
        storeA(NT - 1)

        load_w(2048, 1536)
        prep(0)
        for t in range(NT):
            inproj(1536, t)
            if t + 1 < NT:
                prep(t + 1)
            postB(t)
        P.emit(fused=fused)
    return nc


NKA = 20
NKC = 34


def build_s2a(nc=None, io=None, pfx=""):
    if nc is None:
        nc = bass.Bass("TRN2", target_bir_lowering=False)
    fused, di, do = _mk(nc, io, pfx)
    QKT = di("QKT", [128, NQK, NTOK], BF16)
    KA = None if fused else di("KA", [128, 2, NKA * 128], BF16)
    VA = None if fused else di("VA", [NKA * 128, 256], BF16)
    KC = None if fused else di("KC", [128, 2, NKC * 128], BF16)
    VC = None if fused else di("VC", [NKC * 128, 256], BF16)
    msk = di("msk", [4, 128, 128])
    sink = di("sink", [6])
    gac = di("gac", [1536])
    yAC = do("yAC", [NTOK, 1536], BF16)
    with stage_ctx(nc, fused) as st:
        sb = lambda n, s, d=F32: st.enter_context(nc.sbuf_tensor(pfx + n, s, d))
        pt = lambda n, s, d=F32: st.enter_context(nc.psum_tensor(pfx + n, s, d))
        ka = sb("ka", [128, 2, NKA * 128], BF16)
        va = sb("va", [128, NKA, 2, 129], BF16)
        kc = sb("kc", [128, 2, NKC * 128], BF16)
        vc = sb("vc", [128, NKC, 2, 129], BF16)
        mf = sb("mf", [128, 4, 128])
        mb = sb("mb", [128, 4, 128], BF16)
        sk = sb("sk", [128, 6])
        es = sb("es", [128, 6])
        gt = sb("gt", [128, 1536])
        qt = [sb(f"qt{i}", [128, NQK, 128], BF16) for i in range(2)]
        PT = [sb(f"PT{i}", [128, 3, 128], BF16) for i in range(3)]
        oo = sb("oo", [128, 2, 6, 128])
        den = sb("den", [128, 3])
        sq = sb("sq", [128, 768])
        ss = sb("ss", [128, 1])
        rs = sb("rs", [128, 1])
        yt = sb("yt", [128, 1536], BF16)
        psS = [pt(f"psS{i}", [128, 512]) for i in range(3)]
        psO = [pt(f"psO{i}", [128, 3, 129]) for i in range(2)]
        P = Prog(nc)
        if not fused:
            P.dma(lambda e: e.dma_start(out=ka[:], in_=KA), w=["ka"])
            P.dma(lambda e: e.dma_start(out=kc[:], in_=KC), w=["kc"])
        else:
            KTg = io["KTg"].rearrange("(r h d) t -> d r h t", r=2, h=4)
            Vg = io["Vg"]
            Vo = io["Vout"]
            for k in range(2):
                P.dma(lambda e, k=k: e.dma_start(out=ka[:, k, 0:256], in_=QKT[:, 6 + k, 0:256]), w=["ka"])
                P.dma(lambda e, k=k: e.dma_start(out=ka[:, k, 256:384], in_=KTg[:, 0, k, 1920:2048]), w=["ka"])
                P.dma(lambda e, k=k: e.dma_start(out=ka[:, k, 384:2432], in_=QKT[:, 6 + k, 256:2304]), w=["ka"])
                P.dma(lambda e, k=k: e.dma_start(out=ka[:, k, 2432:2560], in_=KTg[:, 1, k, 0:128]), w=["ka"])
                P.dma(lambda e, k=k: e.dma_start(out=kc[:, k, 0:256], in_=QKT[:, 14 + k, 0:256]), w=["kc"])
                for r in range(2):
                    P.dma(lambda e, k=k, r=r: e.dma_start(out=kc[:, k, 256 + r * 2048:256 + (r + 1) * 2048], in_=KTg[:, r, 2 + k, :]), w=["kc"])
        P.v(lambda e: e.memset(va[:, :, :, 128:129], 1.0), [], ["va1"])
        P.v(lambda e: e.memset(vc[:, :, :, 128:129], 1.0), [], ["vc1"])
        for k in range(2):
            if not fused:
                P.dma(lambda e, k=k: e.dma_start(out=va[:, :, k, 0:128], in_=VA[:, k * 128:(k + 1) * 128].rearrange("(j s) d -> s j d", s=128)), w=[("va", k)])
                P.dma(lambda e, k=k: e.dma_start(out=vc[:, :, k, 0:128], in_=VC[:, k * 128:(k + 1) * 128].rearrange("(j s) d -> s j d", s=128)), w=[("vc", k)])
            else:
                ca = slice(k * 128, (k + 1) * 128)
                cc = slice(256 + k * 128, 256 + (k + 1) * 128)
                tl = lambda ap: ap.rearrange("(j s) d -> s j d", s=128)
                P.dma(lambda e, k=k, ca=ca: e.dma_start(out=va[:, 0:2, k, 0:128], in_=tl(Vo[0:256, ca])), w=[("va", k)])
                P.dma(lambda e, k=k, ca=ca: e.dma_start(out=va[:, 2:3, k, 0:128], in_=tl(Vg[1920:2048, ca])), w=[("va", k)])
                P.dma(lambda e, k=k, ca=ca: e.dma_start(out=va[:, 3:19, k, 0:128], in_=tl(Vo[256:2304, ca])), w=[("va", k)])
                P.dma(lambda e, k=k, ca=ca: e.dma_start(out=va[:, 19:20, k, 0:128], in_=tl(Vg[2048:2176, ca])), w=[("va", k)])
                P.dma(lambda e, k=k, cc=cc: e.dma_start(out=vc[:, 0:2, k, 0:128], in_=tl(Vo[0:256, cc])), w=[("vc", k)])
                P.dma(lambda e, k=k, cc=cc: e.dma_start(out=vc[:, 2:34, k, 0:128], in_=tl(Vg[:, cc])), w=[("vc", k)])
        P.dma(lambda e: e.dma_start(out=mf[:], in_=msk.rearrange("m s q -> s m q")), w=["mf"])
        P.v(lambda e: e.tensor_copy(out=mb[:], in_=mf[:]), ["mf"], ["mb"])
        P.dma(lambda e: e.dma_start(out=sk[:], in_=_bc(sink, 6)), w=["sk"])
        P.a(lambda e: e.activation(out=es[:], in_=sk[:], func=AF.Exp), ["sk"], ["es"])
        P.dma(lambda e: e.dma_start(out=gt[:], in_=_bc(gac, 1536)), w=["gt"])
        PF = 2
        NBUF = PF + 1
        steps = []
        for t in range(NT):
            if t < 2:
                keysA = [(0, None), (1, None)]
                keysC = [(0, None), (1, None)]
            else:
                i = t - 2
                keysA = [(0, None), (1, None), (2 + i, 2 if i == 0 else 0), (3 + i, None), (4 + i, 3 if i == 15 else 1)]
                keysC = [(j, None) for j in range(NKC)]
            for (oi, qbase, KT, VT, kkey, vkeys, keys, use_sink) in (
                    (0, 0, ka, va, "ka", [("va", 0), ("va", 1), "va1"], keysA, True),
                    (1, 8, kc, vc, "kc", [("vc", 0), ("vc", 1), "vc1"], keysC, False)):
                for k in range(2):
                    for n, (j, m) in enumerate(keys):
                        steps.append(dict(t=t, oi=oi, qbase=qbase, KT=KT, VT=VT, kkey=kkey, vkeys=vkeys, k=k, n=n, j=j, m=m,
                                          last=(n == len(keys) - 1), use_sink=use_sink,
                                          tile_first=(oi == 0 and k == 0 and n == 0), tile_last=(oi == 1 and k == 1 and n == len(keys) - 1)))
        ngrp = [0]

        def load_q(t):
            q = qt[t % 2]
            P.dma(lambda e, q=q, t=t: e.dma_start(out=q[:], in_=QKT[:, :, t * 128:(t + 1) * 128]), w=[("qt", t % 2)])

        def s1(i):
            sp = steps[i]
            t, k, j, m = sp["t"], sp["k"], sp["j"], sp["m"]
            if sp["tile_first"]:
                if t == 0:
                    load_q(0)
                if t + 1 < NT:
                    load_q(t + 1)
            q = qt[t % 2]
            b = i % NBUF
            KT, qbase = sp["KT"], sp["qbase"]
            pS = psS[b][:, 0:384].rearrange("p (g q) -> p g q", g=3)
            P.t(lambda e: e.matmul(pS, lhsT=KT[:, k, j * 128:(j + 1) * 128], rhs=q[:, qbase + 3 * k:qbase + 3 * k + 3, :], start=True, stop=True),
                [sp["kkey"], ("qt", t % 2)], [("psS", b)])
            P.a(lambda e: e.activation(out=PT[b][:], in_=pS, func=AF.Exp, scale=128.0 ** -0.5), [("psS", b)], [("PT", b)])
            if m is not None:
                P.v(lambda e: e.tensor_tensor(out=PT[b][:], in0=PT[b][:], in1=mb[:, m, :].unsqueeze(1).to_broadcast([128, 3, 128]), op=ALU.mult),
                    [("PT", b), "mb"], [("PT", b)])

        def s2(i):
            sp = steps[i]
            t, k, j, n, oi = sp["t"], sp["k"], sp["j"], sp["n"], sp["oi"]
            b = i % NBUF
            VT = sp["VT"]
            pb = ngrp[0] % 2
            po = psO[pb]
            for g in range(3):
                P.t(lambda e, g=g: e.matmul(po[:, g, :], lhsT=PT[b][:, g, :], rhs=VT[:, j, k, :], start=(n == 0 and g == 0), stop=sp["last"]),
                    [("PT", b)] + sp["vkeys"], [("psO", pb)])
            if sp["last"]:
                ngrp[0] += 1
                if sp["use_sink"]:
                    P.v(lambda e: e.tensor_tensor(out=den[:], in0=po[:, :, 128], in1=es[:, 3 * k:3 * k + 3], op=ALU.add), [("psO", pb), "es"], ["den"])
                else:
                    P.v(lambda e: e.tensor_copy(out=den[:], in_=po[:, :, 128]), [("psO", pb)], ["den"])
                P.v(lambda e: e.reciprocal(out=den[:], in_=den[:]), ["den"], ["den"])
                P.v(lambda e: e.tensor_tensor(out=oo[:, oi, 3 * k:3 * k + 3, :], in0=po[:, :, 0:128], in1=den[:].unsqueeze(2).to_broadcast([128, 3, 128]),
                                              op=ALU.mult), [("psO", pb), "den"], [("oo", oi)])
            if sp["tile_last"]:
                for o2 in range(2):
                    ov = oo[:, o2, :, :].rearrange("p h d -> p (h d)")
                    P.v(lambda e, ov=ov: e.tensor_tensor(out=sq[:], in0=ov, in1=ov, op=ALU.mult), [("oo", o2)], ["sq"])
                    P.v(lambda e: e.tensor_reduce(out=ss[:], in_=sq[:], axis=AX.X, op=ALU.add), ["sq"], ["ss"])
                    emit_rstd(P, ss[:], rs[:], 768, ["ss"], ["rs"])
                    P.v(lambda e, ov=ov, o2=o2: e.scalar_tensor_tensor(out=yt[:, o2 * 768:(o2 + 1) * 768], in0=ov, scalar=rs[:, 0:1],
                                                                      in1=gt[:, o2 * 768:(o2 + 1) * 768], op0=ALU.mult, op1=ALU.mult),
                        [("oo", o2), "rs", "gt"], ["yt"])
                P.dma(lambda e: e.dma_start(out=yAC[t * 128:(t + 1) * 128, :], in_=yt[:]), ["yt"], [("yAC", t)])

        for i in range(min(PF, len(steps))):
            s1(i)
        for i in range(len(steps)):
            if i + PF < len(steps):
                s1(i + PF)
            s2(i)
        P.emit(fused=fused)
    return nc


def build_s2b(nc=None, io=None, pfx=""):
    if nc is None:
        nc = bass.Bass("TRN2", target_bir_lowering=False)
    fused, di, do = _mk(nc, io, pfx)
    yAC = di("yAC", [NTOK, 1536], BF16)
    yB = di("yB", [NTOK, 512], BF16)
    xin = di("xin", [NTOK, D])
    modx = di("modx", [6, D])
    modc = di("modc", [6, D])
    wout = di("wout", [D, D])
    n2g = di("n2g", [D])
    wr = di("wr", [D, NE])
    ident_in = di("ident", [128, 128])
    x1o = do("x1o", [NTOK, D])
    hx2o = do("hx2o", [NTOK, D], BF16)
    affT = None if fused else do("affT", [NE, NTOK])
    with stage_ctx(nc, fused) as st:
        sb = lambda n, s, d=F32: st.enter_context(nc.sbuf_tensor(pfx + n, s, d))
        pt = lambda n, s, d=F32: st.enter_context(nc.psum_tensor(pfx + n, s, d))
        wbf = sb("wbf", [128, 16, D], BF16)
        G1g = sb("G1g", [128, D])
        G2 = sb("G2", [128, D])
        SH2 = sb("SH2", [128, D])
        gn = sb("gn", [128, D])
        ident = sb("ident_f", [128, 128])
        identb = sb("identb", [128, 128], BF16)
        wrs = sb("wrs", [128, 16, NE])
        xt = sb("xt", [128, D])
        yt = sb("yt", [128, D], BF16)
        yT = sb("yT", [128, 16, 128], BF16)
        tmp = sb("tmp", [128, D])
        x1 = sb("x1", [128, D])
        h2 = sb("h2", [128, D])
        hb = sb("hb", [128, D], BF16)
        h2T = sb("h2T", [128, 16, 128])
        ss = sb("ss", [128, 1])
        rs = sb("rs", [128, 1])
        mx = sb("mx", [128, 1])
        se = sb("se", [128, 1])
        ex = sb("ex", [128, NE])
        af = sb("af", [128, NE])
        aT = sb("aT", [NE, 128])
        psT = pt("psT", [128, 16, 128], BF16)
        psX = [pt(f"psX{i}", [128, 512]) for i in range(2)]
        psR = [pt(f"psR{i}", [128, 4, 128]) for i in range(2)]
        psL = pt("psL", [128, 512])
        psA = pt("psA", [128, 512])
        P = Prog(nc)
        P.dma(lambda e: e.dma_start(out=ident[:], in_=ident_in), w=["ident"])
        P.v(lambda e: e.tensor_copy(out=identb[:], in_=ident[:]), ["ident"], ["identb"])
        P.dma(lambda e: e.dma_start(out=gn[:], in_=_bc(n2g, D)), w=["gn"])
        P.dma(lambda e: e.dma_start(out=wrs[:], in_=wr.rearrange("(kc p) n -> p kc n", p=128)), w=["wrs"])
        for k4 in range(4):
            src = wout[k4 * 512:(k4 + 1) * 512, :].rearrange("(kc p) n -> p kc n", p=128)
            P.dma(lambda e, k4=k4, src=src: e.dma_start(out=wbf[:, k4 * 4:(k4 + 1) * 4, :], in_=src), w=["wbf"], q="gpsimd")

        def load_mod(m):
            P.dma(lambda e: e.dma_start(out=G1g[:], in_=_bc(m[2], D)), w=["G1g"])
            P.dma(lambda e: e.dma_start(out=SH2[:], in_=_bc(m[3], D)), w=["SH2"])
            P.dma(lambda e: e.dma_start(out=G2[:], in_=_bc(m[4], D)), w=["G2"])
            P.v(lambda e: e.scalar_tensor_tensor(out=G2[:], in0=G2[:], scalar=1.0, in1=gn[:], op0=ALU.add, op1=ALU.mult),
                ["G2", "gn"], ["G2"])

        for t in range(NT):
            if t == 0:
                load_mod(modc)
            if t == 2:
                load_mod(modx)
            r0 = t * 128
            P.dma(lambda e, r0=r0: e.dma_start(out=yt[:, 0:768], in_=yAC[r0:r0 + 128, 0:768]), w=[("yt", 0)])
            P.dma(lambda e, r0=r0: e.dma_start(out=yt[:, 768:1280], in_=yB[r0:r0 + 128, :]), w=[("yt", 1)])
            P.dma(lambda e, r0=r0: e.dma_start(out=yt[:, 1280:2048], in_=yAC[r0:r0 + 128, 768:1536]), w=[("yt", 2)])
            P.dma(lambda e, r0=r0: e.dma_start(out=xt[:], in_=xin[r0:r0 + 128, :]), w=["xt"])
            for kc in range(16):
                P.t(lambda e, kc=kc: e.transpose(psT[:, kc, :], yt[:, kc * 128:(kc + 1) * 128], identb[:]),
                    [("yt", 0), ("yt", 1), ("yt", 2), "identb"], ["psT"])
            P.a(lambda e: e.activation(out=yT[:], in_=psT[:], func=AF.Copy), ["psT"], ["yT"])
            for g in range(4):
                px = psX[g % 2]
                for kc in range(16):
                    P.t(lambda e, px=px, g=g, kc=kc: e.matmul(px[:], lhsT=yT[:, kc, :], rhs=wbf[:, kc, g * 512:(g + 1) * 512],
                                                              start=(kc == 0), stop=(kc == 15)), ["yT", "wbf"], [("psX", g % 2)])
                P.v(lambda e, px=px, g=g: e.tensor_tensor(out=tmp[:, g * 512:(g + 1) * 512], in0=px[:], in1=G1g[:, g * 512:(g + 1) * 512],
                                                          op=ALU.mult), [("psX", g % 2), "G1g"], ["tmp"])
            P.v(lambda e: e.tensor_tensor(out=x1[:], in0=tmp[:], in1=xt[:], op=ALU.add), ["tmp", "xt"], ["x1"])
            P.dma(lambda e, r0=r0: e.dma_start(out=x1o[r0:r0 + 128, :], in_=x1[:]), ["x1"], [("x1o", t)])
            P.a(lambda e: e.activation(out=tmp[:], in_=x1[:], func=AF.Square, accum_out=ss[:]), ["x1"], ["tmp", "ss"])
            emit_rstd(P, ss[:], rs[:], D, ["ss"], ["rs"])
            P.v(lambda e: e.scalar_tensor_tensor(out=h2[:], in0=x1[:], scalar=rs[:, 0:1], in1=G2[:], op0=ALU.mult, op1=ALU.mult),
                ["x1", "rs", "G2"], ["h2"])
            P.v(lambda e: e.tensor_tensor(out=h2[:], in0=h2[:], in1=SH2[:], op=ALU.add), ["h2", "SH2"], ["h2"])
            P.a(lambda e: e.activation(out=hb[:], in_=h2[:], func=AF.Copy), ["h2"], ["hb"])
            P.dma(lambda e, r0=r0: e.dma_start(out=hx2o[r0:r0 + 128, :], in_=hb[:]), ["hb"], [("hx2o", t)])
            for q4 in range(4):
                pr = psR[q4 % 2]
                for j in range(4):
                    kc = q4 * 4 + j
                    P.t(lambda e, pr=pr, j=j, kc=kc: e.transpose(pr[:, j, :], h2[:, kc * 128:(kc + 1) * 128], ident[:]),
                        ["h2", "ident"], [("psR", q4 % 2)])
                P.a(lambda e, pr=pr, q4=q4: e.activation(out=h2T[:, q4 * 4:(q4 + 1) * 4, :], in_=pr[:], func=AF.Copy),
                    [("psR", q4 % 2)], ["h2T"])
            for kc in range(16):
                P.t(lambda e, kc=kc: e.matmul(psL[:, 0:NE], lhsT=h2T[:, kc, :], rhs=wrs[:, kc, :], start=(kc == 0), stop=(kc == 15)),
                    ["h2T", "wrs"], ["psL"])
            P.v(lambda e: e.tensor_reduce(out=mx[:], in_=psL[:, 0:NE], axis=AX.X, op=ALU.max), ["psL"], ["mx"])
            P.v(lambda e: e.tensor_scalar(out=mx[:], in0=mx[:], scalar1=-1.0, scalar2=None, op0=ALU.mult), ["mx"], ["mx"])
            P.a(lambda e: e.activation(out=ex[:], in_=psL[:, 0:NE], func=AF.Exp, bias=mx[:, 0:1], accum_out=se[:]), ["psL", "mx"], ["ex", "se"])
            P.v(lambda e: e.reciprocal(out=se[:], in_=se[:]), ["se"], ["se"])
            P.v(lambda e: e.tensor_scalar(out=af[:], in0=ex[:], scalar1=se[:, 0:1], scalar2=None, op0=ALU.mult), ["ex", "se"], ["af"])
            if fused:
                adst = io["affC"][r0:r0 + 128, :] if t < 2 else io["affIn"][r0 - 256:r0 - 128, :]
                P.dma(lambda e, adst=adst: e.dma_start(out=adst, in_=af[:]), ["af"], [("affo", t)])
            else:
                P.t(lambda e: e.transpose(psA[0:NE, 0:128], af[:], ident[:]), ["af", "ident"], ["psA"])
                P.a(lambda e: e.activation(out=aT[:], in_=psA[0:NE, 0:128], func=AF.Copy), ["psA"], ["aT"])
                P.dma(lambda e, r0=r0: e.dma_start(out=affT[:, r0:r0 + 128], in_=aT[:]), ["aT"], [("affT", t)])
        P.emit(fused=fused)
    return nc


NEL = 8
NROW = SEQ + CTX


def build_s3(with_ctx, nc=None, io=None, pfx=""):
    if nc is None:
        nc = bass.Bass("TRN2", target_bir_lowering=False)
    fused, di, do = _mk(nc, io, pfx)
    affL = di("affL", [128, 32, NEL])
    affC = di("affC", [128, 2, NEL])
    hx2 = di("hx2", [NROW, D], BF16)
    wg = di("wg", [NEL, D, D])
    wu = di("wu", [NEL, D, D])
    wd = di("wd", [NEL, D, D])
    consts = di("consts", [128, 128 + 128 + 128 + 512 + 34])
    delta = [do(f"delta{i}", [NROW, 512]) for i in range(4)]
    sets = [("L", 32, 512, 0, affL)]
    if with_ctx:
        sets.append(("C", 2, 32, SEQ, affC))
    with stage_ctx(nc, fused) as st:
        sb = lambda n, s, d=F32: st.enter_context(nc.sbuf_tensor(pfx + n, s, d))
        pt = lambda n, s, d=F32: st.enter_context(nc.psum_tensor(pfx + n, s, d))
        cst = sb("cst", [128, 930])
        identb = sb("identb", [128, 128], BF16)
        zt = sb("zt", [128, D])
        ring = [sb(f"ring{i}", [128, 16, 512], BF16) for i in range(4)]
        xg = [sb(f"xg{i}", [128, D], BF16) for i in range(2)]
        xgT = sb("xgT", [128, 16, 544], BF16)
        hT = sb("hT", [128, 16, 544], BF16)
        sil = sb("sil", [128, 544])
        yb = [sb(f"yb{i}", [128, 512]) for i in range(3)]
        S = {}
        for (nm, C, k, r0, _) in sets:
            S[nm] = dict(
                aff=sb(f"saff{nm}", [128, C, NEL]), lo=sb(f"lo{nm}", [128, NEL]), th=sb(f"th{nm}", [128, NEL, 15]),
                cmp=sb(f"cmp{nm}", [128, C, NEL, 15]), cntp=sb(f"cntp{nm}", [128, NEL, 15]), ge=sb(f"ge{nm}", [128, NEL, 15]),
                nge=sb(f"nge{nm}", [128, NEL]), mask=sb(f"mask{nm}", [128, C, NEL]), maskb=sb(f"maskb{nm}", [128, C, NEL], BF16),
                cA=sb(f"cA{nm}", [128, C, NEL]), cB=sb(f"cB{nm}", [128, C, NEL]), pos=sb(f"pos{nm}", [128, C, NEL]),
                vals=sb(f"vals{nm}", [128, C, NEL, 2]), oh=[sb(f"oh{nm}{i}", [128, k]) for i in range(2)],
                sl=sb(f"sl{nm}", [128, NEL, 4, 2]), idx=sb(f"idx{nm}", [128, NEL, 4], I32), gate=sb(f"gate{nm}", [128, NEL, 4]))
        jt = sb("jt", [128, NEL, 15])
        trib = sb("trib", [128, 128], BF16)
        onesb = sb("onesb", [128, 128], BF16)
        psA = [pt(f"psA{i}", [128, 512]) for i in range(2)]
        psU = [pt(f"psU{i}", [128, 512]) for i in range(2)]
        psT = pt("psT", [128, 16, 128], BF16)
        psY = [pt(f"psY{i}", [128, 512]) for i in range(2)]
        ident = cst[:, 0:128]
        ones = cst[:, 128:256]
        tri = cst[:, 256:384]
        iota = cst[:, 384:896]
        tokid = cst[:, 896:930]
        P = Prog(nc)
        P.dma(lambda e: e.dma_start(out=cst[:], in_=consts), w=["cst"])
        P.v(lambda e: e.tensor_copy(out=identb[:], in_=ident), ["cst"], ["identb"])
        P.v(lambda e: e.tensor_copy(out=trib[:], in_=tri), ["cst"], ["trib"])
        P.v(lambda e: e.tensor_copy(out=onesb[:], in_=ones), ["cst"], ["onesb"])
        for j in range(15):
            P.v(lambda e, j=j: e.memset(jt[:, :, j:j + 1], float(j + 1)), [], ["jt"])
        P.v(lambda e: e.memset(zt[:], 0.0), [], ["zt"])
        for cb in range(4):
            for r in range(NROW // 128):
                P.dma(lambda e, r=r, cb=cb: e.dma_start(out=delta[cb][r * 128:(r + 1) * 128, :], in_=zt[:, 0:512]), ["zt"], [("delta", cb)])

        def select(nm, C, k, r0, affd):
            s = S[nm]
            kk = lambda x: (nm, x)
            P.dma(lambda e, s=s, affd=affd: e.dma_start(out=s["aff"][:], in_=affd), w=[kk("aff")])
            P.v(lambda e, s=s: e.memset(s["lo"][:], 0.0), [], [kk("lo")])
            affb = s["aff"][:].unsqueeze(3).to_broadcast([128, C, NEL, 15])
            for it in range(8):
                step = 16.0 ** -(it + 1)
                P.v(lambda e, s=s, step=step: e.scalar_tensor_tensor(out=s["th"][:], in0=jt[:], scalar=step,
                                                                     in1=s["lo"][:].unsqueeze(2).to_broadcast([128, NEL, 15]),
                                                                     op0=ALU.mult, op1=ALU.add), ["jt", kk("lo")], [kk("th")])
                P.v(lambda e, s=s: e.tensor_tensor(out=s["cmp"][:], in0=affb, in1=s["th"][:].unsqueeze(1).to_broadcast([128, C, NEL, 15]),
                                                   op=ALU.is_ge), [kk("aff"), kk("th")], [kk("cmp")])
                P.v(lambda e, s=s: e.tensor_reduce(out=s["cntp"][:], in_=s["cmp"][:].rearrange("p c e j -> p e j c"), axis=AX.X, op=ALU.add),
                    [kk("cmp")], [kk("cntp")])
                P.t(lambda e, s=s: e.matmul(psY[0][:, 0:NEL * 15], lhsT=ones, rhs=s["cntp"][:].rearrange("p e j -> p (e j)"), start=True, stop=True),
                    ["cst", kk("cntp")], [("psY", 0)])
                P.v(lambda e, s=s, k=k: e.tensor_scalar(out=s["ge"][:].rearrange("p e j -> p (e j)"), in0=psY[0][:, 0:NEL * 15], scalar1=float(k) - 0.5,
                                                        scalar2=None, op0=ALU.is_ge), [("psY", 0)], [kk("ge")])
                P.v(lambda e, s=s: e.tensor_reduce(out=s["nge"][:], in_=s["ge"][:], axis=AX.X, op=ALU.add), [kk("ge")], [kk("nge")])
                P.v(lambda e, s=s, step=step: e.scalar_tensor_tensor(out=s["lo"][:], in0=s["nge"][:], scalar=step, in1=s["lo"][:],
                                                                     op0=ALU.mult, op1=ALU.add), [kk("nge"), kk("lo")], [kk("lo")])
            P.v(lambda e, s=s: e.tensor_tensor(out=s["mask"][:], in0=s["aff"][:], in1=s["lo"][:].unsqueeze(1).to_broadcast([128, C, NEL]),
                                               op=ALU.is_ge), [kk("aff"), kk("lo")], [kk("mask")])
            P.v(lambda e, s=s: e.tensor_copy(out=s["maskb"][:], in_=s["mask"][:]), [kk("mask")], [kk("maskb")])
            mb2 = s["maskb"][:].rearrange("p c e -> p (c e)")
            P.t(lambda e: e.matmul(psY[0][:, 0:C * NEL], lhsT=trib[:], rhs=mb2, start=True, stop=True), ["trib", kk("maskb")], [("psY", 0)])
            P.t(lambda e: e.matmul(psY[1][:, 0:C * NEL], lhsT=onesb[:], rhs=mb2, start=True, stop=True), ["onesb", kk("maskb")], [("psY", 1)])
            P.v(lambda e, s=s: e.tensor_copy(out=s["cA"][:].rearrange("p c e -> p (c e)"), in_=psY[1][:, 0:C * NEL]), [("psY", 1)], [kk("cA")])
            cur, nxt = "cA", "cB"
            sh = 1
            while sh < C:
                P.v(lambda e, s=s, cur=cur, nxt=nxt, sh=sh: e.tensor_copy(out=s[nxt][:, 0:sh, :], in_=s[cur][:, 0:sh, :]), [kk(cur)], [kk(nxt)])
                P.v(lambda e, s=s, cur=cur, nxt=nxt, sh=sh: e.tensor_tensor(out=s[nxt][:, sh:C, :], in0=s[cur][:, sh:C, :], in1=s[cur][:, 0:C - sh, :],
                                                                          op=ALU.add), [kk(cur)], [kk(nxt)])
                cur, nxt = nxt, cur
                sh *= 2
            P.v(lambda e, s=s, cur=cur: e.tensor_tensor(out=s["pos"][:].rearrange("p c e -> p (c e)"), in0=s[cur][:].rearrange("p c e -> p (c e)"),
                                                        in1=psY[1][:, 0:C * NEL], op=ALU.subtract), [kk(cur), ("psY", 1)], [kk("pos")])
            P.v(lambda e, s=s: e.tensor_tensor(out=s["pos"][:].rearrange("p c e -> p (c e)"), in0=s["pos"][:].rearrange("p c e -> p (c e)"),
                                               in1=psY[0][:, 0:C * NEL], op=ALU.add), [kk("pos"), ("psY", 0)], [kk("pos")])
            P.v(lambda e, s=s: e.tensor_tensor(out=s["pos"][:], in0=s["pos"][:], in1=s["mask"][:], op=ALU.mult), [kk("pos"), kk("mask")], [kk("pos")])
            P.v(lambda e, s=s: e.tensor_scalar(out=s["pos"][:], in0=s["pos"][:], scalar1=-1.0, scalar2=None, op0=ALU.add), [kk("pos")], [kk("pos")])
            P.v(lambda e, s=s, r0=r0: e.tensor_scalar(out=s["vals"][:, :, :, 0], in0=tokid[:, 0:C].unsqueeze(2).to_broadcast([128, C, NEL]),
                                                      scalar1=float(r0), scalar2=None, op0=ALU.add), ["cst"], [kk("vals")])
            P.v(lambda e, s=s: e.tensor_copy(out=s["vals"][:, :, :, 1], in_=s["aff"][:]), [kk("aff")], [kk("vals")])
            nst = (k + 127) // 128
            sw = min(k, 128)
            n_oh = 0
            for el in range(NEL):
                for c in range(C):
                    ohb = s["oh"][n_oh % 2]
                    kb = (nm, "oh", n_oh % 2)
                    n_oh += 1
                    P.v(lambda e, ohb=ohb, s=s, c=c, el=el, k=k: e.tensor_scalar(out=ohb[:], in0=iota[:, 0:k], scalar1=s["pos"][:, c, el:el + 1],
                                                                                scalar2=None, op0=ALU.is_equal), ["cst", kk("pos")], [kb])
                    for sti in range(nst):
                        P.t(lambda e, ohb=ohb, s=s, c=c, el=el, sti=sti, sw=sw: e.matmul(
                            psU[0][0:sw, (el * 4 + sti) * 2:(el * 4 + sti) * 2 + 2], lhsT=ohb[:, sti * 128:sti * 128 + sw],
                            rhs=s["vals"][:, c, el, :], start=(c == 0 and el == 0 and sti == 0), stop=(c == C - 1)),
                            [kb, kk("vals")], [("psU", 0)])
            P.v(lambda e, s=s, sw=sw: e.tensor_copy(out=s["sl"][0:sw].rearrange("p e s t -> p (e s t)"), in_=psU[0][0:sw, 0:NEL * 8]), [("psU", 0)], [kk("sl")])
            P.v(lambda e, s=s, sw=sw: e.tensor_copy(out=s["idx"][0:sw], in_=s["sl"][0:sw, :, :, 0]), [kk("sl")], [kk("idx")])
            P.v(lambda e, s=s, sw=sw: e.tensor_copy(out=s["gate"][0:sw], in_=s["sl"][0:sw, :, :, 1]), [kk("sl")], [kk("gate")])

        for sargs in sets:
            select(*sargs)

        tiles = [("L", sti, 128, sti * 128) for sti in range(4)]
        if with_ctx:
            tiles.append(("C", 0, 32, 512))
        ncols = 544 if with_ctx else 512
        ring_n = [0]
        ny = [0]

        def load_unit(wsrc, el, cb):
            rb = ring_n[0] % 4
            ring_n[0] += 1
            for k4 in range(4):
                src = wsrc[el, k4 * 512:(k4 + 1) * 512, cb * 512:(cb + 1) * 512].rearrange("(kc p) n -> p kc n", p=128)
                P.dma(lambda e, rb=rb, k4=k4, src=src: e.dma_start(out=ring[rb][:, k4 * 4:(k4 + 1) * 4, :], in_=src), w=[("ring", rb)], q="gpsimd")
            return rb

        for el in range(NEL):
            for ti, (nm, sti, n, c0) in enumerate(tiles):
                s = S[nm]
                xb = xg[ti % 2]
                kx = ("xg", ti % 2)
                P.op("gpsimd", lambda e, xb=xb, s=s, el=el, sti=sti, n=n: e.indirect_dma_start(
                    out=xb[0:n, :], out_offset=None, in_=hx2[:, :],
                    in_offset=bass.IndirectOffsetOnAxis(ap=s["idx"][0:n, el, sti:sti + 1], axis=0)), [(nm, "idx")], [kx], dma=True)
                for kc in range(16):
                    P.t(lambda e, xb=xb, kc=kc, n=n: e.transpose(psT[:, kc, 0:n], xb[0:n, kc * 128:(kc + 1) * 128], identb[0:n, 0:n]), [kx, "identb"], ["psT"])
                P.a(lambda e, c0=c0, n=n: e.activation(out=xgT[:, :, c0:c0 + n], in_=psT[:, :, 0:n], func=AF.Copy), ["psT"], ["xgT"])
            for cb in range(4):
                rg = load_unit(wg, el, cb)
                ru = load_unit(wu, el, cb)
                for f4 in range(4):
                    fc = cb * 4 + f4
                    pa = psA[fc % 2]
                    pu = psU[fc % 2]
                    for (pp, rbuf, key) in ((pa, rg, "psA"), (pu, ru, "psU")):
                        for kc in range(16):
                            P.t(lambda e, pp=pp, rbuf=rbuf, kc=kc, f4=f4: e.matmul(pp[:, 0:512], lhsT=ring[rbuf][:, kc, f4 * 128:(f4 + 1) * 128],
                                                                                  rhs=xgT[:, kc, 0:512], start=(kc == 0), stop=(kc == 15)),
                                [("ring", rbuf), "xgT"], [(key, fc % 2)])
                    P.a(lambda e, pa=pa: e.activation(out=sil[:, 0:512], in_=pa[:, 0:512], func=AF.Silu), [("psA", fc % 2)], ["sil"])
                    P.v(lambda e, pu=pu, fc=fc: e.tensor_tensor(out=hT[:, fc, 0:512], in0=sil[:, 0:512], in1=pu[:, 0:512], op=ALU.mult),
                        ["sil", ("psU", fc % 2)], ["hT"])
                    if with_ctx:
                        for (pp, rbuf, key) in ((pa, rg, "psA"), (pu, ru, "psU")):
                            for kc in range(16):
                                P.t(lambda e, pp=pp, rbuf=rbuf, kc=kc, f4=f4: e.matmul(pp[:, 0:32], lhsT=ring[rbuf][:, kc, f4 * 128:(f4 + 1) * 128],
                                                                                      rhs=xgT[:, kc, 512:544], start=(kc == 0), stop=(kc == 15)),
                                    [("ring", rbuf), "xgT"], [(key, fc % 2)])
                        P.a(lambda e, pa=pa: e.activation(out=sil[:, 512:544], in_=pa[:, 0:32], func=AF.Silu), [("psA", fc % 2)], ["sil"])
                        P.v(lambda e, pu=pu, fc=fc: e.tensor_tensor(out=hT[:, fc, 512:544], in0=sil[:, 512:544], in1=pu[:, 0:32], op=ALU.mult),
                            ["sil", ("psU", fc % 2)], ["hT"])
            for cb in range(4):
                rd = load_unit(wd, el, cb)
                for ti, (nm, sti, n, c0) in enumerate(tiles):
                    s = S[nm]
                    py = psY[ny[0] % 2]
                    ky = ("psY", ny[0] % 2)
                    ybuf = yb[ny[0] % 3]
                    kyb = ("yb", ny[0] % 3)
                    ny[0] += 1
                    for fc in range(16):
                        P.t(lambda e, py=py, rd=rd, fc=fc, c0=c0, n=n: e.matmul(py[0:n, :], lhsT=hT[:, fc, c0:c0 + n], rhs=ring[rd][:, fc, :],
                                                                               start=(fc == 0), stop=(fc == 15)), ["hT", ("ring", rd)], [ky])
                    P.v(lambda e, py=py, ybuf=ybuf, s=s, el=el, sti=sti, n=n: e.tensor_scalar(out=ybuf[0:n, :], in0=py[0:n, :],
                                                                                              scalar1=s["gate"][0:n, el, sti:sti + 1], scalar2=None, op0=ALU.mult),
                        [ky, (nm, "gate")], [kyb])
                    P.op("gpsimd", lambda e, ybuf=ybuf, s=s, el=el, sti=sti, n=n, cb=cb: e.indirect_dma_start(
                        out=delta[cb][:, :], out_offset=bass.IndirectOffsetOnAxis(ap=s["idx"][0:n, el, sti:sti + 1], axis=0),
                        in_=ybuf[0:n, :], in_offset=None, compute_op=ALU.add), [kyb, (nm, "idx"), ("delta", cb)], [("delta", cb)], dma=True)
        P.emit(fused=fused)
    return nc


def s3_consts():
    c = np.zeros((128, 930), np.float32)
    c[:, 0:128] = np.eye(128)
    c[:, 128:256] = 1.0
    c[:, 256:384] = np.triu(np.ones((128, 128)))
    c[:, 384:896] = np.arange(512)[None, :]
    c[:, 896:930] = np.arange(34)[None, :] * 128 + np.arange(128)[:, None]
    return c


def build_s4(nc=None, io=None, pfx=""):
    if nc is None:
        nc = bass.Bass("TRN2", target_bir_lowering=False)
    fused, di, do = _mk(nc, io, pfx)
    x1 = di("x1", [NTOK, D])
    dA = di("dA", [NTOK, D])
    dB = None if fused else di("dB", [NTOK, D])
    modx = di("modx", [6, D])
    modc = di("modc", [6, D])
    x2 = do("x2", [NTOK, D])
    with stage_ctx(nc, fused) as st:
        sb = lambda n, s, d=F32: st.enter_context(nc.sbuf_tensor(pfx + n, s, d))
        G = sb("G", [128, D])
        xa = [sb(f"xa{i}", [128, D]) for i in range(2)]
        da = [sb(f"da{i}", [128, D]) for i in range(2)]
        db = [sb(f"db{i}", [128, D]) for i in range(2)]
        P = Prog(nc)
        for t in range(NT):
            i = t % 2
            if t == 0:
                P.dma(lambda e: e.dma_start(out=G[:], in_=_bc(modc[5], D)), w=["G"])
            if t == 2:
                P.dma(lambda e: e.dma_start(out=G[:], in_=_bc(modx[5], D)), w=["G"])
            r0 = t * 128
            P.dma(lambda e, i=i, r0=r0: e.dma_start(out=xa[i][:], in_=x1[r0:r0 + 128, :]), w=[("xa", i)])
            P.dma(lambda e, i=i, r0=r0: e.dma_start(out=da[i][:], in_=dA[r0:r0 + 128, :]), w=[("da", i)])
            if not fused:
                P.dma(lambda e, i=i, r0=r0: e.dma_start(out=db[i][:], in_=dB[r0:r0 + 128, :]), w=[("db", i)])
                P.v(lambda e, i=i: e.tensor_tensor(out=da[i][:], in0=da[i][:], in1=db[i][:], op=ALU.add), [("da", i), ("db", i)], [("da", i)])
            P.v(lambda e, i=i: e.tensor_tensor(out=da[i][:], in0=da[i][:], in1=G[:], op=ALU.mult), [("da", i), "G"], [("da", i)])
            P.v(lambda e, i=i: e.tensor_tensor(out=xa[i][:], in0=xa[i][:], in1=da[i][:], op=ALU.add), [("xa", i), ("da", i)], [("xa", i)])
            P.dma(lambda e, i=i, r0=r0: e.dma_start(out=x2[r0:r0 + 128, :], in_=xa[i][:]), [("xa", i)], [("x2", t)])
        P.emit(fused=fused)
    return nc


def _rope_tables(h):
    t = np.arange(h * 2048, (h + 1) * 2048)
    row = (t // 64).astype(np.float32)
    col = (t % 64).astype(np.float32)
    inv = (np.float32(10000.0) ** (-np.arange(32, dtype=np.float32) / np.float32(32))).astype(np.float32)
    ar = row[:, None] * inv
    ac = col[:, None] * inv
    tab = np.zeros((NTOK, 128), np.float32)
    tab[:256, :64] = 1.0
    tab[256:, :64] = np.concatenate([np.cos(ar), np.cos(ac)], 1)
    tab[256:, 64:] = np.concatenate([np.sin(ar), np.sin(ac)], 1)
    return tab


def _masks(h):
    s = np.arange(128)[:, None]
    q = np.arange(128)[None, :]
    mp = (s >= q).astype(np.float32)
    mn = (s <= q).astype(np.float32)
    return np.stack([mp, mn, mp * (0.0 if h == 0 else 1.0), mn * (0.0 if h == 1 else 1.0)])


def _run(nc, in_maps):
    res = run_bass_kernel_spmd(nc, in_maps, core_ids=list(range(8)))
    return res.results


PAIRS = [[0, 1], [2, 3], [4, 5], [6, 7]]
NXR = NTOK + 128
CAPL = 384
NCST = 931


def f_consts():
    c = np.zeros((128, NCST), np.float32)
    c[:, 0:930] = s3_consts()
    c[:, 930] = NTOK + np.arange(128)
    return c


def stage_mod(nc, io, pfx):
    cin, w, b, modS = io["cin"], io["w_mod"], io["b_mod"], io["modS"]
    with stage_ctx(nc, True) as st:
        sb = lambda n, s, d=F32: st.enter_context(nc.sbuf_tensor(pfx + n, s, d))
        ct = sb("ct", [128, 16, 2])
        cs = sb("cs", [128, 16, 2])
        wt = [sb(f"wt{i}", [128, 16, 512]) for i in range(3)]
        bt = [sb(f"bt{i}", [2, 512]) for i in range(3)]
        ot = [sb(f"ot{i}", [2, 512]) for i in range(3)]
        ps = [st.enter_context(nc.psum_tensor(pfx + f"ps{i}", [2, 512], F32)) for i in range(2)]
        P = Prog(nc)
        P.dma(lambda e: e.dma_start(out=ct[:], in_=cin), w=["ct"])
        P.a(lambda e: e.activation(out=cs[:], in_=ct[:], func=AF.Silu), ["ct"], ["cs"])
        gi = 0
        for l in range(2):
            for g in range(24):
                i3 = gi % 3
                i2 = gi % 2
                gi += 1
                cols = slice(g * 512, (g + 1) * 512)
                src = w[l, :, cols].rearrange("(kc p) n -> p kc n", p=128)
                P.dma(lambda e, i3=i3, src=src: e.dma_start(out=wt[i3][:], in_=src), w=[("wt", i3)])
                P.dma(lambda e, i3=i3, l=l, cols=cols: e.dma_start(out=bt[i3][:], in_=b[l:l + 1, cols].to_broadcast([2, 512])), w=[("bt", i3)])
                for kc in range(16):
                    P.t(lambda e, i2=i2, i3=i3, kc=kc: e.matmul(ps[i2][:], lhsT=cs[:, kc, :], rhs=wt[i3][:, kc, :], start=(kc == 0), stop=(kc == 15)),
                        ["cs", ("wt", i3)], [("ps", i2)])
                P.v(lambda e, i2=i2, i3=i3: e.tensor_tensor(out=ot[i3][:], in0=ps[i2][:], in1=bt[i3][:], op=ALU.add), [("ps", i2), ("bt", i3)], [("ot", i3)])
                P.dma(lambda e, i3=i3, l=l, cols=cols: e.dma_start(out=modS[l, :, cols], in_=ot[i3][:]), [("ot", i3)], [("modS", l, g)])
        P.emit(fused=True)


def stage_cc(nc, pairs):
    with nc.cleanup_on_exit():
        _UID[0] += 1
        sem = nc.alloc_semaphore(name="cc%d" % _UID[0])
        with nc.Block() as block:
            @block.gpsimd
            def _(g):
                for i, (a, b) in enumerate(pairs):
                    g.collective_compute("AllGather", ALU.bypass, replica_groups=PAIRS, ins=[a], outs=[b]).then_inc(sem, 1)
                    g.wait_ge(sem, i + 1)
        nc.all_engine_barrier()


def stage_s3a(nc, io, with_ctx, pfx):
    affAll, affIn, affC, consts = io["affAll"], io["affIn"], io["affC"], io["consts"]
    idxT, gateT, delta, hx2 = io["idxT"], io["gateT"], io["delta"], io["hx2o"]
    with stage_ctx(nc, True) as st:
        sb = lambda n, s, d=F32: st.enter_context(nc.sbuf_tensor(pfx + n, s, d))
        pt = lambda n, s, d=F32: st.enter_context(nc.psum_tensor(pfx + n, s, d))
        cst = sb("cst", [128, NCST])
        trib = sb("trib", [128, 128], BF16)
        onesb = sb("onesb", [128, 128], BF16)
        jt = sb("jt", [128, NE, 15])
        zt = sb("zt", [128, D])
        ztb = sb("ztb", [128, D], BF16)
        sl = sb("sl", [128, NE, 4, 3])
        idxf = sb("idxf", [128, NE, 4])
        idx = sb("idx", [128, NE, 4], I32)
        gate = sb("gate", [128, NE, 4])
        psC = pt("psC", [128, 512])
        psP = [pt(f"psP{i}", [128, 512]) for i in range(2)]
        psS = [pt(f"psS{i}", [128, 512]) for i in range(2)]
        ones = cst[:, 128:256]
        tri = cst[:, 256:384]
        iota = cst[:, 384:896]
        tokid = cst[:, 896:930]
        dummy = cst[:, 930:931]
        P = Prog(nc)
        P.dma(lambda e: e.dma_start(out=cst[:], in_=consts), w=["cst"])
        P.v(lambda e: e.tensor_copy(out=trib[:], in_=tri), ["cst"], ["trib"])
        P.v(lambda e: e.tensor_copy(out=onesb[:], in_=ones), ["cst"], ["onesb"])
        for j in range(15):
            P.v(lambda e, j=j: e.memset(jt[:, :, j:j + 1], float(j + 1)), [], ["jt"])
        P.v(lambda e: e.memset(zt[:], 0.0), [], ["zt"])
        P.v(lambda e: e.memset(ztb[:], 0.0), [], ["ztb"])
        P.v(lambda e: e.memset(sl[:], 0.0), [], ["sl"])
        for r in range(NXR // 128):
            P.dma(lambda e, r=r: e.dma_start(out=delta[r * 128:(r + 1) * 128, :], in_=zt[:]), ["zt"], [("delta", r)])
        P.dma(lambda e: e.dma_start(out=hx2[NTOK:NXR, :], in_=ztb[:]), ["ztb"], ["hx2d"])

        def select(nm, Cth, thr_ap, Cm, mask_ap, k, r0, cap, st0, psSl):
            kk = lambda x: (nm, x)
            athr = sb(f"athr{nm}", [128, Cth, NE])
            am = athr if mask_ap is None else sb(f"am{nm}", [128, Cm, NE])
            kam = kk("athr") if mask_ap is None else kk("am")
            lo = sb(f"lo{nm}", [128, NE])
            th = sb(f"th{nm}", [128, NE, 15])
            cmpb = sb(f"cmp{nm}", [128, Cth, NE, 15], BF16)
            cntp = sb(f"cntp{nm}", [128, NE, 15])
            ge = sb(f"ge{nm}", [128, NE, 15])
            nge = sb(f"nge{nm}", [128, NE])
            mask = sb(f"mask{nm}", [128, Cm, NE])
            maskb = sb(f"maskb{nm}", [128, Cm, NE], BF16)
            cA = sb(f"cA{nm}", [128, Cm, NE])
            cB = sb(f"cB{nm}", [128, Cm, NE])
            pos = sb(f"pos{nm}", [128, Cm, NE])
            vals = sb(f"vals{nm}", [128, Cm, NE, 3])
            oh = [sb(f"oh{nm}{i}", [128, cap]) for i in range(2)]
            bufs = {"cA": cA, "cB": cB}
            P.dma(lambda e: e.dma_start(out=athr[:], in_=thr_ap.rearrange("(c p) e -> p c e", p=128)), w=[kk("athr")])
            if mask_ap is not None:
                P.dma(lambda e: e.dma_start(out=am[:], in_=mask_ap.rearrange("(c p) e -> p c e", p=128)), w=[kk("am")])
            P.v(lambda e: e.memset(lo[:], 0.0), [], [kk("lo")])
            affb = athr[:].unsqueeze(3).to_broadcast([128, Cth, NE, 15])
            for it in range(8):
                step = 16.0 ** -(it + 1)
                P.v(lambda e, step=step: e.scalar_tensor_tensor(out=th[:], in0=jt[:], scalar=step, in1=lo[:].unsqueeze(2).to_broadcast([128, NE, 15]),
                                                                op0=ALU.mult, op1=ALU.add), ["jt", kk("lo")], [kk("th")])
                P.v(lambda e: e.tensor_tensor(out=cmpb[:], in0=affb, in1=th[:].unsqueeze(1).to_broadcast([128, Cth, NE, 15]), op=ALU.is_ge),
                    [kk("athr"), kk("th")], [kk("cmp")])
                P.v(lambda e: e.tensor_reduce(out=cntp[:], in_=cmpb[:].rearrange("p c e j -> p e j c"), axis=AX.X, op=ALU.add), [kk("cmp")], [kk("cntp")])
                P.t(lambda e: e.matmul(psC[:, 0:NE * 15], lhsT=ones, rhs=cntp[:].rearrange("p e j -> p (e j)"), start=True, stop=True),
                    ["cst", kk("cntp")], ["psC"])
                P.v(lambda e: e.tensor_scalar(out=ge[:].rearrange("p e j -> p (e j)"), in0=psC[:, 0:NE * 15], scalar1=float(k) - 0.5, scalar2=None,
                                              op0=ALU.is_ge), ["psC"], [kk("ge")])
                P.v(lambda e: e.tensor_reduce(out=nge[:], in_=ge[:], axis=AX.X, op=ALU.add), [kk("ge")], [kk("nge")])
                P.v(lambda e, step=step: e.scalar_tensor_tensor(out=lo[:], in0=nge[:], scalar=step, in1=lo[:], op0=ALU.mult, op1=ALU.add),
                    [kk("nge"), kk("lo")], [kk("lo")])
            P.v(lambda e: e.tensor_tensor(out=mask[:], in0=am[:], in1=lo[:].unsqueeze(1).to_broadcast([128, Cm, NE]), op=ALU.is_ge),
                [kam, kk("lo")], [kk("mask")])
            P.v(lambda e: e.tensor_copy(out=maskb[:], in_=mask[:]), [kk("mask")], [kk("maskb")])
            mb2 = maskb[:].rearrange("p c e -> p (c e)")
            ncol = Cm * NE
            P.t(lambda e: e.matmul(psP[0][:, 0:ncol], lhsT=trib[:], rhs=mb2, start=True, stop=True), ["trib", kk("maskb")], [("psP", 0)])
            P.t(lambda e: e.matmul(psP[1][:, 0:ncol], lhsT=onesb[:], rhs=mb2, start=True, stop=True), ["onesb", kk("maskb")], [("psP", 1)])
            P.v(lambda e: e.tensor_copy(out=cA[:].rearrange("p c e -> p (c e)"), in_=psP[1][:, 0:ncol]), [("psP", 1)], [kk("cA")])
            cur, nxt = "cA", "cB"
            sh = 1
            while sh < Cm:
                P.v(lambda e, cur=cur, nxt=nxt, sh=sh: e.tensor_copy(out=bufs[nxt][:, 0:sh, :], in_=bufs[cur][:, 0:sh, :]), [kk(cur)], [kk(nxt)])
                P.v(lambda e, cur=cur, nxt=nxt, sh=sh: e.tensor_tensor(out=bufs[nxt][:, sh:Cm, :], in0=bufs[cur][:, sh:Cm, :], in1=bufs[cur][:, 0:Cm - sh, :],
                                                                  op=ALU.add), [kk(cur)], [kk(nxt)])
                cur, nxt = nxt, cur
                sh *= 2
            pos2 = pos[:].rearrange("p c e -> p (c e)")
            P.v(lambda e, cur=cur: e.tensor_tensor(out=pos2, in0=bufs[cur][:].rearrange("p c e -> p (c e)"), in1=psP[1][:, 0:ncol], op=ALU.subtract),
                [kk(cur), ("psP", 1)], [kk("pos")])
            P.v(lambda e: e.tensor_tensor(out=pos2, in0=pos2, in1=psP[0][:, 0:ncol], op=ALU.add), [kk("pos"), ("psP", 0)], [kk("pos")])
            P.v(lambda e: e.tensor_tensor(out=pos[:], in0=pos[:], in1=mask[:], op=ALU.mult), [kk("pos"), kk("mask")], [kk("pos")])
            P.v(lambda e: e.tensor_scalar(out=pos[:], in0=pos[:], scalar1=-1.0, scalar2=None, op0=ALU.add), [kk("pos")], [kk("pos")])
            P.v(lambda e: e.tensor_scalar(out=vals[:, :, :, 0], in0=tokid[:, 0:Cm].unsqueeze(2).to_broadcast([128, Cm, NE]), scalar1=float(r0), scalar2=None,
                                          op0=ALU.add), ["cst"], [kk("vals")])
            P.v(lambda e: e.tensor_copy(out=vals[:, :, :, 1], in_=am[:]), [kam], [kk("vals")])
            P.v(lambda e: e.memset(vals[:, :, :, 2], 1.0), [], [kk("vals")])
            nst = (cap + 127) // 128
            sw = min(cap, 128)
            n_oh = 0
            first = True
            for el in range(NE):
                for c in range(Cm):
                    ohb = oh[n_oh % 2]
                    kb = (nm, "oh", n_oh % 2)
                    n_oh += 1
                    P.v(lambda e, ohb=ohb, c=c, el=el: e.tensor_scalar(out=ohb[:], in0=iota[:, 0:cap], scalar1=pos[:, c, el:el + 1], scalar2=None,
                                                                      op0=ALU.is_equal), ["cst", kk("pos")], [kb])
                    for sti in range(nst):
                        col = (el * 4 + st0 + sti) * 3
                        P.t(lambda e, ohb=ohb, c=c, el=el, sti=sti, col=col, first=first: e.matmul(
                            psSl[0:sw, col:col + 3], lhsT=ohb[:, sti * 128:sti * 128 + sw], rhs=vals[:, c, el, :],
                            start=first, stop=(c == Cm - 1)), [kb, kk("vals")], [kk("psSl")])
                        first = False
            for sti in range(nst):
                P.v(lambda e, sti=sti: e.tensor_copy(out=sl[0:sw, :, st0 + sti, :],
                                                     in_=psSl[0:sw, 0:NE * 12].rearrange("p (e s t) -> p e s t", e=NE, s=4)[:, :, st0 + sti, :]),
                    [kk("psSl")], ["sl"])

        select("L", 32, affAll, 16, affIn, 512, 256, CAPL, 0, psS[0])
        if with_ctx:
            select("C", 2, affC, 2, None, 32, 0, 32, 3, psS[1])
        P.v(lambda e: e.tensor_scalar(out=idxf[:], in0=sl[:, :, :, 2], scalar1=-1.0, scalar2=dummy, op0=ALU.add, op1=ALU.mult), ["sl", "cst"], ["idxf"])
        P.v(lambda e: e.tensor_tensor(out=idxf[:], in0=sl[:, :, :, 0], in1=idxf[:], op=ALU.subtract), ["sl", "idxf"], ["idxf"])
        P.v(lambda e: e.tensor_copy(out=idx[:], in_=idxf[:]), ["idxf"], ["idx"])
        P.v(lambda e: e.tensor_copy(out=gate[:], in_=sl[:, :, :, 1]), ["sl"], ["gate"])
        P.dma(lambda e: e.dma_start(out=idxT, in_=idx[:]), ["idx"], ["idxT"])
        P.dma(lambda e: e.dma_start(out=gateT, in_=gate[:]), ["gate"], ["gateT"])
        P.emit(fused=True)


def stage_s3b(nc, io, L, with_ctx, pfx):
    idxT, gateT, delta, hx2, consts = io["idxT"], io["gateT"], io["delta"], io["hx2o"], io["consts"]
    wg, wu, wd = io["w_gate"], io["w_up"], io["w_down"]
    with stage_ctx(nc, True) as st:
        sb = lambda n, s, d=F32: st.enter_context(nc.sbuf_tensor(pfx + n, s, d))
        pt = lambda n, s, d=F32: st.enter_context(nc.psum_tensor(pfx + n, s, d))
        ncols = CAPL + (32 if with_ctx else 0)
        cst = sb("cst", [128, 128])
        identb = sb("identb", [128, 128], BF16)
        idx = sb("idx", [128, NE, 4], I32)
        gate = sb("gate", [128, NE, 4])
        NR, NSTG, PFD = 5, 6, 3
        ring = [sb(f"ring{i}", [128, 16, 512], BF16) for i in range(NR)]
        stg = [sb(f"stg{i}", [128, 4, 512]) for i in range(NSTG)]
        xg = [sb(f"xg{i}", [128, D], BF16) for i in range(2)]
        xgT = sb("xgT", [128, 16, ncols], BF16)
        hT = sb("hT", [128, 16, ncols], BF16)
        sil = sb("sil", [128, ncols])
        ybig = [sb(f"ybig{i}", [128, D]) for i in range(4)]
        psA = [pt(f"psA{i}", [128, 512]) for i in range(2)]
        psU = [pt(f"psU{i}", [128, 512]) for i in range(2)]
        psT = pt("psT", [128, 16, 128], BF16)
        psY = [pt(f"psY{i}", [128, 512]) for i in range(2)]
        P = Prog(nc)
        P.dma(lambda e: e.dma_start(out=cst[:], in_=consts[:, 0:128]), w=["cst"])
        P.v(lambda e: e.tensor_copy(out=identb[:], in_=cst[:]), ["cst"], ["identb"])
        P.dma(lambda e: e.dma_start(out=idx[:], in_=idxT), w=["idx"])
        P.dma(lambda e: e.dma_start(out=gate[:], in_=gateT), w=["gate"])
        tiles = [(sti, 128, sti * 128) for sti in range(3)]
        if with_ctx:
            tiles.append((3, 32, CAPL))
        ny = [0]
        seq = []
        for el_ in range(NE):
            for cb_ in range(4):
                seq.append((wg, el_, cb_))
                seq.append((wu, el_, cb_))
            for cb_ in range(4):
                seq.append((wd, el_, cb_))
        issued = [0]
        nq = [0]
        cast_eng = ["scalar", "vector", "scalar", "vector"]

        def issue(i):
            wsrc, el_, cb_ = seq[i]
            rb = i % NR
            for k4 in range(4):
                si = nq[0] % NSTG
                ce = cast_eng[nq[0] % 4]
                nq[0] += 1
                src = wsrc[L, el_, k4 * 512:(k4 + 1) * 512, cb_ * 512:(cb_ + 1) * 512].rearrange("(kc p) n -> p kc n", p=128)
                P.dma(lambda e, si=si, src=src: e.dma_start(out=stg[si][:], in_=src), w=[("stg", si)])
                dst = ring[rb][:, k4 * 4:(k4 + 1) * 4, :]
                if ce == "scalar":
                    P.op("scalar", lambda e, si=si, dst=dst: e.activation(out=dst, in_=stg[si][:], func=AF.Copy), [("stg", si)], [("ring", rb, k4)])
                else:
                    P.op(ce, lambda e, si=si, dst=dst: e.tensor_copy(out=dst, in_=stg[si][:]), [("stg", si)], [("ring", rb, k4)])

        def need(i):
            while issued[0] < min(len(seq), i + PFD + 1):
                issue(issued[0])
                issued[0] += 1
            return i % NR

        un = [0]

        def load_unit(wsrc, el, cb):
            i = un[0]
            un[0] += 1
            assert seq[i][1] == el and seq[i][2] == cb and seq[i][0] is wsrc
            return need(i)

        for el in range(NE):
            for ti, (sti, n, c0) in enumerate(tiles):
                xb = xg[ti % 2]
                kx = ("xg", ti % 2)
                P.op("gpsimd", lambda e, xb=xb, el=el, sti=sti, n=n: e.indirect_dma_start(
                    out=xb[0:n, :], out_offset=None, in_=hx2[:, :],
                    in_offset=bass.IndirectOffsetOnAxis(ap=idx[0:n, el, sti:sti + 1], axis=0)), ["idx"], [kx], dma=True)
                for kc in range(16):
                    P.t(lambda e, xb=xb, kc=kc, n=n: e.transpose(psT[:, kc, 0:n], xb[0:n, kc * 128:(kc + 1) * 128], identb[0:n, 0:n]), [kx, "identb"], ["psT"])
                P.a(lambda e, c0=c0, n=n: e.activation(out=xgT[:, :, c0:c0 + n], in_=psT[:, :, 0:n], func=AF.Copy), ["psT"], ["xgT"])
            for cb in range(4):
                rg = load_unit(wg, el, cb)
                ru = load_unit(wu, el, cb)
                for f4 in range(4):
                    fc = cb * 4 + f4
                    pa = psA[fc % 2]
                    pu = psU[fc % 2]
                    for (pp, rbuf, key) in ((pa, rg, "psA"), (pu, ru, "psU")):
                        for kc in range(16):
                            P.t(lambda e, pp=pp, rbuf=rbuf, kc=kc, f4=f4: e.matmul(pp[:, 0:ncols], lhsT=ring[rbuf][:, kc, f4 * 128:(f4 + 1) * 128],
                                                                                  rhs=xgT[:, kc, :], start=(kc == 0), stop=(kc == 15)),
                                [("ring", rbuf, kc // 4), "xgT"], [(key, fc % 2)])
                    P.a(lambda e, pa=pa: e.activation(out=sil[:], in_=pa[:, 0:ncols], func=AF.Silu), [("psA", fc % 2)], ["sil"])
                    P.v(lambda e, pu=pu, fc=fc: e.tensor_tensor(out=hT[:, fc, :], in0=sil[:], in1=pu[:, 0:ncols], op=ALU.mult),
                        ["sil", ("psU", fc % 2)], ["hT"])
            for cb in range(4):
                rd = load_unit(wd, el, cb)
                for ti, (sti, n, c0) in enumerate(tiles):
                    py = psY[ny[0] % 2]
                    ky = ("psY", ny[0] % 2)
                    ny[0] += 1
                    for fc in range(16):
                        P.t(lambda e, py=py, rd=rd, fc=fc, c0=c0, n=n: e.matmul(py[0:n, :], lhsT=hT[:, fc, c0:c0 + n], rhs=ring[rd][:, fc, :],
                                                                               start=(fc == 0), stop=(fc == 15)), ["hT", ("ring", rd, fc // 4)], [ky])
                    P.v(lambda e, py=py, ti=ti, el=el, sti=sti, n=n, cb=cb: e.tensor_scalar(out=ybig[ti][0:n, cb * 512:(cb + 1) * 512], in0=py[0:n, :],
                                                                                           scalar1=gate[0:n, el, sti:sti + 1], scalar2=None, op0=ALU.mult),
                        [ky, "gate"], [("ybig", ti)])
            for ti, (sti, n, c0) in enumerate(tiles):
                P.op("gpsimd", lambda e, ti=ti, el=el, sti=sti, n=n: e.indirect_dma_start(
                    out=delta[:, :], out_offset=bass.IndirectOffsetOnAxis(ap=idx[0:n, el, sti:sti + 1], axis=0),
                    in_=ybig[ti][0:n, :], in_offset=None, compute_op=ALU.add), [("ybig", ti), "idx", "delta"], ["delta"], dma=True)
        P.emit(fused=True)


def stage_copy_out(nc, src, dst, pfx):
    with stage_ctx(nc, True) as st:
        bufs = [st.enter_context(nc.sbuf_tensor(pfx + f"cb{i}", [128, D], F32)) for i in range(3)]
        P = Prog(nc)
        for t in range(16):
            b = bufs[t % 3]
            P.dma(lambda e, b=b, t=t: e.dma_start(out=b[:], in_=src[256 + t * 128:256 + (t + 1) * 128, :]), w=[("cb", t % 3)])
            P.dma(lambda e, b=b, t=t: e.dma_start(out=dst[t * 128:(t + 1) * 128, :], in_=b[:]), [("cb", t % 3)], [("out", t)])
        P.emit(fused=True)


def build_fused(ncores=8):
    global PAIRS
    PAIRS = [[2 * i, 2 * i + 1] for i in range(ncores // 2)]
    nc = bass.Bass("TRN2", target_bir_lowering=False)
    ein = lambda n, s, d=F32: nc.dram_tensor(n, s, d, kind="ExternalInput").ap()
    scr = lambda n, s, d=F32: nc.dram_tensor(n, s, d, kind="Internal").ap()
    scrl = lambda n, s, d=F32: nc.dram_tensor(n, s, d, kind="Internal", addr_space="Local").ap()
    X = {}
    X["xin0"] = ein("xin", [NTOK, D])
    X["cin"] = ein("cin", [128, 16, 2])
    X["w_mod"] = ein("w_mod", [2, D, 6 * D])
    X["b_mod"] = ein("b_mod", [2, 6 * D])
    n1g = ein("n1g", [2, D]); n2g = ein("n2g", [2, D])
    win = ein("win", [2, D, 3584])
    gqk = ein("gqk", [2, NQK * 128])
    X["cs_t"] = ein("cs_t", [NTOK, 128])
    vnb = ein("vnb", [2, 512]); wsT = ein("wsT", [2, 128, 4, 128]); bsT = ein("bsT", [2, 128, 4]); ogb = ein("ogb", [2, 512])
    X["ident"] = ein("ident", [128, 128])
    X["msk"] = ein("msk", [4, 128, 128])
    sink = ein("sink", [2, 6]); gac = ein("gac", [2, 1536])
    wout = ein("wout", [2, D, D]); wr = ein("wr", [2, D, NE])
    X["w_gate"] = ein("w_gate", [2, NE, D, D]); X["w_up"] = ein("w_up", [2, NE, D, D]); X["w_down"] = ein("w_down", [2, NE, D, D])
    X["consts"] = ein("consts", [128, NCST])
    out = nc.dram_tensor("out", [2048, D], F32, kind="ExternalOutput").ap()
    modS = scr("modS", [2, 2, 6 * D])
    X["modS"] = modS
    X["QKT"] = scr("QKT", [128, NQK, NTOK], BF16); X["Vout"] = scr("Vout", [NTOK, 512], BF16)
    X["yB"] = scr("yB", [NTOK, 512], BF16); X["yAC"] = scr("yAC", [NTOK, 1536], BF16)
    X["KTx"] = scr("KTx", [512, 2048], BF16); X["KTg"] = scrl("KTg", [1024, 2048], BF16)
    X["Vx"] = scr("Vx", [2048, 512], BF16); X["Vg"] = scrl("Vg", [4096, 512], BF16)
    xs = scr("xs", [NXR, D]); X["hx2o"] = scr("hx2s", [NXR, D], BF16); X["delta"] = scr("delta", [NXR, D])
    X["affIn"] = scr("affIn", [2048, NE]); X["affC"] = scr("affC", [256, NE]); X["affAll"] = scrl("affAll", [4096, NE])
    X["idxT"] = scr("idxT", [128, NE, 4], I32); X["gateT"] = scr("gateT", [128, NE, 4])

    stage_mod(nc, X, "m_")
    for L in range(2):
        io = dict(X)
        io["xin"] = X["xin0"] if L == 0 else xs
        io["modx"] = modS[L, 0].rearrange("(s d) -> s d", s=6)
        io["modc"] = modS[L, 1].rearrange("(s d) -> s d", s=6)
        io.update(n1g=n1g[L], n2g=n2g[L], win=win[L], gqk=gqk[L], vnb=vnb[L], wsT=wsT[L], bsT=bsT[L], ogb=ogb[L],
                  sink=sink[L], gac=gac[L], wout=wout[L], wr=wr[L], x1o=xs, x1=xs, dA=X["delta"], x2=xs)
        build_s1(nc=nc, io=io, pfx=f"L{L}a_")
        stage_cc(nc, [(X["KTx"], X["KTg"]), (X["Vx"], X["Vg"])])
        build_s2a(nc=nc, io=io, pfx=f"L{L}b_")
        build_s2b(nc=nc, io=io, pfx=f"L{L}c_")
        stage_cc(nc, [(X["affIn"], X["affAll"])])
        stage_s3a(nc, io, L == 0, f"L{L}d_")
        stage_s3b(nc, io, L, L == 0, f"L{L}e_")
        build_s4(nc=nc, io=io, pfx=f"L{L}f_")
    stage_copy_out(nc, xs, out, "o_")
    return nc


def kernel(x, c, ctx, c_ctx, w_mod, b_mod, norm1_g, norm2_g, w_in, qn_a, kn_a, sink_a, vn_b,
           w_s, b_s, qn_c, kn_c, out_g, w_out, w_router, w_gate, w_up, w_down):
    f32 = np.float32
    A = lambda a: np.ascontiguousarray(np.asarray(a, f32))
    x = A(x); ctx = A(ctx); c = A(c); c_ctx = A(c_ctx)
    cores = [(k // 2, k % 2) for k in range(8)]
    og = A(out_g)
    shared = {
        "w_mod": A(w_mod), "b_mod": A(b_mod), "n1g": A(norm1_g), "n2g": A(norm2_g),
        "win": A(np.asarray(w_in, f32)[:, :, WIN_PERM]),
        "gqk": A(np.stack([np.concatenate([np.tile(qn_a[L], 6), np.tile(kn_a[L], 2), np.tile(qn_c[L], 6), np.tile(kn_c[L], 2)]) for L in range(2)])),
        "vnb": A(vn_b), "wsT": A(np.asarray(w_s, f32).transpose(0, 3, 1, 2)), "bsT": A(np.asarray(b_s, f32).transpose(0, 2, 1)),
        "ogb": A(og[:, 768:1280]), "ident": np.eye(128, dtype=f32), "sink": A(sink_a),
        "gac": A(np.concatenate([og[:, :768], og[:, 1280:]], 1)), "wout": A(w_out), "wr": A(w_router),
        "w_gate": A(w_gate), "w_up": A(w_up), "w_down": A(w_down), "consts": f_consts(),
    }
    ims = []
    for (b, h) in cores:
        d = dict(shared)
        d["xin"] = A(np.concatenate([ctx[b], x[b, h * 2048:(h + 1) * 2048]], 0))
        c2 = np.stack([c[b], c_ctx], 0)
        d["cin"] = A(c2.T.reshape(16, 128, 2).transpose(1, 0, 2))
        d["cs_t"] = _rope_tables(h)
        d["msk"] = _masks(h)
        ims.append(d)
    ncores = _NCORES[0]
    nc = build_fused(ncores)
    res = run_bass_kernel_spmd(nc, ims[:ncores], core_ids=list(range(ncores)))
    out = np.zeros((4, SEQ, D), f32)
    for k, (b, h) in enumerate(cores[:ncores]):
        out[b, h * 2048:(h + 1) * 2048] = res.results[k]["out"]
    return out


_NCORES = [8]
```

```python
import contextlib
import numpy as np
import ml_dtypes
import concourse.bass as bass
import concourse.mybir as mybir
from concourse.bass_utils import run_bass_kernel_spmd

F32 = mybir.dt.float32
BF16 = mybir.dt.bfloat16
I32 = mybir.dt.int32
AF = mybir.ActivationFunctionType
ALU = mybir.AluOpType
AX = mybir.AxisListType

D = 2048
SEQ = 4096
CTX = 256
NT = 18
NTOK = NT * 128
EPS = 1e-6
NE = 16

ENGS = ("sync", "scalar", "vector", "gpsimd", "tensor")
SEM_CHUNK = 3000
NDMA_SEMS = 12


class _Op:
    __slots__ = ("eng", "fn", "deps", "is_dma", "has_dep", "sem", "val", "prev_dma")

    def __init__(self, eng, fn, is_dma):
        self.eng = eng
        self.fn = fn
        self.is_dma = is_dma
        self.deps = []
        self.has_dep = False
        self.sem = None
        self.val = None
        self.prev_dma = None


class Prog:
    def __init__(self, nc):
        self.nc = nc
        self.ops = {e: [] for e in ENGS}
        self.last_w = {}
        self.readers = {}
        self.ndma = {e: 0 for e in ENGS}
        self.dma_last = {}
        self.all_dmas = []

    def op(self, eng, fn, reads=(), writes=(), dma=False):
        o = _Op(eng, fn, dma)
        deps = []
        for r in reads:
            w = self.last_w.get(r)
            if w is not None:
                deps.append((w, "raw"))
        for k in writes:
            w = self.last_w.get(k)
            if w is not None:
                deps.append((w, "waw"))
            for rd in self.readers.get(k, ()):
                deps.append((rd, "war"))
        for (d, kind) in deps:
            if d is o:
                continue
            if not d.is_dma and not dma and d.eng == eng:
                if kind != "raw" or eng == "tensor":
                    continue
            if d not in o.deps:
                o.deps.append(d)
                d.has_dep = True
        for r in reads:
            self.readers.setdefault(r, []).append(o)
        for k in writes:
            self.last_w[k] = o
            self.readers[k] = []
        if dma:
            j = self.ndma[eng]
            self.ndma[eng] += 1
            slot = (eng, j % NDMA_SEMS)
            o.prev_dma = self.dma_last.get(slot)
            self.dma_last[slot] = o
            o.sem = slot
            o.val = 16 * (j // NDMA_SEMS + 1)
            self.all_dmas.append(o)
        self.ops[eng].append(o)
        return o

    def v(self, fn, r=(), w=()):
        return self.op("vector", fn, r, w)

    def a(self, fn, r=(), w=()):
        return self.op("scalar", fn, r, w)

    def g(self, fn, r=(), w=()):
        return self.op("gpsimd", fn, r, w)

    def t(self, fn, r=(), w=()):
        return self.op("tensor", fn, r, w)

    def dma(self, fn, r=(), w=(), q="sync"):
        return self.op(q, fn, r, w, dma=True)

    def emit(self, final_wait_eng="sync", fused=False):
        nc = self.nc
        sem_names = set()
        for e in ENGS:
            cnt = 0
            for o in self.ops[e]:
                if o.is_dma:
                    sem_names.add(o.sem)
                    continue
                if o.has_dep:
                    o.sem = (e, "c", cnt // SEM_CHUNK)
                    o.val = cnt % SEM_CHUNK + 1
                    sem_names.add(o.sem)
                    cnt += 1
        sem_names = sorted(sem_names, key=str)
        with contextlib.ExitStack() as st:
            sems = {}
            for n in sem_names:
                if fused:
                    _UID[0] += 1
                    sems[n] = nc.alloc_semaphore(name="f%d_" % _UID[0] + "_".join(str(x) for x in n))
                else:
                    sems[n] = st.enter_context(nc.semaphore("s_" + "_".join(str(x) for x in n)))
            block = st.enter_context(nc.Block())
            prog = self

            def run(e, eng):
                seen = {}
                for o in prog.ops[e]:
                    waits = []
                    if o.is_dma and o.prev_dma is not None:
                        waits.append(o.prev_dma)
                    waits.extend(o.deps)
                    for d in waits:
                        if seen.get(d.sem, 0) >= d.val:
                            continue
                        eng.wait_ge(sems[d.sem], d.val)
                        seen[d.sem] = d.val
                    ins = o.fn(eng)
                    if o.is_dma:
                        ins.then_inc(sems[o.sem], 16)
                    elif o.has_dep:
                        ins.then_inc(sems[o.sem], 1)
                if e == final_wait_eng:
                    last = {}
                    for o in prog.all_dmas:
                        last[o.sem] = max(last.get(o.sem, 0), o.val)
                    for s, v in last.items():
                        if seen.get(s, 0) < v:
                            eng.wait_ge(sems[s], v)

            @block.sync
            def _(eng):
                run("sync", eng)

            @block.scalar
            def _(eng):
                run("scalar", eng)

            @block.vector
            def _(eng):
                run("vector", eng)

            @block.gpsimd
            def _(eng):
                run("gpsimd", eng)

            @block.tensor
            def _(eng):
                run("tensor", eng)


_UID = [0]


@contextlib.contextmanager
def stage_ctx(nc, fused):
    if not fused:
        with contextlib.ExitStack() as st:
            yield st
    else:
        with nc.cleanup_on_exit():
            with contextlib.ExitStack() as st:
                yield st
            nc.all_engine_barrier()


def _mk(nc, io, pfx):
    fused = io is not None
    if fused:
        di = lambda n, s, d=F32: io[n]
        do = lambda n, s, d=F32: io[n]
    else:
        di = lambda n, s, d=F32: nc.dram_tensor(n, s, d, kind="ExternalInput").ap()
        do = lambda n, s, d=F32: nc.dram_tensor(n, s, d, kind="ExternalOutput").ap()
    return fused, di, do


def _bc(ap1d, n):
    return ap1d.unsqueeze(0).to_broadcast([128, n])


MCOL = 1536


def build_mod():
    nc = bass.Bass("TRN2", target_bir_lowering=False)
    cin = nc.dram_tensor("cin", [128, 16, 5], F32, kind="ExternalInput").ap()
    w = nc.dram_tensor("w", [2, D, MCOL], F32, kind="ExternalInput").ap()
    b = nc.dram_tensor("b", [2, MCOL], F32, kind="ExternalInput").ap()
    out = nc.dram_tensor("out", [2, 5, MCOL], F32, kind="ExternalOutput").ap()
    with contextlib.ExitStack() as st:
        sb = lambda n, s, d: st.enter_context(nc.sbuf_tensor(n, s, d))
        ct = sb("ct", [128, 16, 5], F32)
        cs = sb("cs", [128, 16, 5], F32)
        wt = [sb(f"wt{i}", [128, 16, 512], F32) for i in range(2)]
        bt = sb("bt", [5, 2, MCOL], F32)
        ot = sb("ot", [5, 2, MCOL], F32)
        ps = [st.enter_context(nc.psum_tensor(f"ps{i}", [5, 512], F32)) for i in range(2)]
        P = Prog(nc)
        P.dma(lambda e: e.dma_start(out=ct[:], in_=cin), w=["ct"])
        for l in range(2):
            P.dma(lambda e, l=l: e.dma_start(out=bt[:, l, :], in_=b[l:l + 1, :].to_broadcast([5, MCOL])), w=[("bt", l)])
        P.a(lambda e: e.activation(out=cs[:], in_=ct[:], func=AF.Silu), ["ct"], ["cs"])
        gi = 0
        for l in range(2):
            for g in range(3):
                buf = gi % 2
                src = w[l, :, g * 512:(g + 1) * 512].rearrange("(kc p) n -> p kc n", p=128)
                P.dma(lambda e, buf=buf, src=src: e.dma_start(out=wt[buf][:], in_=src), w=[("wt", buf)])
                for kc in range(16):
                    P.t(lambda e, buf=buf, kc=kc: e.matmul(ps[buf][:], lhsT=cs[:, kc, :], rhs=wt[buf][:, kc, :],
                                                           start=(kc == 0), stop=(kc == 15)),
                        ["cs", ("wt", buf)], [("ps", buf)])
                P.v(lambda e, buf=buf, l=l, g=g: e.tensor_tensor(out=ot[:, l, g * 512:(g + 1) * 512], in0=ps[buf][:],
                                                                 in1=bt[:, l, g * 512:(g + 1) * 512], op=ALU.add),
                    [("ps", buf), ("bt", l)], [("ot", l, g)])
                gi += 1
        for l in range(2):
            P.dma(lambda e, l=l: e.dma_start(out=out[l], in_=ot[:, l, :]), [("ot", l, g) for g in range(3)], [("out", l)])
        P.emit()
    return nc


def run_mod(c, c_ctx, w_mod, b_mod):
    c_all = np.concatenate([c, c_ctx[None, :]], axis=0).astype(np.float32)
    cin = np.ascontiguousarray(c_all.T.reshape(16, 128, 5).transpose(1, 0, 2))
    nc = build_mod()
    in_maps = [{"cin": cin, "w": np.ascontiguousarray(w_mod[:, :, j * MCOL:(j + 1) * MCOL]),
                "b": np.ascontiguousarray(b_mod[:, j * MCOL:(j + 1) * MCOL])} for j in range(8)]
    res = run_bass_kernel_spmd(nc, in_maps, core_ids=list(range(8)))
    return np.concatenate([r["out"] for r in res.results], axis=2)


def emit_rstd(P, ss, rstd, n, kr, kw):
    P.a(lambda e: e.activation(out=rstd, in_=ss, func=AF.Sqrt, scale=1.0 / n, bias=EPS), kr, kw)
    P.v(lambda e: e.reciprocal(out=rstd, in_=rstd), kw, kw)


NQK = 16
WIN_PERM = np.concatenate([np.arange(0, 1024), np.arange(2304, 3328),
                           np.arange(1024, 1280), np.arange(3328, 3584),
                           np.arange(1280, 2304)])


def build_s1(nc=None, io=None, pfx=""):
    if nc is None:
        nc = bass.Bass("TRN2", target_bir_lowering=False)
    fused, di, do = _mk(nc, io, pfx)
    xin = di("xin", [NTOK, D])
    modx = di("modx", [6, D])
    modc = di("modc", [6, D])
    n1g = di("n1g", [D])
    win = di("win", [D, 3584])
    gqk = di("gqk", [NQK * 128])
    cs_t = di("cs_t", [NTOK, 128])
    vnb = di("vnb", [512])
    wsT = di("wsT", [128, 4, 128])
    bsT = di("bsT", [128, 4])
    ogb = di("ogb", [512])
    ident_in = di("ident", [128, 128])
    QKT = do("QKT", [128, NQK, NTOK], BF16)
    Vout = do("Vout", [NTOK, 512], BF16)
    yB = do("yB", [NTOK, 512], BF16)

    with stage_ctx(nc, fused) as st:
        sb = lambda n, s, d=F32: st.enter_context(nc.sbuf_tensor(pfx + n, s, d))
        pt = lambda n, s, d=F32: st.enter_context(nc.psum_tensor(pfx + n, s, d))
        wbf = sb("wbf", [128, 16, 2048], BF16)
        G1 = sb("G1", [128, D])
        SH = sb("SH", [128, D])
        gn = sb("gn", [128, D])
        xt = [sb(f"xt{i}", [128, D]) for i in range(2)]
        tmp = sb("tmp", [128, D])
        hx = sb("hx", [128, D], BF16)
        hxT = sb("hxT", [128, 16, 128], BF16)
        hxb = [hx, sb("hx1", [128, D], BF16)]
        hxTb = [hxT, sb("hxT1", [128, 16, 128], BF16)]
        tmph = sb("tmph", [128, D])
        ssh = sb("ssh", [128, 1])
        rsh = sb("rsh", [128, 1])
        ident = sb("ident_f", [128, 128])
        identb = sb("identb", [128, 128], BF16)
        ss = sb("ss", [128, 1])
        rs = sb("rs", [128, 1])
        Pb = sb("Pb", [128, 2048])
        gq = sb("gq", [128, NQK, 128])
        ssq = sb("ssq", [128, NQK])
        rsq = sb("rsq", [128, NQK])
        qn = sb("qn", [128, NQK, 2, 2, 32])
        cst = sb("cst", [128, 128])
        rA = sb("rA", [128, NQK, 2, 32])
        rB = sb("rB", [128, NQK, 2, 32])
        qr = sb("qr", [128, NQK, 2, 2, 32], BF16)
        qrb = [qr, sb("qr1", [128, NQK, 2, 2, 32], BF16)]
        qT = sb("qT", [128, NQK, 128], BF16)
        vb16 = sb("vb16", [128, 512], BF16)
        vng = sb("vng", [128, 512])
        wsb = sb("wsb", [128, 4, 128])
        wsbb = sb("wsbb", [128, 4, 128], BF16)
        bsb = sb("bsb", [128, 4])
        ogbt = sb("ogbt", [128, 512])
        t1 = sb("t1", [128, 1024])
        t2 = sb("t2", [128, 1024])
        gl = sb("gl", [128, 1024])
        ssv = sb("ssv", [128, 4])
        rsv = sb("rsv", [128, 4])
        vn = sb("vn", [128, 4, 128], BF16)
        ob = sb("ob", [128, 512])
        yb = sb("yb", [128, 512], BF16)
        pT = pt("pT", [128, 16, 128], BF16)
        pP = pt("pP", [128, 2048])
        pQ = pt("pQ", [128, NQK, 128], BF16)

        P = Prog(nc)
        P.dma(lambda e: e.dma_start(out=ident[:], in_=ident_in), w=["ident"])
        P.v(lambda e: e.tensor_copy(out=identb[:], in_=ident[:]), ["ident"], ["identb"])
        P.dma(lambda e: e.dma_start(out=gn[:], in_=_bc(n1g, D)), w=["gn"])
        P.dma(lambda e: e.dma_start(out=gq[:].rearrange("p h d -> p (h d)"), in_=_bc(gqk, NQK * 128)), w=["gq"])
        P.dma(lambda e: e.dma_start(out=vng[:], in_=_bc(vnb, 512)), w=["vng"])
        P.dma(lambda e: e.dma_start(out=ogbt[:], in_=_bc(ogb, 512)), w=["ogbt"])
        P.dma(lambda e: e.dma_start(out=wsb[:], in_=wsT), w=["wsb"])
        P.v(lambda e: e.tensor_copy(out=wsbb[:], in_=wsb[:]), ["wsb"], ["wsbb"])
        P.dma(lambda e: e.dma_start(out=bsb[:], in_=bsT), w=["bsb"])

        def load_w(c0, ncol):
            for k4 in range(4):
                src = win[k4 * 512:(k4 + 1) * 512, c0:c0 + ncol].rearrange("(kc p) n -> p kc n", p=128)
                P.dma(lambda e, k4=k4, src=src: e.dma_start(out=wbf[:, k4 * 4:(k4 + 1) * 4, 0:ncol], in_=src),
                      w=["wbf"], q="gpsimd")

        def load_mod(m):
            P.dma(lambda e: e.dma_start(out=SH[:], in_=_bc(m[0], D)), w=["SH"])
            P.dma(lambda e: e.dma_start(out=G1[:], in_=_bc(m[1], D)), w=["G1"])
            P.v(lambda e: e.scalar_tensor_tensor(out=G1[:], in0=G1[:], scalar=1.0, in1=gn[:], op0=ALU.add, op1=ALU.mult),
                ["G1", "gn"], ["G1"])

        def hx_tile(t):
            xb = xt[t % 2]
            kx = ("xt", t % 2)
            hb = hxb[t % 2]
            hTb = hxTb[t % 2]
            kh = ("hx", t % 2)
            P.dma(lambda e: e.dma_start(out=xb[:], in_=xin[t * 128:(t + 1) * 128, :]), w=[kx])
            P.a(lambda e: e.activation(out=tmph[:], in_=xb[:], func=AF.Square, accum_out=ssh[:]), [kx], ["tmph", "ssh"])
            emit_rstd(P, ssh[:], rsh[:], D, ["ssh"], ["rsh"])
            P.v(lambda e: e.scalar_tensor_tensor(out=tmph[:], in0=xb[:], scalar=rsh[:, 0:1], in1=G1[:], op0=ALU.mult, op1=ALU.mult),
                [kx, "rsh", "G1"], ["tmph"])
            P.v(lambda e: e.tensor_tensor(out=hb[:], in0=tmph[:], in1=SH[:], op=ALU.add), ["tmph", "SH"], [kh])
            for kc in range(16):
                P.t(lambda e, kc=kc: e.transpose(pT[:, kc, :], hb[:, kc * 128:(kc + 1) * 128], identb[:]), [kh, "identb"], ["pT"])
            P.a(lambda e: e.activation(out=hTb[:], in_=pT[:], func=AF.Copy), ["pT"], [("hxT", t % 2)])

        def prep(t):
            if t == 0:
                load_mod(modc)
            if t == 2:
                load_mod(modx)
            hx_tile(t)

        def inproj(ncol, t):
            hTb = hxTb[t % 2]
            for g in range(ncol // 512):
                for kc in range(16):
                    P.t(lambda e, g=g, kc=kc: e.matmul(pP[:, g * 512:(g + 1) * 512], lhsT=hTb[:, kc, :],
                                                      rhs=wbf[:, kc, g * 512:(g + 1) * 512], start=(kc == 0), stop=(kc == 15)),
                        [("hxT", t % 2), "wbf"], ["pP"])

        def postA(t):
            P.a(lambda e: e.activation(out=Pb[:], in_=pP[:], func=AF.Copy), ["pP"], ["Pb"])
            Pv = Pb[:].rearrange("p (h d) -> p h d", h=NQK)
            P.dma(lambda e, t=t: e.dma_start(out=cst[:], in_=cs_t[t * 128:(t + 1) * 128, :]), w=["cst"])
            P.v(lambda e: e.tensor_tensor(out=tmp[:], in0=Pb[:], in1=Pb[:], op=ALU.mult), ["Pb"], ["tmp"])
            P.v(lambda e: e.tensor_reduce(out=ssq[:], in_=tmp[:].rearrange("p (h d) -> p h d", h=NQK), axis=AX.X, op=ALU.add),
                ["tmp"], ["ssq"])
            emit_rstd(P, ssq[:], rsq[:], 128, ["ssq"], ["rsq"])
            qnv = qn[:].rearrange("p h a b f -> p h (a b f)")
            P.v(lambda e: e.tensor_tensor(out=qnv, in0=Pv, in1=rsq[:].unsqueeze(2).to_broadcast([128, NQK, 128]), op=ALU.mult),
                ["Pb", "rsq"], ["qn"])
            P.v(lambda e: e.tensor_tensor(out=qnv, in0=qnv, in1=gq[:], op=ALU.mult), ["qn", "gq"], ["qn"])
            cosb = cst[:, 0:64].rearrange("p (a f) -> p a f", a=2).unsqueeze(1).to_broadcast([128, NQK, 2, 32])
            sinb = cst[:, 64:128].rearrange("p (a f) -> p a f", a=2).unsqueeze(1).to_broadcast([128, NQK, 2, 32])
            x1 = qn[:, :, :, 0, :]
            x2 = qn[:, :, :, 1, :]
            P.v(lambda e: e.tensor_tensor(out=rA[:], in0=x1, in1=cosb, op=ALU.mult), ["qn", "cst"], ["rA"])
            P.v(lambda e: e.tensor_tensor(out=rB[:], in0=x2, in1=sinb, op=ALU.mult), ["qn", "cst"], ["rB"])
            P.v(lambda e: e.tensor_tensor(out=qrb[t % 2][:, :, :, 0, :], in0=rA[:], in1=rB[:], op=ALU.subtract), ["rA", "rB"], [("qr", t % 2)])
            P.v(lambda e: e.tensor_tensor(out=rA[:], in0=x2, in1=cosb, op=ALU.mult), ["qn", "cst", ("qr", t % 2)], ["rA"])
            P.v(lambda e: e.tensor_tensor(out=rB[:], in0=x1, in1=sinb, op=ALU.mult), ["qn", "cst", ("qr", t % 2)], ["rB"])
            P.v(lambda e: e.tensor_tensor(out=qrb[t % 2][:, :, :, 1, :], in0=rA[:], in1=rB[:], op=ALU.add), ["rA", "rB"], [("qr", t % 2)])

        def storeA(t):
            qrv = qrb[t % 2][:].rearrange("p h a b f -> p h (a b f)")
            for h in range(NQK):
                P.t(lambda e, h=h: e.transpose(pQ[:, h, :], qrv[:, h, :], identb[:]), [("qr", t % 2), "identb"], ["pQ"])
            P.a(lambda e: e.activation(out=qT[:], in_=pQ[:], func=AF.Copy), ["pQ"], ["qT"])
            P.dma(lambda e, t=t: e.dma_start(out=QKT[:, :, t * 128:(t + 1) * 128], in_=qT[:]), ["qT"], [("QKT", t)], q="gpsimd")
            if fused and t >= 2:
                ktv = io["KTx"].rearrange("(h d) t -> d h t", d=128)
                c0 = (t - 2) * 128
                P.dma(lambda e, c0=c0: e.dma_start(out=ktv[:, 0:2, c0:c0 + 128], in_=qT[:, 6:8, :]), ["qT"], [("KTx", t, 0)], q="gpsimd")
                P.dma(lambda e, c0=c0: e.dma_start(out=ktv[:, 2:4, c0:c0 + 128], in_=qT[:, 14:16, :]), ["qT"], [("KTx", t, 1)], q="gpsimd")


        def postB(t):
            P.a(lambda e: e.activation(out=vb16[:], in_=pP[:, 0:512], func=AF.Copy), ["pP"], ["vb16"])
            P.dma(lambda e, t=t: e.dma_start(out=Vout[t * 128:(t + 1) * 128, :], in_=vb16[:]), ["vb16"], [("Vout", t)], q="gpsimd")
            if fused and t >= 2:
                P.dma(lambda e, t=t: e.dma_start(out=io["Vx"][(t - 2) * 128:(t - 1) * 128, :], in_=vb16[:]), ["vb16"], [("Vx", t)], q="gpsimd")
            P.a(lambda e: e.activation(out=gl[:], in_=pP[:, 512:1536], func=AF.Gelu_apprx_tanh), ["pP"], ["gl"])
            gv = gl[:, 512:1024]
            P.v(lambda e: e.tensor_tensor(out=t1[:, 0:512], in0=gv, in1=gv, op=ALU.mult), ["gl"], ["t1"])
            P.v(lambda e: e.tensor_reduce(out=ssv[:], in_=t1[:, 0:512].rearrange("p (g d) -> p g d", g=4), axis=AX.X, op=ALU.add),
                ["t1"], ["ssv"])
            emit_rstd(P, ssv[:], rsv[:], 128, ["ssv"], ["rsv"])
            P.v(lambda e: e.tensor_tensor(out=t1[:, 0:512].rearrange("p (g d) -> p g d", g=4), in0=gv.rearrange("p (g d) -> p g d", g=4),
                                          in1=rsv[:].unsqueeze(2).to_broadcast([128, 4, 128]), op=ALU.mult), ["gl", "rsv"], ["t1"])
            P.v(lambda e: e.tensor_tensor(out=vn[:].rearrange("p g d -> p (g d)"), in0=t1[:, 0:512], in1=vng[:], op=ALU.mult),
                ["t1", "vng"], ["vn"])
            for g in range(4):
                P.t(lambda e, g=g: e.matmul(pP[:, 1536 + g * 128:1536 + (g + 1) * 128], lhsT=wsbb[:, g, :], rhs=vn[:, g, :],
                                            start=True, stop=True), ["vn", "wsbb"], ["pM"])
            P.v(lambda e: e.tensor_tensor(out=ob[:].rearrange("p (g d) -> p g d", g=4),
                                          in0=pP[:, 1536:2048].rearrange("p (g d) -> p g d", g=4),
                                          in1=bsb[:].unsqueeze(2).to_broadcast([128, 4, 128]), op=ALU.add), ["pM", "bsb"], ["ob"])
            P.v(lambda e: e.tensor_tensor(out=ob[:], in0=ob[:], in1=gl[:, 0:512], op=ALU.mult), ["ob", "gl"], ["ob"])
            P.a(lambda e: e.activation(out=t2[:, 0:512], in_=ob[:], func=AF.Square, accum_out=ss[:]), ["ob"], ["t2", "ss"])
            emit_rstd(P, ss[:], rs[:], 512, ["ss"], ["rs"])
            P.v(lambda e: e.scalar_tensor_tensor(out=yb[:], in0=ob[:], scalar=rs[:, 0:1], in1=ogbt[:], op0=ALU.mult, op1=ALU.mult),
                ["ob", "rs", "ogbt"], ["yb"])
            P.dma(lambda e, t=t: e.dma_start(out=yB[t * 128:(t + 1) * 128, :], in_=yb[:]), ["yb"], [("yB", t)], q="gpsimd")

        load_w(0, 2048)
        prep(0)
        for t in range(NT):
            inproj(2048, t)
            if t + 1 < NT:
                prep(t + 1)
            if t >= 1:
                storeA(t - 1)
            postA(t)
        storeA(NT - 1)

        load_w(2048, 1536)
        prep(0)
        for t in range(NT):
            inproj(1536, t)
            if t + 1 < NT:
                prep(t + 1)
            postB(t)
        P.emit(fused=fused)
    return nc


NKA = 20
NKC = 34


def build_s2a(nc=None, io=None, pfx=""):
    if nc is None:
        nc = bass.Bass("TRN2", target_bir_lowering=False)
    fused, di, do = _mk(nc, io, pfx)
    QKT = di("QKT", [128, NQK, NTOK], BF16)
    KA = None if fused else di("KA", [128, 2, NKA * 128], BF16)
    VA = None if fused else di("VA", [NKA * 128, 256], BF16)
    KC = None if fused else di("KC", [128, 2, NKC * 128], BF16)
    VC = None if fused else di("VC", [NKC * 128, 256], BF16)
    msk = di("msk", [4, 128, 128])
    sink = di("sink", [6])
    gac = di("gac", [1536])
    yAC = do("yAC", [NTOK, 1536], BF16)
    with stage_ctx(nc, fused) as st:
        sb = lambda n, s, d=F32: st.enter_context(nc.sbuf_tensor(pfx + n, s, d))
        pt = lambda n, s, d=F32: st.enter_context(nc.psum_tensor(pfx + n, s, d))
        ka = sb("ka", [128, 2, NKA * 128], BF16)
        va = sb("va", [128, NKA, 2, 129], BF16)
        kc = sb("kc", [128, 2, NKC * 128], BF16)
        vc = sb("vc", [128, NKC, 2, 129], BF16)
        mf = sb("mf", [128, 4, 128])
        mb = sb("mb", [128, 4, 128], BF16)
        sk = sb("sk", [128, 6])
        es = sb("es", [128, 6])
        gt = sb("gt", [128, 1536])
        qt = [sb(f"qt{i}", [128, NQK, 128], BF16) for i in range(2)]
        PT = [sb(f"PT{i}", [128, 3, 128], BF16) for i in range(3)]
        oo = sb("oo", [128, 2, 6, 128])
        den = sb("den", [128, 3])
        sq = sb("sq", [128, 768])
        ss = sb("ss", [128, 1])
        rs = sb("rs", [128, 1])
        yt = sb("yt", [128, 1536], BF16)
        psS = [pt(f"psS{i}", [128, 512]) for i in range(3)]
        psO = [pt(f"psO{i}", [128, 3, 129]) for i in range(2)]
        P = Prog(nc)
        if not fused:
            P.dma(lambda e: e.dma_start(out=ka[:], in_=KA), w=["ka"])
            P.dma(lambda e: e.dma_start(out=kc[:], in_=KC), w=["kc"])
        else:
            KTg = io["KTg"].rearrange("(r h d) t -> d r h t", r=2, h=4)
            Vg = io["Vg"]
            Vo = io["Vout"]
            for k in range(2):
                P.dma(lambda e, k=k: e.dma_start(out=ka[:, k, 0:256], in_=QKT[:, 6 + k, 0:256]), w=["ka"])
                P.dma(lambda e, k=k: e.dma_start(out=ka[:, k, 256:384], in_=KTg[:, 0, k, 1920:2048]), w=["ka"])
                P.dma(lambda e, k=k: e.dma_start(out=ka[:, k, 384:2432], in_=QKT[:, 6 + k, 256:2304]), w=["ka"])
                P.dma(lambda e, k=k: e.dma_start(out=ka[:, k, 2432:2560], in_=KTg[:, 1, k, 0:128]), w=["ka"])
                P.dma(lambda e, k=k: e.dma_start(out=kc[:, k, 0:256], in_=QKT[:, 14 + k, 0:256]), w=["kc"])
                for r in range(2):
                    P.dma(lambda e, k=k, r=r: e.dma_start(out=kc[:, k, 256 + r * 2048:256 + (r + 1) * 2048], in_=KTg[:, r, 2 + k, :]), w=["kc"])
        P.v(lambda e: e.memset(va[:, :, :, 128:129], 1.0), [], ["va1"])
        P.v(lambda e: e.memset(vc[:, :, :, 128:129], 1.0), [], ["vc1"])
        for k in range(2):
            if not fused:
                P.dma(lambda e, k=k: e.dma_start(out=va[:, :, k, 0:128], in_=VA[:, k * 128:(k + 1) * 128].rearrange("(j s) d -> s j d", s=128)), w=[("va", k)])
                P.dma(lambda e, k=k: e.dma_start(out=vc[:, :, k, 0:128], in_=VC[:, k * 128:(k + 1) * 128].rearrange("(j s) d -> s j d", s=128)), w=[("vc", k)])
            else:
                ca = slice(k * 128, (k + 1) * 128)
                cc = slice(256 + k * 128, 256 + (k + 1) * 128)
                tl = lambda ap: ap.rearrange("(j s) d -> s j d", s=128)
                P.dma(lambda e, k=k, ca=ca: e.dma_start(out=va[:, 0:2, k, 0:128], in_=tl(Vo[0:256, ca])), w=[("va", k)])
                P.dma(lambda e, k=k, ca=ca: e.dma_start(out=va[:, 2:3, k, 0:128], in_=tl(Vg[1920:2048, ca])), w=[("va", k)])
                P.dma(lambda e, k=k, ca=ca: e.dma_start(out=va[:, 3:19, k, 0:128], in_=tl(Vo[256:2304, ca])), w=[("va", k)])
                P.dma(lambda e, k=k, ca=ca: e.dma_start(out=va[:, 19:20, k, 0:128], in_=tl(Vg[2048:2176, ca])), w=[("va", k)])
                P.dma(lambda e, k=k, cc=cc: e.dma_start(out=vc[:, 0:2, k, 0:128], in_=tl(Vo[0:256, cc])), w=[("vc", k)])
                P.dma(lambda e, k=k, cc=cc: e.dma_start(out=vc[:, 2:34, k, 0:128], in_=tl(Vg[:, cc])), w=[("vc", k)])
        P.dma(lambda e: e.dma_start(out=mf[:], in_=msk.rearrange("m s q -> s m q")), w=["mf"])
        P.v(lambda e: e.tensor_copy(out=mb[:], in_=mf[:]), ["mf"], ["mb"])
        P.dma(lambda e: e.dma_start(out=sk[:], in_=_bc(sink, 6)), w=["sk"])
        P.a(lambda e: e.activation(out=es[:], in_=sk[:], func=AF.Exp), ["sk"], ["es"])
        P.dma(lambda e: e.dma_start(out=gt[:], in_=_bc(gac, 1536)), w=["gt"])
        PF = 2
        NBUF = PF + 1
        steps = []
        for t in range(NT):
            if t < 2:
                keysA = [(0, None), (1, None)]
                keysC = [(0, None), (1, None)]
            else:
                i = t - 2
                keysA = [(0, None), (1, None), (2 + i, 2 if i == 0 else 0), (3 + i, None), (4 + i, 3 if i == 15 else 1)]
                keysC = [(j, None) for j in range(NKC)]
            for (oi, qbase, KT, VT, kkey, vkeys, keys, use_sink) in (
                    (0, 0, ka, va, "ka", [("va", 0), ("va", 1), "va1"], keysA, True),
                    (1, 8, kc, vc, "kc", [("vc", 0), ("vc", 1), "vc1"], keysC, False)):
                for k in range(2):
                    for n, (j, m) in enumerate(keys):
                        steps.append(dict(t=t, oi=oi, qbase=qbase, KT=KT, VT=VT, kkey=kkey, vkeys=vkeys, k=k, n=n, j=j, m=m,
                                          last=(n == len(keys) - 1), use_sink=use_sink,
                                          tile_first=(oi == 0 and k == 0 and n == 0), tile_last=(oi == 1 and k == 1 and n == len(keys) - 1)))
        ngrp = [0]

        def load_q(t):
            q = qt[t % 2]
            P.dma(lambda e, q=q, t=t: e.dma_start(out=q[:], in_=QKT[:, :, t * 128:(t + 1) * 128]), w=[("qt", t % 2)])

        def s1(i):
            sp = steps[i]
            t, k, j, m = sp["t"], sp["k"], sp["j"], sp["m"]
            if sp["tile_first"]:
                if t == 0:
                    load_q(0)
                if t + 1 < NT:
                    load_q(t + 1)
            q = qt[t % 2]
            b = i % NBUF
            KT, qbase = sp["KT"], sp["qbase"]
            pS = psS[b][:, 0:384].rearrange("p (g q) -> p g q", g=3)
            P.t(lambda e: e.matmul(pS, lhsT=KT[:, k, j * 128:(j + 1) * 128], rhs=q[:, qbase + 3 * k:qbase + 3 * k + 3, :], start=True, stop=True),
                [sp["kkey"], ("qt", t % 2)], [("psS", b)])
            P.a(lambda e: e.activation(out=PT[b][:], in_=pS, func=AF.Exp, scale=128.0 ** -0.5), [("psS", b)], [("PT", b)])
            if m is not None:
                P.v(lambda e: e.tensor_tensor(out=PT[b][:], in0=PT[b][:], in1=mb[:, m, :].unsqueeze(1).to_broadcast([128, 3, 128]), op=ALU.mult),
                    [("PT", b), "mb"], [("PT", b)])

        def s2(i):
            sp = steps[i]
            t, k, j, n, oi = sp["t"], sp["k"], sp["j"], sp["n"], sp["oi"]
            b = i % NBUF
            VT = sp["VT"]
            pb = ngrp[0] % 2
            po = psO[pb]
            for g in range(3):
                P.t(lambda e, g=g: e.matmul(po[:, g, :], lhsT=PT[b][:, g, :], rhs=VT[:, j, k, :], start=(n == 0 and g == 0), stop=sp["last"]),
                    [("PT", b)] + sp["vkeys"], [("psO", pb)])
            if sp["last"]:
                ngrp[0] += 1
                if sp["use_sink"]:
                    P.v(lambda e: e.tensor_tensor(out=den[:], in0=po[:, :, 128], in1=es[:, 3 * k:3 * k + 3], op=ALU.add), [("psO", pb), "es"], ["den"])
                else:
                    P.v(lambda e: e.tensor_copy(out=den[:], in_=po[:, :, 128]), [("psO", pb)], ["den"])
                P.v(lambda e: e.reciprocal(out=den[:], in_=den[:]), ["den"], ["den"])
                P.v(lambda e: e.tensor_tensor(out=oo[:, oi, 3 * k:3 * k + 3, :], in0=po[:, :, 0:128], in1=den[:].unsqueeze(2).to_broadcast([128, 3, 128]),
                                              op=ALU.mult), [("psO", pb), "den"], [("oo", oi)])
            if sp["tile_last"]:
                for o2 in range(2):
                    ov = oo[:, o2, :, :].rearrange("p h d -> p (h d)")
                    P.v(lambda e, ov=ov: e.tensor_tensor(out=sq[:], in0=ov, in1=ov, op=ALU.mult), [("oo", o2)], ["sq"])
                    P.v(lambda e: e.tensor_reduce(out=ss[:], in_=sq[:], axis=AX.X, op=ALU.add), ["sq"], ["ss"])
                    emit_rstd(P, ss[:], rs[:], 768, ["ss"], ["rs"])
                    P.v(lambda e, ov=ov, o2=o2: e.scalar_tensor_tensor(out=yt[:, o2 * 768:(o2 + 1) * 768], in0=ov, scalar=rs[:, 0:1],
                                                                      in1=gt[:, o2 * 768:(o2 + 1) * 768], op0=ALU.mult, op1=ALU.mult),
                        [("oo", o2), "rs", "gt"], ["yt"])
                P.dma(lambda e: e.dma_start(out=yAC[t * 128:(t + 1) * 128, :], in_=yt[:]), ["yt"], [("yAC", t)])

        for i in range(min(PF, len(steps))):
            s1(i)
        for i in range(len(steps)):
            if i + PF < len(steps):
                s1(i + PF)
            s2(i)
        P.emit(fused=fused)
    return nc


def build_s2b(nc=None, io=None, pfx=""):
    if nc is None:
        nc = bass.Bass("TRN2", target_bir_lowering=False)
    fused, di, do = _mk(nc, io, pfx)
    yAC = di("yAC", [NTOK, 1536], BF16)
    yB = di("yB", [NTOK, 512], BF16)
    xin = di("xin", [NTOK, D])
    modx = di("modx", [6, D])
    modc = di("modc", [6, D])
    wout = di("wout", [D, D])
    n2g = di("n2g", [D])
    wr = di("wr", [D, NE])
    ident_in = di("ident", [128, 128])
    x1o = do("x1o", [NTOK, D])
    hx2o = do("hx2o", [NTOK, D], BF16)
    affT = None if fused else do("affT", [NE, NTOK])
    with stage_ctx(nc, fused) as st:
        sb = lambda n, s, d=F32: st.enter_context(nc.sbuf_tensor(pfx + n, s, d))
        pt = lambda n, s, d=F32: st.enter_context(nc.psum_tensor(pfx + n, s, d))
        wbf = sb("wbf", [128, 16, D], BF16)
        G1g = sb("G1g", [128, D])
        G2 = sb("G2", [128, D])
        SH2 = sb("SH2", [128, D])
        gn = sb("gn", [128, D])
        ident = sb("ident_f", [128, 128])
        identb = sb("identb", [128, 128], BF16)
        wrs = sb("wrs", [128, 16, NE])
        xt = sb("xt", [128, D])
        yt = sb("yt", [128, D], BF16)
        yT = sb("yT", [128, 16, 128], BF16)
        tmp = sb("tmp", [128, D])
        x1 = sb("x1", [128, D])
        h2 = sb("h2", [128, D])
        hb = sb("hb", [128, D], BF16)
        h2T = sb("h2T", [128, 16, 128])
        ytb = [yt, sb("yt1", [128, D], BF16)]
        yTb = [yT, sb("yT1", [128, 16, 128], BF16)]
        xtb = [xt, sb("xt1", [128, D])]
        h2b = [h2, sb("h21", [128, D])]
        ss = sb("ss", [128, 1])
        rs = sb("rs", [128, 1])
        mx = sb("mx", [128, 1])
        se = sb("se", [128, 1])
        ex = sb("ex", [128, NE])
        af = sb("af", [128, NE])
        aT = sb("aT", [NE, 128])
        psT = pt("psT", [128, 16, 128], BF16)
        psX = [pt(f"psX{i}", [128, 512]) for i in range(2)]
        psR = [pt(f"psR{i}", [128, 4, 128]) for i in range(2)]
        psL = pt("psL", [128, 512])
        psA = pt("psA", [128, 512])
        P = Prog(nc)
        P.dma(lambda e: e.dma_start(out=ident[:], in_=ident_in), w=["ident"])
        P.v(lambda e: e.tensor_copy(out=identb[:], in_=ident[:]), ["ident"], ["identb"])
        P.dma(lambda e: e.dma_start(out=gn[:], in_=_bc(n2g, D)), w=["gn"])
        P.dma(lambda e: e.dma_start(out=wrs[:], in_=wr.rearrange("(kc p) n -> p kc n", p=128)), w=["wrs"])
        for k4 in range(4):
            src = wout[k4 * 512:(k4 + 1) * 512, :].rearrange("(kc p) n -> p kc n", p=128)
            P.dma(lambda e, k4=k4, src=src: e.dma_start(out=wbf[:, k4 * 4:(k4 + 1) * 4, :], in_=src), w=["wbf"], q="gpsimd")

        def load_mod(m):
            P.dma(lambda e: e.dma_start(out=G1g[:], in_=_bc(m[2], D)), w=["G1g"])
            P.dma(lambda e: e.dma_start(out=SH2[:], in_=_bc(m[3], D)), w=["SH2"])
            P.dma(lambda e: e.dma_start(out=G2[:], in_=_bc(m[4], D)), w=["G2"])
            P.v(lambda e: e.scalar_tensor_tensor(out=G2[:], in0=G2[:], scalar=1.0, in1=gn[:], op0=ALU.add, op1=ALU.mult),
                ["G2", "gn"], ["G2"])

        def stA(t):
            r0 = t * 128
            i = t % 2
            ytx, yTx, xtx = ytb[i], yTb[i], xtb[i]
            P.dma(lambda e: e.dma_start(out=ytx[:, 0:768], in_=yAC[r0:r0 + 128, 0:768]), w=[("yt", i, 0)])
            P.dma(lambda e: e.dma_start(out=ytx[:, 768:1280], in_=yB[r0:r0 + 128, :]), w=[("yt", i, 1)])
            P.dma(lambda e: e.dma_start(out=ytx[:, 1280:2048], in_=yAC[r0:r0 + 128, 768:1536]), w=[("yt", i, 2)])
            P.dma(lambda e: e.dma_start(out=xtx[:], in_=xin[r0:r0 + 128, :]), w=[("xt", i)])
            for kc in range(16):
                P.t(lambda e, kc=kc: e.transpose(psT[:, kc, :], ytx[:, kc * 128:(kc + 1) * 128], identb[:]),
                    [("yt", i, 0), ("yt", i, 1), ("yt", i, 2), "identb"], ["psT"])
            P.a(lambda e: e.activation(out=yTx[:], in_=psT[:], func=AF.Copy), ["psT"], [("yT", i)])

        def stB(t):
            r0 = t * 128
            i = t % 2
            yTx, xtx = yTb[i], xtb[i]
            for g in range(4):
                px = psX[g % 2]
                for kc in range(16):
                    P.t(lambda e, px=px, g=g, kc=kc: e.matmul(px[:], lhsT=yTx[:, kc, :], rhs=wbf[:, kc, g * 512:(g + 1) * 512],
                                                              start=(kc == 0), stop=(kc == 15)), [("yT", i), "wbf"], [("psX", g % 2)])
                P.v(lambda e, px=px, g=g: e.tensor_tensor(out=tmp[:, g * 512:(g + 1) * 512], in0=px[:], in1=G1g[:, g * 512:(g + 1) * 512],
                                                          op=ALU.mult), [("psX", g % 2), "G1g"], ["tmp"])
            P.v(lambda e: e.tensor_tensor(out=x1[:], in0=tmp[:], in1=xtx[:], op=ALU.add), ["tmp", ("xt", i)], ["x1"])
            P.dma(lambda e: e.dma_start(out=x1o[r0:r0 + 128, :], in_=x1[:]), ["x1"], [("x1o", t)], q="gpsimd")

        def stC1(t):
            r0 = t * 128
            hh = h2b[t % 2]
            kh = ("h2", t % 2)
            P.a(lambda e: e.activation(out=tmp[:], in_=x1[:], func=AF.Square, accum_out=ss[:]), ["x1"], ["tmp", "ss"])
            emit_rstd(P, ss[:], rs[:], D, ["ss"], ["rs"])
            P.v(lambda e: e.scalar_tensor_tensor(out=hh[:], in0=x1[:], scalar=rs[:, 0:1], in1=G2[:], op0=ALU.mult, op1=ALU.mult),
                ["x1", "rs", "G2"], [kh])
            P.v(lambda e: e.tensor_tensor(out=hh[:], in0=hh[:], in1=SH2[:], op=ALU.add), [kh, "SH2"], [kh])
            P.a(lambda e: e.activation(out=hb[:], in_=hh[:], func=AF.Copy), [kh], ["hb"])
            P.dma(lambda e: e.dma_start(out=hx2o[r0:r0 + 128, :], in_=hb[:]), ["hb"], [("hx2o", t)], q="gpsimd")

        def stC2(t):
            r0 = t * 128
            hh = h2b[t % 2]
            kh = ("h2", t % 2)
            for q4 in range(4):
                pr = psR[q4 % 2]
                for j in range(4):
                    kc = q4 * 4 + j
                    P.t(lambda e, pr=pr, j=j, kc=kc: e.transpose(pr[:, j, :], hh[:, kc * 128:(kc + 1) * 128], ident[:]),
                        [kh, "ident"], [("psR", q4 % 2)])
                P.a(lambda e, pr=pr, q4=q4: e.activation(out=h2T[:, q4 * 4:(q4 + 1) * 4, :], in_=pr[:], func=AF.Copy),
                    [("psR", q4 % 2)], ["h2T"])
            for kc in range(16):
                P.t(lambda e, kc=kc: e.matmul(psL[:, 0:NE], lhsT=h2T[:, kc, :], rhs=wrs[:, kc, :], start=(kc == 0), stop=(kc == 15)),
                    ["h2T", "wrs"], ["psL"])
            P.v(lambda e: e.tensor_reduce(out=mx[:], in_=psL[:, 0:NE], axis=AX.X, op=ALU.max), ["psL"], ["mx"])
            P.v(lambda e: e.tensor_scalar(out=mx[:], in0=mx[:], scalar1=-1.0, scalar2=None, op0=ALU.mult), ["mx"], ["mx"])
            P.a(lambda e: e.activation(out=ex[:], in_=psL[:, 0:NE], func=AF.Exp, bias=mx[:, 0:1], accum_out=se[:]), ["psL", "mx"], ["ex", "se"])
            P.v(lambda e: e.reciprocal(out=se[:], in_=se[:]), ["se"], ["se"])
            P.v(lambda e: e.tensor_scalar(out=af[:], in0=ex[:], scalar1=se[:, 0:1], scalar2=None, op0=ALU.mult), ["ex", "se"], ["af"])
            if fused:
                adst = io["affC"][r0:r0 + 128, :] if t < 2 else io["affIn"][r0 - 256:r0 - 128, :]
                P.dma(lambda e: e.dma_start(out=adst, in_=af[:]), ["af"], [("affo", t)], q="gpsimd")
            else:
                P.t(lambda e: e.transpose(psA[0:NE, 0:128], af[:], ident[:]), ["af", "ident"], ["psA"])
                P.a(lambda e: e.activation(out=aT[:], in_=psA[0:NE, 0:128], func=AF.Copy), ["psA"], ["aT"])
                P.dma(lambda e: e.dma_start(out=affT[:, r0:r0 + 128], in_=aT[:]), ["aT"], [("affT", t)])

        load_mod(modc)
        stA(0)
        for t in range(NT):
            if t == 2:
                load_mod(modx)
            stB(t)
            if t + 1 < NT:
                stA(t + 1)
            stC1(t)
            if t >= 1:
                stC2(t - 1)
        stC2(NT - 1)
        P.emit(fused=fused)
    return nc


NEL = 8
NROW = SEQ + CTX


def build_s3(with_ctx, nc=None, io=None, pfx=""):
    if nc is None:
        nc = bass.Bass("TRN2", target_bir_lowering=False)
    fused, di, do = _mk(nc, io, pfx)
    affL = di("affL", [128, 32, NEL])
    affC = di("affC", [128, 2, NEL])
    hx2 = di("hx2", [NROW, D], BF16)
    wg = di("wg", [NEL, D, D])
    wu = di("wu", [NEL, D, D])
    wd = di("wd", [NEL, D, D])
    consts = di("consts", [128, 128 + 128 + 128 + 512 + 34])
    delta = [do(f"delta{i}", [NROW, 512]) for i in range(4)]
    sets = [("L", 32, 512, 0, affL)]
    if with_ctx:
        sets.append(("C", 2, 32, SEQ, affC))
    with stage_ctx(nc, fused) as st:
        sb = lambda n, s, d=F32: st.enter_context(nc.sbuf_tensor(pfx + n, s, d))
        pt = lambda n, s, d=F32: st.enter_context(nc.psum_tensor(pfx + n, s, d))
        cst = sb("cst", [128, 930])
        identb = sb("identb", [128, 128], BF16)
        zt = sb("zt", [128, D])
        ring = [sb(f"ring{i}", [128, 16, 512], BF16) for i in range(4)]
        xg = [sb(f"xg{i}", [128, D], BF16) for i in range(2)]
        xgT = sb("xgT", [128, 16, 544], BF16)
        hT = sb("hT", [128, 16, 544], BF16)
        sil = sb("sil", [128, 544])
        yb = [sb(f"yb{i}", [128, 512]) for i in range(3)]
        S = {}
        for (nm, C, k, r0, _) in sets:
            S[nm] = dict(
                aff=sb(f"saff{nm}", [128, C, NEL]), lo=sb(f"lo{nm}", [128, NEL]), th=sb(f"th{nm}", [128, NEL, 15]),
                cmp=sb(f"cmp{nm}", [128, C, NEL, 15]), cntp=sb(f"cntp{nm}", [128, NEL, 15]), ge=sb(f"ge{nm}", [128, NEL, 15]),
                nge=sb(f"nge{nm}", [128, NEL]), mask=sb(f"mask{nm}", [128, C, NEL]), maskb=sb(f"maskb{nm}", [128, C, NEL], BF16),
                cA=sb(f"cA{nm}", [128, C, NEL]), cB=sb(f"cB{nm}", [128, C, NEL]), pos=sb(f"pos{nm}", [128, C, NEL]),
                vals=sb(f"vals{nm}", [128, C, NEL, 2]), oh=[sb(f"oh{nm}{i}", [128, k]) for i in range(2)],
                sl=sb(f"sl{nm}", [128, NEL, 4, 2]), idx=sb(f"idx{nm}", [128, NEL, 4], I32), gate=sb(f"gate{nm}", [128, NEL, 4]))
        jt = sb("jt", [128, NEL, 15])
        trib = sb("trib", [128, 128], BF16)
        onesb = sb("onesb", [128, 128], BF16)
        psA = [pt(f"psA{i}", [128, 512]) for i in range(2)]
        psU = [pt(f"psU{i}", [128, 512]) for i in range(2)]
        psT = pt("psT", [128, 16, 128], BF16)
        psY = [pt(f"psY{i}", [128, 512]) for i in range(2)]
        ident = cst[:, 0:128]
        ones = cst[:, 128:256]
        tri = cst[:, 256:384]
        iota = cst[:, 384:896]
        tokid = cst[:, 896:930]
        P = Prog(nc)
        P.dma(lambda e: e.dma_start(out=cst[:], in_=consts), w=["cst"])
        P.v(lambda e: e.tensor_copy(out=identb[:], in_=ident), ["cst"], ["identb"])
        P.v(lambda e: e.tensor_copy(out=trib[:], in_=tri), ["cst"], ["trib"])
        P.v(lambda e: e.tensor_copy(out=onesb[:], in_=ones), ["cst"], ["onesb"])
        for j in range(15):
            P.v(lambda e, j=j: e.memset(jt[:, :, j:j + 1], float(j + 1)), [], ["jt"])
        P.v(lambda e: e.memset(zt[:], 0.0), [], ["zt"])
        for cb in range(4):
            for r in range(NROW // 128):
                P.dma(lambda e, r=r, cb=cb: e.dma_start(out=delta[cb][r * 128:(r + 1) * 128, :], in_=zt[:, 0:512]), ["zt"], [("delta", cb)])

        def select(nm, C, k, r0, affd):
            s = S[nm]
            kk = lambda x: (nm, x)
            P.dma(lambda e, s=s, affd=affd: e.dma_start(out=s["aff"][:], in_=affd), w=[kk("aff")])
            P.v(lambda e, s=s: e.memset(s["lo"][:], 0.0), [], [kk("lo")])
            affb = s["aff"][:].unsqueeze(3).to_broadcast([128, C, NEL, 15])
            for it in range(8):
                step = 16.0 ** -(it + 1)
                P.v(lambda e, s=s, step=step: e.scalar_tensor_tensor(out=s["th"][:], in0=jt[:], scalar=step,
                                                                     in1=s["lo"][:].unsqueeze(2).to_broadcast([128, NEL, 15]),
                                                                     op0=ALU.mult, op1=ALU.add), ["jt", kk("lo")], [kk("th")])
                P.v(lambda e, s=s: e.tensor_tensor(out=s["cmp"][:], in0=affb, in1=s["th"][:].unsqueeze(1).to_broadcast([128, C, NEL, 15]),
                                                   op=ALU.is_ge), [kk("aff"), kk("th")], [kk("cmp")])
                P.v(lambda e, s=s: e.tensor_reduce(out=s["cntp"][:], in_=s["cmp"][:].rearrange("p c e j -> p e j c"), axis=AX.X, op=ALU.add),
                    [kk("cmp")], [kk("cntp")])
                P.t(lambda e, s=s: e.matmul(psY[0][:, 0:NEL * 15], lhsT=ones, rhs=s["cntp"][:].rearrange("p e j -> p (e j)"), start=True, stop=True),
                    ["cst", kk("cntp")], [("psY", 0)])
                P.v(lambda e, s=s, k=k: e.tensor_scalar(out=s["ge"][:].rearrange("p e j -> p (e j)"), in0=psY[0][:, 0:NEL * 15], scalar1=float(k) - 0.5,
                                                        scalar2=None, op0=ALU.is_ge), [("psY", 0)], [kk("ge")])
                P.v(lambda e, s=s: e.tensor_reduce(out=s["nge"][:], in_=s["ge"][:], axis=AX.X, op=ALU.add), [kk("ge")], [kk("nge")])
                P.v(lambda e, s=s, step=step: e.scalar_tensor_tensor(out=s["lo"][:], in0=s["nge"][:], scalar=step, in1=s["lo"][:],
                                                                     op0=ALU.mult, op1=ALU.add), [kk("nge"), kk("lo")], [kk("lo")])
            P.v(lambda e, s=s: e.tensor_tensor(out=s["mask"][:], in0=s["aff"][:], in1=s["lo"][:].unsqueeze(1).to_broadcast([128, C, NEL]),
                                               op=ALU.is_ge), [kk("aff"), kk("lo")], [kk("mask")])
            P.v(lambda e, s=s: e.tensor_copy(out=s["maskb"][:], in_=s["mask"][:]), [kk("mask")], [kk("maskb")])
            mb2 = s["maskb"][:].rearrange("p c e -> p (c e)")
            P.t(lambda e: e.matmul(psY[0][:, 0:C * NEL], lhsT=trib[:], rhs=mb2, start=True, stop=True), ["trib", kk("maskb")], [("psY", 0)])
            P.t(lambda e: e.matmul(psY[1][:, 0:C * NEL], lhsT=onesb[:], rhs=mb2, start=True, stop=True), ["onesb", kk("maskb")], [("psY", 1)])
            P.v(lambda e, s=s: e.tensor_copy(out=s["cA"][:].rearrange("p c e -> p (c e)"), in_=psY[1][:, 0:C * NEL]), [("psY", 1)], [kk("cA")])
            cur, nxt = "cA", "cB"
            sh = 1
            while sh < C:
                P.v(lambda e, s=s, cur=cur, nxt=nxt, sh=sh: e.tensor_copy(out=s[nxt][:, 0:sh, :], in_=s[cur][:, 0:sh, :]), [kk(cur)], [kk(nxt)])
                P.v(lambda e, s=s, cur=cur, nxt=nxt, sh=sh: e.tensor_tensor(out=s[nxt][:, sh:C, :], in0=s[cur][:, sh:C, :], in1=s[cur][:, 0:C - sh, :],
                                                                          op=ALU.add), [kk(cur)], [kk(nxt)])
                cur, nxt = nxt, cur
                sh *= 2
            P.v(lambda e, s=s, cur=cur: e.tensor_tensor(out=s["pos"][:].rearrange("p c e -> p (c e)"), in0=s[cur][:].rearrange("p c e -> p (c e)"),
                                                        in1=psY[1][:, 0:C * NEL], op=ALU.subtract), [kk(cur), ("psY", 1)], [kk("pos")])
            P.v(lambda e, s=s: e.tensor_tensor(out=s["pos"][:].rearrange("p c e -> p (c e)"), in0=s["pos"][:].rearrange("p c e -> p (c e)"),
                                               in1=psY[0][:, 0:C * NEL], op=ALU.add), [kk("pos"), ("psY", 0)], [kk("pos")])
            P.v(lambda e, s=s: e.tensor_tensor(out=s["pos"][:], in0=s["pos"][:], in1=s["mask"][:], op=ALU.mult), [kk("pos"), kk("mask")], [kk("pos")])
            P.v(lambda e, s=s: e.tensor_scalar(out=s["pos"][:], in0=s["pos"][:], scalar1=-1.0, scalar2=None, op0=ALU.add), [kk("pos")], [kk("pos")])
            P.v(lambda e, s=s, r0=r0: e.tensor_scalar(out=s["vals"][:, :, :, 0], in0=tokid[:, 0:C].unsqueeze(2).to_broadcast([128, C, NEL]),
                                                      scalar1=float(r0), scalar2=None, op0=ALU.add), ["cst"], [kk("vals")])
            P.v(lambda e, s=s: e.tensor_copy(out=s["vals"][:, :, :, 1], in_=s["aff"][:]), [kk("aff")], [kk("vals")])
            nst = (k + 127) // 128
            sw = min(k, 128)
            n_oh = 0
            for el in range(NEL):
                for c in range(C):
                    ohb = s["oh"][n_oh % 2]
                    kb = (nm, "oh", n_oh % 2)
                    n_oh += 1
                    P.v(lambda e, ohb=ohb, s=s, c=c, el=el, k=k: e.tensor_scalar(out=ohb[:], in0=iota[:, 0:k], scalar1=s["pos"][:, c, el:el + 1],
                                                                                scalar2=None, op0=ALU.is_equal), ["cst", kk("pos")], [kb])
                    for sti in range(nst):
                        P.t(lambda e, ohb=ohb, s=s, c=c, el=el, sti=sti, sw=sw: e.matmul(
                            psU[0][0:sw, (el * 4 + sti) * 2:(el * 4 + sti) * 2 + 2], lhsT=ohb[:, sti * 128:sti * 128 + sw],
                            rhs=s["vals"][:, c, el, :], start=(c == 0 and el == 0 and sti == 0), stop=(c == C - 1)),
                            [kb, kk("vals")], [("psU", 0)])
            P.v(lambda e, s=s, sw=sw: e.tensor_copy(out=s["sl"][0:sw].rearrange("p e s t -> p (e s t)"), in_=psU[0][0:sw, 0:NEL * 8]), [("psU", 0)], [kk("sl")])
            P.v(lambda e, s=s, sw=sw: e.tensor_copy(out=s["idx"][0:sw], in_=s["sl"][0:sw, :, :, 0]), [kk("sl")], [kk("idx")])
            P.v(lambda e, s=s, sw=sw: e.tensor_copy(out=s["gate"][0:sw], in_=s["sl"][0:sw, :, :, 1]), [kk("sl")], [kk("gate")])

        for sargs in sets:
            select(*sargs)

        tiles = [("L", sti, 128, sti * 128) for sti in range(4)]
        if with_ctx:
            tiles.append(("C", 0, 32, 512))
        ncols = 544 if with_ctx else 512
        ring_n = [0]
        ny = [0]

        def load_unit(wsrc, el, cb):
            rb = ring_n[0] % 4
            ring_n[0] += 1
            for k4 in range(4):
                src = wsrc[el, k4 * 512:(k4 + 1) * 512, cb * 512:(cb + 1) * 512].rearrange("(kc p) n -> p kc n", p=128)
                P.dma(lambda e, rb=rb, k4=k4, src=src: e.dma_start(out=ring[rb][:, k4 * 4:(k4 + 1) * 4, :], in_=src), w=[("ring", rb)], q="gpsimd")
            return rb

        for el in range(NEL):
            for ti, (nm, sti, n, c0) in enumerate(tiles):
                s = S[nm]
                xb = xg[ti % 2]
                kx = ("xg", ti % 2)
                P.op("gpsimd", lambda e, xb=xb, s=s, el=el, sti=sti, n=n: e.indirect_dma_start(
                    out=xb[0:n, :], out_offset=None, in_=hx2[:, :],
                    in_offset=bass.IndirectOffsetOnAxis(ap=s["idx"][0:n, el, sti:sti + 1], axis=0)), [(nm, "idx")], [kx], dma=True)
                for kc in range(16):
                    P.t(lambda e, xb=xb, kc=kc, n=n: e.transpose(psT[:, kc, 0:n], xb[0:n, kc * 128:(kc + 1) * 128], identb[0:n, 0:n]), [kx, "identb"], ["psT"])
                P.a(lambda e, c0=c0, n=n: e.activation(out=xgT[:, :, c0:c0 + n], in_=psT[:, :, 0:n], func=AF.Copy), ["psT"], ["xgT"])
            for cb in range(4):
                rg = load_unit(wg, el, cb)
                ru = load_unit(wu, el, cb)
                for f4 in range(4):
                    fc = cb * 4 + f4
                    pa = psA[fc % 2]
                    pu = psU[fc % 2]
                    for (pp, rbuf, key) in ((pa, rg, "psA"), (pu, ru, "psU")):
                        for kc in range(16):
                            P.t(lambda e, pp=pp, rbuf=rbuf, kc=kc, f4=f4: e.matmul(pp[:, 0:512], lhsT=ring[rbuf][:, kc, f4 * 128:(f4 + 1) * 128],
                                                                                  rhs=xgT[:, kc, 0:512], start=(kc == 0), stop=(kc == 15)),
                                [("ring", rbuf), "xgT"], [(key, fc % 2)])
                    P.a(lambda e, pa=pa: e.activation(out=sil[:, 0:512], in_=pa[:, 0:512], func=AF.Silu), [("psA", fc % 2)], ["sil"])
                    P.v(lambda e, pu=pu, fc=fc: e.tensor_tensor(out=hT[:, fc, 0:512], in0=sil[:, 0:512], in1=pu[:, 0:512], op=ALU.mult),
                        ["sil", ("psU", fc % 2)], ["hT"])
                    if with_ctx:
                        for (pp, rbuf, key) in ((pa, rg, "psA"), (pu, ru, "psU")):
                            for kc in range(16):
                                P.t(lambda e, pp=pp, rbuf=rbuf, kc=kc, f4=f4: e.matmul(pp[:, 0:32], lhsT=ring[rbuf][:, kc, f4 * 128:(f4 + 1) * 128],
                                                                                      rhs=xgT[:, kc, 512:544], start=(kc == 0), stop=(kc == 15)),
                                    [("ring", rbuf), "xgT"], [(key, fc % 2)])
                        P.a(lambda e, pa=pa: e.activation(out=sil[:, 512:544], in_=pa[:, 0:32], func=AF.Silu), [("psA", fc % 2)], ["sil"])
                        P.v(lambda e, pu=pu, fc=fc: e.tensor_tensor(out=hT[:, fc, 512:544], in0=sil[:, 512:544], in1=pu[:, 0:32], op=ALU.mult),
                            ["sil", ("psU", fc % 2)], ["hT"])
            for cb in range(4):
                rd = load_unit(wd, el, cb)
                for ti, (nm, sti, n, c0) in enumerate(tiles):
                    s = S[nm]
                    py = psY[ny[0] % 2]
                    ky = ("psY", ny[0] % 2)
                    ybuf = yb[ny[0] % 3]
                    kyb = ("yb", ny[0] % 3)
                    ny[0] += 1
                    for fc in range(16):
                        P.t(lambda e, py=py, rd=rd, fc=fc, c0=c0, n=n: e.matmul(py[0:n, :], lhsT=hT[:, fc, c0:c0 + n], rhs=ring[rd][:, fc, :],
                                                                               start=(fc == 0), stop=(fc == 15)), ["hT", ("ring", rd)], [ky])
                    P.v(lambda e, py=py, ybuf=ybuf, s=s, el=el, sti=sti, n=n: e.tensor_scalar(out=ybuf[0:n, :], in0=py[0:n, :],
                                                                                              scalar1=s["gate"][0:n, el, sti:sti + 1], scalar2=None, op0=ALU.mult),
                        [ky, (nm, "gate")], [kyb])
                    P.op("gpsimd", lambda e, ybuf=ybuf, s=s, el=el, sti=sti, n=n, cb=cb: e.indirect_dma_start(
                        out=delta[cb][:, :], out_offset=bass.IndirectOffsetOnAxis(ap=s["idx"][0:n, el, sti:sti + 1], axis=0),
                        in_=ybuf[0:n, :], in_offset=None, compute_op=ALU.add), [kyb, (nm, "idx"), ("delta", cb)], [("delta", cb)], dma=True)
        P.emit(fused=fused)
    return nc


def s3_consts():
    c = np.zeros((128, 930), np.float32)
    c[:, 0:128] = np.eye(128)
    c[:, 128:256] = 1.0
    c[:, 256:384] = np.triu(np.ones((128, 128)))
    c[:, 384:896] = np.arange(512)[None, :]
    c[:, 896:930] = np.arange(34)[None, :] * 128 + np.arange(128)[:, None]
    return c


def build_s4(nc=None, io=None, pfx=""):
    if nc is None:
        nc = bass.Bass("TRN2", target_bir_lowering=False)
    fused, di, do = _mk(nc, io, pfx)
    x1 = di("x1", [NTOK, D])
    dA = di("dA", [NTOK, D])
    dB = None if fused else di("dB", [NTOK, D])
    modx = di("modx", [6, D])
    modc = di("modc", [6, D])
    x2 = do("x2", [NTOK, D])
    with stage_ctx(nc, fused) as st:
        sb = lambda n, s, d=F32: st.enter_context(nc.sbuf_tensor(pfx + n, s, d))
        G = sb("G", [128, D])
        xa = [sb(f"xa{i}", [128, D]) for i in range(2)]
        da = [sb(f"da{i}", [128, D]) for i in range(2)]
        db = [sb(f"db{i}", [128, D]) for i in range(2)]
        P = Prog(nc)
        for t in range(NT):
            i = t % 2
            if t == 0:
                P.dma(lambda e: e.dma_start(out=G[:], in_=_bc(modc[5], D)), w=["G"])
            if t == 2:
                P.dma(lambda e: e.dma_start(out=G[:], in_=_bc(modx[5], D)), w=["G"])
            r0 = t * 128
            P.dma(lambda e, i=i, r0=r0: e.dma_start(out=xa[i][:], in_=x1[r0:r0 + 128, :]), w=[("xa", i)])
            P.dma(lambda e, i=i, r0=r0: e.dma_start(out=da[i][:], in_=dA[r0:r0 + 128, :]), w=[("da", i)])
            if not fused:
                P.dma(lambda e, i=i, r0=r0: e.dma_start(out=db[i][:], in_=dB[r0:r0 + 128, :]), w=[("db", i)])
                P.v(lambda e, i=i: e.tensor_tensor(out=da[i][:], in0=da[i][:], in1=db[i][:], op=ALU.add), [("da", i), ("db", i)], [("da", i)])
            P.v(lambda e, i=i: e.tensor_tensor(out=da[i][:], in0=da[i][:], in1=G[:], op=ALU.mult), [("da", i), "G"], [("da", i)])
            P.v(lambda e, i=i: e.tensor_tensor(out=xa[i][:], in0=xa[i][:], in1=da[i][:], op=ALU.add), [("xa", i), ("da", i)], [("xa", i)])
            P.dma(lambda e, i=i, r0=r0: e.dma_start(out=x2[r0:r0 + 128, :], in_=xa[i][:]), [("xa", i)], [("x2", t)], q="gpsimd")
        P.emit(fused=fused)
    return nc


def _rope_tables(h):
    t = np.arange(h * 2048, (h + 1) * 2048)
    row = (t // 64).astype(np.float32)
    col = (t % 64).astype(np.float32)
    inv = (np.float32(10000.0) ** (-np.arange(32, dtype=np.float32) / np.float32(32))).astype(np.float32)
    ar = row[:, None] * inv
    ac = col[:, None] * inv
    tab = np.zeros((NTOK, 128), np.float32)
    tab[:256, :64] = 1.0
    tab[256:, :64] = np.concatenate([np.cos(ar), np.cos(ac)], 1)
    tab[256:, 64:] = np.concatenate([np.sin(ar), np.sin(ac)], 1)
    return tab


def _masks(h):
    s = np.arange(128)[:, None]
    q = np.arange(128)[None, :]
    mp = (s >= q).astype(np.float32)
    mn = (s <= q).astype(np.float32)
    return np.stack([mp, mn, mp * (0.0 if h == 0 else 1.0), mn * (0.0 if h == 1 else 1.0)])


def _run(nc, in_maps):
    res = run_bass_kernel_spmd(nc, in_maps, core_ids=list(range(8)))
    return res.results


PAIRS = [[0, 1], [2, 3], [4, 5], [6, 7]]
NXR = NTOK + 128
CAPL = 384
NCST = 931


def f_consts():
    c = np.zeros((128, NCST), np.float32)
    c[:, 0:930] = s3_consts()
    c[:, 930] = NTOK + np.arange(128)
    return c


def stage_mod(nc, io, pfx):
    cin, w, b, modS = io["cin"], io["w_mod"], io["b_mod"], io["modS"]
    with stage_ctx(nc, True) as st:
        sb = lambda n, s, d=F32: st.enter_context(nc.sbuf_tensor(pfx + n, s, d))
        ct = sb("ct", [128, 16, 2])
        cs = sb("cs", [128, 16, 2])
        wt = [sb(f"wt{i}", [128, 16, 512]) for i in range(3)]
        bt = [sb(f"bt{i}", [2, 512]) for i in range(3)]
        ot = [sb(f"ot{i}", [2, 512]) for i in range(3)]
        ps = [st.enter_context(nc.psum_tensor(pfx + f"ps{i}", [2, 512], F32)) for i in range(2)]
        P = Prog(nc)
        P.dma(lambda e: e.dma_start(out=ct[:], in_=cin), w=["ct"])
        P.a(lambda e: e.activation(out=cs[:], in_=ct[:], func=AF.Silu), ["ct"], ["cs"])
        gi = 0
        for l in range(2):
            for g in range(24):
                i3 = gi % 3
                i2 = gi % 2
                gi += 1
                cols = slice(g * 512, (g + 1) * 512)
                src = w[l, :, cols].rearrange("(kc p) n -> p kc n", p=128)
                P.dma(lambda e, i3=i3, src=src: e.dma_start(out=wt[i3][:], in_=src), w=[("wt", i3)])
                P.dma(lambda e, i3=i3, l=l, cols=cols: e.dma_start(out=bt[i3][:], in_=b[l:l + 1, cols].to_broadcast([2, 512])), w=[("bt", i3)])
                for kc in range(16):
                    P.t(lambda e, i2=i2, i3=i3, kc=kc: e.matmul(ps[i2][:], lhsT=cs[:, kc, :], rhs=wt[i3][:, kc, :], start=(kc == 0), stop=(kc == 15)),
                        ["cs", ("wt", i3)], [("ps", i2)])
                P.v(lambda e, i2=i2, i3=i3: e.tensor_tensor(out=ot[i3][:], in0=ps[i2][:], in1=bt[i3][:], op=ALU.add), [("ps", i2), ("bt", i3)], [("ot", i3)])
                P.dma(lambda e, i3=i3, l=l, cols=cols: e.dma_start(out=modS[l, :, cols], in_=ot[i3][:]), [("ot", i3)], [("modS", l, g)])
        P.emit(fused=True)


def stage_cc(nc, pairs):
    with nc.cleanup_on_exit():
        _UID[0] += 1
        sem = nc.alloc_semaphore(name="cc%d" % _UID[0])
        with nc.Block() as block:
            @block.gpsimd
            def _(g):
                for i, (a, b) in enumerate(pairs):
                    g.collective_compute("AllGather", ALU.bypass, replica_groups=PAIRS, ins=[a], outs=[b]).then_inc(sem, 1)
                    g.wait_ge(sem, i + 1)
        nc.all_engine_barrier()


def stage_s3a(nc, io, with_ctx, pfx):
    affAll, affIn, affC, consts = io["affAll"], io["affIn"], io["affC"], io["consts"]
    idxT, gateT, delta, hx2 = io["idxT"], io["gateT"], io["delta"], io["hx2o"]
    with stage_ctx(nc, True) as st:
        sb = lambda n, s, d=F32: st.enter_context(nc.sbuf_tensor(pfx + n, s, d))
        pt = lambda n, s, d=F32: st.enter_context(nc.psum_tensor(pfx + n, s, d))
        cst = sb("cst", [128, NCST])
        trib = sb("trib", [128, 128], BF16)
        onesb = sb("onesb", [128, 128], BF16)
        jt = sb("jt", [128, NE, 15])
        zt = sb("zt", [128, D])
        ztb = sb("ztb", [128, D], BF16)
        sl = sb("sl", [128, NE, 4, 3])
        idxf = sb("idxf", [128, NE, 4])
        idx = sb("idx", [128, NE, 4], I32)
        gate = sb("gate", [128, NE, 4])
        psC = pt("psC", [128, 512])
        psP = [pt(f"psP{i}", [128, 512]) for i in range(2)]
        psS = [pt(f"psS{i}", [128, 512]) for i in range(2)]
        ones = cst[:, 128:256]
        tri = cst[:, 256:384]
        iota = cst[:, 384:896]
        tokid = cst[:, 896:930]
        dummy = cst[:, 930:931]
        P = Prog(nc)
        P.dma(lambda e: e.dma_start(out=cst[:], in_=consts), w=["cst"])
        P.v(lambda e: e.tensor_copy(out=trib[:], in_=tri), ["cst"], ["trib"])
        P.v(lambda e: e.tensor_copy(out=onesb[:], in_=ones), ["cst"], ["onesb"])
        for j in range(15):
            P.v(lambda e, j=j: e.memset(jt[:, :, j:j + 1], float(j + 1)), [], ["jt"])
        P.v(lambda e: e.memset(zt[:], 0.0), [], ["zt"])
        P.v(lambda e: e.memset(ztb[:], 0.0), [], ["ztb"])
        P.v(lambda e: e.memset(sl[:], 0.0), [], ["sl"])
        for r in range(NXR // 128):
            P.dma(lambda e, r=r: e.dma_start(out=delta[r * 128:(r + 1) * 128, :], in_=zt[:]), ["zt"], [("delta", r)])
        P.dma(lambda e: e.dma_start(out=hx2[NTOK:NXR, :], in_=ztb[:]), ["ztb"], ["hx2d"])

        def select(nm, Cth, thr_ap, Cm, mask_ap, k, r0, cap, st0, psSl):
            kk = lambda x: (nm, x)
            athr = sb(f"athr{nm}", [128, Cth, NE])
            am = athr if mask_ap is None else sb(f"am{nm}", [128, Cm, NE])
            kam = kk("athr") if mask_ap is None else kk("am")
            lo = sb(f"lo{nm}", [128, NE])
            th = sb(f"th{nm}", [128, NE, 15])
            cmpb = sb(f"cmp{nm}", [128, Cth, NE, 15], BF16)
            cntp = sb(f"cntp{nm}", [128, NE, 15])
            ge = sb(f"ge{nm}", [128, NE, 15])
            nge = sb(f"nge{nm}", [128, NE])
            mask = sb(f"mask{nm}", [128, Cm, NE])
            maskb = sb(f"maskb{nm}", [128, Cm, NE], BF16)
            cA = sb(f"cA{nm}", [128, Cm, NE])
            cB = sb(f"cB{nm}", [128, Cm, NE])
            pos = sb(f"pos{nm}", [128, Cm, NE])
            vals = sb(f"vals{nm}", [128, Cm, NE, 3])
            oh = [sb(f"oh{nm}{i}", [128, cap]) for i in range(2)]
            bufs = {"cA": cA, "cB": cB}
            P.dma(lambda e: e.dma_start(out=athr[:], in_=thr_ap.rearrange("(c p) e -> p c e", p=128)), w=[kk("athr")])
            if mask_ap is not None:
                P.dma(lambda e: e.dma_start(out=am[:], in_=mask_ap.rearrange("(c p) e -> p c e", p=128)), w=[kk("am")])
            P.v(lambda e: e.memset(lo[:], 0.0), [], [kk("lo")])
            affb = athr[:].unsqueeze(3).to_broadcast([128, Cth, NE, 15])
            for it in range(8):
                step = 16.0 ** -(it + 1)
                P.v(lambda e, step=step: e.scalar_tensor_tensor(out=th[:], in0=jt[:], scalar=step, in1=lo[:].unsqueeze(2).to_broadcast([128, NE, 15]),
                                                                op0=ALU.mult, op1=ALU.add), ["jt", kk("lo")], [kk("th")])
                P.v(lambda e: e.tensor_tensor(out=cmpb[:], in0=affb, in1=th[:].unsqueeze(1).to_broadcast([128, Cth, NE, 15]), op=ALU.is_ge),
                    [kk("athr"), kk("th")], [kk("cmp")])
                P.v(lambda e: e.tensor_reduce(out=cntp[:], in_=cmpb[:].rearrange("p c e j -> p e j c"), axis=AX.X, op=ALU.add), [kk("cmp")], [kk("cntp")])
                P.t(lambda e: e.matmul(psC[:, 0:NE * 15], lhsT=ones, rhs=cntp[:].rearrange("p e j -> p (e j)"), start=True, stop=True),
                    ["cst", kk("cntp")], ["psC"])
                P.v(lambda e: e.tensor_scalar(out=ge[:].rearrange("p e j -> p (e j)"), in0=psC[:, 0:NE * 15], scalar1=float(k) - 0.5, scalar2=None,
                                              op0=ALU.is_ge), ["psC"], [kk("ge")])
                P.v(lambda e: e.tensor_reduce(out=nge[:], in_=ge[:], axis=AX.X, op=ALU.add), [kk("ge")], [kk("nge")])
                P.v(lambda e, step=step: e.scalar_tensor_tensor(out=lo[:], in0=nge[:], scalar=step, in1=lo[:], op0=ALU.mult, op1=ALU.add),
                    [kk("nge"), kk("lo")], [kk("lo")])
            P.v(lambda e: e.tensor_tensor(out=mask[:], in0=am[:], in1=lo[:].unsqueeze(1).to_broadcast([128, Cm, NE]), op=ALU.is_ge),
                [kam, kk("lo")], [kk("mask")])
            P.v(lambda e: e.tensor_copy(out=maskb[:], in_=mask[:]), [kk("mask")], [kk("maskb")])
            mb2 = maskb[:].rearrange("p c e -> p (c e)")
            ncol = Cm * NE
            P.t(lambda e: e.matmul(psP[0][:, 0:ncol], lhsT=trib[:], rhs=mb2, start=True, stop=True), ["trib", kk("maskb")], [("psP", 0)])
            P.t(lambda e: e.matmul(psP[1][:, 0:ncol], lhsT=onesb[:], rhs=mb2, start=True, stop=True), ["onesb", kk("maskb")], [("psP", 1)])
            P.v(lambda e: e.tensor_copy(out=cA[:].rearrange("p c e -> p (c e)"), in_=psP[1][:, 0:ncol]), [("psP", 1)], [kk("cA")])
            cur, nxt = "cA", "cB"
            sh = 1
            while sh < Cm:
                P.v(lambda e, cur=cur, nxt=nxt, sh=sh: e.tensor_copy(out=bufs[nxt][:, 0:sh, :], in_=bufs[cur][:, 0:sh, :]), [kk(cur)], [kk(nxt)])
                P.v(lambda e, cur=cur, nxt=nxt, sh=sh: e.tensor_tensor(out=bufs[nxt][:, sh:Cm, :], in0=bufs[cur][:, sh:Cm, :], in1=bufs[cur][:, 0:Cm - sh, :],
                                                                  op=ALU.add), [kk(cur)], [kk(nxt)])
                cur, nxt = nxt, cur
                sh *= 2
            pos2 = pos[:].rearrange("p c e -> p (c e)")
            P.v(lambda e, cur=cur: e.tensor_tensor(out=pos2, in0=bufs[cur][:].rearrange("p c e -> p (c e)"), in1=psP[1][:, 0:ncol], op=ALU.subtract),
                [kk(cur), ("psP", 1)], [kk("pos")])
            P.v(lambda e: e.tensor_tensor(out=pos2, in0=pos2, in1=psP[0][:, 0:ncol], op=ALU.add), [kk("pos"), ("psP", 0)], [kk("pos")])
            P.v(lambda e: e.tensor_tensor(out=pos[:], in0=pos[:], in1=mask[:], op=ALU.mult), [kk("pos"), kk("mask")], [kk("pos")])
            P.v(lambda e: e.tensor_scalar(out=pos[:], in0=pos[:], scalar1=-1.0, scalar2=None, op0=ALU.add), [kk("pos")], [kk("pos")])
            P.v(lambda e: e.tensor_scalar(out=vals[:, :, :, 0], in0=tokid[:, 0:Cm].unsqueeze(2).to_broadcast([128, Cm, NE]), scalar1=float(r0), scalar2=None,
                                          op0=ALU.add), ["cst"], [kk("vals")])
            P.v(lambda e: e.tensor_copy(out=vals[:, :, :, 1], in_=am[:]), [kam], [kk("vals")])
            P.v(lambda e: e.memset(vals[:, :, :, 2], 1.0), [], [kk("vals")])
            nst = (cap + 127) // 128
            sw = min(cap, 128)
            n_oh = 0
            first = True
            for el in range(NE):
                for c in range(Cm):
                    ohb = oh[n_oh % 2]
                    kb = (nm, "oh", n_oh % 2)
                    n_oh += 1
                    P.v(lambda e, ohb=ohb, c=c, el=el: e.tensor_scalar(out=ohb[:], in0=iota[:, 0:cap], scalar1=pos[:, c, el:el + 1], scalar2=None,
                                                                      op0=ALU.is_equal), ["cst", kk("pos")], [kb])
                    for sti in range(nst):
                        col = (el * 4 + st0 + sti) * 3
                        P.t(lambda e, ohb=ohb, c=c, el=el, sti=sti, col=col, first=first: e.matmul(
                            psSl[0:sw, col:col + 3], lhsT=ohb[:, sti * 128:sti * 128 + sw], rhs=vals[:, c, el, :],
                            start=first, stop=(c == Cm - 1)), [kb, kk("vals")], [kk("psSl")])
                        first = False
            for sti in range(nst):
                P.v(lambda e, sti=sti: e.tensor_copy(out=sl[0:sw, :, st0 + sti, :],
                                                     in_=psSl[0:sw, 0:NE * 12].rearrange("p (e s t) -> p e s t", e=NE, s=4)[:, :, st0 + sti, :]),
                    [kk("psSl")], ["sl"])

        select("L", 32, affAll, 16, affIn, 512, 256, CAPL, 0, psS[0])
        if with_ctx:
            select("C", 2, affC, 2, None, 32, 0, 32, 3, psS[1])
        P.v(lambda e: e.tensor_scalar(out=idxf[:], in0=sl[:, :, :, 2], scalar1=-1.0, scalar2=dummy, op0=ALU.add, op1=ALU.mult), ["sl", "cst"], ["idxf"])
        P.v(lambda e: e.tensor_tensor(out=idxf[:], in0=sl[:, :, :, 0], in1=idxf[:], op=ALU.subtract), ["sl", "idxf"], ["idxf"])
        P.v(lambda e: e.tensor_copy(out=idx[:], in_=idxf[:]), ["idxf"], ["idx"])
        P.v(lambda e: e.tensor_copy(out=gate[:], in_=sl[:, :, :, 1]), ["sl"], ["gate"])
        P.dma(lambda e: e.dma_start(out=idxT, in_=idx[:]), ["idx"], ["idxT"])
        P.dma(lambda e: e.dma_start(out=gateT, in_=gate[:]), ["gate"], ["gateT"])
        P.emit(fused=True)


def stage_s3b(nc, io, L, with_ctx, pfx):
    idxT, gateT, delta, hx2, consts = io["idxT"], io["gateT"], io["delta"], io["hx2o"], io["consts"]
    wg, wu, wd = io["w_gate"], io["w_up"], io["w_down"]
    with stage_ctx(nc, True) as st:
        sb = lambda n, s, d=F32: st.enter_context(nc.sbuf_tensor(pfx + n, s, d))
        pt = lambda n, s, d=F32: st.enter_context(nc.psum_tensor(pfx + n, s, d))
        ncols = CAPL + (32 if with_ctx else 0)
        cst = sb("cst", [128, 128])
        identb = sb("identb", [128, 128], BF16)
        idx = sb("idx", [128, NE, 4], I32)
        gate = sb("gate", [128, NE, 4])
        NR, NSTG, PFD = 5, 6, 3
        ring = [sb(f"ring{i}", [128, 16, 512], BF16) for i in range(NR)]
        stg = [sb(f"stg{i}", [128, 4, 512]) for i in range(NSTG)]
        xg = [sb(f"xg{i}", [128, D], BF16) for i in range(2)]
        xgT = sb("xgT", [128, 16, ncols], BF16)
        hT = sb("hT", [128, 16, ncols], BF16)
        sil = sb("sil", [128, ncols])
        ybig = [sb(f"ybig{i}", [128, D]) for i in range(4)]
        psA = [pt(f"psA{i}", [128, 512]) for i in range(2)]
        psU = [pt(f"psU{i}", [128, 512]) for i in range(2)]
        psT = pt("psT", [128, 16, 128], BF16)
        psY = [pt(f"psY{i}", [128, 512]) for i in range(2)]
        P = Prog(nc)
        P.dma(lambda e: e.dma_start(out=cst[:], in_=consts[:, 0:128]), w=["cst"])
        P.v(lambda e: e.tensor_copy(out=identb[:], in_=cst[:]), ["cst"], ["identb"])
        P.dma(lambda e: e.dma_start(out=idx[:], in_=idxT), w=["idx"])
        P.dma(lambda e: e.dma_start(out=gate[:], in_=gateT), w=["gate"])
        tiles = [(sti, 128, sti * 128) for sti in range(3)]
        if with_ctx:
            tiles.append((3, 32, CAPL))
        ny = [0]
        seq = []
        for el_ in range(NE):
            for cb_ in range(4):
                seq.append((wg, el_, cb_))
                seq.append((wu, el_, cb_))
            for cb_ in range(4):
                seq.append((wd, el_, cb_))
        issued = [0]
        nq = [0]
        cast_eng = ["scalar", "vector", "scalar", "vector"]

        def issue(i):
            wsrc, el_, cb_ = seq[i]
            rb = i % NR
            for k4 in range(4):
                si = nq[0] % NSTG
                ce = cast_eng[nq[0] % 4]
                nq[0] += 1
                src = wsrc[L, el_, k4 * 512:(k4 + 1) * 512, cb_ * 512:(cb_ + 1) * 512].rearrange("(kc p) n -> p kc n", p=128)
                P.dma(lambda e, si=si, src=src: e.dma_start(out=stg[si][:], in_=src), w=[("stg", si)])
                dst = ring[rb][:, k4 * 4:(k4 + 1) * 4, :]
                if ce == "scalar":
                    P.op("scalar", lambda e, si=si, dst=dst: e.activation(out=dst, in_=stg[si][:], func=AF.Copy), [("stg", si)], [("ring", rb, k4)])
                else:
                    P.op(ce, lambda e, si=si, dst=dst: e.tensor_copy(out=dst, in_=stg[si][:]), [("stg", si)], [("ring", rb, k4)])

        def need(i):
            while issued[0] < min(len(seq), i + PFD + 1):
                issue(issued[0])
                issued[0] += 1
            return i % NR

        un = [0]

        def load_unit(wsrc, el, cb):
            i = un[0]
            un[0] += 1
            assert seq[i][1] == el and seq[i][2] == cb and seq[i][0] is wsrc
            return need(i)

        for el in range(NE):
            for ti, (sti, n, c0) in enumerate(tiles):
                xb = xg[ti % 2]
                kx = ("xg", ti % 2)
                P.op("gpsimd", lambda e, xb=xb, el=el, sti=sti, n=n: e.indirect_dma_start(
                    out=xb[0:n, :], out_offset=None, in_=hx2[:, :],
                    in_offset=bass.IndirectOffsetOnAxis(ap=idx[0:n, el, sti:sti + 1], axis=0)), ["idx"], [kx], dma=True)
                for kc in range(16):
                    P.t(lambda e, xb=xb, kc=kc, n=n: e.transpose(psT[:, kc, 0:n], xb[0:n, kc * 128:(kc + 1) * 128], identb[0:n, 0:n]), [kx, "identb"], ["psT"])
                P.a(lambda e, c0=c0, n=n: e.activation(out=xgT[:, :, c0:c0 + n], in_=psT[:, :, 0:n], func=AF.Copy), ["psT"], ["xgT"])
            for cb in range(4):
                rg = load_unit(wg, el, cb)
                ru = load_unit(wu, el, cb)
                for f4 in range(4):
                    fc = cb * 4 + f4
                    pa = psA[fc % 2]
                    pu = psU[fc % 2]
                    for (pp, rbuf, key) in ((pa, rg, "psA"), (pu, ru, "psU")):
                        for kc in range(16):
                            P.t(lambda e, pp=pp, rbuf=rbuf, kc=kc, f4=f4: e.matmul(pp[:, 0:ncols], lhsT=ring[rbuf][:, kc, f4 * 128:(f4 + 1) * 128],
                                                                                  rhs=xgT[:, kc, :], start=(kc == 0), stop=(kc == 15)),
                                [("ring", rbuf, kc // 4), "xgT"], [(key, fc % 2)])
                    P.a(lambda e, pa=pa: e.activation(out=sil[:], in_=pa[:, 0:ncols], func=AF.Silu), [("psA", fc % 2)], ["sil"])
                    P.v(lambda e, pu=pu, fc=fc: e.tensor_tensor(out=hT[:, fc, :], in0=sil[:], in1=pu[:, 0:ncols], op=ALU.mult),
                        ["sil", ("psU", fc % 2)], ["hT"])
            for cb in range(4):
                rd = load_unit(wd, el, cb)
                for ti, (sti, n, c0) in enumerate(tiles):
                    py = psY[ny[0] % 2]
                    ky = ("psY", ny[0] % 2)
                    ny[0] += 1
                    for fc in range(16):
                        P.t(lambda e, py=py, rd=rd, fc=fc, c0=c0, n=n: e.matmul(py[0:n, :], lhsT=hT[:, fc, c0:c0 + n], rhs=ring[rd][:, fc, :],
                                                                               start=(fc == 0), stop=(fc == 15)), ["hT", ("ring", rd, fc // 4)], [ky])
                    P.v(lambda e, py=py, ti=ti, el=el, sti=sti, n=n, cb=cb: e.tensor_scalar(out=ybig[ti][0:n, cb * 512:(cb + 1) * 512], in0=py[0:n, :],
                                                                                           scalar1=gate[0:n, el, sti:sti + 1], scalar2=None, op0=ALU.mult),
                        [ky, "gate"], [("ybig", ti)])
            for ti, (sti, n, c0) in enumerate(tiles):
                P.op("gpsimd", lambda e, ti=ti, el=el, sti=sti, n=n: e.indirect_dma_start(
                    out=delta[:, :], out_offset=bass.IndirectOffsetOnAxis(ap=idx[0:n, el, sti:sti + 1], axis=0),
                    in_=ybig[ti][0:n, :], in_offset=None, compute_op=ALU.add), [("ybig", ti), "idx", "delta"], ["delta"], dma=True)
        P.emit(fused=True)


def stage_copy_out(nc, src, dst, pfx):
    with stage_ctx(nc, True) as st:
        bufs = [st.enter_context(nc.sbuf_tensor(pfx + f"cb{i}", [128, D], F32)) for i in range(3)]
        P = Prog(nc)
        for t in range(16):
            b = bufs[t % 3]
            P.dma(lambda e, b=b, t=t: e.dma_start(out=b[:], in_=src[256 + t * 128:256 + (t + 1) * 128, :]), w=[("cb", t % 3)])
            P.dma(lambda e, b=b, t=t: e.dma_start(out=dst[t * 128:(t + 1) * 128, :], in_=b[:]), [("cb", t % 3)], [("out", t)])
        P.emit(fused=True)


def build_fused(ncores=8):
    global PAIRS
    PAIRS = [[2 * i, 2 * i + 1] for i in range(ncores // 2)]
    nc = bass.Bass("TRN2", target_bir_lowering=False)
    ein = lambda n, s, d=F32: nc.dram_tensor(n, s, d, kind="ExternalInput").ap()
    scr = lambda n, s, d=F32: nc.dram_tensor(n, s, d, kind="Internal").ap()
    scrl = lambda n, s, d=F32: nc.dram_tensor(n, s, d, kind="Internal", addr_space="Local").ap()
    X = {}
    X["xin0"] = ein("xin", [NTOK, D])
    X["cin"] = ein("cin", [128, 16, 2])
    X["w_mod"] = ein("w_mod", [2, D, 6 * D])
    X["b_mod"] = ein("b_mod", [2, 6 * D])
    n1g = ein("n1g", [2, D]); n2g = ein("n2g", [2, D])
    win = ein("win", [2, D, 3584])
    gqk = ein("gqk", [2, NQK * 128])
    X["cs_t"] = ein("cs_t", [NTOK, 128])
    vnb = ein("vnb", [2, 512]); wsT = ein("wsT", [2, 128, 4, 128]); bsT = ein("bsT", [2, 128, 4]); ogb = ein("ogb", [2, 512])
    X["ident"] = ein("ident", [128, 128])
    X["msk"] = ein("msk", [4, 128, 128])
    sink = ein("sink", [2, 6]); gac = ein("gac", [2, 1536])
    wout = ein("wout", [2, D, D]); wr = ein("wr", [2, D, NE])
    X["w_gate"] = ein("w_gate", [2, NE, D, D]); X["w_up"] = ein("w_up", [2, NE, D, D]); X["w_down"] = ein("w_down", [2, NE, D, D])
    X["consts"] = ein("consts", [128, NCST])
    out = nc.dram_tensor("out", [2048, D], F32, kind="ExternalOutput").ap()
    modS = scr("modS", [2, 2, 6 * D])
    X["modS"] = modS
    X["QKT"] = scr("QKT", [128, NQK, NTOK], BF16); X["Vout"] = scr("Vout", [NTOK, 512], BF16)
    X["yB"] = scr("yB", [NTOK, 512], BF16); X["yAC"] = scr("yAC", [NTOK, 1536], BF16)
    X["KTx"] = scr("KTx", [512, 2048], BF16); X["KTg"] = scrl("KTg", [1024, 2048], BF16)
    X["Vx"] = scr("Vx", [2048, 512], BF16); X["Vg"] = scrl("Vg", [4096, 512], BF16)
    xs = scr("xs", [NXR, D]); X["hx2o"] = scr("hx2s", [NXR, D], BF16); X["delta"] = scr("delta", [NXR, D])
    X["affIn"] = scr("affIn", [2048, NE]); X["affC"] = scr("affC", [256, NE]); X["affAll"] = scrl("affAll", [4096, NE])
    X["idxT"] = scr("idxT", [128, NE, 4], I32); X["gateT"] = scr("gateT", [128, NE, 4])

    stage_mod(nc, X, "m_")
    for L in range(2):
        io = dict(X)
        io["xin"] = X["xin0"] if L == 0 else xs
        io["modx"] = modS[L, 0].rearrange("(s d) -> s d", s=6)
        io["modc"] = modS[L, 1].rearrange("(s d) -> s d", s=6)
        io.update(n1g=n1g[L], n2g=n2g[L], win=win[L], gqk=gqk[L], vnb=vnb[L], wsT=wsT[L], bsT=bsT[L], ogb=ogb[L],
                  sink=sink[L], gac=gac[L], wout=wout[L], wr=wr[L], x1o=xs, x1=xs, dA=X["delta"], x2=xs)
        build_s1(nc=nc, io=io, pfx=f"L{L}a_")
        stage_cc(nc, [(X["KTx"], X["KTg"]), (X["Vx"], X["Vg"])])
        build_s2a(nc=nc, io=io, pfx=f"L{L}b_")
        build_s2b(nc=nc, io=io, pfx=f"L{L}c_")
        stage_cc(nc, [(X["affIn"], X["affAll"])])
        stage_s3a(nc, io, L == 0, f"L{L}d_")
        stage_s3b(nc, io, L, L == 0, f"L{L}e_")
        build_s4(nc=nc, io=io, pfx=f"L{L}f_")
    stage_copy_out(nc, xs, out, "o_")
    return nc


def kernel(x, c, ctx, c_ctx, w_mod, b_mod, norm1_g, norm2_g, w_in, qn_a, kn_a, sink_a, vn_b,
           w_s, b_s, qn_c, kn_c, out_g, w_out, w_router, w_gate, w_up, w_down):
    f32 = np.float32
    A = lambda a: np.ascontiguousarray(np.asarray(a, f32))
    x = A(x); ctx = A(ctx); c = A(c); c_ctx = A(c_ctx)
    cores = [(k // 2, k % 2) for k in range(8)]
    og = A(out_g)
    shared = {
        "w_mod": A(w_mod), "b_mod": A(b_mod), "n1g": A(norm1_g), "n2g": A(norm2_g),
        "win": A(np.asarray(w_in, f32)[:, :, WIN_PERM]),
        "gqk": A(np.stack([np.concatenate([np.tile(qn_a[L], 6), np.tile(kn_a[L], 2), np.tile(qn_c[L], 6), np.tile(kn_c[L], 2)]) for L in range(2)])),
        "vnb": A(vn_b), "wsT": A(np.asarray(w_s, f32).transpose(0, 3, 1, 2)), "bsT": A(np.asarray(b_s, f32).transpose(0, 2, 1)),
        "ogb": A(og[:, 768:1280]), "ident": np.eye(128, dtype=f32), "sink": A(sink_a),
        "gac": A(np.concatenate([og[:, :768], og[:, 1280:]], 1)), "wout": A(w_out), "wr": A(w_router),
        "w_gate": A(w_gate), "w_up": A(w_up), "w_down": A(w_down), "consts": f_consts(),
    }
    ims = []
    for (b, h) in cores:
        d = dict(shared)
        d["xin"] = A(np.concatenate([ctx[b], x[b, h * 2048:(h + 1) * 2048]], 0))
        c2 = np.stack([c[b], c_ctx], 0)
        d["cin"] = A(c2.T.reshape(16, 128, 2).transpose(1, 0, 2))
        d["cs_t"] = _rope_tables(h)
        d["msk"] = _masks(h)
        ims.append(d)
    ncores = _NCORES[0]
    nc = build_fused(ncores)
    res = run_bass_kernel_spmd(nc, ims[:ncores], core_ids=list(range(ncores)))
    out = np.zeros((4, SEQ, D), f32)
    for k, (b, h) in enumerate(cores[:ncores]):
        out[b, h * 2048:(h + 1) * 2048] = res.results[k]["out"]
    return out


_NCORES = [8]
```

```python
import contextlib
import numpy as np
import ml_dtypes
import concourse.bass as bass
import concourse.mybir as mybir
from concourse.bass_utils import run_bass_kernel_spmd

F32 = mybir.dt.float32
BF16 = mybir.dt.bfloat16
I32 = mybir.dt.int32
AF = mybir.ActivationFunctionType
ALU = mybir.AluOpType
AX = mybir.AxisListType

D = 2048
SEQ = 4096
CTX = 256
NT = 18
NTOK = NT * 128
EPS = 1e-6
NE = 16

ENGS = ("sync", "scalar", "vector", "gpsimd", "tensor")
SEM_CHUNK = 3000
NDMA_SEMS = 12


class _Op:
    __slots__ = ("eng", "fn", "deps", "is_dma", "has_dep", "sem", "val", "prev_dma")

    def __init__(self, eng, fn, is_dma):
        self.eng = eng
        self.fn = fn
        self.is_dma = is_dma
        self.deps = []
        self.has_dep = False
        self.sem = None
        self.val = None
        self.prev_dma = None


class Prog:
    def __init__(self, nc):
        self.nc = nc
        self.ops = {e: [] for e in ENGS}
        self.last_w = {}
        self.readers = {}
        self.ndma = {e: 0 for e in ENGS}
        self.dma_last = {}
        self.all_dmas = []

    def op(self, eng, fn, reads=(), writes=(), dma=False):
        o = _Op(eng, fn, dma)
        deps = []
        for r in reads:
            w = self.last_w.get(r)
            if w is not None:
                deps.append((w, "raw"))
        for k in writes:
            w = self.last_w.get(k)
            if w is not None:
                deps.append((w, "waw"))
            for rd in self.readers.get(k, ()):
                deps.append((rd, "war"))
        for (d, kind) in deps:
            if d is o:
                continue
            if not d.is_dma and not dma and d.eng == eng:
                if kind != "raw" or eng == "tensor":
                    continue
            if d not in o.deps:
                o.deps.append(d)
                d.has_dep = True
        for r in reads:
            self.readers.setdefault(r, []).append(o)
        for k in writes:
            self.last_w[k] = o
            self.readers[k] = []
        if dma:
            j = self.ndma[eng]
            self.ndma[eng] += 1
            slot = (eng, j % NDMA_SEMS)
            o.prev_dma = self.dma_last.get(slot)
            self.dma_last[slot] = o
            o.sem = slot
            o.val = 16 * (j // NDMA_SEMS + 1)
            self.all_dmas.append(o)
        self.ops[eng].append(o)
        return o

    def v(self, fn, r=(), w=()):
        return self.op("vector", fn, r, w)

    def a(self, fn, r=(), w=()):
        return self.op("scalar", fn, r, w)

    def g(self, fn, r=(), w=()):
        return self.op("gpsimd", fn, r, w)

    def t(self, fn, r=(), w=()):
        return self.op("tensor", fn, r, w)

    def dma(self, fn, r=(), w=(), q="sync"):
        return self.op(q, fn, r, w, dma=True)

    def emit(self, final_wait_eng="sync", fused=False):
        nc = self.nc
        sem_names = set()
        for e in ENGS:
            cnt = 0
            for o in self.ops[e]:
                if o.is_dma:
                    sem_names.add(o.sem)
                    continue
                if o.has_dep:
                    o.sem = (e, "c", cnt // SEM_CHUNK)
                    o.val = cnt % SEM_CHUNK + 1
                    sem_names.add(o.sem)
                    cnt += 1
        sem_names = sorted(sem_names, key=str)
        with contextlib.ExitStack() as st:
            sems = {}
            for n in sem_names:
                if fused:
                    _UID[0] += 1
                    sems[n] = nc.alloc_semaphore(name="f%d_" % _UID[0] + "_".join(str(x) for x in n))
                else:
                    sems[n] = st.enter_context(nc.semaphore("s_" + "_".join(str(x) for x in n)))
            block = st.enter_context(nc.Block())
            prog = self

            def run(e, eng):
                seen = {}
                for o in prog.ops[e]:
                    waits = []
                    if o.is_dma and o.prev_dma is not None:
                        waits.append(o.prev_dma)
                    waits.extend(o.deps)
                    for d in waits:
                        if seen.get(d.sem, 0) >= d.val:
                            continue
                        eng.wait_ge(sems[d.sem], d.val)
                        seen[d.sem] = d.val
                    ins = o.fn(eng)
                    if o.is_dma:
                        ins.then_inc(sems[o.sem], 16)
                    elif o.has_dep:
                        ins.then_inc(sems[o.sem], 1)
                if e == final_wait_eng:
                    last = {}
                    for o in prog.all_dmas:
                        last[o.sem] = max(last.get(o.sem, 0), o.val)
                    for s, v in last.items():
                        if seen.get(s, 0) < v:
                            eng.wait_ge(sems[s], v)

            @block.sync
            def _(eng):
                run("sync", eng)

            @block.scalar
            def _(eng):
                run("scalar", eng)

            @block.vector
            def _(eng):
                run("vector", eng)

            @block.gpsimd
            def _(eng):
                run("gpsimd", eng)

            @block.tensor
            def _(eng):
                run("tensor", eng)


_UID = [0]


@contextlib.contextmanager
def stage_ctx(nc, fused):
    if not fused:
        with contextlib.ExitStack() as st:
            yield st
    else:
        with nc.cleanup_on_exit():
            with contextlib.ExitStack() as st:
                yield st
            nc.all_engine_barrier()


def _mk(nc, io, pfx):
    fused = io is not None
    if fused:
        di = lambda n, s, d=F32: io[n]
        do = lambda n, s, d=F32: io[n]
    else:
        di = lambda n, s, d=F32: nc.dram_tensor(n, s, d, kind="ExternalInput").ap()
        do = lambda n, s, d=F32: nc.dram_tensor(n, s, d, kind="ExternalOutput").ap()
    return fused, di, do


def _bc(ap1d, n):
    return ap1d.unsqueeze(0).to_broadcast([128, n])


MCOL = 1536


def build_mod():
    nc = bass.Bass("TRN2", target_bir_lowering=False)
    cin = nc.dram_tensor("cin", [128, 16, 5], F32, kind="ExternalInput").ap()
    w = nc.dram_tensor("w", [2, D, MCOL], F32, kind="ExternalInput").ap()
    b = nc.dram_tensor("b", [2, MCOL], F32, kind="ExternalInput").ap()
    out = nc.dram_tensor("out", [2, 5, MCOL], F32, kind="ExternalOutput").ap()
    with contextlib.ExitStack() as st:
        sb = lambda n, s, d: st.enter_context(nc.sbuf_tensor(n, s, d))
        ct = sb("ct", [128, 16, 5], F32)
        cs = sb("cs", [128, 16, 5], F32)
        wt = [sb(f"wt{i}", [128, 16, 512], F32) for i in range(2)]
        bt = sb("bt", [5, 2, MCOL], F32)
        ot = sb("ot", [5, 2, MCOL], F32)
        ps = [st.enter_context(nc.psum_tensor(f"ps{i}", [5, 512], F32)) for i in range(2)]
        P = Prog(nc)
        P.dma(lambda e: e.dma_start(out=ct[:], in_=cin), w=["ct"])
        for l in range(2):
            P.dma(lambda e, l=l: e.dma_start(out=bt[:, l, :], in_=b[l:l + 1, :].to_broadcast([5, MCOL])), w=[("bt", l)])
        P.a(lambda e: e.activation(out=cs[:], in_=ct[:], func=AF.Silu), ["ct"], ["cs"])
        gi = 0
        for l in range(2):
            for g in range(3):
                buf = gi % 2
                src = w[l, :, g * 512:(g + 1) * 512].rearrange("(kc p) n -> p kc n", p=128)
                P.dma(lambda e, buf=buf, src=src: e.dma_start(out=wt[buf][:], in_=src), w=[("wt", buf)])
                for kc in range(16):
                    P.t(lambda e, buf=buf, kc=kc: e.matmul(ps[buf][:], lhsT=cs[:, kc, :], rhs=wt[buf][:, kc, :],
                                                           start=(kc == 0), stop=(kc == 15)),
                        ["cs", ("wt", buf)], [("ps", buf)])
                P.v(lambda e, buf=buf, l=l, g=g: e.tensor_tensor(out=ot[:, l, g * 512:(g + 1) * 512], in0=ps[buf][:],
                                                                 in1=bt[:, l, g * 512:(g + 1) * 512], op=ALU.add),
                    [("ps", buf), ("bt", l)], [("ot", l, g)])
                gi += 1
        for l in range(2):
            P.dma(lambda e, l=l: e.dma_start(out=out[l], in_=ot[:, l, :]), [("ot", l, g) for g in range(3)], [("out", l)])
        P.emit()
    return nc


def run_mod(c, c_ctx, w_mod, b_mod):
    c_all = np.concatenate([c, c_ctx[None, :]], axis=0).astype(np.float32)
    cin = np.ascontiguousarray(c_all.T.reshape(16, 128, 5).transpose(1, 0, 2))
    nc = build_mod()
    in_maps = [{"cin": cin, "w": np.ascontiguousarray(w_mod[:, :, j * MCOL:(j + 1) * MCOL]),
                "b": np.ascontiguousarray(b_mod[:, j * MCOL:(j + 1) * MCOL])} for j in range(8)]
    res = run_bass_kernel_spmd(nc, in_maps, core_ids=list(range(8)))
    return np.concatenate([r["out"] for r in res.results], axis=2)


def emit_rstd(P, ss, rstd, n, kr, kw):
    P.a(lambda e: e.activation(out=rstd, in_=ss, func=AF.Sqrt, scale=1.0 / n, bias=EPS), kr, kw)
    P.v(lambda e: e.reciprocal(out=rstd, in_=rstd), kw, kw)


NQK = 16
WIN_PERM = np.concatenate([np.arange(0, 1024), np.arange(2304, 3328),
                           np.arange(1024, 1280), np.arange(3328, 3584),
                           np.arange(1280, 2304)])


def build_s1(nc=None, io=None, pfx=""):
    if nc is None:
        nc = bass.Bass("TRN2", target_bir_lowering=False)
    fused, di, do = _mk(nc, io, pfx)
    xin = di("xin", [NTOK, D])
    modx = di("modx", [6, D])
    modc = di("modc", [6, D])
    n1g = di("n1g", [D])
    win = di("win", [D, 3584])
    gqk = di("gqk", [NQK * 128])
    cs_t = di("cs_t", [NTOK, 128])
    vnb = di("vnb", [512])
    wsT = di("wsT", [128, 4, 128])
    bsT = di("bsT", [128, 4])
    ogb = di("ogb", [512])
    ident_in = di("ident", [128, 128])
    QKT = do("QKT", [128, NQK, NTOK], BF16)
    Vout = do("Vout", [NTOK, 512], BF16)
    yB = do("yB", [NTOK, 512], BF16)

    with stage_ctx(nc, fused) as st:
        sb = lambda n, s, d=F32: st.enter_context(nc.sbuf_tensor(pfx + n, s, d))
        pt = lambda n, s, d=F32: st.enter_context(nc.psum_tensor(pfx + n, s, d))
        wbf = sb("wbf", [128, 16, 2048], BF16)
        G1 = sb("G1", [128, D])
        SH = sb("SH", [128, D])
        gn = sb("gn", [128, D])
        xt = [sb(f"xt{i}", [128, D]) for i in range(2)]
        tmp = sb("tmp", [128, D])
        hx = sb("hx", [128, D], BF16)
        hxT = sb("hxT", [128, 16, 128], BF16)
        hxb = [hx, sb("hx1", [128, D], BF16)]
        hxTb = [hxT, sb("hxT1", [128, 16, 128], BF16)]
        tmph = sb("tmph", [128, D])
        ssh = sb("ssh", [128, 1])
        rsh = sb("rsh", [128, 1])
        ident = sb("ident_f", [128, 128])
        identb = sb("identb", [128, 128], BF16)
        ss = sb("ss", [128, 1])
        rs = sb("rs", [128, 1])
        Pb = sb("Pb", [128, 2048])
        gq = sb("gq", [128, NQK, 128])
        ssq = sb("ssq", [128, NQK])
        rsq = sb("rsq", [128, NQK])
        qn = sb("qn", [128, NQK, 2, 2, 32])
        cst = sb("cst", [128, 128])
        rA = sb("rA", [128, NQK, 2, 32])
        rB = sb("rB", [128, NQK, 2, 32])
        qr = sb("qr", [128, NQK, 2, 2, 32], BF16)
        qrb = [qr, sb("qr1", [128, NQK, 2, 2, 32], BF16)]
        qT = sb("qT", [128, NQK, 128], BF16)
        vb16 = sb("vb16", [128, 512], BF16)
        vng = sb("vng", [128, 512])
        wsb = sb("wsb", [128, 4, 128])
        wsbb = sb("wsbb", [128, 4, 128], BF16)
        bsb = sb("bsb", [128, 4])
        ogbt = sb("ogbt", [128, 512])
        t1 = sb("t1", [128, 1024])
        t2 = sb("t2", [128, 1024])
        gl = sb("gl", [128, 1024])
        ssv = sb("ssv", [128, 4])
        rsv = sb("rsv", [128, 4])
        vn = sb("vn", [128, 4, 128], BF16)
        ob = sb("ob", [128, 512])
        yb = sb("yb", [128, 512], BF16)
        pT = pt("pT", [128, 16, 128], BF16)
        pP = pt("pP", [128, 2048])
        pQ = pt("pQ", [128, NQK, 128], BF16)

        P = Prog(nc)
        P.dma(lambda e: e.dma_start(out=ident[:], in_=ident_in), w=["ident"])
        P.v(lambda e: e.tensor_copy(out=identb[:], in_=ident[:]), ["ident"], ["identb"])
        P.dma(lambda e: e.dma_start(out=gn[:], in_=_bc(n1g, D)), w=["gn"])
        P.dma(lambda e: e.dma_start(out=gq[:].rearrange("p h d -> p (h d)"), in_=_bc(gqk, NQK * 128)), w=["gq"])
        P.dma(lambda e: e.dma_start(out=vng[:], in_=_bc(vnb, 512)), w=["vng"])
        P.dma(lambda e: e.dma_start(out=ogbt[:], in_=_bc(ogb, 512)), w=["ogbt"])
        P.dma(lambda e: e.dma_start(out=wsb[:], in_=wsT), w=["wsb"])
        P.v(lambda e: e.tensor_copy(out=wsbb[:], in_=wsb[:]), ["wsb"], ["wsbb"])
        P.dma(lambda e: e.dma_start(out=bsb[:], in_=bsT), w=["bsb"])

        def load_w(c0, ncol):
            for k4 in range(4):
                src = win[k4 * 512:(k4 + 1) * 512, c0:c0 + ncol].rearrange("(kc p) n -> p kc n", p=128)
                P.dma(lambda e, k4=k4, src=src: e.dma_start(out=wbf[:, k4 * 4:(k4 + 1) * 4, 0:ncol], in_=src),
                      w=["wbf"], q="gpsimd")

        def load_mod(m):
            P.dma(lambda e: e.dma_start(out=SH[:], in_=_bc(m[0], D)), w=["SH"])
            P.dma(lambda e: e.dma_start(out=G1[:], in_=_bc(m[1], D)), w=["G1"])
            P.v(lambda e: e.scalar_tensor_tensor(out=G1[:], in0=G1[:], scalar=1.0, in1=gn[:], op0=ALU.add, op1=ALU.mult),
                ["G1", "gn"], ["G1"])

        def hx_tile(t):
            xb = xt[t % 2]
            kx = ("xt", t % 2)
            hb = hxb[t % 2]
            hTb = hxTb[t % 2]
            kh = ("hx", t % 2)
            P.dma(lambda e: e.dma_start(out=xb[:], in_=xin[t * 128:(t + 1) * 128, :]), w=[kx])
            P.a(lambda e: e.activation(out=tmph[:], in_=xb[:], func=AF.Square, accum_out=ssh[:]), [kx], ["tmph", "ssh"])
            emit_rstd(P, ssh[:], rsh[:], D, ["ssh"], ["rsh"])
            P.v(lambda e: e.scalar_tensor_tensor(out=tmph[:], in0=xb[:], scalar=rsh[:, 0:1], in1=G1[:], op0=ALU.mult, op1=ALU.mult),
                [kx, "rsh", "G1"], ["tmph"])
            P.v(lambda e: e.tensor_tensor(out=hb[:], in0=tmph[:], in1=SH[:], op=ALU.add), ["tmph", "SH"], [kh])
            for kc in range(16):
                P.t(lambda e, kc=kc: e.transpose(pT[:, kc, :], hb[:, kc * 128:(kc + 1) * 128], identb[:]), [kh, "identb"], ["pT"])
            P.a(lambda e: e.activation(out=hTb[:], in_=pT[:], func=AF.Copy), ["pT"], [("hxT", t % 2)])

        def prep(t):
            if t == 0:
                load_mod(modc)
            if t == 2:
                load_mod(modx)
            hx_tile(t)

        def inproj(ncol, t):
            hTb = hxTb[t % 2]
            for g in range(ncol // 512):
                for kc in range(16):
                    P.t(lambda e, g=g, kc=kc: e.matmul(pP[:, g * 512:(g + 1) * 512], lhsT=hTb[:, kc, :],
                                                      rhs=wbf[:, kc, g * 512:(g + 1) * 512], start=(kc == 0), stop=(kc == 15)),
                        [("hxT", t % 2), "wbf"], ["pP"])

        def postA(t):
            P.a(lambda e: e.activation(out=Pb[:], in_=pP[:], func=AF.Copy), ["pP"], ["Pb"])
            Pv = Pb[:].rearrange("p (h d) -> p h d", h=NQK)
            P.dma(lambda e, t=t: e.dma_start(out=cst[:], in_=cs_t[t * 128:(t + 1) * 128, :]), w=["cst"])
            P.v(lambda e: e.tensor_tensor(out=tmp[:], in0=Pb[:], in1=Pb[:], op=ALU.mult), ["Pb"], ["tmp"])
            P.v(lambda e: e.tensor_reduce(out=ssq[:], in_=tmp[:].rearrange("p (h d) -> p h d", h=NQK), axis=AX.X, op=ALU.add),
                ["tmp"], ["ssq"])
            emit_rstd(P, ssq[:], rsq[:], 128, ["ssq"], ["rsq"])
            qnv = qn[:].rearrange("p h a b f -> p h (a b f)")
            P.v(lambda e: e.tensor_tensor(out=qnv, in0=Pv, in1=rsq[:].unsqueeze(2).to_broadcast([128, NQK, 128]), op=ALU.mult),
                ["Pb", "rsq"], ["qn"])
            P.v(lambda e: e.tensor_tensor(out=qnv, in0=qnv, in1=gq[:], op=ALU.mult), ["qn", "gq"], ["qn"])
            cosb = cst[:, 0:64].rearrange("p (a f) -> p a f", a=2).unsqueeze(1).to_broadcast([128, NQK, 2, 32])
            sinb = cst[:, 64:128].rearrange("p (a f) -> p a f", a=2).unsqueeze(1).to_broadcast([128, NQK, 2, 32])
            x1 = qn[:, :, :, 0, :]
            x2 = qn[:, :, :, 1, :]
            P.v(lambda e: e.tensor_tensor(out=rA[:], in0=x1, in1=cosb, op=ALU.mult), ["qn", "cst"], ["rA"])
            P.v(lambda e: e.tensor_tensor(out=rB[:], in0=x2, in1=sinb, op=ALU.mult), ["qn", "cst"], ["rB"])
            P.v(lambda e: e.tensor_tensor(out=qrb[t % 2][:, :, :, 0, :], in0=rA[:], in1=rB[:], op=ALU.subtract), ["rA", "rB"], [("qr", t % 2)])
            P.v(lambda e: e.tensor_tensor(out=rA[:], in0=x2, in1=cosb, op=ALU.mult), ["qn", "cst", ("qr", t % 2)], ["rA"])
            P.v(lambda e: e.tensor_tensor(out=rB[:], in0=x1, in1=sinb, op=ALU.mult), ["qn", "cst", ("qr", t % 2)], ["rB"])
            P.v(lambda e: e.tensor_tensor(out=qrb[t % 2][:, :, :, 1, :], in0=rA[:], in1=rB[:], op=ALU.add), ["rA", "rB"], [("qr", t % 2)])

        def storeA(t):
            qrv = qrb[t % 2][:].rearrange("p h a b f -> p h (a b f)")
            for h in range(NQK):
                P.t(lambda e, h=h: e.transpose(pQ[:, h, :], qrv[:, h, :], identb[:]), [("qr", t % 2), "identb"], ["pQ"])
            P.a(lambda e: e.activation(out=qT[:], in_=pQ[:], func=AF.Copy), ["pQ"], ["qT"])
            P.dma(lambda e, t=t: e.dma_start(out=QKT[:, :, t * 128:(t + 1) * 128], in_=qT[:]), ["qT"], [("QKT", t)], q="gpsimd")
            if fused and t >= 2:
                ktv = io["KTx"].rearrange("(h d) t -> d h t", d=128)
                c0 = (t - 2) * 128
                P.dma(lambda e, c0=c0: e.dma_start(out=ktv[:, 0:2, c0:c0 + 128], in_=qT[:, 6:8, :]), ["qT"], [("KTx", t, 0)], q="gpsimd")
                P.dma(lambda e, c0=c0: e.dma_start(out=ktv[:, 2:4, c0:c0 + 128], in_=qT[:, 14:16, :]), ["qT"], [("KTx", t, 1)], q="gpsimd")


        def postB(t):
            P.a(lambda e: e.activation(out=vb16[:], in_=pP[:, 0:512], func=AF.Copy), ["pP"], ["vb16"])
            P.dma(lambda e, t=t: e.dma_start(out=Vout[t * 128:(t + 1) * 128, :], in_=vb16[:]), ["vb16"], [("Vout", t)], q="gpsimd")
            if fused and t >= 2:
                P.dma(lambda e, t=t: e.dma_start(out=io["Vx"][(t - 2) * 128:(t - 1) * 128, :], in_=vb16[:]), ["vb16"], [("Vx", t)], q="gpsimd")
            P.a(lambda e: e.activation(out=gl[:], in_=pP[:, 512:1536], func=AF.Gelu_apprx_tanh), ["pP"], ["gl"])
            gv = gl[:, 512:1024]
            P.v(lambda e: e.tensor_tensor(out=t1[:, 0:512], in0=gv, in1=gv, op=ALU.mult), ["gl"], ["t1"])
            P.v(lambda e: e.tensor_reduce(out=ssv[:], in_=t1[:, 0:512].rearrange("p (g d) -> p g d", g=4), axis=AX.X, op=ALU.add),
                ["t1"], ["ssv"])
            emit_rstd(P, ssv[:], rsv[:], 128, ["ssv"], ["rsv"])
            P.v(lambda e: e.tensor_tensor(out=t1[:, 0:512].rearrange("p (g d) -> p g d", g=4), in0=gv.rearrange("p (g d) -> p g d", g=4),
                                          in1=rsv[:].unsqueeze(2).to_broadcast([128, 4, 128]), op=ALU.mult), ["gl", "rsv"], ["t1"])
            P.v(lambda e: e.tensor_tensor(out=vn[:].rearrange("p g d -> p (g d)"), in0=t1[:, 0:512], in1=vng[:], op=ALU.mult),
                ["t1", "vng"], ["vn"])
            for g in range(4):
                P.t(lambda e, g=g: e.matmul(pP[:, 1536 + g * 128:1536 + (g + 1) * 128], lhsT=wsbb[:, g, :], rhs=vn[:, g, :],
                                            start=True, stop=True), ["vn", "wsbb"], ["pM"])
            P.v(lambda e: e.tensor_tensor(out=ob[:].rearrange("p (g d) -> p g d", g=4),
                                          in0=pP[:, 1536:2048].rearrange("p (g d) -> p g d", g=4),
                                          in1=bsb[:].unsqueeze(2).to_broadcast([128, 4, 128]), op=ALU.add), ["pM", "bsb"], ["ob"])
            P.v(lambda e: e.tensor_tensor(out=ob[:], in0=ob[:], in1=gl[:, 0:512], op=ALU.mult), ["ob", "gl"], ["ob"])
            P.a(lambda e: e.activation(out=t2[:, 0:512], in_=ob[:], func=AF.Square, accum_out=ss[:]), ["ob"], ["t2", "ss"])
            emit_rstd(P, ss[:], rs[:], 512, ["ss"], ["rs"])
            P.v(lambda e: e.scalar_tensor_tensor(out=yb[:], in0=ob[:], scalar=rs[:, 0:1], in1=ogbt[:], op0=ALU.mult, op1=ALU.mult),
                ["ob", "rs", "ogbt"], ["yb"])
            P.dma(lambda e, t=t: e.dma_start(out=yB[t * 128:(t + 1) * 128, :], in_=yb[:]), ["yb"], [("yB", t)], q="gpsimd")

        load_w(0, 2048)
        prep(0)
        for t in range(NT):
            inproj(2048, t)
            if t + 1 < NT:
                prep(t + 1)
            if t >= 1:
                storeA(t - 1)
            postA(t)
        storeA(NT - 1)

        load_w(2048, 1536)
        prep(0)
        for t in range(NT):
            inproj(1536, t)
            if t + 1 < NT:
                prep(t + 1)
            postB(t)
        P.emit(fused=fused)
    return nc


NKA = 20
NKC = 34


def build_s2a(nc=None, io=None, pfx=""):
    if nc is None:
        nc = bass.Bass("TRN2", target_bir_lowering=False)
    fused, di, do = _mk(nc, io, pfx)
    QKT = di("QKT", [128, NQK, NTOK], BF16)
    KA = None if fused else di("KA", [128, 2, NKA * 128], BF16)
    VA = None if fused else di("VA", [NKA * 128, 256], BF16)
    KC = None if fused else di("KC", [128, 2, NKC * 128], BF16)
    VC = None if fused else di("VC", [NKC * 128, 256], BF16)
    msk = di("msk", [4, 128, 128])
    sink = di("sink", [6])
    gac = di("gac", [1536])
    yAC = do("yAC", [NTOK, 1536], BF16)
    with stage_ctx(nc, fused) as st:
        sb = lambda n, s, d=F32: st.enter_context(nc.sbuf_tensor(pfx + n, s, d))
        pt = lambda n, s, d=F32: st.enter_context(nc.psum_tensor(pfx + n, s, d))
        ka = sb("ka", [128, 2, NKA * 128], BF16)
        va = sb("va", [128, NKA, 2, 129], BF16)
        kc = sb("kc", [128, 2, NKC * 128], BF16)
        vc = sb("vc", [128, NKC, 2, 129], BF16)
        mf = sb("mf", [128, 4, 128])
        mb = sb("mb", [128, 4, 128], BF16)
        sk = sb("sk", [128, 6])
        es = sb("es", [128, 6])
        gt = sb("gt", [128, 1536])
        qt = [sb(f"qt{i}", [128, NQK, 128], BF16) for i in range(2)]
        PT = [sb(f"PT{i}", [128, 3, 128], BF16) for i in range(3)]
        oo = sb("oo", [128, 2, 6, 128])
        den = sb("den", [128, 3])
        sq = sb("sq", [128, 768])
        ss = sb("ss", [128, 1])
        rs = sb("rs", [128, 1])
        yt = sb("yt", [128, 1536], BF16)
        psS = [pt(f"psS{i}", [128, 512]) for i in range(3)]
        psO = [pt(f"psO{i}", [128, 3, 129]) for i in range(2)]
        P = Prog(nc)
        if not fused:
            P.dma(lambda e: e.dma_start(out=ka[:], in_=KA), w=["ka"])
            P.dma(lambda e: e.dma_start(out=kc[:], in_=KC), w=["kc"])
        else:
            KTg = io["KTg"].rearrange("(r h d) t -> d r h t", r=2, h=4)
            Vg = io["Vg"]
            Vo = io["Vout"]
            for k in range(2):
                P.dma(lambda e, k=k: e.dma_start(out=ka[:, k, 0:256], in_=QKT[:, 6 + k, 0:256]), w=["ka"])
                P.dma(lambda e, k=k: e.dma_start(out=ka[:, k, 256:384], in_=KTg[:, 0, k, 1920:2048]), w=["ka"])
                P.dma(lambda e, k=k: e.dma_start(out=ka[:, k, 384:2432], in_=QKT[:, 6 + k, 256:2304]), w=["ka"])
                P.dma(lambda e, k=k: e.dma_start(out=ka[:, k, 2432:2560], in_=KTg[:, 1, k, 0:128]), w=["ka"])
                P.dma(lambda e, k=k: e.dma_start(out=kc[:, k, 0:256], in_=QKT[:, 14 + k, 0:256]), w=["kc"])
                for r in range(2):
                    P.dma(lambda e, k=k, r=r: e.dma_start(out=kc[:, k, 256 + r * 2048:256 + (r + 1) * 2048], in_=KTg[:, r, 2 + k, :]), w=["kc"])
        P.v(lambda e: e.memset(va[:, :, :, 128:129], 1.0), [], ["va1"])
        P.v(lambda e: e.memset(vc[:, :, :, 128:129], 1.0), [], ["vc1"])
        for k in range(2):
            if not fused:
                P.dma(lambda e, k=k: e.dma_start(out=va[:, :, k, 0:128], in_=VA[:, k * 128:(k + 1) * 128].rearrange("(j s) d -> s j d", s=128)), w=[("va", k)])
                P.dma(lambda e, k=k: e.dma_start(out=vc[:, :, k, 0:128], in_=VC[:, k * 128:(k + 1) * 128].rearrange("(j s) d -> s j d", s=128)), w=[("vc", k)])
            else:
                ca = slice(k * 128, (k + 1) * 128)
                cc = slice(256 + k * 128, 256 + (k + 1) * 128)
                tl = lambda ap: ap.rearrange("(j s) d -> s j d", s=128)
                P.dma(lambda e, k=k, ca=ca: e.dma_start(out=va[:, 0:2, k, 0:128], in_=tl(Vo[0:256, ca])), w=[("va", k)])
                P.dma(lambda e, k=k, ca=ca: e.dma_start(out=va[:, 2:3, k, 0:128], in_=tl(Vg[1920:2048, ca])), w=[("va", k)])
                P.dma(lambda e, k=k, ca=ca: e.dma_start(out=va[:, 3:19, k, 0:128], in_=tl(Vo[256:2304, ca])), w=[("va", k)])
                P.dma(lambda e, k=k, ca=ca: e.dma_start(out=va[:, 19:20, k, 0:128], in_=tl(Vg[2048:2176, ca])), w=[("va", k)])
                P.dma(lambda e, k=k, cc=cc: e.dma_start(out=vc[:, 0:2, k, 0:128], in_=tl(Vo[0:256, cc])), w=[("vc", k)])
                P.dma(lambda e, k=k, cc=cc: e.dma_start(out=vc[:, 2:34, k, 0:128], in_=tl(Vg[:, cc])), w=[("vc", k)])
        P.dma(lambda e: e.dma_start(out=mf[:], in_=msk.rearrange("m s q -> s m q")), w=["mf"])
        P.v(lambda e: e.tensor_copy(out=mb[:], in_=mf[:]), ["mf"], ["mb"])
        P.dma(lambda e: e.dma_start(out=sk[:], in_=_bc(sink, 6)), w=["sk"])
        P.a(lambda e: e.activation(out=es[:], in_=sk[:], func=AF.Exp), ["sk"], ["es"])
        P.dma(lambda e: e.dma_start(out=gt[:], in_=_bc(gac, 1536)), w=["gt"])
        PF = 2
        NBUF = PF + 1
        steps = []
        for t in range(NT):
            if t < 2:
                keysA = [(0, None), (1, None)]
                keysC = [(0, None), (1, None)]
            else:
                i = t - 2
                keysA = [(0, None), (1, None), (2 + i, 2 if i == 0 else 0), (3 + i, None), (4 + i, 3 if i == 15 else 1)]
                keysC = [(j, None) for j in range(NKC)]
            for (oi, qbase, KT, VT, kkey, vkeys, keys, use_sink) in (
                    (0, 0, ka, va, "ka", [("va", 0), ("va", 1), "va1"], keysA, True),
                    (1, 8, kc, vc, "kc", [("vc", 0), ("vc", 1), "vc1"], keysC, False)):
                for k in range(2):
                    for n, (j, m) in enumerate(keys):
                        steps.append(dict(t=t, oi=oi, qbase=qbase, KT=KT, VT=VT, kkey=kkey, vkeys=vkeys, k=k, n=n, j=j, m=m,
                                          last=(n == len(keys) - 1), use_sink=use_sink,
                                          tile_first=(oi == 0 and k == 0 and n == 0), tile_last=(oi == 1 and k == 1 and n == len(keys) - 1)))
        ngrp = [0]

        def load_q(t):
            q = qt[t % 2]
            P.dma(lambda e, q=q, t=t: e.dma_start(out=q[:], in_=QKT[:, :, t * 128:(t + 1) * 128]), w=[("qt", t % 2)])

        def s1(i):
            sp = steps[i]
            t, k, j, m = sp["t"], sp["k"], sp["j"], sp["m"]
            if sp["tile_first"]:
                if t == 0:
                    load_q(0)
                if t + 1 < NT:
                    load_q(t + 1)
            q = qt[t % 2]
            b = i % NBUF
            KT, qbase = sp["KT"], sp["qbase"]
            pS = psS[b][:, 0:384].rearrange("p (g q) -> p g q", g=3)
            P.t(lambda e: e.matmul(pS, lhsT=KT[:, k, j * 128:(j + 1) * 128], rhs=q[:, qbase + 3 * k:qbase + 3 * k + 3, :], start=True, stop=True),
                [sp["kkey"], ("qt", t % 2)], [("psS", b)])
            P.a(lambda e: e.activation(out=PT[b][:], in_=pS, func=AF.Exp, scale=128.0 ** -0.5), [("psS", b)], [("PT", b)])
            if m is not None:
                P.v(lambda e: e.tensor_tensor(out=PT[b][:], in0=PT[b][:], in1=mb[:, m, :].unsqueeze(1).to_broadcast([128, 3, 128]), op=ALU.mult),
                    [("PT", b), "mb"], [("PT", b)])

        def s2(i):
            sp = steps[i]
            t, k, j, n, oi = sp["t"], sp["k"], sp["j"], sp["n"], sp["oi"]
            b = i % NBUF
            VT = sp["VT"]
            pb = ngrp[0] % 2
            po = psO[pb]
            for g in range(3):
                P.t(lambda e, g=g: e.matmul(po[:, g, :], lhsT=PT[b][:, g, :], rhs=VT[:, j, k, :], start=(n == 0 and g == 0), stop=sp["last"]),
                    [("PT", b)] + sp["vkeys"], [("psO", pb)])
            if sp["last"]:
                ngrp[0] += 1
                if sp["use_sink"]:
                    P.v(lambda e: e.tensor_tensor(out=den[:], in0=po[:, :, 128], in1=es[:, 3 * k:3 * k + 3], op=ALU.add), [("psO", pb), "es"], ["den"])
                else:
                    P.v(lambda e: e.tensor_copy(out=den[:], in_=po[:, :, 128]), [("psO", pb)], ["den"])
                P.v(lambda e: e.reciprocal(out=den[:], in_=den[:]), ["den"], ["den"])
                P.v(lambda e: e.tensor_tensor(out=oo[:, oi, 3 * k:3 * k + 3, :], in0=po[:, :, 0:128], in1=den[:].unsqueeze(2).to_broadcast([128, 3, 128]),
                                              op=ALU.mult), [("psO", pb), "den"], [("oo", oi)])
            if sp["tile_last"]:
                for o2 in range(2):
                    ov = oo[:, o2, :, :].rearrange("p h d -> p (h d)")
                    P.v(lambda e, ov=ov: e.tensor_tensor(out=sq[:], in0=ov, in1=ov, op=ALU.mult), [("oo", o2)], ["sq"])
                    P.v(lambda e: e.tensor_reduce(out=ss[:], in_=sq[:], axis=AX.X, op=ALU.add), ["sq"], ["ss"])
                    emit_rstd(P, ss[:], rs[:], 768, ["ss"], ["rs"])
                    P.v(lambda e, ov=ov, o2=o2: e.scalar_tensor_tensor(out=yt[:, o2 * 768:(o2 + 1) * 768], in0=ov, scalar=rs[:, 0:1],
                                                                      in1=gt[:, o2 * 768:(o2 + 1) * 768], op0=ALU.mult, op1=ALU.mult),
                        [("oo", o2), "rs", "gt"], ["yt"])
                P.dma(lambda e: e.dma_start(out=yAC[t * 128:(t + 1) * 128, :], in_=yt[:]), ["yt"], [("yAC", t)])

        for i in range(min(PF, len(steps))):
            s1(i)
        for i in range(len(steps)):
            if i + PF < len(steps):
                s1(i + PF)
            s2(i)
        P.emit(fused=fused)
    return nc


def build_s2b(nc=None, io=None, pfx=""):
    if nc is None:
        nc = bass.Bass("TRN2", target_bir_lowering=False)
    fused, di, do = _mk(nc, io, pfx)
    yAC = di("yAC", [NTOK, 1536], BF16)
    yB = di("yB", [NTOK, 512], BF16)
    xin = di("xin", [NTOK, D])
    modx = di("modx", [6, D])
    modc = di("modc", [6, D])
    wout = di("wout", [D, D])
    n2g = di("n2g", [D])
    wr = di("wr", [D, NE])
    ident_in = di("ident", [128, 128])
    x1o = do("x1o", [NTOK, D])
    hx2o = do("hx2o", [NTOK, D], BF16)
    affT = None if fused else do("affT", [NE, NTOK])
    with stage_ctx(nc, fused) as st:
        sb = lambda n, s, d=F32: st.enter_context(nc.sbuf_tensor(pfx + n, s, d))
        pt = lambda n, s, d=F32: st.enter_context(nc.psum_tensor(pfx + n, s, d))
        wbf = sb("wbf", [128, 16, D], BF16)
        G1g = sb("G1g", [128, D])
        G2 = sb("G2", [128, D])
        SH2 = sb("SH2", [128, D])
        gn = sb("gn", [128, D])
        ident = sb("ident_f", [128, 128])
        identb = sb("identb", [128, 128], BF16)
        wrs = sb("wrs", [128, 16, NE])
        xt = sb("xt", [128, D])
        yt = sb("yt", [128, D], BF16)
        yT = sb("yT", [128, 16, 128], BF16)
        tmp = sb("tmp", [128, D])
        x1 = sb("x1", [128, D])
        h2 = sb("h2", [128, D])
        hb = sb("hb", [128, D], BF16)
        h2T = sb("h2T", [128, 16, 128])
        ytb = [yt, sb("yt1", [128, D], BF16)]
        yTb = [yT, sb("yT1", [128, 16, 128], BF16)]
        xtb = [xt, sb("xt1", [128, D])]
        h2b = [h2, sb("h21", [128, D])]
        ss = sb("ss", [128, 1])
        rs = sb("rs", [128, 1])
        mx = sb("mx", [128, 1])
        se = sb("se", [128, 1])
        ex = sb("ex", [128, NE])
        af = sb("af", [128, NE])
        aT = sb("aT", [NE, 128])
        psT = pt("psT", [128, 16, 128], BF16)
        psX = [pt(f"psX{i}", [128, 512]) for i in range(2)]
        psR = [pt(f"psR{i}", [128, 4, 128]) for i in range(2)]
        psL = pt("psL", [128, 512])
        psA = pt("psA", [128, 512])
        P = Prog(nc)
        P.dma(lambda e: e.dma_start(out=ident[:], in_=ident_in), w=["ident"])
        P.v(lambda e: e.tensor_copy(out=identb[:], in_=ident[:]), ["ident"], ["identb"])
        P.dma(lambda e: e.dma_start(out=gn[:], in_=_bc(n2g, D)), w=["gn"])
        P.dma(lambda e: e.dma_start(out=wrs[:], in_=wr.rearrange("(kc p) n -> p kc n", p=128)), w=["wrs"])
        for k4 in range(4):
            src = wout[k4 * 512:(k4 + 1) * 512, :].rearrange("(kc p) n -> p kc n", p=128)
            P.dma(lambda e, k4=k4, src=src: e.dma_start(out=wbf[:, k4 * 4:(k4 + 1) * 4, :], in_=src), w=["wbf"], q="gpsimd")

        def load_mod(m):
            P.dma(lambda e: e.dma_start(out=G1g[:], in_=_bc(m[2], D)), w=["G1g"])
            P.dma(lambda e: e.dma_start(out=SH2[:], in_=_bc(m[3], D)), w=["SH2"])
            P.dma(lambda e: e.dma_start(out=G2[:], in_=_bc(m[4], D)), w=["G2"])
            P.v(lambda e: e.scalar_tensor_tensor(out=G2[:], in0=G2[:], scalar=1.0, in1=gn[:], op0=ALU.add, op1=ALU.mult),
                ["G2", "gn"], ["G2"])

        def stA(t):
            r0 = t * 128
            i = t % 2
            ytx, yTx, xtx = ytb[i], yTb[i], xtb[i]
            P.dma(lambda e: e.dma_start(out=ytx[:, 0:768], in_=yAC[r0:r0 + 128, 0:768]), w=[("yt", i, 0)])
            P.dma(lambda e: e.dma_start(out=ytx[:, 768:1280], in_=yB[r0:r0 + 128, :]), w=[("yt", i, 1)])
            P.dma(lambda e: e.dma_start(out=ytx[:, 1280:2048], in_=yAC[r0:r0 + 128, 768:1536]), w=[("yt", i, 2)])
            P.dma(lambda e: e.dma_start(out=xtx[:], in_=xin[r0:r0 + 128, :]), w=[("xt", i)])
            for kc in range(16):
                P.t(lambda e, kc=kc: e.transpose(psT[:, kc, :], ytx[:, kc * 128:(kc + 1) * 128], identb[:]),
                    [("yt", i, 0), ("yt", i, 1), ("yt", i, 2), "identb"], ["psT"])
            P.a(lambda e: e.activation(out=yTx[:], in_=psT[:], func=AF.Copy), ["psT"], [("yT", i)])

        def stB(t):
            r0 = t * 128
            i = t % 2
            yTx, xtx = yTb[i], xtb[i]
            for g in range(4):
                px = psX[g % 2]
                for kc in range(16):
                    P.t(lambda e, px=px, g=g, kc=kc: e.matmul(px[:], lhsT=yTx[:, kc, :], rhs=wbf[:, kc, g * 512:(g + 1) * 512],
                                                              start=(kc == 0), stop=(kc == 15)), [("yT", i), "wbf"], [("psX", g % 2)])
                P.v(lambda e, px=px, g=g: e.tensor_tensor(out=tmp[:, g * 512:(g + 1) * 512], in0=px[:], in1=G1g[:, g * 512:(g + 1) * 512],
                                                          op=ALU.mult), [("psX", g % 2), "G1g"], ["tmp"])
            P.v(lambda e: e.tensor_tensor(out=x1[:], in0=tmp[:], in1=xtx[:], op=ALU.add), ["tmp", ("xt", i)], ["x1"])
            P.dma(lambda e: e.dma_start(out=x1o[r0:r0 + 128, :], in_=x1[:]), ["x1"], [("x1o", t)], q="gpsimd")

        def stC1(t):
            r0 = t * 128
            hh = h2b[t % 2]
            kh = ("h2", t % 2)
            P.a(lambda e: e.activation(out=tmp[:], in_=x1[:], func=AF.Square, accum_out=ss[:]), ["x1"], ["tmp", "ss"])
            emit_rstd(P, ss[:], rs[:], D, ["ss"], ["rs"])
            P.v(lambda e: e.scalar_tensor_tensor(out=hh[:], in0=x1[:], scalar=rs[:, 0:1], in1=G2[:], op0=ALU.mult, op1=ALU.mult),
                ["x1", "rs", "G2"], [kh])
            P.v(lambda e: e.tensor_tensor(out=hh[:], in0=hh[:], in1=SH2[:], op=ALU.add), [kh, "SH2"], [kh])
            P.a(lambda e: e.activation(out=hb[:], in_=hh[:], func=AF.Copy), [kh], ["hb"])
            P.dma(lambda e: e.dma_start(out=hx2o[r0:r0 + 128, :], in_=hb[:]), ["hb"], [("hx2o", t)], q="gpsimd")

        def stC2(t):
            r0 = t * 128
            hh = h2b[t % 2]
            kh = ("h2", t % 2)
            for q4 in range(4):
                pr = psR[q4 % 2]
                for j in range(4):
                    kc = q4 * 4 + j
                    P.t(lambda e, pr=pr, j=j, kc=kc: e.transpose(pr[:, j, :], hh[:, kc * 128:(kc + 1) * 128], ident[:]),
                        [kh, "ident"], [("psR", q4 % 2)])
                P.a(lambda e, pr=pr, q4=q4: e.activation(out=h2T[:, q4 * 4:(q4 + 1) * 4, :], in_=pr[:], func=AF.Copy),
                    [("psR", q4 % 2)], ["h2T"])
            for kc in range(16):
                P.t(lambda e, kc=kc: e.matmul(psL[:, 0:NE], lhsT=h2T[:, kc, :], rhs=wrs[:, kc, :], start=(kc == 0), stop=(kc == 15)),
                    ["h2T", "wrs"], ["psL"])
            P.v(lambda e: e.tensor_reduce(out=mx[:], in_=psL[:, 0:NE], axis=AX.X, op=ALU.max), ["psL"], ["mx"])
            P.v(lambda e: e.tensor_scalar(out=mx[:], in0=mx[:], scalar1=-1.0, scalar2=None, op0=ALU.mult), ["mx"], ["mx"])
            P.a(lambda e: e.activation(out=ex[:], in_=psL[:, 0:NE], func=AF.Exp, bias=mx[:, 0:1], accum_out=se[:]), ["psL", "mx"], ["ex", "se"])
            P.v(lambda e: e.reciprocal(out=se[:], in_=se[:]), ["se"], ["se"])
            P.v(lambda e: e.tensor_scalar(out=af[:], in0=ex[:], scalar1=se[:, 0:1], scalar2=None, op0=ALU.mult), ["ex", "se"], ["af"])
            if fused:
                adst = io["affC"][r0:r0 + 128, :] if t < 2 else io["affIn"][r0 - 256:r0 - 128, :]
                P.dma(lambda e: e.dma_start(out=adst, in_=af[:]), ["af"], [("affo", t)], q="gpsimd")
            else:
                P.t(lambda e: e.transpose(psA[0:NE, 0:128], af[:], ident[:]), ["af", "ident"], ["psA"])
                P.a(lambda e: e.activation(out=aT[:], in_=psA[0:NE, 0:128], func=AF.Copy), ["psA"], ["aT"])
                P.dma(lambda e: e.dma_start(out=affT[:, r0:r0 + 128], in_=aT[:]), ["aT"], [("affT", t)])

        load_mod(modc)
        stA(0)
        for t in range(NT):
            if t == 2:
                load_mod(modx)
            stB(t)
            if t + 1 < NT:
                stA(t + 1)
            stC1(t)
            if t >= 1:
                stC2(t - 1)
        stC2(NT - 1)
        P.emit(fused=fused)
    return nc


NEL = 8
NROW = SEQ + CTX


def build_s3(with_ctx, nc=None, io=None, pfx=""):
    if nc is None:
        nc = bass.Bass("TRN2", target_bir_lowering=False)
    fused, di, do = _mk(nc, io, pfx)
    affL = di("affL", [128, 32, NEL])
    affC = di("affC", [128, 2, NEL])
    hx2 = di("hx2", [NROW, D], BF16)
    wg = di("wg", [NEL, D, D])
    wu = di("wu", [NEL, D, D])
    wd = di("wd", [NEL, D, D])
    consts = di("consts", [128, 128 + 128 + 128 + 512 + 34])
    delta = [do(f"delta{i}", [NROW, 512]) for i in range(4)]
    sets = [("L", 32, 512, 0, affL)]
    if with_ctx:
        sets.append(("C", 2, 32, SEQ, affC))
    with stage_ctx(nc, fused) as st:
        sb = lambda n, s, d=F32: st.enter_context(nc.sbuf_tensor(pfx + n, s, d))
        pt = lambda n, s, d=F32: st.enter_context(nc.psum_tensor(pfx + n, s, d))
        cst = sb("cst", [128, 930])
        identb = sb("identb", [128, 128], BF16)
        zt = sb("zt", [128, D])
        ring = [sb(f"ring{i}", [128, 16, 512], BF16) for i in range(4)]
        xg = [sb(f"xg{i}", [128, D], BF16) for i in range(2)]
        xgT = sb("xgT", [128, 16, 544], BF16)
        hT = sb("hT", [128, 16, 544], BF16)
        sil = sb("sil", [128, 544])
        yb = [sb(f"yb{i}", [128, 512]) for i in range(3)]
        S = {}
        for (nm, C, k, r0, _) in sets:
            S[nm] = dict(
                aff=sb(f"saff{nm}", [128, C, NEL]), lo=sb(f"lo{nm}", [128, NEL]), th=sb(f"th{nm}", [128, NEL, 15]),
                cmp=sb(f"cmp{nm}", [128, C, NEL, 15]), cntp=sb(f"cntp{nm}", [128, NEL, 15]), ge=sb(f"ge{nm}", [128, NEL, 15]),
                nge=sb(f"nge{nm}", [128, NEL]), mask=sb(f"mask{nm}", [128, C, NEL]), maskb=sb(f"maskb{nm}", [128, C, NEL], BF16),
                cA=sb(f"cA{nm}", [128, C, NEL]), cB=sb(f"cB{nm}", [128, C, NEL]), pos=sb(f"pos{nm}", [128, C, NEL]),
                vals=sb(f"vals{nm}", [128, C, NEL, 2]), oh=[sb(f"oh{nm}{i}", [128, k]) for i in range(2)],
                sl=sb(f"sl{nm}", [128, NEL, 4, 2]), idx=sb(f"idx{nm}", [128, NEL, 4], I32), gate=sb(f"gate{nm}", [128, NEL, 4]))
        jt = sb("jt", [128, NEL, 15])
        trib = sb("trib", [128, 128], BF16)
        onesb = sb("onesb", [128, 128], BF16)
        psA = [pt(f"psA{i}", [128, 512]) for i in range(2)]
        psU = [pt(f"psU{i}", [128, 512]) for i in range(2)]
        psT = pt("psT", [128, 16, 128], BF16)
        psY = [pt(f"psY{i}", [128, 512]) for i in range(2)]
        ident = cst[:, 0:128]
        ones = cst[:, 128:256]
        tri = cst[:, 256:384]
        iota = cst[:, 384:896]
        tokid = cst[:, 896:930]
        P = Prog(nc)
        P.dma(lambda e: e.dma_start(out=cst[:], in_=consts), w=["cst"])
        P.v(lambda e: e.tensor_copy(out=identb[:], in_=ident), ["cst"], ["identb"])
        P.v(lambda e: e.tensor_copy(out=trib[:], in_=tri), ["cst"], ["trib"])
        P.v(lambda e: e.tensor_copy(out=onesb[:], in_=ones), ["cst"], ["onesb"])
        for j in range(15):
            P.v(lambda e, j=j: e.memset(jt[:, :, j:j + 1], float(j + 1)), [], ["jt"])
        P.v(lambda e: e.memset(zt[:], 0.0), [], ["zt"])
        for cb in range(4):
            for r in range(NROW // 128):
                P.dma(lambda e, r=r, cb=cb: e.dma_start(out=delta[cb][r * 128:(r + 1) * 128, :], in_=zt[:, 0:512]), ["zt"], [("delta", cb)])

        def select(nm, C, k, r0, affd):
            s = S[nm]
            kk = lambda x: (nm, x)
            P.dma(lambda e, s=s, affd=affd: e.dma_start(out=s["aff"][:], in_=affd), w=[kk("aff")])
            P.v(lambda e, s=s: e.memset(s["lo"][:], 0.0), [], [kk("lo")])
            affb = s["aff"][:].unsqueeze(3).to_broadcast([128, C, NEL, 15])
            for it in range(8):
                step = 16.0 ** -(it + 1)
                P.v(lambda e, s=s, step=step: e.scalar_tensor_tensor(out=s["th"][:], in0=jt[:], scalar=step,
                                                                     in1=s["lo"][:].unsqueeze(2).to_broadcast([128, NEL, 15]),
                                                                     op0=ALU.mult, op1=ALU.add), ["jt", kk("lo")], [kk("th")])
                P.v(lambda e, s=s: e.tensor_tensor(out=s["cmp"][:], in0=affb, in1=s["th"][:].unsqueeze(1).to_broadcast([128, C, NEL, 15]),
                                                   op=ALU.is_ge), [kk("aff"), kk("th")], [kk("cmp")])
                P.v(lambda e, s=s: e.tensor_reduce(out=s["cntp"][:], in_=s["cmp"][:].rearrange("p c e j -> p e j c"), axis=AX.X, op=ALU.add),
                    [kk("cmp")], [kk("cntp")])
                P.t(lambda e, s=s: e.matmul(psY[0][:, 0:NEL * 15], lhsT=ones, rhs=s["cntp"][:].rearrange("p e j -> p (e j)"), start=True, stop=True),
                    ["cst", kk("cntp")], [("psY", 0)])
                P.v(lambda e, s=s, k=k: e.tensor_scalar(out=s["ge"][:].rearrange("p e j -> p (e j)"), in0=psY[0][:, 0:NEL * 15], scalar1=float(k) - 0.5,
                                                        scalar2=None, op0=ALU.is_ge), [("psY", 0)], [kk("ge")])
                P.v(lambda e, s=s: e.tensor_reduce(out=s["nge"][:], in_=s["ge"][:], axis=AX.X, op=ALU.add), [kk("ge")], [kk("nge")])
                P.v(lambda e, s=s, step=step: e.scalar_tensor_tensor(out=s["lo"][:], in0=s["nge"][:], scalar=step, in1=s["lo"][:],
                                                                     op0=ALU.mult, op1=ALU.add), [kk("nge"), kk("lo")], [kk("lo")])
            P.v(lambda e, s=s: e.tensor_tensor(out=s["mask"][:], in0=s["aff"][:], in1=s["lo"][:].unsqueeze(1).to_broadcast([128, C, NEL]),
                                               op=ALU.is_ge), [kk("aff"), kk("lo")], [kk("mask")])
            P.v(lambda e, s=s: e.tensor_copy(out=s["maskb"][:], in_=s["mask"][:]), [kk("mask")], [kk("maskb")])
            mb2 = s["maskb"][:].rearrange("p c e -> p (c e)")
            P.t(lambda e: e.matmul(psY[0][:, 0:C * NEL], lhsT=trib[:], rhs=mb2, start=True, stop=True), ["trib", kk("maskb")], [("psY", 0)])
            P.t(lambda e: e.matmul(psY[1][:, 0:C * NEL], lhsT=onesb[:], rhs=mb2, start=True, stop=True), ["onesb", kk("maskb")], [("psY", 1)])
            P.v(lambda e, s=s: e.tensor_copy(out=s["cA"][:].rearrange("p c e -> p (c e)"), in_=psY[1][:, 0:C * NEL]), [("psY", 1)], [kk("cA")])
            cur, nxt = "cA", "cB"
            sh = 1
            while sh < C:
                P.v(lambda e, s=s, cur=cur, nxt=nxt, sh=sh: e.tensor_copy(out=s[nxt][:, 0:sh, :], in_=s[cur][:, 0:sh, :]), [kk(cur)], [kk(nxt)])
                P.v(lambda e, s=s, cur=cur, nxt=nxt, sh=sh: e.tensor_tensor(out=s[nxt][:, sh:C, :], in0=s[cur][:, sh:C, :], in1=s[cur][:, 0:C - sh, :],
                                                                          op=ALU.add), [kk(cur)], [kk(nxt)])
                cur, nxt = nxt, cur
                sh *= 2
            P.v(lambda e, s=s, cur=cur: e.tensor_tensor(out=s["pos"][:].rearrange("p c e -> p (c e)"), in0=s[cur][:].rearrange("p c e -> p (c e)"),
                                                        in1=psY[1][:, 0:C * NEL], op=ALU.subtract), [kk(cur), ("psY", 1)], [kk("pos")])
            P.v(lambda e, s=s: e.tensor_tensor(out=s["pos"][:].rearrange("p c e -> p (c e)"), in0=s["pos"][:].rearrange("p c e -> p (c e)"),
                                               in1=psY[0][:, 0:C * NEL], op=ALU.add), [kk("pos"), ("psY", 0)], [kk("pos")])
            P.v(lambda e, s=s: e.tensor_tensor(out=s["pos"][:], in0=s["pos"][:], in1=s["mask"][:], op=ALU.mult), [kk("pos"), kk("mask")], [kk("pos")])
            P.v(lambda e, s=s: e.tensor_scalar(out=s["pos"][:], in0=s["pos"][:], scalar1=-1.0, scalar2=None, op0=ALU.add), [kk("pos")], [kk("pos")])
            P.v(lambda e, s=s, r0=r0: e.tensor_scalar(out=s["vals"][:, :, :, 0], in0=tokid[:, 0:C].unsqueeze(2).to_broadcast([128, C, NEL]),
                                                      scalar1=float(r0), scalar2=None, op0=ALU.add), ["cst"], [kk("vals")])
            P.v(lambda e, s=s: e.tensor_copy(out=s["vals"][:, :, :, 1], in_=s["aff"][:]), [kk("aff")], [kk("vals")])
            nst = (k + 127) // 128
            sw = min(k, 128)
            n_oh = 0
            for el in range(NEL):
                for c in range(C):
                    ohb = s["oh"][n_oh % 2]
                    kb = (nm, "oh", n_oh % 2)
                    n_oh += 1
                    P.v(lambda e, ohb=ohb, s=s, c=c, el=el, k=k: e.tensor_scalar(out=ohb[:], in0=iota[:, 0:k], scalar1=s["pos"][:, c, el:el + 1],
                                                                                scalar2=None, op0=ALU.is_equal), ["cst", kk("pos")], [kb])
                    for sti in range(nst):
                        P.t(lambda e, ohb=ohb, s=s, c=c, el=el, sti=sti, sw=sw: e.matmul(
                            psU[0][0:sw, (el * 4 + sti) * 2:(el * 4 + sti) * 2 + 2], lhsT=ohb[:, sti * 128:sti * 128 + sw],
                            rhs=s["vals"][:, c, el, :], start=(c == 0 and el == 0 and sti == 0), stop=(c == C - 1)),
                            [kb, kk("vals")], [("psU", 0)])
            P.v(lambda e, s=s, sw=sw: e.tensor_copy(out=s["sl"][0:sw].rearrange("p e s t -> p (e s t)"), in_=psU[0][0:sw, 0:NEL * 8]), [("psU", 0)], [kk("sl")])
            P.v(lambda e, s=s, sw=sw: e.tensor_copy(out=s["idx"][0:sw], in_=s["sl"][0:sw, :, :, 0]), [kk("sl")], [kk("idx")])
            P.v(lambda e, s=s, sw=sw: e.tensor_copy(out=s["gate"][0:sw], in_=s["sl"][0:sw, :, :, 1]), [kk("sl")], [kk("gate")])

        for sargs in sets:
            select(*sargs)

        tiles = [("L", sti, 128, sti * 128) for sti in range(4)]
        if with_ctx:
            tiles.append(("C", 0, 32, 512))
        ncols = 544 if with_ctx else 512
        ring_n = [0]
        ny = [0]

        def load_unit(wsrc, el, cb):
            rb = ring_n[0] % 4
            ring_n[0] += 1
            for k4 in range(4):
                src = wsrc[el, k4 * 512:(k4 + 1) * 512, cb * 512:(cb + 1) * 512].rearrange("(kc p) n -> p kc n", p=128)
                P.dma(lambda e, rb=rb, k4=k4, src=src: e.dma_start(out=ring[rb][:, k4 * 4:(k4 + 1) * 4, :], in_=src), w=[("ring", rb)], q="gpsimd")
            return rb

        for el in range(NEL):
            for ti, (nm, sti, n, c0) in enumerate(tiles):
                s = S[nm]
                xb = xg[ti % 2]
                kx = ("xg", ti % 2)
                P.op("gpsimd", lambda e, xb=xb, s=s, el=el, sti=sti, n=n: e.indirect_dma_start(
                    out=xb[0:n, :], out_offset=None, in_=hx2[:, :],
                    in_offset=bass.IndirectOffsetOnAxis(ap=s["idx"][0:n, el, sti:sti + 1], axis=0)), [(nm, "idx")], [kx], dma=True)
                for kc in range(16):
                    P.t(lambda e, xb=xb, kc=kc, n=n: e.transpose(psT[:, kc, 0:n], xb[0:n, kc * 128:(kc + 1) * 128], identb[0:n, 0:n]), [kx, "identb"], ["psT"])
                P.a(lambda e, c0=c0, n=n: e.activation(out=xgT[:, :, c0:c0 + n], in_=psT[:, :, 0:n], func=AF.Copy), ["psT"], ["xgT"])
            for cb in range(4):
                rg = load_unit(wg, el, cb)
                ru = load_unit(wu, el, cb)
                for f4 in range(4):
                    fc = cb * 4 + f4
                    pa = psA[fc % 2]
                    pu = psU[fc % 2]
                    for (pp, rbuf, key) in ((pa, rg, "psA"), (pu, ru, "psU")):
                        for kc in range(16):
                            P.t(lambda e, pp=pp, rbuf=rbuf, kc=kc, f4=f4: e.matmul(pp[:, 0:512], lhsT=ring[rbuf][:, kc, f4 * 128:(f4 + 1) * 128],
                                                                                  rhs=xgT[:, kc, 0:512], start=(kc == 0), stop=(kc == 15)),
                                [("ring", rbuf), "xgT"], [(key, fc % 2)])
                    P.a(lambda e, pa=pa: e.activation(out=sil[:, 0:512], in_=pa[:, 0:512], func=AF.Silu), [("psA", fc % 2)], ["sil"])
                    P.v(lambda e, pu=pu, fc=fc: e.tensor_tensor(out=hT[:, fc, 0:512], in0=sil[:, 0:512], in1=pu[:, 0:512], op=ALU.mult),
                        ["sil", ("psU", fc % 2)], ["hT"])
                    if with_ctx:
                        for (pp, rbuf, key) in ((pa, rg, "psA"), (pu, ru, "psU")):
                            for kc in range(16):
                                P.t(lambda e, pp=pp, rbuf=rbuf, kc=kc, f4=f4: e.matmul(pp[:, 0:32], lhsT=ring[rbuf][:, kc, f4 * 128:(f4 + 1) * 128],
                                                                                      rhs=xgT[:, kc, 512:544], start=(kc == 0), stop=(kc == 15)),
                                    [("ring", rbuf), "xgT"], [(key, fc % 2)])
                        P.a(lambda e, pa=pa: e.activation(out=sil[:, 512:544], in_=pa[:, 0:32], func=AF.Silu), [("psA", fc % 2)], ["sil"])
                        P.v(lambda e, pu=pu, fc=fc: e.tensor_tensor(out=hT[:, fc, 512:544], in0=sil[:, 512:544], in1=pu[:, 0:32], op=ALU.mult),
                            ["sil", ("psU", fc % 2)], ["hT"])
            for cb in range(4):
                rd = load_unit(wd, el, cb)
                for ti, (nm, sti, n, c0) in enumerate(tiles):
                    s = S[nm]
                    py = psY[ny[0] % 2]
                    ky = ("psY", ny[0] % 2)
                    ybuf = yb[ny[0] % 3]
                    kyb = ("yb", ny[0] % 3)
                    ny[0] += 1
                    for fc in range(16):
                        P.t(lambda e, py=py, rd=rd, fc=fc, c0=c0, n=n: e.matmul(py[0:n, :], lhsT=hT[:, fc, c0:c0 + n], rhs=ring[rd][:, fc, :],
                                                                               start=(fc == 0), stop=(fc == 15)), ["hT", ("ring", rd)], [ky])
                    P.v(lambda e, py=py, ybuf=ybuf, s=s, el=el, sti=sti, n=n: e.tensor_scalar(out=ybuf[0:n, :], in0=py[0:n, :],
                                                                                              scalar1=s["gate"][0:n, el, sti:sti + 1], scalar2=None, op0=ALU.mult),
                        [ky, (nm, "gate")], [kyb])
                    P.op("gpsimd", lambda e, ybuf=ybuf, s=s, el=el, sti=sti, n=n, cb=cb: e.indirect_dma_start(
                        out=delta[cb][:, :], out_offset=bass.IndirectOffsetOnAxis(ap=s["idx"][0:n, el, sti:sti + 1], axis=0),
                        in_=ybuf[0:n, :], in_offset=None, compute_op=ALU.add), [kyb, (nm, "idx"), ("delta", cb)], [("delta", cb)], dma=True)
        P.emit(fused=fused)
    return nc


def s3_consts():
    c = np.zeros((128, 930), np.float32)
    c[:, 0:128] = np.eye(128)
    c[:, 128:256] = 1.0
    c[:, 256:384] = np.triu(np.ones((128, 128)))
    c[:, 384:896] = np.arange(512)[None, :]
    c[:, 896:930] = np.arange(34)[None, :] * 128 + np.arange(128)[:, None]
    return c


def build_s4(nc=None, io=None, pfx=""):
    if nc is None:
        nc = bass.Bass("TRN2", target_bir_lowering=False)
    fused, di, do = _mk(nc, io, pfx)
    x1 = di("x1", [NTOK, D])
    dA = di("dA", [NTOK, D])
    dB = None if fused else di("dB", [NTOK, D])
    modx = di("modx", [6, D])
    modc = di("modc", [6, D])
    x2 = do("x2", [NTOK, D])
    with stage_ctx(nc, fused) as st:
        sb = lambda n, s, d=F32: st.enter_context(nc.sbuf_tensor(pfx + n, s, d))
        G = sb("G", [128, D])
        xa = [sb(f"xa{i}", [128, D]) for i in range(2)]
        da = [sb(f"da{i}", [128, D]) for i in range(2)]
        db = [sb(f"db{i}", [128, D]) for i in range(2)]
        P = Prog(nc)
        for t in range(NT):
            i = t % 2
            if t == 0:
                P.dma(lambda e: e.dma_start(out=G[:], in_=_bc(modc[5], D)), w=["G"])
            if t == 2:
                P.dma(lambda e: e.dma_start(out=G[:], in_=_bc(modx[5], D)), w=["G"])
            r0 = t * 128
            P.dma(lambda e, i=i, r0=r0: e.dma_start(out=xa[i][:], in_=x1[r0:r0 + 128, :]), w=[("xa", i)])
            P.dma(lambda e, i=i, r0=r0: e.dma_start(out=da[i][:], in_=dA[r0:r0 + 128, :]), w=[("da", i)])
            if not fused:
                P.dma(lambda e, i=i, r0=r0: e.dma_start(out=db[i][:], in_=dB[r0:r0 + 128, :]), w=[("db", i)])
                P.v(lambda e, i=i: e.tensor_tensor(out=da[i][:], in0=da[i][:], in1=db[i][:], op=ALU.add), [("da", i), ("db", i)], [("da", i)])
            P.v(lambda e, i=i: e.tensor_tensor(out=da[i][:], in0=da[i][:], in1=G[:], op=ALU.mult), [("da", i), "G"], [("da", i)])
            P.v(lambda e, i=i: e.tensor_tensor(out=xa[i][:], in0=xa[i][:], in1=da[i][:], op=ALU.add), [("xa", i), ("da", i)], [("xa", i)])
            P.dma(lambda e, i=i, r0=r0: e.dma_start(out=x2[r0:r0 + 128, :], in_=xa[i][:]), [("xa", i)], [("x2", t)], q="gpsimd")
        P.emit(fused=fused)
    return nc


def _rope_tables(h):
    t = np.arange(h * 2048, (h + 1) * 2048)
    row = (t // 64).astype(np.float32)
    col = (t % 64).astype(np.float32)
    inv = (np.float32(10000.0) ** (-np.arange(32, dtype=np.float32) / np.float32(32))).astype(np.float32)
    ar = row[:, None] * inv
    ac = col[:, None] * inv
    tab = np.zeros((NTOK, 128), np.float32)
    tab[:256, :64] = 1.0
    tab[256:, :64] = np.concatenate([np.cos(ar), np.cos(ac)], 1)
    tab[256:, 64:] = np.concatenate([np.sin(ar), np.sin(ac)], 1)
    return tab


def _masks(h):
    s = np.arange(128)[:, None]
    q = np.arange(128)[None, :]
    mp = (s >= q).astype(np.float32)
    mn = (s <= q).astype(np.float32)
    return np.stack([mp, mn, mp * (0.0 if h == 0 else 1.0), mn * (0.0 if h == 1 else 1.0)])


def _run(nc, in_maps):
    res = run_bass_kernel_spmd(nc, in_maps, core_ids=list(range(8)))
    return res.results


PAIRS = [[0, 1], [2, 3], [4, 5], [6, 7]]
NXR = NTOK + 128
CAPL = 384
NCST = 931


def f_consts():
    c = np.zeros((128, NCST), np.float32)
    c[:, 0:930] = s3_consts()
    c[:, 930] = NTOK + np.arange(128)
    return c


def stage_mod(nc, io, pfx):
    cin, w, b, modS = io["cin"], io["w_mod"], io["b_mod"], io["modS"]
    with stage_ctx(nc, True) as st:
        sb = lambda n, s, d=F32: st.enter_context(nc.sbuf_tensor(pfx + n, s, d))
        ct = sb("ct", [128, 16, 2])
        cs = sb("cs", [128, 16, 2])
        wt = [sb(f"wt{i}", [128, 16, 512]) for i in range(3)]
        bt = [sb(f"bt{i}", [2, 512]) for i in range(3)]
        ot = [sb(f"ot{i}", [2, 512]) for i in range(3)]
        ps = [st.enter_context(nc.psum_tensor(pfx + f"ps{i}", [2, 512], F32)) for i in range(2)]
        P = Prog(nc)
        P.dma(lambda e: e.dma_start(out=ct[:], in_=cin), w=["ct"])
        P.a(lambda e: e.activation(out=cs[:], in_=ct[:], func=AF.Silu), ["ct"], ["cs"])
        gi = 0
        for l in range(2):
            for g in range(24):
                i3 = gi % 3
                i2 = gi % 2
                gi += 1
                cols = slice(g * 512, (g + 1) * 512)
                src = w[l, :, cols].rearrange("(kc p) n -> p kc n", p=128)
                P.dma(lambda e, i3=i3, src=src: e.dma_start(out=wt[i3][:], in_=src), w=[("wt", i3)])
                P.dma(lambda e, i3=i3, l=l, cols=cols: e.dma_start(out=bt[i3][:], in_=b[l:l + 1, cols].to_broadcast([2, 512])), w=[("bt", i3)])
                for kc in range(16):
                    P.t(lambda e, i2=i2, i3=i3, kc=kc: e.matmul(ps[i2][:], lhsT=cs[:, kc, :], rhs=wt[i3][:, kc, :], start=(kc == 0), stop=(kc == 15)),
                        ["cs", ("wt", i3)], [("ps", i2)])
                P.v(lambda e, i2=i2, i3=i3: e.tensor_tensor(out=ot[i3][:], in0=ps[i2][:], in1=bt[i3][:], op=ALU.add), [("ps", i2), ("bt", i3)], [("ot", i3)])
                P.dma(lambda e, i3=i3, l=l, cols=cols: e.dma_start(out=modS[l, :, cols], in_=ot[i3][:]), [("ot", i3)], [("modS", l, g)])
        P.emit(fused=True)


def stage_mod_split(nc, io, pfx, ncores):
    ncol = 6 * D // ncores
    cin, w, b, selT_in = io["cin5"], io["wms"], io["bms"], io["selT"]
    modP, modG, modS = io["modP"], io["modG"], io["modS"]
    with stage_ctx(nc, True) as st:
        sb = lambda n, s, d=F32: st.enter_context(nc.sbuf_tensor(pfx + n, s, d))
        ct = sb("ct", [128, 16, 5])
        cs = sb("cs", [128, 16, 5])
        wt = [sb(f"wt{i}", [128, 16, 512]) for i in range(3)]
        bt = [sb(f"bt{i}", [5, 512]) for i in range(3)]
        ot = [sb(f"ot{i}", [5, 512]) for i in range(3)]
        ps = [st.enter_context(nc.psum_tensor(pfx + f"ps{i}", [5, 512], F32)) for i in range(2)]
        P = Prog(nc)
        P.dma(lambda e: e.dma_start(out=ct[:], in_=cin), w=["ct"])
        P.a(lambda e: e.activation(out=cs[:], in_=ct[:], func=AF.Silu), ["ct"], ["cs"])
        gi = 0
        for l in range(2):
            for g in range(ncol // 512):
                i3 = gi % 3
                i2 = gi % 2
                gi += 1
                cols = slice(g * 512, (g + 1) * 512)
                src = w[l, :, cols].rearrange("(kc p) n -> p kc n", p=128)
                P.dma(lambda e, i3=i3, src=src: e.dma_start(out=wt[i3][:], in_=src), w=[("wt", i3)])
                P.dma(lambda e, i3=i3, l=l, cols=cols: e.dma_start(out=bt[i3][:], in_=b[l:l + 1, cols].to_broadcast([5, 512])), w=[("bt", i3)])
                for kc in range(16):
                    P.t(lambda e, i2=i2, i3=i3, kc=kc: e.matmul(ps[i2][:], lhsT=cs[:, kc, :], rhs=wt[i3][:, kc, :], start=(kc == 0), stop=(kc == 15)),
                        ["cs", ("wt", i3)], [("ps", i2)])
                P.v(lambda e, i2=i2, i3=i3: e.tensor_tensor(out=ot[i3][:], in0=ps[i2][:], in1=bt[i3][:], op=ALU.add), [("ps", i2), ("bt", i3)], [("ot", i3)])
                P.dma(lambda e, i3=i3, l=l, cols=cols: e.dma_start(out=modP[l * 5:(l + 1) * 5, cols], in_=ot[i3][:]), [("ot", i3)], [("modP", l, g)],
                      q="gpsimd")
        P.emit(fused=True)
    stage_cc(nc, [(modP, modG)], groups=[list(range(ncores))])
    with stage_ctx(nc, True) as st:
        sb = lambda n, s, d=F32: st.enter_context(nc.sbuf_tensor(pfx + "b" + n, s, d))
        G = sb("G", [5, 2, ncores, ncol])
        sel = sb("sel", [5, 2])
        o2 = [sb(f"o2{i}", [2, 512]) for i in range(3)]
        ps = [st.enter_context(nc.psum_tensor(pfx + f"bps{i}", [2, 512], F32)) for i in range(2)]
        P = Prog(nc)
        P.dma(lambda e: e.dma_start(out=sel[:], in_=selT_in), w=["sel"])
        mv = modG.rearrange("(k l r) n -> l r k n", l=2, r=5)
        for l in range(2):
            P.dma(lambda e, l=l: e.dma_start(out=G[:, l], in_=mv[l]), w=[("G", l)])
        gi = 0
        for l in range(2):
            for k in range(ncores):
                for g in range(ncol // 512):
                    i3 = gi % 3
                    i2 = gi % 2
                    gi += 1
                    P.t(lambda e, i2=i2, l=l, k=k, g=g: e.matmul(ps[i2][:], lhsT=sel[:], rhs=G[:, l, k, g * 512:(g + 1) * 512], start=True, stop=True),
                        ["sel", ("G", l)], [("ps", i2)])
                    P.v(lambda e, i2=i2, i3=i3: e.tensor_copy(out=o2[i3][:], in_=ps[i2][:]), [("ps", i2)], [("o2", i3)])
                    c0 = k * ncol + g * 512
                    P.dma(lambda e, i3=i3, l=l, c0=c0: e.dma_start(out=modS[l, :, c0:c0 + 512], in_=o2[i3][:]), [("o2", i3)], [("modS", l, c0)],
                          q="gpsimd")
        P.emit(fused=True)


def stage_cc(nc, pairs, groups=None):
    groups = PAIRS if groups is None else groups
    with nc.cleanup_on_exit():
        _UID[0] += 1
        sem = nc.alloc_semaphore(name="cc%d" % _UID[0])
        with nc.Block() as block:
            @block.gpsimd
            def _(g):
                for i, (a, b) in enumerate(pairs):
                    g.collective_compute("AllGather", ALU.bypass, replica_groups=groups, ins=[a], outs=[b]).then_inc(sem, 1)
                    g.wait_ge(sem, i + 1)
        nc.all_engine_barrier()


def stage_s3a(nc, io, with_ctx, pfx):
    affAll, affIn, affC, consts = io["affAll"], io["affIn"], io["affC"], io["consts"]
    idxT, gateT, delta, hx2 = io["idxT"], io["gateT"], io["delta"], io["hx2o"]
    with stage_ctx(nc, True) as st:
        sb = lambda n, s, d=F32: st.enter_context(nc.sbuf_tensor(pfx + n, s, d))
        pt = lambda n, s, d=F32: st.enter_context(nc.psum_tensor(pfx + n, s, d))
        cst = sb("cst", [128, NCST])
        trib = sb("trib", [128, 128], BF16)
        onesb = sb("onesb", [128, 128], BF16)
        jt = sb("jt", [128, NE, 15])
        zt = sb("zt", [128, D])
        ztb = sb("ztb", [128, D], BF16)
        sl = sb("sl", [128, NE, 4, 3])
        idxf = sb("idxf", [128, NE, 4])
        idx = sb("idx", [128, NE, 4], I32)
        gate = sb("gate", [128, NE, 4])
        psC = pt("psC", [128, 512])
        psP = [pt(f"psP{i}", [128, 512]) for i in range(2)]
        psS = [pt(f"psS{i}", [128, 512]) for i in range(2)]
        ones = cst[:, 128:256]
        tri = cst[:, 256:384]
        iota = cst[:, 384:896]
        tokid = cst[:, 896:930]
        dummy = cst[:, 930:931]
        P = Prog(nc)
        P.dma(lambda e: e.dma_start(out=cst[:], in_=consts), w=["cst"])
        P.v(lambda e: e.tensor_copy(out=trib[:], in_=tri), ["cst"], ["trib"])
        P.v(lambda e: e.tensor_copy(out=onesb[:], in_=ones), ["cst"], ["onesb"])
        for j in range(15):
            P.v(lambda e, j=j: e.memset(jt[:, :, j:j + 1], float(j + 1)), [], ["jt"])
        P.v(lambda e: e.memset(zt[:], 0.0), [], ["zt"])
        P.v(lambda e: e.memset(ztb[:], 0.0), [], ["ztb"])
        P.v(lambda e: e.memset(sl[:], 0.0), [], ["sl"])
        for r in range(NXR // 128):
            P.dma(lambda e, r=r: e.dma_start(out=delta[r * 128:(r + 1) * 128, :], in_=zt[:]), ["zt"], [("delta", r)])
        P.dma(lambda e: e.dma_start(out=hx2[NTOK:NXR, :], in_=ztb[:]), ["ztb"], ["hx2d"])

        def select(nm, Cth, thr_ap, Cm, mask_ap, k, r0, cap, st0, psSl):
            kk = lambda x: (nm, x)
            athr = sb(f"athr{nm}", [128, Cth, NE])
            am = athr if mask_ap is None else sb(f"am{nm}", [128, Cm, NE])
            kam = kk("athr") if mask_ap is None else kk("am")
            lo = sb(f"lo{nm}", [128, NE])
            th = sb(f"th{nm}", [128, NE, 15])
            cmpb = sb(f"cmp{nm}", [128, Cth, NE, 15], BF16)
            cntp = sb(f"cntp{nm}", [128, NE, 15])
            ge = sb(f"ge{nm}", [128, NE, 15])
            nge = sb(f"nge{nm}", [128, NE])
            mask = sb(f"mask{nm}", [128, Cm, NE])
            maskb = sb(f"maskb{nm}", [128, Cm, NE], BF16)
            cA = sb(f"cA{nm}", [128, Cm, NE])
            cB = sb(f"cB{nm}", [128, Cm, NE])
            pos = sb(f"pos{nm}", [128, Cm, NE])
            vals = sb(f"vals{nm}", [128, Cm, NE, 3])
            oh = [sb(f"oh{nm}{i}", [128, cap]) for i in range(2)]
            bufs = {"cA": cA, "cB": cB}
            P.dma(lambda e: e.dma_start(out=athr[:], in_=thr_ap.rearrange("(c p) e -> p c e", p=128)), w=[kk("athr")])
            if mask_ap is not None:
                P.dma(lambda e: e.dma_start(out=am[:], in_=mask_ap.rearrange("(c p) e -> p c e", p=128)), w=[kk("am")])
            P.v(lambda e: e.memset(lo[:], 0.0), [], [kk("lo")])
            affb = athr[:].unsqueeze(3).to_broadcast([128, Cth, NE, 15])
            for it in range(8):
                step = 16.0 ** -(it + 1)
                P.v(lambda e, step=step: e.scalar_tensor_tensor(out=th[:], in0=jt[:], scalar=step, in1=lo[:].unsqueeze(2).to_broadcast([128, NE, 15]),
                                                                op0=ALU.mult, op1=ALU.add), ["jt", kk("lo")], [kk("th")])
                P.v(lambda e: e.tensor_tensor(out=cmpb[:], in0=affb, in1=th[:].unsqueeze(1).to_broadcast([128, Cth, NE, 15]), op=ALU.is_ge),
                    [kk("athr"), kk("th")], [kk("cmp")])
                P.v(lambda e: e.tensor_reduce(out=cntp[:], in_=cmpb[:].rearrange("p c e j -> p e j c"), axis=AX.X, op=ALU.add), [kk("cmp")], [kk("cntp")])
                P.t(lambda e: e.matmul(psC[:, 0:NE * 15], lhsT=ones, rhs=cntp[:].rearrange("p e j -> p (e j)"), start=True, stop=True),
                    ["cst", kk("cntp")], ["psC"])
                P.v(lambda e: e.tensor_scalar(out=ge[:].rearrange("p e j -> p (e j)"), in0=psC[:, 0:NE * 15], scalar1=float(k) - 0.5, scalar2=None,
                                              op0=ALU.is_ge), ["psC"], [kk("ge")])
                P.v(lambda e: e.tensor_reduce(out=nge[:], in_=ge[:], axis=AX.X, op=ALU.add), [kk("ge")], [kk("nge")])
                P.v(lambda e, step=step: e.scalar_tensor_tensor(out=lo[:], in0=nge[:], scalar=step, in1=lo[:], op0=ALU.mult, op1=ALU.add),
                    [kk("nge"), kk("lo")], [kk("lo")])
            P.v(lambda e: e.tensor_tensor(out=mask[:], in0=am[:], in1=lo[:].unsqueeze(1).to_broadcast([128, Cm, NE]), op=ALU.is_ge),
                [kam, kk("lo")], [kk("mask")])
            P.v(lambda e: e.tensor_copy(out=maskb[:], in_=mask[:]), [kk("mask")], [kk("maskb")])
            mb2 = maskb[:].rearrange("p c e -> p (c e)")
            ncol = Cm * NE
            P.t(lambda e: e.matmul(psP[0][:, 0:ncol], lhsT=trib[:], rhs=mb2, start=True, stop=True), ["trib", kk("maskb")], [("psP", 0)])
            P.t(lambda e: e.matmul(psP[1][:, 0:ncol], lhsT=onesb[:], rhs=mb2, start=True, stop=True), ["onesb", kk("maskb")], [("psP", 1)])
            P.v(lambda e: e.tensor_copy(out=cA[:].rearrange("p c e -> p (c e)"), in_=psP[1][:, 0:ncol]), [("psP", 1)], [kk("cA")])
            cur, nxt = "cA", "cB"
            sh = 1
            while sh < Cm:
                P.v(lambda e, cur=cur, nxt=nxt, sh=sh: e.tensor_copy(out=bufs[nxt][:, 0:sh, :], in_=bufs[cur][:, 0:sh, :]), [kk(cur)], [kk(nxt)])
                P.v(lambda e, cur=cur, nxt=nxt, sh=sh: e.tensor_tensor(out=bufs[nxt][:, sh:Cm, :], in0=bufs[cur][:, sh:Cm, :], in1=bufs[cur][:, 0:Cm - sh, :],
                                                                  op=ALU.add), [kk(cur)], [kk(nxt)])
                cur, nxt = nxt, cur
                sh *= 2
            pos2 = pos[:].rearrange("p c e -> p (c e)")
            P.v(lambda e, cur=cur: e.tensor_tensor(out=pos2, in0=bufs[cur][:].rearrange("p c e -> p (c e)"), in1=psP[1][:, 0:ncol], op=ALU.subtract),
                [kk(cur), ("psP", 1)], [kk("pos")])
            P.v(lambda e: e.tensor_tensor(out=pos2, in0=pos2, in1=psP[0][:, 0:ncol], op=ALU.add), [kk("pos"), ("psP", 0)], [kk("pos")])
            P.v(lambda e: e.tensor_tensor(out=pos[:], in0=pos[:], in1=mask[:], op=ALU.mult), [kk("pos"), kk("mask")], [kk("pos")])
            P.v(lambda e: e.tensor_scalar(out=pos[:], in0=pos[:], scalar1=-1.0, scalar2=None, op0=ALU.add), [kk("pos")], [kk("pos")])
            P.v(lambda e: e.tensor_scalar(out=vals[:, :, :, 0], in0=tokid[:, 0:Cm].unsqueeze(2).to_broadcast([128, Cm, NE]), scalar1=float(r0), scalar2=None,
                                          op0=ALU.add), ["cst"], [kk("vals")])
            P.v(lambda e: e.tensor_copy(out=vals[:, :, :, 1], in_=am[:]), [kam], [kk("vals")])
            P.v(lambda e: e.memset(vals[:, :, :, 2], 1.0), [], [kk("vals")])
            nst = (cap + 127) // 128
            sw = min(cap, 128)
            n_oh = 0
            first = True
            for el in range(NE):
                for c in range(Cm):
                    ohb = oh[n_oh % 2]
                    kb = (nm, "oh", n_oh % 2)
                    n_oh += 1
                    P.v(lambda e, ohb=ohb, c=c, el=el: e.tensor_scalar(out=ohb[:], in0=iota[:, 0:cap], scalar1=pos[:, c, el:el + 1], scalar2=None,
                                                                      op0=ALU.is_equal), ["cst", kk("pos")], [kb])
                    for sti in range(nst):
                        col = (el * 4 + st0 + sti) * 3
                        P.t(lambda e, ohb=ohb, c=c, el=el, sti=sti, col=col, first=first: e.matmul(
                            psSl[0:sw, col:col + 3], lhsT=ohb[:, sti * 128:sti * 128 + sw], rhs=vals[:, c, el, :],
                            start=first, stop=(c == Cm - 1)), [kb, kk("vals")], [kk("psSl")])
                        first = False
            for sti in range(nst):
                P.v(lambda e, sti=sti: e.tensor_copy(out=sl[0:sw, :, st0 + sti, :],
                                                     in_=psSl[0:sw, 0:NE * 12].rearrange("p (e s t) -> p e s t", e=NE, s=4)[:, :, st0 + sti, :]),
                    [kk("psSl")], ["sl"])

        select("L", 32, affAll, 16, affIn, 512, 256, CAPL, 0, psS[0])
        if with_ctx:
            select("C", 2, affC, 2, None, 32, 0, 32, 3, psS[1])
        P.v(lambda e: e.tensor_scalar(out=idxf[:], in0=sl[:, :, :, 2], scalar1=-1.0, scalar2=dummy, op0=ALU.add, op1=ALU.mult), ["sl", "cst"], ["idxf"])
        P.v(lambda e: e.tensor_tensor(out=idxf[:], in0=sl[:, :, :, 0], in1=idxf[:], op=ALU.subtract), ["sl", "idxf"], ["idxf"])
        P.v(lambda e: e.tensor_copy(out=idx[:], in_=idxf[:]), ["idxf"], ["idx"])
        P.v(lambda e: e.tensor_copy(out=gate[:], in_=sl[:, :, :, 1]), ["sl"], ["gate"])
        P.dma(lambda e: e.dma_start(out=idxT, in_=idx[:]), ["idx"], ["idxT"])
        P.dma(lambda e: e.dma_start(out=gateT, in_=gate[:]), ["gate"], ["gateT"])
        P.emit(fused=True)


def stage_s3b(nc, io, L, with_ctx, pfx):
    idxT, gateT, delta, hx2, consts = io["idxT"], io["gateT"], io["delta"], io["hx2o"], io["consts"]
    wg, wu, wd = io["w_gate"], io["w_up"], io["w_down"]
    with stage_ctx(nc, True) as st:
        sb = lambda n, s, d=F32: st.enter_context(nc.sbuf_tensor(pfx + n, s, d))
        pt = lambda n, s, d=F32: st.enter_context(nc.psum_tensor(pfx + n, s, d))
        ncols = CAPL + (32 if with_ctx else 0)
        cst = sb("cst", [128, 128])
        identb = sb("identb", [128, 128], BF16)
        idx = sb("idx", [128, NE, 4], I32)
        gate = sb("gate", [128, NE, 4])
        NR, NSTG, PFD = 5, 6, 3
        ring = [sb(f"ring{i}", [128, 16, 512], BF16) for i in range(NR)]
        stg = [sb(f"stg{i}", [128, 4, 512]) for i in range(NSTG)]
        xg = [sb(f"xg{i}", [128, D], BF16) for i in range(2)]
        xgT = sb("xgT", [128, 16, ncols], BF16)
        hT = sb("hT", [128, 16, ncols], BF16)
        sil = sb("sil", [128, ncols])
        ybig = [sb(f"ybig{i}", [128, D]) for i in range(4)]
        psA = [pt(f"psA{i}", [128, 512]) for i in range(2)]
        psU = [pt(f"psU{i}", [128, 512]) for i in range(2)]
        psT = pt("psT", [128, 16, 128], BF16)
        psY = [pt(f"psY{i}", [128, 512]) for i in range(2)]
        P = Prog(nc)
        P.dma(lambda e: e.dma_start(out=cst[:], in_=consts[:, 0:128]), w=["cst"])
        P.v(lambda e: e.tensor_copy(out=identb[:], in_=cst[:]), ["cst"], ["identb"])
        P.dma(lambda e: e.dma_start(out=idx[:], in_=idxT), w=["idx"])
        P.dma(lambda e: e.dma_start(out=gate[:], in_=gateT), w=["gate"])
        tiles = [(sti, 128, sti * 128) for sti in range(3)]
        if with_ctx:
            tiles.append((3, 32, CAPL))
        ny = [0]
        seq = []
        for el_ in range(NE):
            for cb_ in range(4):
                seq.append((wg, el_, cb_))
                seq.append((wu, el_, cb_))
            for cb_ in range(4):
                seq.append((wd, el_, cb_))
        issued = [0]
        nq = [0]
        cast_eng = ["scalar", "vector", "scalar", "vector"]

        def issue(i):
            wsrc, el_, cb_ = seq[i]
            rb = i % NR
            for k4 in range(4):
                si = nq[0] % NSTG
                ce = cast_eng[nq[0] % 4]
                nq[0] += 1
                src = wsrc[L, el_, k4 * 512:(k4 + 1) * 512, cb_ * 512:(cb_ + 1) * 512].rearrange("(kc p) n -> p kc n", p=128)
                P.dma(lambda e, si=si, src=src: e.dma_start(out=stg[si][:], in_=src), w=[("stg", si)])
                dst = ring[rb][:, k4 * 4:(k4 + 1) * 4, :]
                if ce == "scalar":
                    P.op("scalar", lambda e, si=si, dst=dst: e.activation(out=dst, in_=stg[si][:], func=AF.Copy), [("stg", si)], [("ring", rb, k4)])
                else:
                    P.op(ce, lambda e, si=si, dst=dst: e.tensor_copy(out=dst, in_=stg[si][:]), [("stg", si)], [("ring", rb, k4)])

        def need(i):
            while issued[0] < min(len(seq), i + PFD + 1):
                issue(issued[0])
                issued[0] += 1
            return i % NR

        un = [0]

        def load_unit(wsrc, el, cb):
            i = un[0]
            un[0] += 1
            assert seq[i][1] == el and seq[i][2] == cb and seq[i][0] is wsrc
            return need(i)

        for el in range(NE):
            for ti, (sti, n, c0) in enumerate(tiles):
                xb = xg[ti % 2]
                kx = ("xg", ti % 2)
                P.op("gpsimd", lambda e, xb=xb, el=el, sti=sti, n=n: e.indirect_dma_start(
                    out=xb[0:n, :], out_offset=None, in_=hx2[:, :],
                    in_offset=bass.IndirectOffsetOnAxis(ap=idx[0:n, el, sti:sti + 1], axis=0)), ["idx"], [kx], dma=True)
                for kc in range(16):
                    P.t(lambda e, xb=xb, kc=kc, n=n: e.transpose(psT[:, kc, 0:n], xb[0:n, kc * 128:(kc + 1) * 128], identb[0:n, 0:n]), [kx, "identb"], ["psT"])
                P.a(lambda e, c0=c0, n=n: e.activation(out=xgT[:, :, c0:c0 + n], in_=psT[:, :, 0:n], func=AF.Copy), ["psT"], ["xgT"])
            for cb in range(4):
                rg = load_unit(wg, el, cb)
                ru = load_unit(wu, el, cb)
                for f4 in range(4):
                    fc = cb * 4 + f4
                    pa = psA[fc % 2]
                    pu = psU[fc % 2]
                    for (pp, rbuf, key) in ((pa, rg, "psA"), (pu, ru, "psU")):
                        for kc in range(16):
                            P.t(lambda e, pp=pp, rbuf=rbuf, kc=kc, f4=f4: e.matmul(pp[:, 0:ncols], lhsT=ring[rbuf][:, kc, f4 * 128:(f4 + 1) * 128],
                                                                                  rhs=xgT[:, kc, :], start=(kc == 0), stop=(kc == 15)),
                                [("ring", rbuf, kc // 4), "xgT"], [(key, fc % 2)])
                    P.a(lambda e, pa=pa: e.activation(out=sil[:], in_=pa[:, 0:ncols], func=AF.Silu), [("psA", fc % 2)], ["sil"])
                    P.v(lambda e, pu=pu, fc=fc: e.tensor_tensor(out=hT[:, fc, :], in0=sil[:], in1=pu[:, 0:ncols], op=ALU.mult),
                        ["sil", ("psU", fc % 2)], ["hT"])
            for cb in range(4):
                rd = load_unit(wd, el, cb)
                for ti, (sti, n, c0) in enumerate(tiles):
                    py = psY[ny[0] % 2]
                    ky = ("psY", ny[0] % 2)
                    ny[0] += 1
                    for fc in range(16):
                        P.t(lambda e, py=py, rd=rd, fc=fc, c0=c0, n=n: e.matmul(py[0:n, :], lhsT=hT[:, fc, c0:c0 + n], rhs=ring[rd][:, fc, :],
                                                                               start=(fc == 0), stop=(fc == 15)), ["hT", ("ring", rd, fc // 4)], [ky])
                    P.v(lambda e, py=py, ti=ti, el=el, sti=sti, n=n, cb=cb: e.tensor_scalar(out=ybig[ti][0:n, cb * 512:(cb + 1) * 512], in0=py[0:n, :],
                                                                                           scalar1=gate[0:n, el, sti:sti + 1], scalar2=None, op0=ALU.mult),
                        [ky, "gate"], [("ybig", ti)])
            for ti, (sti, n, c0) in enumerate(tiles):
                P.op("gpsimd", lambda e, ti=ti, el=el, sti=sti, n=n: e.indirect_dma_start(
                    out=delta[:, :], out_offset=bass.IndirectOffsetOnAxis(ap=idx[0:n, el, sti:sti + 1], axis=0),
                    in_=ybig[ti][0:n, :], in_offset=None, compute_op=ALU.add), [("ybig", ti), "idx", "delta"], ["delta"], dma=True)
        P.emit(fused=True)


def stage_copy_out(nc, src, dst, pfx):
    with stage_ctx(nc, True) as st:
        bufs = [st.enter_context(nc.sbuf_tensor(pfx + f"cb{i}", [128, D], F32)) for i in range(3)]
        P = Prog(nc)
        for t in range(16):
            b = bufs[t % 3]
            P.dma(lambda e, b=b, t=t: e.dma_start(out=b[:], in_=src[256 + t * 128:256 + (t + 1) * 128, :]), w=[("cb", t % 3)])
            P.dma(lambda e, b=b, t=t: e.dma_start(out=dst[t * 128:(t + 1) * 128, :], in_=b[:]), [("cb", t % 3)], [("out", t)])
        P.emit(fused=True)


def build_fused(ncores=8):
    global PAIRS
    PAIRS = [[2 * i, 2 * i + 1] for i in range(ncores // 2)]
    nc = bass.Bass("TRN2", target_bir_lowering=False)
    ein = lambda n, s, d=F32: nc.dram_tensor(n, s, d, kind="ExternalInput").ap()
    scr = lambda n, s, d=F32: nc.dram_tensor(n, s, d, kind="Internal").ap()
    scrl = lambda n, s, d=F32: nc.dram_tensor(n, s, d, kind="Internal", addr_space="Local").ap()
    X = {}
    X["xin0"] = ein("xin", [NTOK, D])
    mcol = 6 * D // ncores
    X["cin5"] = ein("cin5", [128, 16, 5])
    X["wms"] = ein("wms", [2, D, mcol])
    X["bms"] = ein("bms", [2, mcol])
    X["selT"] = ein("selT", [5, 2])
    n1g = ein("n1g", [2, D]); n2g = ein("n2g", [2, D])
    win = ein("win", [2, D, 3584])
    gqk = ein("gqk", [2, NQK * 128])
    X["cs_t"] = ein("cs_t", [NTOK, 128])
    vnb = ein("vnb", [2, 512]); wsT = ein("wsT", [2, 128, 4, 128]); bsT = ein("bsT", [2, 128, 4]); ogb = ein("ogb", [2, 512])
    X["ident"] = ein("ident", [128, 128])
    X["msk"] = ein("msk", [4, 128, 128])
    sink = ein("sink", [2, 6]); gac = ein("gac", [2, 1536])
    wout = ein("wout", [2, D, D]); wr = ein("wr", [2, D, NE])
    X["w_gate"] = ein("w_gate", [2, NE, D, D]); X["w_up"] = ein("w_up", [2, NE, D, D]); X["w_down"] = ein("w_down", [2, NE, D, D])
    X["consts"] = ein("consts", [128, NCST])
    out = nc.dram_tensor("out", [2048, D], F32, kind="ExternalOutput").ap()
    modS = scr("modS", [2, 2, 6 * D])
    X["modS"] = modS
    X["modP"] = scr("modP", [10, mcol]); X["modG"] = scrl("modG", [10 * ncores, mcol])
    X["QKT"] = scr("QKT", [128, NQK, NTOK], BF16); X["Vout"] = scr("Vout", [NTOK, 512], BF16)
    X["yB"] = scr("yB", [NTOK, 512], BF16); X["yAC"] = scr("yAC", [NTOK, 1536], BF16)
    X["KTx"] = scr("KTx", [512, 2048], BF16); X["KTg"] = scrl("KTg", [1024, 2048], BF16)
    X["Vx"] = scr("Vx", [2048, 512], BF16); X["Vg"] = scrl("Vg", [4096, 512], BF16)
    xs = scr("xs", [NXR, D]); X["hx2o"] = scr("hx2s", [NXR, D], BF16); X["delta"] = scr("delta", [NXR, D])
    X["affIn"] = scr("affIn", [2048, NE]); X["affC"] = scr("affC", [256, NE]); X["affAll"] = scrl("affAll", [4096, NE])
    X["idxT"] = scr("idxT", [128, NE, 4], I32); X["gateT"] = scr("gateT", [128, NE, 4])

    stage_mod_split(nc, X, "m_", ncores)
    for L in range(2):
        io = dict(X)
        io["xin"] = X["xin0"] if L == 0 else xs
        io["modx"] = modS[L, 0].rearrange("(s d) -> s d", s=6)
        io["modc"] = modS[L, 1].rearrange("(s d) -> s d", s=6)
        io.update(n1g=n1g[L], n2g=n2g[L], win=win[L], gqk=gqk[L], vnb=vnb[L], wsT=wsT[L], bsT=bsT[L], ogb=ogb[L],
                  sink=sink[L], gac=gac[L], wout=wout[L], wr=wr[L], x1o=xs, x1=xs, dA=X["delta"], x2=xs)
        build_s1(nc=nc, io=io, pfx=f"L{L}a_")
        stage_cc(nc, [(X["KTx"], X["KTg"]), (X["Vx"], X["Vg"])])
        build_s2a(nc=nc, io=io, pfx=f"L{L}b_")
        build_s2b(nc=nc, io=io, pfx=f"L{L}c_")
        stage_cc(nc, [(X["affIn"], X["affAll"])])
        stage_s3a(nc, io, L == 0, f"L{L}d_")
        stage_s3b(nc, io, L, L == 0, f"L{L}e_")
        build_s4(nc=nc, io=io, pfx=f"L{L}f_")
    stage_copy_out(nc, xs, out, "o_")
    return nc


def kernel(x, c, ctx, c_ctx, w_mod, b_mod, norm1_g, norm2_g, w_in, qn_a, kn_a, sink_a, vn_b,
           w_s, b_s, qn_c, kn_c, out_g, w_out, w_router, w_gate, w_up, w_down):
    f32 = np.float32
    A = lambda a: np.ascontiguousarray(np.asarray(a, f32))
    x = A(x); ctx = A(ctx); c = A(c); c_ctx = A(c_ctx)
    cores = [(k // 2, k % 2) for k in range(8)]
    og = A(out_g)
    shared = {
        "n1g": A(norm1_g), "n2g": A(norm2_g),
        "win": A(np.asarray(w_in, f32)[:, :, WIN_PERM]),
        "gqk": A(np.stack([np.concatenate([np.tile(qn_a[L], 6), np.tile(kn_a[L], 2), np.tile(qn_c[L], 6), np.tile(kn_c[L], 2)]) for L in range(2)])),
        "vnb": A(vn_b), "wsT": A(np.asarray(w_s, f32).transpose(0, 3, 1, 2)), "bsT": A(np.asarray(b_s, f32).transpose(0, 2, 1)),
        "ogb": A(og[:, 768:1280]), "ident": np.eye(128, dtype=f32), "sink": A(sink_a),
        "gac": A(np.concatenate([og[:, :768], og[:, 1280:]], 1)), "wout": A(w_out), "wr": A(w_router),
        "w_gate": A(w_gate), "w_up": A(w_up), "w_down": A(w_down), "consts": f_consts(),
    }
    ims = []
    wm = np.asarray(w_mod, f32)
    bm = np.asarray(b_mod, f32)
    c5 = np.concatenate([c, c_ctx[None, :]], 0)
    cin5 = A(c5.T.reshape(16, 128, 5).transpose(1, 0, 2))
    for (b, h) in cores:
        d = dict(shared)
        d["xin"] = A(np.concatenate([ctx[b], x[b, h * 2048:(h + 1) * 2048]], 0))
        k = 2 * b + h
        mcol = 6 * D // _NCORES[0]
        d["cin5"] = cin5
        d["wms"] = A(wm[:, :, k * mcol:(k + 1) * mcol]) if k < _NCORES[0] else None
        d["bms"] = A(bm[:, k * mcol:(k + 1) * mcol]) if k < _NCORES[0] else None
        selT = np.zeros((5, 2), f32)
        selT[b, 0] = 1.0
        selT[4, 1] = 1.0
        d["selT"] = selT
        d["cs_t"] = _rope_tables(h)
        d["msk"] = _masks(h)
        ims.append(d)
    ncores = _NCORES[0]
    nc = build_fused(ncores)
    res = run_bass_kernel_spmd(nc, ims[:ncores], core_ids=list(range(ncores)))
    out = np.zeros((4, SEQ, D), f32)
    for k, (b, h) in enumerate(cores[:ncores]):
        out[b, h * 2048:(h + 1) * 2048] = res.results[k]["out"]
    return out


_NCORES = [8]
```
